# Optimizing a Trainium2 kernel written in Bass

```python
import jax, jax.numpy as jnp
from jax import lax
import numpy as np

D_MODEL = 2048
BATCH = 2
SEQ = 4096
DEPTH = 2

N_MIXERS = 2
HEAD_DIM = 128
ATTN_HEADS = D_MODEL // HEAD_DIM
DILATED_GROUPS = ((128, 1), (512, 4), (2048, 16))
N_DIL = len(DILATED_GROUPS)
ROT_DIM = HEAD_DIM // 4
ROPE_THETA = 500000.0
FNET_WIDTH = D_MODEL
FNET_GROUPS = 4
FNET_GROUP_CH = FNET_WIDTH // FNET_GROUPS
N_EXPERT_GROUPS = 4
EXPERTS_PER_GROUP = 8
N_EXPERTS = N_EXPERT_GROUPS * EXPERTS_PER_GROUP
EXPERT_DIM = D_MODEL // 4
TOP_K_FINE = 2
MOE_BLOCK = 128
LN_EPS = 1e-5
NEG_INF = -1e30
DEEPNORM_ALPHA = (2 * DEPTH) ** 0.25
DEEPNORM_BETA = (8 * DEPTH) ** -0.25
N_ATTN_LAYERS = (DEPTH + 1) // 2
N_FNET_LAYERS = DEPTH // 2

kernel_name = "hybrid_dilated_attn_fnet_hier_moe_deepnorm_adaln"


def layer_norm(x, g, b):
    xf = x.astype(jnp.float32)
    mu = jnp.mean(xf, axis=-1, keepdims=True)
    var = jnp.mean(jnp.square(xf - mu), axis=-1, keepdims=True)
    y = (xf - mu) * lax.rsqrt(var + LN_EPS)
    return (y * g.astype(jnp.float32) + b.astype(jnp.float32)).astype(x.dtype)


def adaln(c, w, b):
    m = jax.nn.silu(c) @ w + b
    shift, scale, gate = jnp.split(m, 3, axis=-1)
    return shift[:, None, :], scale[:, None, :], gate[:, None, :]


def rotary_tables(positions, dtype):
    inv_freq = ROPE_THETA ** (-jnp.arange(0, ROT_DIM, 2, dtype=jnp.float32) / ROT_DIM)
    ang = positions.astype(jnp.float32)[..., None] * inv_freq
    return (jnp.cos(ang)[:, :, None, :].astype(dtype),
            jnp.sin(ang)[:, :, None, :].astype(dtype))


def partial_rotary(t, cos, sin):
    half = ROT_DIM // 2
    t1, t2, rest = t[..., :half], t[..., half:ROT_DIM], t[..., ROT_DIM:]
    return jnp.concatenate([t1 * cos - t2 * sin, t2 * cos + t1 * sin, rest], axis=-1)


def stride_split(t, d):
    bsz, s = t.shape[:2]
    rest = t.shape[2:]
    t = jnp.moveaxis(t.reshape(bsz, s // d, d, *rest), 2, 1)
    return t.reshape(bsz * d, s // d, *rest)


def stride_merge(t, bsz, d):
    n, l = t.shape[:2]
    rest = t.shape[2:]
    t = jnp.moveaxis(t.reshape(bsz, d, l, *rest), 1, 2)
    return t.reshape(bsz, l * d, *rest)


def banded_attention(q, k, v, radius):
    n, l, h, dh = q.shape
    blk = radius
    nb = -(-l // blk)
    lp = nb * blk
    qb = jnp.pad(q, ((0, 0), (0, lp - l), (0, 0), (0, 0))).reshape(n, nb, blk, h, dh)

    def windows(t):
        tp = jnp.pad(t, ((0, 0), (blk, lp - l + blk), (0, 0), (0, 0))).reshape(n, nb + 2, blk, h, dh)
        return jnp.concatenate([tp[:, :-2], tp[:, 1:-1], tp[:, 2:]], axis=2)

    kw, vw = windows(k), windows(v)
    s = jnp.einsum('nbqhd,nbkhd->nbhqk', qb, kw, preferred_element_type=jnp.float32) * (dh ** -0.5)
    qpos = jnp.arange(nb)[:, None] * blk + jnp.arange(blk)[None, :]
    kpos = jnp.arange(nb)[:, None] * blk - blk + jnp.arange(3 * blk)[None, :]
    valid = ((jnp.abs(qpos[:, :, None] - kpos[:, None, :]) <= radius)
             & (kpos >= 0)[:, None, :] & (kpos < l)[:, None, :])
    s = jnp.where(valid[None, :, None], s, NEG_INF)
    m = jnp.max(s, axis=-1, keepdims=True)
    p = jnp.exp(s - m)
    den = jnp.sum(p, axis=-1, keepdims=True)
    o = jnp.einsum('nbhqk,nbkhd->nbqhd', p, vw.astype(jnp.float32)) / jnp.swapaxes(den, 2, 3)
    lse = jnp.swapaxes((m + jnp.log(den))[..., 0], 2, 3)
    return o.reshape(n, lp, h, dh)[:, :l], lse.reshape(n, lp, h)[:, :l]


def dilated_attention(h, w_qkv, w_o, cos, sin):
    bsz, s, _ = h.shape
    qkv = (h @ w_qkv).reshape(bsz, s, N_DIL, 3, ATTN_HEADS, HEAD_DIM)
    outs, lses = [], []
    for g, (window, dil) in enumerate(DILATED_GROUPS):
        radius = window // (2 * dil)
        q = partial_rotary(qkv[:, :, g, 0], cos, sin)
        k = partial_rotary(qkv[:, :, g, 1], cos, sin)
        v = qkv[:, :, g, 2]
        o_g, lse_g = banded_attention(stride_split(q, dil), stride_split(k, dil),
                                      stride_split(v, dil), radius)
        outs.append(stride_merge(o_g, bsz, dil))
        lses.append(stride_merge(lse_g, bsz, dil))
    wts = jax.nn.softmax(jnp.stack(lses), axis=0)
    o = jnp.einsum('gbsh,gbshd->bshd', wts, jnp.stack(outs)).astype(h.dtype)
    return o.reshape(bsz, s, ATTN_HEADS * HEAD_DIM) @ w_o


def fourier_mixer(h, w_in, w_out):
    bsz, s, _ = h.shape
    u = (h @ w_in).reshape(bsz, s, FNET_GROUPS, FNET_GROUP_CH).astype(jnp.float32)
    y = jnp.fft.fft2(u, axes=(1, 3), norm='ortho').real
    return y.astype(h.dtype).reshape(bsz, s, FNET_WIDTH) @ w_out


def hier_moe(h, wr1, br1, wr2, br2, w_gate, w_up, w_down):
    bsz, s, d = h.shape
    t = h.reshape(-1, d)
    n_tok = t.shape[0]
    pc = jax.nn.softmax((t @ wr1).astype(jnp.float32) + br1.astype(jnp.float32), axis=-1)
    pg, gi = lax.top_k(pc, 1)
    lf = ((t @ wr2).astype(jnp.float32) + br2.astype(jnp.float32)).reshape(n_tok, N_EXPERT_GROUPS, EXPERTS_PER_GROUP)
    lf_sel = jnp.take_along_axis(lf, gi[:, :, None], axis=1)[:, 0]
    pk, ek = lax.top_k(jax.nn.softmax(lf_sel, axis=-1), TOP_K_FINE)
    gates = pg * (pk / jnp.sum(pk, axis=-1, keepdims=True))
    eid = gi * EXPERTS_PER_GROUP + ek

    tk = n_tok * TOP_K_FINE
    e_flat = eid.reshape(-1)
    g_flat = gates.reshape(-1)
    tok = jnp.arange(tk, dtype=jnp.int32) // TOP_K_FINE
    order = jnp.argsort(e_flat)
    e_s, tok_s, g_s = e_flat[order], tok[order], g_flat[order]
    counts = jnp.bincount(e_flat, length=N_EXPERTS)
    starts = jnp.cumsum(counts) - counts
    padded = (counts + MOE_BLOCK - 1) // MOE_BLOCK * MOE_BLOCK
    pends = jnp.cumsum(padded)
    pstarts = pends - padded
    dest = pstarts[e_s] + (jnp.arange(tk) - starts[e_s])
    n_blocks = -(-tk // MOE_BLOCK) + N_EXPERTS
    n_rows = n_blocks * MOE_BLOCK
    row_tok = jnp.zeros((n_rows,), jnp.int32).at[dest].set(tok_s)
    row_gate = jnp.zeros((n_rows,), t.dtype).at[dest].set(g_s.astype(t.dtype))
    block_e = jnp.clip(jnp.searchsorted(pends, jnp.arange(n_blocks) * MOE_BLOCK, side='right'), 0, N_EXPERTS - 1)

    def run_block(args):
        tok_b, gate_b, e = args
        xb = t[tok_b]
        a = jax.nn.silu(xb @ w_gate[e]) * (xb @ w_up[e])
        return (a @ w_down[e]) * gate_b[:, None]

    ys = lax.map(run_block, (row_tok.reshape(n_blocks, MOE_BLOCK),
                             row_gate.reshape(n_blocks, MOE_BLOCK), block_e))
    out = jnp.zeros_like(t).at[row_tok].add(ys.reshape(n_rows, d))
    return out.reshape(bsz, s, d)


def setup_inputs(seed: int = 0) -> dict:
    key = jax.random.key(seed)
    ks = jax.random.split(key, 20)

    def nrm(k, shape, scale):
        return jax.random.normal(k, shape, jnp.float32) * scale

    hd_total = ATTN_HEADS * HEAD_DIM
    qkv_scale = jnp.array([1.0, 1.0, DEEPNORM_BETA], jnp.float32).reshape(1, 1, 1, 3, 1)
    return {
        "x": nrm(ks[0], (BATCH, SEQ, D_MODEL), 1.0),
        "c": nrm(ks[1], (BATCH, D_MODEL), 1.0),
        "positions": jnp.broadcast_to(jnp.arange(SEQ, dtype=jnp.int32), (BATCH, SEQ)),
        "ada_w": nrm(ks[2], (DEPTH, 2, D_MODEL, 3 * D_MODEL), D_MODEL ** -0.5),
        "ada_b": nrm(ks[3], (DEPTH, 2, 3 * D_MODEL), 0.02),
        "attn_w_qkv": (nrm(ks[4], (N_ATTN_LAYERS, D_MODEL, N_DIL, 3, hd_total), D_MODEL ** -0.5)
                       * qkv_scale).reshape(N_ATTN_LAYERS, D_MODEL, N_DIL * 3 * hd_total),
        "attn_w_o": nrm(ks[5], (N_ATTN_LAYERS, hd_total, D_MODEL), hd_total ** -0.5 * DEEPNORM_BETA),
        "fnet_w_in": nrm(ks[6], (N_FNET_LAYERS, D_MODEL, FNET_WIDTH), D_MODEL ** -0.5 * DEEPNORM_BETA),
        "fnet_w_out": nrm(ks[7], (N_FNET_LAYERS, FNET_WIDTH, D_MODEL), FNET_WIDTH ** -0.5 * DEEPNORM_BETA),
        "ln_g": 1.0 + nrm(ks[8], (DEPTH, 2, D_MODEL), 0.02),
        "ln_b": nrm(ks[9], (DEPTH, 2, D_MODEL), 0.02),
        "router_coarse_w": nrm(ks[10], (DEPTH, D_MODEL, N_EXPERT_GROUPS), D_MODEL ** -0.5),
        "router_coarse_b": nrm(ks[11], (DEPTH, N_EXPERT_GROUPS), 0.01),
        "router_fine_w": nrm(ks[12], (DEPTH, D_MODEL, N_EXPERTS), D_MODEL ** -0.5),
        "router_fine_b": nrm(ks[13], (DEPTH, N_EXPERTS), 0.01),
        "expert_w_gate": nrm(ks[14], (DEPTH, N_EXPERTS, D_MODEL, EXPERT_DIM), D_MODEL ** -0.5 * DEEPNORM_BETA),
        "expert_w_up": nrm(ks[15], (DEPTH, N_EXPERTS, D_MODEL, EXPERT_DIM), D_MODEL ** -0.5 * DEEPNORM_BETA),
        "expert_w_down": nrm(ks[16], (DEPTH, N_EXPERTS, EXPERT_DIM, D_MODEL), EXPERT_DIM ** -0.5 * DEEPNORM_BETA),
    }


def reference(x, c, positions, ada_w, ada_b, attn_w_qkv, attn_w_o, fnet_w_in, fnet_w_out,
              ln_g, ln_b, router_coarse_w, router_coarse_b, router_fine_w, router_fine_b,
              expert_w_gate, expert_w_up, expert_w_down):
    cos, sin = rotary_tables(positions, x.dtype)
    for i in range(DEPTH):
        j = i // N_MIXERS
        shift, scale, gate = adaln(c, ada_w[i, 0], ada_b[i, 0])
        h = x * (1.0 + scale) + shift
        if i % N_MIXERS == 0:
            y = dilated_attention(h, attn_w_qkv[j], attn_w_o[j], cos, sin)
        else:
            y = fourier_mixer(h, fnet_w_in[j], fnet_w_out[j])
        x = layer_norm(DEEPNORM_ALPHA * x + gate * y, ln_g[i, 0], ln_b[i, 0])
        shift, scale, gate = adaln(c, ada_w[i, 1], ada_b[i, 1])
        h = x * (1.0 + scale) + shift
        y = hier_moe(h, router_coarse_w[i], router_coarse_b[i], router_fine_w[i], router_fine_b[i],
                     expert_w_gate[i], expert_w_up[i], expert_w_down[i])
        x = layer_norm(DEEPNORM_ALPHA * x + gate * y, ln_g[i, 1], ln_b[i, 1])
    return x
```

```python
import numpy as np
from contextlib import ExitStack
import concourse.bass as bass
import concourse.mybir as mybir
from concourse.bass_utils import run_bass_kernel_spmd

F32 = mybir.dt.float32
F32R = mybir.dt.float32r
BF16 = mybir.dt.bfloat16
I32 = mybir.dt.int32
AF = mybir.ActivationFunctionType
ALU = mybir.AluOpType
AX = mybir.AxisListType

NCORES = 8
D = 2048
KC = D // 128
B = 2
S = 4096
NTOK = B * S
HD = 128
NH = 16
NG = 3
DILS = (1, 4, 16)
RADIUS = 64
ROT = 32
THETA = 500000.0
NE = 32
EPG = 8
NGRP = 4
ED = 512
EPS = 1e-5
ALPHA = 4.0 ** 0.25

SAME_ENG_SYNC = True


class Buf:
    __slots__ = ("t", "name", "w", "r", "dsem", "dcnt")

    def __init__(self, t, name):
        self.t = t
        self.name = name
        self.w = None
        self.r = []
        self.dsem = None
        self.dcnt = 0

    def __getitem__(self, k):
        return self.t[k]


class Sched:
    def __init__(self, nc, es):
        self.nc = nc
        self.es = es
        self.eng = {"pe": nc.tensor, "dve": nc.vector, "act": nc.scalar,
                    "pool": nc.gpsimd, "sp": nc.sync}
        self.sem = {}
        self.cnt = {}
        self.seen = {}
        for k in self.eng:
            self.sem[k] = es.enter_context(nc.semaphore("s_" + k))
            self.cnt[k] = 0
            self.seen[k] = {}
        self.nbuf = 0
        self.out_bufs = []

    def sbuf(self, shape, dtype=F32, name=None):
        self.nbuf += 1
        name = name or f"sb{self.nbuf}"
        t = self.es.enter_context(self.nc.sbuf_tensor(name, list(shape), dtype))
        return Buf(t, name)

    def psum(self, shape, dtype=F32, name=None):
        self.nbuf += 1
        name = name or f"ps{self.nbuf}"
        t = self.es.enter_context(self.nc.psum_tensor(name, list(shape), dtype))
        return Buf(t, name)

    def view(self, name="v"):
        return Buf(None, name)

    def _collect(self, eng, reads, writes):
        need = {}

        def add(dep, raw):
            if dep is None:
                return
            if dep[0] == "dma":
                key = ("d", dep[1])
                sem, val = dep[1], dep[2]
            else:
                e2, val = dep
                if e2 == eng:
                    if eng == "pe" or not raw or not SAME_ENG_SYNC:
                        return
                key = ("e", e2)
                sem = self.sem[e2]
            if need.get(key, (None, 0))[1] < val:
                need[key] = (sem, val)

        for b in reads:
            add(b.w, True)
        for b in writes:
            add(b.w, True)
            for r in b.r:
                add(r, False)
        return need

    def _emit_waits(self, eng, need):
        E = self.eng[eng]
        seen = self.seen[eng]
        for key, (sem, val) in need.items():
            if seen.get(key, 0) < val:
                E.wait_ge(sem, val)
                seen[key] = val

    def op(self, eng, fn, reads=(), writes=()):
        need = self._collect(eng, reads, writes)
        self._emit_waits(eng, need)
        ins = fn(self.eng[eng])
        self.cnt[eng] += 1
        ins.then_inc(self.sem[eng], 1)
        me = (eng, self.cnt[eng])
        for b in reads:
            b.r.append(me)
        for b in writes:
            b.w = me
            b.r = []
        return ins

    def dma(self, q, out, in_, reads=(), writes=(), track=None, **kw):
        need = self._collect("dmaq_" + q, reads, writes)
        self._emit_waits(q, need)
        b = track or (writes[0] if writes else reads[0])
        if b.dsem is None:
            b.dsem = self.es.enter_context(self.nc.semaphore("d_" + b.name))
            self.out_bufs.append(b)
        ins = self.eng[q].dma_start(out=out, in_=in_, **kw)
        b.dcnt += 16
        ins.then_inc(b.dsem, 16)
        me = ("dma", b.dsem, b.dcnt)
        for x in reads:
            x.r.append(me)
        for x in writes:
            x.w = me
            x.r = []
        return ins

    def mark_output(self, b):
        if b not in self.out_bufs:
            self.out_bufs.append(b)

    def finish(self):
        E = self.eng["sp"]
        for b in self.out_bufs:
            if b.dsem is not None:
                E.wait_ge(b.dsem, b.dcnt)


def r32(ap):
    return ap.bitcast(F32R)


L1_COLS = 3072


def build_l1():
    nc = bass.Bass("TRN2", target_bir_lowering=False)
    cT = nc.dram_tensor("cT", [128, KC, B], F32, kind="ExternalInput").ap()
    w = nc.dram_tensor("w", [KC, 128, L1_COLS], F32, kind="ExternalInput").ap()
    bias = nc.dram_tensor("bias", [B, L1_COLS], F32, kind="ExternalInput").ap()
    out = nc.dram_tensor("out", [B, L1_COLS], F32, kind="ExternalOutput").ap()
    with ExitStack() as es:
        S_ = Sched(nc, es)
        sc = S_.sbuf([128, KC, B], name="sc")
        bt = S_.sbuf([B, L1_COLS], name="bt")
        ot = S_.sbuf([B, L1_COLS], name="ot")
        NB = 4
        wb = [S_.sbuf([128, L1_COLS], name=f"wb{i}") for i in range(NB)]
        ps = [S_.psum([128, 512], name=f"ps{i}") for i in range(6)]
        S_.dma("sp", sc[:], cT, writes=[sc])
        S_.dma("sp", bt[:], bias, writes=[bt])
        S_.op("act", lambda e: e.activation(out=sc[:], in_=sc[:], func=AF.Silu),
              reads=[sc], writes=[sc])
        for k in range(KC):
            wk = wb[k % NB]
            S_.dma("sp" if k % 2 == 0 else "pool", wk[:], w[k], writes=[wk])
            for n in range(6):
                S_.op("pe", lambda e, n=n, k=k, wk=wk: e.matmul(
                    ps[n][0:B, :], lhsT=sc[:, k, :], rhs=wk[:, n * 512:(n + 1) * 512],
                    start=(k == 0), stop=(k == KC - 1)),
                    reads=[sc, wk], writes=[ps[n]])
        for n in range(6):
            S_.op("dve", lambda e, n=n: e.tensor_tensor(
                out=ot[:, n * 512:(n + 1) * 512], in0=ps[n][0:B, :],
                in1=bt[:, n * 512:(n + 1) * 512], op=ALU.add),
                reads=[ps[n], bt], writes=[ot])
        S_.dma("sp", out, ot[:], reads=[ot])
        S_.mark_output(ot)
        S_.finish()
    return nc


def run_l1(c, ada_w, ada_b):
    nc = build_l1()
    cT = np.ascontiguousarray(c.T.reshape(KC, 128, B).transpose(1, 0, 2))
    in_maps = []
    for core in range(NCORES):
        s = core // 2
        i, j = s // 2, s % 2
        c0 = (core % 2) * L1_COLS
        wsl = np.ascontiguousarray(ada_w[i, j][:, c0:c0 + L1_COLS]).reshape(KC, 128, L1_COLS)
        bsl = np.ascontiguousarray(np.broadcast_to(ada_b[i, j][c0:c0 + L1_COLS], (B, L1_COLS)))
        in_maps.append({"cT": cT, "w": wsl, "bias": bsl})
    res = run_bass_kernel_spmd(nc, in_maps, core_ids=list(range(NCORES)))
    mod = np.zeros((2, 2, B, 3 * D), np.float32)
    for core in range(NCORES):
        s = core // 2
        i, j = s // 2, s % 2
        c0 = (core % 2) * L1_COLS
        mod[i, j][:, c0:c0 + L1_COLS] = res.results[core]["out"]
    return mod


NPASS = 2
TP = 512
TT = TP // 128
P_GATE1, P_LNG1, P_LNB1, P_SC2, P_SH2, P_GATE2, P_LNG2, P_LNB2 = range(8)


def build_l3(n_experts=NE):
    nc = bass.Bass("TRN2", target_bir_lowering=False)
    aT_d = nc.dram_tensor("aT", [NPASS, 128, KC, TP], F32R, kind="ExternalInput").ap()
    wp_d = nc.dram_tensor("wp", [4, 128, KC * 512], F32R, kind="ExternalInput").ap()
    x_d = nc.dram_tensor("x", [NPASS, TT, 128, D], F32, kind="ExternalInput").ap()
    par_d = nc.dram_tensor("par", [8, 128, D], F32, kind="ExternalInput").ap()
    wr_d = nc.dram_tensor("wr", [128, KC, 36], F32, kind="ExternalInput").ap()
    br_d = nc.dram_tensor("br", [128, 36], F32, kind="ExternalInput").ap()
    wgu_d = nc.dram_tensor("wgu", [NE * 4, 128, 2 * KC * 128], F32R, kind="ExternalInput").ap()
    wd_d = nc.dram_tensor("wd", [NE, 128, 4 * D], F32R, kind="ExternalInput").ap()
    id_d = nc.dram_tensor("ident", [128, 128], F32, kind="ExternalInput").ap()
    out_d = nc.dram_tensor("out", [NPASS, TT, 128, D], F32, kind="ExternalOutput").ap()

    with ExitStack() as es:
        S_ = Sched(nc, es)
        hT = S_.sbuf([128, KC, TP], name="hT")
        acc = [S_.sbuf([128, D], name=f"acc{t}") for t in range(TT)]
        wdb = [S_.sbuf([128, 4 * D], name=f"wdb{i}") for i in range(2)]
        gub = [S_.sbuf([128, 2 * KC * 128], name=f"gub{i}") for i in range(2)]
        aTb = [S_.sbuf([128, TP], name=f"aTb{i}") for i in range(8)]
        par = [S_.sbuf([128, D], name=f"par{i}") for i in range(3)]
        scr = gub[0]
        sh2 = gub[1]
        wr = S_.sbuf([128, KC, 36], name="wr_sb")
        br = S_.sbuf([128, 36], name="br_sb")
        ident = S_.sbuf([128, 128], name="ident_sb")
        G = S_.sbuf([128, TT, NE], name="G")
        sm = [S_.sbuf([128, 64], name=f"sm{i}") for i in range(2)]
        ps = [S_.psum([128, 512], name=f"psb{i}") for i in range(8)]
        pg, pu, py = ps[0:2], ps[2:4], ps[4:8]

        S_.dma("sp", wr[:], wr_d, writes=[wr])
        S_.dma("sp", br[:], br_d, writes=[br])
        S_.dma("sp", ident[:], id_d, writes=[ident])

        def ln_tile(xb, gb, gap, bb, bap, smb):
            s1, s2, mean, msq, var, std, rstd, nmr = [smb[:, i:i + 1] for i in range(8)]
            S_.op("act", lambda e: e.activation(out=r32(scr[:, 0:D]), in_=xb[:], func=AF.Identity, accum_out=s1),
                  reads=[xb], writes=[scr, smb])
            S_.op("act", lambda e: e.activation(out=r32(scr[:, 0:D]), in_=xb[:], func=AF.Square, accum_out=s2),
                  reads=[xb], writes=[scr, smb])
            S_.op("dve", lambda e: e.tensor_scalar(out=mean, in0=s1, scalar1=1.0 / D, scalar2=None, op0=ALU.mult),
                  reads=[smb], writes=[smb])
            S_.op("dve", lambda e: e.tensor_tensor(out=msq, in0=mean, in1=mean, op=ALU.mult),
                  reads=[smb], writes=[smb])
            S_.op("dve", lambda e: e.scalar_tensor_tensor(out=var, in0=s2, scalar=1.0 / D, in1=msq,
                                                          op0=ALU.mult, op1=ALU.subtract),
                  reads=[smb], writes=[smb])
            S_.op("dve", lambda e: e.tensor_scalar(out=var, in0=var, scalar1=EPS, scalar2=None, op0=ALU.add),
                  reads=[smb], writes=[smb])
            S_.op("act", lambda e: e.activation(out=std, in_=var, func=AF.Sqrt), reads=[smb], writes=[smb])
            S_.op("dve", lambda e: e.reciprocal(out=rstd, in_=std), reads=[smb], writes=[smb])
            S_.op("dve", lambda e: e.tensor_scalar(out=nmr, in0=mean, scalar1=rstd, scalar2=-1.0,
                                                   op0=ALU.mult, op1=ALU.mult), reads=[smb], writes=[smb])
            S_.op("act", lambda e: e.activation(out=xb[:], in_=xb[:], func=AF.Identity, scale=rstd, bias=nmr),
                  reads=[xb, smb], writes=[xb])
            S_.op("dve", lambda e: e.tensor_tensor(out=xb[:], in0=xb[:], in1=gap, op=ALU.mult),
                  reads=[xb, gb], writes=[xb])
            S_.op("dve", lambda e: e.tensor_tensor(out=xb[:], in0=xb[:], in1=bap, op=ALU.add),
                  reads=[xb, bb], writes=[xb])

        pyi = 0
        for pz in range(NPASS):
            S_.dma("pool", r32(hT[:]), aT_d[pz], writes=[hT])
            for t in range(TT):
                S_.dma("sp", acc[t][:], x_d[pz, t], writes=[acc[t]])
            S_.dma("sp", par[0][:], par_d[P_GATE1], writes=[par[0]])
            S_.dma("sp", par[1][:], par_d[P_LNG1], writes=[par[1]])
            S_.dma("sp", par[2][:], par_d[P_LNB1], writes=[par[2]])
            for n in range(4):
                wb = wdb[n % 2]
                S_.dma("pool", r32(wb[:]), wp_d[n], writes=[wb])
                wv = wb[:].rearrange("p (k c) -> p k c", k=KC)
                for t in range(TT):
                    pb = py[pyi % 4]
                    pyi += 1
                    for k in range(KC):
                        S_.op("pe", lambda e, k=k, t=t, pb=pb, wv=wv: e.matmul(
                            pb[:], lhsT=r32(hT[:, k, t * 128:(t + 1) * 128]), rhs=r32(wv[:, k, :]),
                            start=(k == 0), stop=(k == KC - 1)), reads=[hT, wb], writes=[pb])
                    cs = slice(n * 512, (n + 1) * 512)
                    sg = aTb[(n * TT + t) % 8]
                    S_.op("dve", lambda e, pb=pb, sg=sg, cs=cs: e.tensor_tensor(
                        out=r32(sg[:]), in0=pb[:], in1=par[0][:, cs], op=ALU.mult),
                        reads=[pb, par[0]], writes=[sg])
                    S_.op("dve", lambda e, t=t, sg=sg, cs=cs: e.scalar_tensor_tensor(
                        out=acc[t][:, cs], in0=acc[t][:, cs], scalar=ALPHA, in1=sg[:],
                        op0=ALU.mult, op1=ALU.add), reads=[acc[t], sg], writes=[acc[t]])
            for t in range(TT):
                ln_tile(acc[t], par[1], par[1][:], par[2], par[2][:], sm[t % 2])
            S_.dma("sp", par[0][:], par_d[P_SC2], writes=[par[0]])
            S_.dma("pool", r32(sh2[:, 0:D]), par_d[P_SH2], writes=[sh2])
            S_.op("dve", lambda e: e.tensor_scalar(out=par[0][:], in0=par[0][:], scalar1=1.0, scalar2=None,
                                                    op0=ALU.add), reads=[par[0]], writes=[par[0]])
            for t in range(TT):
                S_.op("dve", lambda e, t=t: e.tensor_tensor(out=r32(scr[:, 0:D]), in0=acc[t][:], in1=par[0][:], op=ALU.mult),
                      reads=[acc[t], par[0]], writes=[scr])
                S_.op("dve", lambda e: e.tensor_tensor(out=r32(scr[:, 0:D]), in0=scr[:, 0:D], in1=sh2[:, 0:D], op=ALU.add),
                      reads=[scr, sh2], writes=[scr])
                S_.op("dve", lambda e, t=t: e.tensor_scalar(out=acc[t][:], in0=acc[t][:], scalar1=ALPHA,
                                                             scalar2=None, op0=ALU.mult),
                      reads=[acc[t]], writes=[acc[t]])
                for k0 in range(0, KC, 4):
                    pb = py[pyi % 4]
                    pyi += 1
                    for j in range(4):
                        k = k0 + j
                        S_.op("pe", lambda e, j=j, k=k, pb=pb: e.transpose(
                            out=pb[:, j * 128:(j + 1) * 128], in_=scr[:, k * 128:(k + 1) * 128],
                            identity=ident[:]), reads=[scr, ident], writes=[pb])
                    S_.op("act", lambda e, k0=k0, t=t, pb=pb: e.activation(
                        out=r32(hT[:, k0:k0 + 4, t * 128:(t + 1) * 128]),
                        in_=pb[:].rearrange("p (a b) -> p a b", a=4), func=AF.Identity),
                        reads=[pb], writes=[hT])
            for t in range(TT):
                pb = py[pyi % 4]
                pyi += 1
                for k in range(KC):
                    S_.op("pe", lambda e, k=k, t=t, pb=pb: e.matmul(
                        pb[:, 0:36], lhsT=hT[:, k, t * 128:(t + 1) * 128], rhs=wr[:, k, :],
                        start=(k == 0), stop=(k == KC - 1)), reads=[hT, wr], writes=[pb])
                s = sm[t % 2]
                lg = s[:, 0:36]
                m4, nm4, s4, pgp = s[:, 36:37], s[:, 37:38], s[:, 38:39], s[:, 39:40]
                oh4 = s[:, 40:44]
                e4 = s[:, 44:48]
                sel = s[:, 48:56]
                S_.op("dve", lambda e: e.tensor_tensor(out=lg, in0=pb[:, 0:36], in1=br[:], op=ALU.add),
                      reads=[pb, br], writes=[s])
                S_.op("dve", lambda e: e.reduce_max(out=m4, in_=s[:, 0:4], axis=AX.X), reads=[s], writes=[s])
                S_.op("dve", lambda e: e.tensor_scalar(out=nm4, in0=m4, scalar1=-1.0, scalar2=None, op0=ALU.mult),
                      reads=[s], writes=[s])
                S_.op("act", lambda e: e.activation(out=e4, in_=s[:, 0:4], func=AF.Exp, bias=nm4, accum_out=s4),
                      reads=[s], writes=[s])
                S_.op("dve", lambda e: e.reciprocal(out=pgp, in_=s4), reads=[s], writes=[s])
                S_.op("dve", lambda e: e.tensor_scalar(out=oh4, in0=s[:, 0:4], scalar1=m4, scalar2=None,
                                                       op0=ALU.is_equal), reads=[s], writes=[s])
                S_.op("dve", lambda e: e.tensor_scalar(out=sel, in0=s[:, 4:12], scalar1=s[:, 40:41], scalar2=None,
                                                       op0=ALU.mult), reads=[s], writes=[s])
                for g in range(1, 4):
                    S_.op("dve", lambda e, g=g: e.scalar_tensor_tensor(
                        out=sel, in0=s[:, 4 + 8 * g:12 + 8 * g], scalar=s[:, 40 + g:41 + g], in1=sel,
                        op0=ALU.mult, op1=ALU.add), reads=[s], writes=[s])
                s2b = sm[t % 2]
                m1, m2, nm1, dd, p1, p2 = [s[:, 56 + i:57 + i] for i in range(6)]
                g1, g2 = s[:, 62:63], s[:, 63:64]
                o1 = G[:, t, 0:8]
                o2 = G[:, t, 8:16]
                sel2 = G[:, t, 16:24]
                g8 = G[:, t, 24:32]
                S_.op("dve", lambda e: e.reduce_max(out=m1, in_=sel, axis=AX.X), reads=[s], writes=[s])
                S_.op("dve", lambda e: e.tensor_scalar(out=o1, in0=sel, scalar1=m1, scalar2=None, op0=ALU.is_equal),
                      reads=[s], writes=[G])
                S_.op("dve", lambda e: e.scalar_tensor_tensor(out=sel2, in0=o1, scalar=-1e30, in1=sel,
                                                              op0=ALU.mult, op1=ALU.add), reads=[s, G], writes=[G])
                S_.op("dve", lambda e: e.reduce_max(out=m2, in_=sel2, axis=AX.X), reads=[G], writes=[s])
                S_.op("dve", lambda e: e.tensor_scalar(out=o2, in0=sel2, scalar1=m2, scalar2=None, op0=ALU.is_equal),
                      reads=[s, G], writes=[G])
                S_.op("dve", lambda e: e.tensor_scalar(out=nm1, in0=m1, scalar1=-1.0, scalar2=None, op0=ALU.mult),
                      reads=[s], writes=[s])
                S_.op("act", lambda e: e.activation(out=dd, in_=m2, func=AF.Exp, bias=nm1), reads=[s], writes=[s])
                S_.op("dve", lambda e: e.tensor_scalar(out=p1, in0=dd, scalar1=1.0, scalar2=None, op0=ALU.add),
                      reads=[s], writes=[s])
                S_.op("dve", lambda e: e.reciprocal(out=p1, in_=p1), reads=[s], writes=[s])
                S_.op("dve", lambda e: e.tensor_tensor(out=p2, in0=dd, in1=p1, op=ALU.mult), reads=[s], writes=[s])
                S_.op("dve", lambda e: e.tensor_tensor(out=g1, in0=p1, in1=pgp, op=ALU.mult), reads=[s], writes=[s])
                S_.op("dve", lambda e: e.tensor_tensor(out=g2, in0=p2, in1=pgp, op=ALU.mult), reads=[s], writes=[s])
                S_.op("dve", lambda e: e.tensor_scalar(out=g8, in0=o1, scalar1=g1, scalar2=None, op0=ALU.mult),
                      reads=[s, G], writes=[G])
                S_.op("dve", lambda e: e.scalar_tensor_tensor(out=g8, in0=o2, scalar=g2, in1=g8,
                                                              op0=ALU.mult, op1=ALU.add), reads=[s, G], writes=[G])
                S_.op("dve", lambda e: e.tensor_copy(out=sel, in_=g8), reads=[G], writes=[s])
                for g in range(4):
                    S_.op("dve", lambda e, g=g: e.tensor_scalar(
                        out=G[:, t, 8 * g:8 * g + 8], in0=sel, scalar1=s[:, 40 + g:41 + g], scalar2=None,
                        op0=ALU.mult), reads=[s], writes=[G])
            S_.dma("sp", par[0][:], par_d[P_GATE2], writes=[par[0]])
            ui = 0
            S_.dma("pool", r32(wdb[0][:]), wd_d[0], writes=[wdb[0]])
            for ex in range(n_experts):
                wb = wdb[ex % 2]
                pre = {}
                for hc in range(2):
                    gbp = gub[(ui + hc) % 2]
                    S_.dma("pool", r32(gbp[:]), wgu_d[ex * 4 + hc], writes=[gbp])
                    pre[hc] = gbp
                for hc in range(4):
                    S_.op("pool", lambda e, hc=hc, wb=wb: e.tensor_tensor(
                        out=r32(wb[:, hc * D:(hc + 1) * D]), in0=wb[:, hc * D:(hc + 1) * D], in1=par[0][:], op=ALU.mult),
                        reads=[wb, par[0]], writes=[wb])
                if ex + 1 < n_experts:
                    wbn = wdb[(ex + 1) % 2]
                    S_.dma("pool", r32(wbn[:]), wd_d[ex + 1], writes=[wbn])
                for hc in range(4):
                    gb = gub[ui % 2]
                    if hc not in pre:
                        S_.dma("pool", r32(gb[:]), wgu_d[ex * 4 + hc], writes=[gb])
                    gv = gb[:].rearrange("p (j k c) -> p j k c", j=2, k=KC)
                    pgb, pub = pg[ui % 2], pu[ui % 2]
                    for k in range(KC):
                        S_.op("pe", lambda e, k=k, gv=gv, pgb=pgb: e.matmul(
                            pgb[:], lhsT=r32(gv[:, 0, k, :]), rhs=r32(hT[:, k, :]),
                            start=(k == 0), stop=(k == KC - 1)), reads=[gb, hT], writes=[pgb])
                    for k in range(KC):
                        S_.op("pe", lambda e, k=k, gv=gv, pub=pub: e.matmul(
                            pub[:], lhsT=r32(gv[:, 1, k, :]), rhs=r32(hT[:, k, :]),
                            start=(k == 0), stop=(k == KC - 1)), reads=[gb, hT], writes=[pub])
                    ab = aTb[(ex % 2) * 4 + hc]
                    S_.op("act", lambda e, ab=ab, pgb=pgb: e.activation(out=r32(ab[:]), in_=pgb[:], func=AF.Silu),
                          reads=[pgb], writes=[ab])
                    S_.op("dve", lambda e, pub=pub, ab=ab: e.tensor_tensor(
                        out=r32(ab[:]), in0=ab[:], in1=pub[:], op=ALU.mult), reads=[ab, pub], writes=[ab])
                    ui += 1
                abs_ = [aTb[(ex % 2) * 4 + hc] for hc in range(4)]
                for t in range(TT):
                    for n in range(4):
                        pb = py[pyi % 4]
                        pyi += 1
                        for hc in range(4):
                            S_.op("pe", lambda e, hc=hc, t=t, n=n, pb=pb, wb=wb, abs_=abs_: e.matmul(
                                pb[:], lhsT=r32(abs_[hc][:, t * 128:(t + 1) * 128]),
                                rhs=r32(wb[:, hc * D + n * 512: hc * D + (n + 1) * 512]),
                                start=(hc == 0), stop=(hc == 3)), reads=[abs_[hc], wb], writes=[pb])
                        cs = slice(n * 512, (n + 1) * 512)
                        S_.op("dve", lambda e, t=t, cs=cs, pb=pb, ex=ex: e.scalar_tensor_tensor(
                            out=acc[t][:, cs], in0=pb[:], scalar=G[:, t, ex:ex + 1], in1=acc[t][:, cs],
                            op0=ALU.mult, op1=ALU.add), reads=[pb, G, acc[t]], writes=[acc[t]])
            S_.dma("sp", par[1][:], par_d[P_LNG2], writes=[par[1]])
            S_.dma("sp", par[2][:], par_d[P_LNB2], writes=[par[2]])
            for t in range(TT):
                ln_tile(acc[t], par[1], par[1][:], par[2], par[2][:], sm[t % 2])
                S_.dma("act", out_d[pz, t], acc[t][:], reads=[acc[t]])
                S_.mark_output(acc[t])
        S_.finish()
    return nc


def pack_experts(wg, wu, wd):
    st = np.stack([wg, wu], axis=1).reshape(NE, 2, KC, 128, 4, 128)
    wgu = np.ascontiguousarray(st.transpose(0, 4, 3, 1, 2, 5)).reshape(NE * 4, 128, 2 * KC * 128)
    wdp = np.ascontiguousarray(wd.reshape(NE, 4, 128, D).transpose(0, 2, 1, 3)).reshape(NE, 128, 4 * D)
    return wgu, wdp


def bc128(v):
    return np.ascontiguousarray(np.broadcast_to(v, (128,) + v.shape))


_NC_CACHE = {}


def run_l3(a_tok, wp, x_tok, rows, wrc, brc, wrf, brf, wgu, wdp, trace=False):
    if "l3" not in _NC_CACHE:
        _NC_CACHE["l3"] = build_l3()
    nc = _NC_CACHE["l3"]
    wpp = np.ascontiguousarray(wp.reshape(KC, 128, 4, 512).transpose(2, 1, 0, 3)).reshape(4, 128, KC * 512)
    wr = np.ascontiguousarray(np.concatenate([wrc, wrf], axis=1).reshape(KC, 128, 36).transpose(1, 0, 2))
    br = bc128(np.concatenate([brc, brf]))
    ident = np.eye(128, dtype=np.float32)
    pars = [np.ascontiguousarray(np.stack([bc128(v) for v in rows[b]])) for b in range(B)]
    in_maps = []
    TC = NTOK // NCORES
    for c in range(NCORES):
        b = c // (NCORES // B)
        a_c = a_tok[c * TC:(c + 1) * TC]
        aT = np.ascontiguousarray(a_c.reshape(NPASS, TP, KC, 128).transpose(0, 3, 2, 1))
        xc = np.ascontiguousarray(x_tok[c * TC:(c + 1) * TC]).reshape(NPASS, TT, 128, D)
        in_maps.append({"aT": aT, "wp": wpp, "x": xc, "par": pars[b], "wr": wr, "br": br,
                        "wgu": wgu, "wd": wdp, "ident": ident})
    res = run_bass_kernel_spmd(nc, in_maps, core_ids=list(range(NCORES)), trace=trace)
    out = np.concatenate([res.results[c]["out"].reshape(TC, D) for c in range(NCORES)], axis=0)
    if trace:
        return out, res
    return out


HPC = 4
BLK = 256
NBLK = S // BLK
QB = 512
SM_SCALE = HD ** -0.5
TWO_PI_HI = 6.28125
TWO_PI_LO = 2.0 * np.pi - 6.28125


def build_l2(hpc=HPC, do_tab=True, do_proj=True, do_attn=True, nblk=NBLK, nqb=S // QB, do_sel=True, do_rot=True, do_v=True):
    nc = bass.Bass("TRN2", target_bir_lowering=False)
    xT_d = nc.dram_tensor("xT", [NBLK, 128, KC * BLK], F32R, kind="ExternalInput").ap()
    mod_d = nc.dram_tensor("mod", [128, 2 * KC], F32, kind="ExternalInput").ap()
    wqk_d = nc.dram_tensor("wqk", [HPC, 128, 6 * KC * 128], F32R, kind="ExternalInput").ap()
    wv_d = nc.dram_tensor("wv", [HPC, 128, KC * 384], F32R, kind="ExternalInput").ap()
    pos_d = nc.dram_tensor("pos", [32, S], I32, kind="ExternalInput").ap()
    invf_d = nc.dram_tensor("invf", [32, 1], F32, kind="ExternalInput").ap()
    rot_d = nc.dram_tensor("rot", [128, 128], F32, kind="ExternalInput").ap()
    msk_d = nc.dram_tensor("msk", [2, 128, QB], F32, kind="ExternalInput").ap()
    o_d = nc.dram_tensor("o", [HPC, S // 128, 128, HD], F32, kind="ExternalOutput").ap()

    with ExitStack() as es:
        S_ = Sched(nc, es)
        xb = [S_.sbuf([128, KC * BLK], name=f"xb{i}") for i in range(2)]
        wqk = S_.sbuf([128, 6 * KC * 128], name="wqk_sb")
        wv = S_.sbuf([128, KC * 384], name="wv_sb")
        QT = [S_.sbuf([128, S], BF16, name=f"QT{g}") for g in range(NG)]
        KT = [S_.sbuf([128, S], BF16, name=f"KT{g}") for g in range(NG)]
        V = S_.sbuf([128, S // 128, NG, HD + 1], BF16, name="V")
        cosT = S_.sbuf([128, S], BF16, name="cosT")
        sinT = S_.sbuf([128, S], BF16, name="sinT")
        mod = S_.sbuf([128, 2 * KC], name="mod_sb")
        invf = S_.sbuf([32, 1], name="invf_sb")
        rotf = S_.sbuf([128, 128], name="rotf")
        rotb = S_.sbuf([128, 128], BF16, name="rotb")
        mskf = S_.sbuf([128, QB], name="mskf")
        msk = [S_.sbuf([128, QB], BF16, name=f"msk{i}") for i in range(2)]
        PT = [S_.sbuf([128, QB], BF16, name=f"PT{i}") for i in range(4)]
        t1 = [S_.sbuf([128, BLK], name=f"t1_{i}") for i in range(2)]
        t2 = [S_.sbuf([128, BLK], name=f"t2_{i}") for i in range(2)]
        osb = [S_.sbuf([128, HD], name=f"osb{i}") for i in range(2)]
        rden = [S_.sbuf([128, 1], name=f"rden{i}") for i in range(2)]
        ps = [S_.psum([128, 512], name=f"psb{i}") for i in range(8)]

        S_.dma("sp", mod[:], mod_d, writes=[mod])
        S_.dma("sp", invf[:], invf_d, writes=[invf])
        S_.dma("sp", rotf[:], rot_d, writes=[rotf])
        S_.op("dve", lambda e: e.tensor_copy(out=rotb[:], in_=rotf[:]), reads=[rotf], writes=[rotb])
        for i in range(2):
            S_.dma("sp", mskf[:], msk_d[i], writes=[mskf])
            S_.op("dve", lambda e, i=i: e.tensor_copy(out=msk[i][:], in_=mskf[:]), reads=[mskf], writes=[msk[i]])
        S_.op("pool", lambda e: e.memset(V[:], 1.0), writes=[V])
        S_.op("pool", lambda e: e.memset(cosT[:], 1.0), writes=[cosT])
        S_.op("pool", lambda e: e.memset(sinT[:], 0.0), writes=[sinT])
        S_.op("dve", lambda e: e.tensor_scalar(out=mod[:, 0:KC], in0=mod[:, 0:KC], scalar1=1.0, scalar2=None,
                                               op0=ALU.add), reads=[mod], writes=[mod])

        for ch in range(S // BLK if do_tab else 0):
            cs = slice(ch * BLK, (ch + 1) * BLK)
            bA, bB, bC, bM = t1[0], t1[1], t2[0], t2[1]
            posi = bA[0:32, :].bitcast(I32)
            ang = bA[0:32, :]
            tq = bB[0:32, :]
            ni = bB[0:32, :].bitcast(I32)
            nf = bB[0:32, :]
            rr = bC[0:32, :]
            mk = bM[0:32, :]
            S_.dma("sp", posi, pos_d[:, cs], writes=[bA])
            S_.op("dve", lambda e: e.tensor_copy(out=ang, in_=posi), reads=[bA], writes=[bA])
            S_.op("dve", lambda e: e.tensor_scalar(out=ang, in0=ang, scalar1=invf[:, 0:1], scalar2=None, op0=ALU.mult),
                  reads=[bA, invf], writes=[bA])
            for which, tab in ((0, sinT), (1, cosT)):
                off = 0.0 if which == 0 else float(np.pi / 2)
                S_.op("dve", lambda e, off=off: e.tensor_scalar(out=tq, in0=ang, scalar1=off, scalar2=float(1 / (2 * np.pi)),
                                                                op0=ALU.add, op1=ALU.mult), reads=[bA], writes=[bB])
                S_.op("dve", lambda e: e.tensor_copy(out=ni, in_=tq), reads=[bB], writes=[bB])
                S_.op("dve", lambda e: e.tensor_copy(out=nf, in_=ni), reads=[bB], writes=[bB])
                S_.op("dve", lambda e, off=off: e.tensor_scalar(out=rr, in0=ang, scalar1=off, scalar2=None, op0=ALU.add),
                      reads=[bA], writes=[bC])
                S_.op("dve", lambda e: e.scalar_tensor_tensor(out=rr, in0=nf, scalar=-TWO_PI_HI, in1=rr,
                                                              op0=ALU.mult, op1=ALU.add), reads=[bC, bB], writes=[bC])
                S_.op("dve", lambda e: e.scalar_tensor_tensor(out=rr, in0=nf, scalar=-TWO_PI_LO, in1=rr,
                                                              op0=ALU.mult, op1=ALU.add), reads=[bC, bB], writes=[bC])
                S_.op("dve", lambda e: e.tensor_scalar(out=mk, in0=rr, scalar1=float(np.pi), scalar2=float(-2 * np.pi),
                                                       op0=ALU.is_gt, op1=ALU.mult), reads=[bC], writes=[bM])
                S_.op("dve", lambda e: e.tensor_tensor(out=rr, in0=rr, in1=mk, op=ALU.add), reads=[bC, bM], writes=[bC])
                S_.op("dve", lambda e: e.tensor_scalar(out=mk, in0=rr, scalar1=float(-np.pi), scalar2=float(2 * np.pi),
                                                       op0=ALU.is_lt, op1=ALU.mult), reads=[bC], writes=[bM])
                S_.op("dve", lambda e: e.tensor_tensor(out=rr, in0=rr, in1=mk, op=ALU.add), reads=[bC, bM], writes=[bC])
                S_.op("dve", lambda e: e.tensor_scalar(out=rr, in0=rr, scalar1=float(np.pi), scalar2=float(-np.pi),
                                                       op0=ALU.min, op1=ALU.max), reads=[bC], writes=[bC])
                S_.op("act", lambda e, tab=tab, cs=cs: e.activation(out=tab[0:32, cs], in_=rr, func=AF.Sin),
                      reads=[bC], writes=[tab])

        pi_ = 0
        pti = 0
        oi = 0
        for hl in range(hpc):
            S_.dma("pool", r32(wqk[:]), wqk_d[hl], writes=[wqk])
            S_.dma("pool", r32(wv[:]), wv_d[hl], writes=[wv])
            wq = wqk[:].rearrange("p (j k c) -> p j k c", j=6, k=KC)
            wvv = wv[:].rearrange("p (k c) -> p k c", k=KC)
            for blk in range(nblk if do_proj else 0):
                x_ = xb[blk % 2]
                S_.dma("pool", r32(x_[:]), xT_d[blk], writes=[x_])
                xv = x_[:].rearrange("p (k t) -> p k t", k=KC)
                for k in range(KC):
                    if k % 2 == 0:
                        S_.op("act", lambda e, k=k, xv=xv: e.activation(
                            out=r32(xv[:, k, :]), in_=xv[:, k, :], func=AF.Identity,
                            scale=mod[:, k:k + 1], bias=mod[:, KC + k:KC + k + 1]), reads=[x_, mod], writes=[x_])
                    else:
                        S_.op("dve", lambda e, k=k, xv=xv: e.tensor_scalar(
                            out=r32(xv[:, k, :]), in0=xv[:, k, :], scalar1=mod[:, k:k + 1],
                            scalar2=mod[:, KC + k:KC + k + 1], op0=ALU.mult, op1=ALU.add),
                            reads=[x_, mod], writes=[x_])
                cs = slice(blk * BLK, (blk + 1) * BLK)
                for j in range(6):
                    g, isk = j // 2, j % 2
                    dst = (KT if isk else QT)[g]
                    pq = ps[pi_ % 4]
                    pr = ps[4 + (pi_ % 2)]
                    pi_ += 1
                    for k in range(KC):
                        S_.op("pe", lambda e, k=k, j=j, pq=pq, xv=xv: e.matmul(
                            pq[:, 0:BLK], lhsT=r32(wq[:, j, k, :]), rhs=r32(xv[:, k, :]),
                            start=(k == 0), stop=(k == KC - 1)), reads=[wqk, x_], writes=[pq])
                    S_.op("act", lambda e, dst=dst, pq=pq, cs=cs: e.activation(
                        out=dst[:, cs], in_=pq[:, 0:BLK], func=AF.Identity), reads=[pq], writes=[dst])
                    if not do_rot:
                        continue
                    S_.op("pe", lambda e, dst=dst, pr=pr, cs=cs: e.matmul(
                        pr[:, 0:BLK], lhsT=rotb[:], rhs=dst[:, cs], start=True, stop=True),
                        reads=[rotb, dst], writes=[pr])
                    a1, a2 = t1[j % 2], t2[j % 2]
                    S_.op("dve", lambda e, pq=pq, a1=a1, cs=cs: e.tensor_tensor(
                        out=a1[:], in0=pq[:, 0:BLK], in1=cosT[:, cs], op=ALU.mult),
                        reads=[pq, cosT, dst], writes=[a1])
                    S_.op("dve", lambda e, pr=pr, a2=a2, cs=cs: e.tensor_tensor(
                        out=a2[:], in0=pr[:, 0:BLK], in1=sinT[:, cs], op=ALU.mult),
                        reads=[pr, sinT], writes=[a2])
                    S_.op("dve", lambda e, dst=dst, a1=a1, a2=a2, cs=cs: e.tensor_tensor(
                        out=dst[:, cs], in0=a1[:], in1=a2[:], op=ALU.add), reads=[a1, a2], writes=[dst])
                for tt in range(BLK // 128 if do_v else 0):
                    pv = ps[6 + tt % 2]
                    for k in range(KC):
                        S_.op("pe", lambda e, k=k, tt=tt, pv=pv, xv=xv: e.matmul(
                            pv[:, 0:384], lhsT=r32(xv[:, k, tt * 128:(tt + 1) * 128]), rhs=r32(wvv[:, k, :]),
                            start=(k == 0), stop=(k == KC - 1)), reads=[x_, wv], writes=[pv])
                    tile_i = blk * (BLK // 128) + tt
                    S_.op("act", lambda e, pv=pv, tile_i=tile_i: e.activation(
                        out=V[:, tile_i, :, 0:HD], in_=pv[:, 0:384].rearrange("p (g c) -> p g c", g=NG),
                        func=AF.Identity), reads=[pv], writes=[V])
            if not do_attn and hl == 0:
                S_.op("dve", lambda e: e.tensor_copy(out=osb[0][:], in_=QT[0][:, 0:HD]), reads=[QT[0], KT[0], V, cosT, sinT], writes=[osb[0]])
                S_.dma("sp", o_d[0, 0], osb[0][:], reads=[osb[0]])
            for qb in range(nqb if do_attn else 0):
                work = []
                for g, d in enumerate(DILS):
                    W_ = RADIUS * d
                    for kt in range(S // 128):
                        dl = kt * 128 - qb * QB
                        if dl - (QB - 1) <= W_ and dl + 127 >= -W_:
                            work.append((g, d, kt, dl))
                po = ps[4:8]
                qs = slice(qb * QB, (qb + 1) * QB)
                for wi, (g, d, kt, dl) in enumerate(work):
                    W_ = RADIUS * d
                    pS = ps[wi % 4]
                    S_.op("pe", lambda e, g=g, kt=kt, pS=pS, qs=qs: e.matmul(
                        pS[:], lhsT=KT[g][:, kt * 128:(kt + 1) * 128], rhs=QT[g][:, qs], start=True, stop=True),
                        reads=[KT[g], QT[g]], writes=[pS])
                    P_ = PT[pti % 4]
                    pti += 1
                    S_.op("act", lambda e, P_=P_, pS=pS: e.activation(out=P_[:], in_=pS[:], func=AF.Exp, scale=SM_SCALE),
                          reads=[pS], writes=[P_])
                    if g > 0:
                        S_.op("dve", lambda e, P_=P_, g=g: e.tensor_tensor(out=P_[:], in0=P_[:], in1=msk[g - 1][:],
                                                                           op=ALU.mult), reads=[P_, msk[g - 1]], writes=[P_])
                    if do_sel and dl - (QB - 1) < -W_:
                        S_.op("pool", lambda e, P_=P_, dl=dl, W_=W_: e.affine_select(
                            out=P_[:], in_=P_[:], pattern=[[-1, QB]], compare_op=ALU.is_ge, fill=0.0,
                            base=dl + W_, channel_multiplier=1), reads=[P_], writes=[P_])
                    if do_sel and dl + 127 > W_:
                        S_.op("pool", lambda e, P_=P_, dl=dl, W_=W_: e.affine_select(
                            out=P_[:], in_=P_[:], pattern=[[1, QB]], compare_op=ALU.is_ge, fill=0.0,
                            base=W_ - dl, channel_multiplier=-1), reads=[P_], writes=[P_])
                    for sub in range(4):
                        S_.op("pe", lambda e, P_=P_, sub=sub, g=g, kt=kt, wi=wi: e.matmul(
                            po[sub][:, 0:HD + 1], lhsT=P_[:, sub * 128:(sub + 1) * 128], rhs=V[:, kt, g, :],
                            start=(wi == 0), stop=(wi == len(work) - 1)), reads=[P_, V], writes=[po[sub]])
                for sub in range(4):
                    ob, rd = osb[oi % 2], rden[oi % 2]
                    oi += 1
                    S_.op("dve", lambda e, rd=rd, sub=sub: e.reciprocal(out=rd[:], in_=po[sub][:, HD:HD + 1]),
                          reads=[po[sub]], writes=[rd])
                    S_.op("dve", lambda e, rd=rd, ob=ob, sub=sub: e.tensor_scalar(
                        out=ob[:], in0=po[sub][:, 0:HD], scalar1=rd[:, 0:1], scalar2=None, op0=ALU.mult),
                        reads=[po[sub], rd], writes=[ob])
                    S_.dma("sp", o_d[hl, qb * 4 + sub], ob[:], reads=[ob])
                    S_.mark_output(ob)
        S_.finish()
    return nc


def rot_matrix():
    R = np.zeros((128, 128), np.float32)
    for i in range(16):
        R[16 + i, i] = -1.0
        R[i, 16 + i] = 1.0
    return R


def run_l2(x, positions, w_qkv, mod0, trace=False):
    if "l2" not in _NC_CACHE:
        _NC_CACHE["l2"] = build_l2()
    nc = _NC_CACHE["l2"]
    invf = (THETA ** (-np.arange(0, ROT, 2, dtype=np.float32) / ROT)).astype(np.float32)
    invf32 = np.concatenate([invf, invf]).reshape(32, 1).astype(np.float32)
    kl = np.arange(128)[:, None]
    ql = np.arange(QB)[None, :]
    msk = np.stack([((kl - ql) % d == 0).astype(np.float32) for d in DILS[1:]])
    rot = rot_matrix()
    w5 = w_qkv.reshape(KC, 128, NG, 3, NH, HD)
    xTs = []
    for b in range(B):
        xt = x[b].T.reshape(KC, 128, NBLK, BLK)
        xTs.append(np.ascontiguousarray(xt.transpose(2, 1, 0, 3)).reshape(NBLK, 128, KC * BLK))
    in_maps = []
    for c in range(NCORES):
        b, hg = c // 4, c % 4
        hs = slice(hg * HPC, (hg + 1) * HPC)
        wqk = w5[:, :, :, 0:2, hs, :]
        wqk = np.ascontiguousarray(wqk.transpose(4, 1, 2, 3, 0, 5)).reshape(HPC, 128, 6 * KC * 128)
        wv = w5[:, :, :, 2, hs, :]
        wv = np.ascontiguousarray(wv.transpose(3, 1, 0, 2, 4)).reshape(HPC, 128, KC * 384)
        shift, scale = mod0[b, 0:D], mod0[b, D:2 * D]
        modc = np.concatenate([scale.reshape(KC, 128).T, shift.reshape(KC, 128).T], axis=1).astype(np.float32)
        pos = np.ascontiguousarray(np.broadcast_to(positions[b].astype(np.int32), (32, S)))
        in_maps.append({"xT": xTs[b], "mod": np.ascontiguousarray(modc), "wqk": wqk, "wv": wv, "pos": pos,
                        "invf": invf32, "rot": rot, "msk": msk})
    res = run_bass_kernel_spmd(nc, in_maps, core_ids=list(range(NCORES)), trace=trace)
    o = np.zeros((B, S, NH, HD), np.float32)
    for c in range(NCORES):
        b, hg = c // 4, c % 4
        oc = res.results[c]["o"].reshape(HPC, S, HD)
        o[b, :, hg * HPC:(hg + 1) * HPC, :] = oc.transpose(1, 0, 2)
    o = o.reshape(NTOK, D)
    if trace:
        return o, res
    return o


def sched_barrier(S_):
    engs = list(S_.eng.keys())
    for en in engs:
        E = S_.eng[en]
        for x in engs:
            if x != en and S_.cnt[x] > 0 and S_.seen[en].get(("e", x), 0) < S_.cnt[x]:
                E.wait_ge(S_.sem[x], S_.cnt[x])
                S_.seen[en][("e", x)] = S_.cnt[x]
        for b in S_.out_bufs:
            if b.dsem is not None and S_.seen[en].get(("d", b.dsem), 0) < b.dcnt:
                E.wait_ge(b.dsem, b.dcnt)
                S_.seen[en][("d", b.dsem)] = b.dcnt


GC = 512
NST = S // 128


def build_l4():
    nc = bass.Bass("TRN2", target_bir_lowering=False)
    xT_d = nc.dram_tensor("xT", [NBLK, 128, KC * BLK], F32R, kind="ExternalInput").ap()
    mod_d = nc.dram_tensor("mod", [128, 2 * KC], F32, kind="ExternalInput").ap()
    win_d = nc.dram_tensor("win", [128, KC * GC], F32R, kind="ExternalInput").ap()
    cc_d = nc.dram_tensor("cc", [2, 128, 4 * GC], F32R, kind="ExternalInput").ap()
    dft_d = nc.dram_tensor("dft", [S // QB, 16, 128, 4 * QB], F32R, kind="ExternalInput").ap()
    y_d = nc.dram_tensor("yT", [4, 128, S], F32, kind="ExternalOutput").ap()
    with ExitStack() as es:
        S_ = Sched(nc, es)
        AB = [S_.sbuf([128, NST, GC], name=f"AB{i}") for i in range(2)]
        win = S_.sbuf([128, KC * GC], name="win_sb")
        xb = [S_.sbuf([128, KC * BLK], name=f"xb{i}") for i in range(1)]
        uT = S_.sbuf([128, 4, BLK], name="uT")
        ccs = [S_.sbuf([128, 4 * GC], name=f"ccs{i}") for i in range(2)]
        mod = S_.sbuf([128, 2 * KC], name="mod_sb")
        ysb = [S_.sbuf([128, QB], name=f"ysb{i}") for i in range(2)]
        ps = [S_.psum([128, 512], name=f"psb{i}") for i in range(8)]
        S_.dma("sp", mod[:], mod_d, writes=[mod])
        S_.op("dve", lambda e: e.tensor_scalar(out=mod[:, 0:KC], in0=mod[:, 0:KC], scalar1=1.0, scalar2=None,
                                               op0=ALU.add), reads=[mod], writes=[mod])
        S_.dma("pool", r32(win[:]), win_d, writes=[win])
        for i in range(2):
            S_.dma("pool", r32(ccs[i][:]), cc_d[i], writes=[ccs[i]])
        wv = win[:].rearrange("p (k c) -> p k c", k=KC)
        pi_ = 0
        for blk in range(NBLK):
            x_ = xb[0]
            S_.dma("pool", r32(x_[:]), xT_d[blk], writes=[x_])
            xv = x_[:].rearrange("p (k t) -> p k t", k=KC)
            for k in range(KC):
                if k % 2 == 0:
                    S_.op("act", lambda e, k=k, xv=xv: e.activation(
                        out=r32(xv[:, k, :]), in_=xv[:, k, :], func=AF.Identity,
                        scale=mod[:, k:k + 1], bias=mod[:, KC + k:KC + k + 1]), reads=[x_, mod], writes=[x_])
                else:
                    S_.op("dve", lambda e, k=k, xv=xv: e.tensor_scalar(
                        out=r32(xv[:, k, :]), in0=xv[:, k, :], scalar1=mod[:, k:k + 1],
                        scalar2=mod[:, KC + k:KC + k + 1], op0=ALU.mult, op1=ALU.add),
                        reads=[x_, mod], writes=[x_])
            for cc in range(4):
                pu = ps[pi_ % 4]
                pi_ += 1
                for k in range(KC):
                    S_.op("pe", lambda e, k=k, cc=cc, pu=pu, xv=xv: e.matmul(
                        pu[:, 0:BLK], lhsT=r32(wv[:, k, cc * 128:(cc + 1) * 128]), rhs=r32(xv[:, k, :]),
                        start=(k == 0), stop=(k == KC - 1)), reads=[win, x_], writes=[pu])
                S_.op("act", lambda e, cc=cc, pu=pu: e.activation(out=r32(uT[:, cc, :]), in_=pu[:, 0:BLK],
                                                                  func=AF.Identity), reads=[pu], writes=[uT])
            for tt in range(BLK // 128):
                st = blk * (BLK // 128) + tt
                for i in range(2):
                    pa = ps[4 + (2 * tt + i) % 4]
                    cv = ccs[i][:].rearrange("p (c m) -> p c m", c=4)
                    for cc in range(4):
                        S_.op("pe", lambda e, cc=cc, tt=tt, pa=pa, cv=cv: e.matmul(
                            pa[:], lhsT=r32(uT[:, cc, tt * 128:(tt + 1) * 128]), rhs=r32(cv[:, cc, :]),
                            start=(cc == 0), stop=(cc == 3)), reads=[uT, ccs[i]], writes=[pa])
                    if i == 0:
                        S_.op("act", lambda e, pa=pa, st=st: e.activation(out=r32(AB[0][:, st, :]), in_=pa[:],
                                                                          func=AF.Identity), reads=[pa], writes=[AB[0]])
                    else:
                        S_.op("dve", lambda e, pa=pa, st=st: e.tensor_copy(out=r32(AB[1][:, st, :]), in_=pa[:]),
                              reads=[pa], writes=[AB[1]])
        sched_barrier(S_)
        dpc = [Buf(win.t, f"dpc{i}") for i in range(4)]
        yi = 0
        di = 0
        for kb in range(S // QB):
            for pc in range(16):
                sg, cs_ = pc // 2, pc % 2
                db = dpc[di % 4]
                dcol = slice((di % 4) * 2048, (di % 4 + 1) * 2048)
                di += 1
                S_.dma("pool", r32(win.t[:, dcol]), dft_d[kb, pc], writes=[db])
                dv = win.t[:, dcol].rearrange("p (s k) -> p s k", s=4)
                for s4 in range(4):
                    st = sg * 4 + s4
                    for mc in range(4):
                        first = (pc == 0 and s4 == 0)
                        last = (pc == 15 and s4 == 3)
                        S_.op("pe", lambda e, mc=mc, st=st, s4=s4, cs_=cs_, dv=dv, first=first, last=last: e.matmul(
                            ps[mc][:], lhsT=r32(AB[cs_][:, st, mc * 128:(mc + 1) * 128]), rhs=r32(dv[:, s4, :]),
                            start=first, stop=last), reads=[AB[cs_], db], writes=[ps[mc]])
            for mc in range(4):
                yb = ysb[yi % 2]
                yi += 1
                if mc % 2 == 0:
                    S_.op("act", lambda e, mc=mc, yb=yb: e.activation(out=yb[:], in_=ps[mc][:], func=AF.Identity),
                          reads=[ps[mc]], writes=[yb])
                else:
                    S_.op("dve", lambda e, mc=mc, yb=yb: e.tensor_copy(out=yb[:], in_=ps[mc][:]),
                          reads=[ps[mc]], writes=[yb])
                S_.dma("sp", y_d[mc, :, kb * QB:(kb + 1) * QB], yb[:], reads=[yb])
        S_.finish()
    return nc


def dft_tables():
    n = np.arange(S)
    angS = 2 * np.pi * ((n[:, None] * n[None, :]) % S) / S
    CS = (np.cos(angS) / np.sqrt(S)).astype(np.float32)
    SS = (-np.sin(angS) / np.sqrt(S)).astype(np.float32)
    m = np.arange(GC)
    angC = 2 * np.pi * ((m[:, None] * m[None, :]) % GC) / GC
    CC = (np.cos(angC) / np.sqrt(GC)).astype(np.float32)
    SC = (np.sin(angC) / np.sqrt(GC)).astype(np.float32)
    M = np.stack([CS, SS])
    M = M.reshape(2, 8, 4, 128, S // QB, QB)
    dft = np.ascontiguousarray(M.transpose(4, 1, 0, 3, 2, 5)).reshape(S // QB, 16, 128, 4 * QB)
    cc = np.stack([CC, SC]).reshape(2, 4, 128, GC)
    cc = np.ascontiguousarray(cc.transpose(0, 2, 1, 3)).reshape(2, 128, 4 * GC)
    return dft, cc


def run_l4(x_tok, w_in, mod0):
    if "l4" not in _NC_CACHE:
        _NC_CACHE["l4"] = build_l4()
    nc = _NC_CACHE["l4"]
    dft, cc = dft_tables()
    x = x_tok.reshape(B, S, D)
    xTs = []
    for b in range(B):
        xt = x[b].T.reshape(KC, 128, NBLK, BLK)
        xTs.append(np.ascontiguousarray(xt.transpose(2, 1, 0, 3)).reshape(NBLK, 128, KC * BLK))
    in_maps = []
    for c in range(NCORES):
        b, g = c // 4, c % 4
        wi = w_in[:, g * GC:(g + 1) * GC].reshape(KC, 128, GC)
        wi = np.ascontiguousarray(wi.transpose(1, 0, 2)).reshape(128, KC * GC)
        shift, scale = mod0[b, 0:D], mod0[b, D:2 * D]
        modc = np.ascontiguousarray(np.concatenate([scale.reshape(KC, 128).T, shift.reshape(KC, 128).T], axis=1))
        in_maps.append({"xT": xTs[b], "mod": modc.astype(np.float32), "win": wi, "cc": cc, "dft": dft})
    res = run_bass_kernel_spmd(nc, in_maps, core_ids=list(range(NCORES)))
    y = np.zeros((B, S, D), np.float32)
    for c in range(NCORES):
        b, g = c // 4, c % 4
        yT = res.results[c]["yT"].reshape(GC, S)
        y[b, :, g * GC:(g + 1) * GC] = yT.T
    return y.reshape(NTOK, D)


def kernel(x, c, positions, ada_w, ada_b, attn_w_qkv, attn_w_o, fnet_w_in, fnet_w_out,
           ln_g, ln_b, router_coarse_w, router_coarse_b, router_fine_w, router_fine_b,
           expert_w_gate, expert_w_up, expert_w_down):
    f = lambda a: np.asarray(a, dtype=np.float32)
    x, c = f(x), f(c)
    positions = np.asarray(positions).astype(np.int32)
    ada_w, ada_b = f(ada_w), f(ada_b)
    ln_g, ln_b = f(ln_g), f(ln_b)
    mod = run_l1(c, ada_w, ada_b)
    x_tok = x.reshape(NTOK, D)
    for i in range(2):
        m0, m1 = mod[i, 0], mod[i, 1]
        if i == 0:
            a_tok = run_l2(x, positions, f(attn_w_qkv[0]), m0)
            wp = f(attn_w_o[0])
        else:
            a_tok = run_l4(x_tok, f(fnet_w_in[0]), m0)
            wp = f(fnet_w_out[0])
        rows = [[m0[b, 2 * D:3 * D], ln_g[i, 0], ln_b[i, 0], m1[b, D:2 * D], m1[b, 0:D], m1[b, 2 * D:3 * D],
                 ln_g[i, 1], ln_b[i, 1]] for b in range(B)]
        wgu, wdp = pack_experts(f(expert_w_gate[i]), f(expert_w_up[i]), f(expert_w_down[i]))
        x_tok = run_l3(a_tok, wp, x_tok, rows, f(router_coarse_w[i]), f(router_coarse_b[i]),
                       f(router_fine_w[i]), f(router_fine_b[i]), wgu, wdp)
        del wgu, wdp
    return x_tok.reshape(B, S, D).astype(np.float32)


TB = 1024
TTB = TB // 128
DMA_CAST = dict(max_dma_last_dim=4096)


def build_l3b(n_experts=NE):
    nc = bass.Bass("TRN2", target_bir_lowering=False)
    aT_d = nc.dram_tensor("aT", [128, KC, TB], F32, kind="ExternalInput").ap()
    wp_d = nc.dram_tensor("wp", [4, 128, KC * 512], F32, kind="ExternalInput").ap()
    x_d = nc.dram_tensor("x", [TTB, 128, D], F32, kind="ExternalInput").ap()
    par_d = nc.dram_tensor("par", [8, 128, D], F32, kind="ExternalInput").ap()
    wr_d = nc.dram_tensor("wr", [128, KC, 36], F32, kind="ExternalInput").ap()
    br_d = nc.dram_tensor("br", [128, 36], F32, kind="ExternalInput").ap()
    wgu_d = nc.dram_tensor("wgu", [n_experts * 4, 128, 2 * KC * 128], F32, kind="ExternalInput").ap()
    wd_d = nc.dram_tensor("wd", [n_experts, 128, 4 * D], F32, kind="ExternalInput").ap()
    id_d = nc.dram_tensor("ident", [128, 128], F32, kind="ExternalInput").ap()
    out_d = nc.dram_tensor("out", [TTB, 128, D], F32, kind="ExternalOutput").ap()

    with ExitStack() as es:
        S_ = Sched(nc, es)
        hT = S_.sbuf([128, KC, TB], BF16, name="hT")
        acc = [S_.sbuf([128, D], name=f"acc{t}") for t in range(TTB)]
        wdb = [S_.sbuf([128, 4 * D], BF16, name=f"wdb{i}") for i in range(2)]
        gub = [S_.sbuf([128, 2 * KC * 128], BF16, name=f"gub{i}") for i in range(2)]
        aTb = [S_.sbuf([128, TB], BF16, name=f"aTb{i}") for i in range(8)]
        par = [S_.sbuf([128, D], name=f"par{i}") for i in range(3)]
        scr = S_.sbuf([128, D], name="scr")
        sh2 = par[1]
        hTfb = par[2]
        tmpy = [S_.sbuf([128, 512], name=f"tmpy{i}") for i in range(2)]
        wr = S_.sbuf([128, KC, 36], name="wr_sb")
        br = S_.sbuf([128, 36], name="br_sb")
        ident = S_.sbuf([128, 128], name="ident_sb")
        G = S_.sbuf([128, TTB, NE], name="G")
        sm = [S_.sbuf([128, 64], name=f"sm{i}") for i in range(2)]
        ps = [S_.psum([128, 512], name=f"psb{i}") for i in range(8)]
        pgu, py = ps[0:4], ps[4:8]

        S_.dma("sp", wr[:], wr_d, writes=[wr])
        S_.dma("sp", br[:], br_d, writes=[br])
        S_.dma("sp", ident[:], id_d, writes=[ident])

        def ln_tile(xb, gb, gap, bb, bap, smb):
            s1, s2, mean, msq, var, std, rstd, nmr = [smb[:, i:i + 1] for i in range(8)]
            S_.op("act", lambda e: e.activation(out=scr[:], in_=xb[:], func=AF.Identity, accum_out=s1),
                  reads=[xb], writes=[scr, smb])
            S_.op("act", lambda e: e.activation(out=scr[:], in_=xb[:], func=AF.Square, accum_out=s2),
                  reads=[xb], writes=[scr, smb])
            S_.op("dve", lambda e: e.tensor_scalar(out=mean, in0=s1, scalar1=1.0 / D, scalar2=None, op0=ALU.mult),
                  reads=[smb], writes=[smb])
            S_.op("dve", lambda e: e.tensor_tensor(out=msq, in0=mean, in1=mean, op=ALU.mult),
                  reads=[smb], writes=[smb])
            S_.op("dve", lambda e: e.scalar_tensor_tensor(out=var, in0=s2, scalar=1.0 / D, in1=msq,
                                                          op0=ALU.mult, op1=ALU.subtract),
                  reads=[smb], writes=[smb])
            S_.op("dve", lambda e: e.tensor_scalar(out=var, in0=var, scalar1=EPS, scalar2=None, op0=ALU.add),
                  reads=[smb], writes=[smb])
            S_.op("act", lambda e: e.activation(out=std, in_=var, func=AF.Sqrt), reads=[smb], writes=[smb])
            S_.op("dve", lambda e: e.reciprocal(out=rstd, in_=std), reads=[smb], writes=[smb])
            S_.op("dve", lambda e: e.tensor_scalar(out=nmr, in0=mean, scalar1=rstd, scalar2=-1.0,
                                                   op0=ALU.mult, op1=ALU.mult), reads=[smb], writes=[smb])
            S_.op("act", lambda e: e.activation(out=xb[:], in_=xb[:], func=AF.Identity, scale=rstd, bias=nmr),
                  reads=[xb, smb], writes=[xb])
            S_.op("pool", lambda e: e.tensor_tensor(out=xb[:], in0=xb[:], in1=gap, op=ALU.mult),
                  reads=[xb, gb], writes=[xb])
            S_.op("pool", lambda e: e.tensor_tensor(out=xb[:], in0=xb[:], in1=bap, op=ALU.add),
                  reads=[xb, bb], writes=[xb])

        pyi = 0
        for k in range(KC):
            S_.dma("pool", hT[:, k, :], aT_d[:, k, :], writes=[hT], **DMA_CAST)
        for t in range(TTB):
            S_.dma("sp", acc[t][:], x_d[t], writes=[acc[t]])
        S_.dma("sp", par[0][:], par_d[P_GATE1], writes=[par[0]])
        S_.dma("sp", par[1][:], par_d[P_LNG1], writes=[par[1]])
        S_.dma("sp", par[2][:], par_d[P_LNB1], writes=[par[2]])
        for n in range(4):
            wb = wdb[n % 2]
            S_.dma("pool", wb[:], wp_d[n], writes=[wb], **DMA_CAST)
            wv = wb[:].rearrange("p (k c) -> p k c", k=KC)
            for t in range(TTB):
                pb = py[pyi % 4]
                pyi += 1
                for k in range(KC):
                    S_.op("pe", lambda e, k=k, t=t, pb=pb, wv=wv: e.matmul(
                        pb[:], lhsT=hT[:, k, t * 128:(t + 1) * 128], rhs=wv[:, k, :],
                        start=(k == 0), stop=(k == KC - 1)), reads=[hT, wb], writes=[pb])
                cs = slice(n * 512, (n + 1) * 512)
                sg = tmpy[(n * TTB + t) % 2]
                S_.op("dve", lambda e, pb=pb, sg=sg, cs=cs: e.tensor_tensor(
                    out=sg[:], in0=pb[:], in1=par[0][:, cs], op=ALU.mult),
                    reads=[pb, par[0]], writes=[sg])
                S_.op("pool", lambda e, t=t, sg=sg, cs=cs: e.scalar_tensor_tensor(
                    out=acc[t][:, cs], in0=acc[t][:, cs], scalar=ALPHA, in1=sg[:],
                    op0=ALU.mult, op1=ALU.add), reads=[acc[t], sg], writes=[acc[t]]) if False else \
                    S_.op("dve", lambda e, t=t, sg=sg, cs=cs: e.scalar_tensor_tensor(
                        out=acc[t][:, cs], in0=acc[t][:, cs], scalar=ALPHA, in1=sg[:],
                        op0=ALU.mult, op1=ALU.add), reads=[acc[t], sg], writes=[acc[t]])
        for t in range(TTB):
            ln_tile(acc[t], par[1], par[1][:], par[2], par[2][:], sm[t % 2])
        S_.dma("sp", par[0][:], par_d[P_SC2], writes=[par[0]])
        S_.dma("sp", sh2[:], par_d[P_SH2], writes=[sh2])
        S_.op("pool", lambda e: e.tensor_scalar(out=par[0][:], in0=par[0][:], scalar1=1.0, scalar2=None,
                                                op0=ALU.add), reads=[par[0]], writes=[par[0]])
        for t in range(TTB):
            S_.op("dve", lambda e, t=t: e.tensor_tensor(out=scr[:], in0=acc[t][:], in1=par[0][:], op=ALU.mult),
                  reads=[acc[t], par[0]], writes=[scr])
            S_.op("dve", lambda e: e.tensor_tensor(out=scr[:], in0=scr[:], in1=sh2[:], op=ALU.add),
                  reads=[scr, sh2], writes=[scr])
            S_.op("pool", lambda e, t=t: e.tensor_scalar(out=acc[t][:], in0=acc[t][:], scalar1=ALPHA,
                                                         scalar2=None, op0=ALU.mult),
                  reads=[acc[t]], writes=[acc[t]])
            for k0 in range(0, KC, 4):
                pb = py[pyi % 4]
                pyi += 1
                for j in range(4):
                    k = k0 + j
                    S_.op("pe", lambda e, j=j, k=k, pb=pb: e.transpose(
                        out=pb[:, j * 128:(j + 1) * 128], in_=scr[:, k * 128:(k + 1) * 128],
                        identity=ident[:]), reads=[scr, ident], writes=[pb])
                S_.op("act", lambda e, k0=k0, t=t, pb=pb: e.activation(
                    out=hT[:, k0:k0 + 4, t * 128:(t + 1) * 128],
                    in_=pb[:].rearrange("p (a b) -> p a b", a=4), func=AF.Identity),
                    reads=[pb], writes=[hT])
                S_.op("act", lambda e, k0=k0, pb=pb: e.activation(
                    out=hTfb[:].rearrange("p (k t) -> p k t", k=KC)[:, k0:k0 + 4, :],
                    in_=pb[:].rearrange("p (a b) -> p a b", a=4), func=AF.Identity),
                    reads=[pb], writes=[hTfb])
            pb = py[pyi % 4]
            pyi += 1
            for k in range(KC):
                S_.op("pe", lambda e, k=k, pb=pb: e.matmul(
                    pb[:, 0:36], lhsT=hTfb[:, k * 128:(k + 1) * 128], rhs=wr[:, k, :],
                    start=(k == 0), stop=(k == KC - 1)), reads=[hTfb, wr], writes=[pb])
            s = sm[t % 2]
            lg = s[:, 0:36]
            m4, nm4, s4, pgp = s[:, 36:37], s[:, 37:38], s[:, 38:39], s[:, 39:40]
            oh4 = s[:, 40:44]
            e4 = s[:, 44:48]
            sel = s[:, 48:56]
            S_.op("dve", lambda e: e.tensor_tensor(out=lg, in0=pb[:, 0:36], in1=br[:], op=ALU.add),
                  reads=[pb, br], writes=[s])
            S_.op("dve", lambda e: e.reduce_max(out=m4, in_=s[:, 0:4], axis=AX.X), reads=[s], writes=[s])
            S_.op("dve", lambda e: e.tensor_scalar(out=nm4, in0=m4, scalar1=-1.0, scalar2=None, op0=ALU.mult),
                  reads=[s], writes=[s])
            S_.op("act", lambda e: e.activation(out=e4, in_=s[:, 0:4], func=AF.Exp, bias=nm4, accum_out=s4),
                  reads=[s], writes=[s])
            S_.op("dve", lambda e: e.reciprocal(out=pgp, in_=s4), reads=[s], writes=[s])
            S_.op("dve", lambda e: e.tensor_scalar(out=oh4, in0=s[:, 0:4], scalar1=m4, scalar2=None,
                                                   op0=ALU.is_equal), reads=[s], writes=[s])
            S_.op("dve", lambda e: e.tensor_scalar(out=sel, in0=s[:, 4:12], scalar1=s[:, 40:41], scalar2=None,
                                                   op0=ALU.mult), reads=[s], writes=[s])
            for g in range(1, 4):
                S_.op("dve", lambda e, g=g: e.scalar_tensor_tensor(
                    out=sel, in0=s[:, 4 + 8 * g:12 + 8 * g], scalar=s[:, 40 + g:41 + g], in1=sel,
                    op0=ALU.mult, op1=ALU.add), reads=[s], writes=[s])
            m1, m2, nm1, dd, p1, p2 = [s[:, 56 + i:57 + i] for i in range(6)]
            g1, g2 = s[:, 62:63], s[:, 63:64]
            o1 = G[:, t, 0:8]
            o2 = G[:, t, 8:16]
            sel2 = G[:, t, 16:24]
            g8 = G[:, t, 24:32]
            S_.op("dve", lambda e: e.reduce_max(out=m1, in_=sel, axis=AX.X), reads=[s], writes=[s])
            S_.op("dve", lambda e: e.tensor_scalar(out=o1, in0=sel, scalar1=m1, scalar2=None, op0=ALU.is_equal),
                  reads=[s], writes=[G])
            S_.op("dve", lambda e: e.scalar_tensor_tensor(out=sel2, in0=o1, scalar=-1e30, in1=sel,
                                                          op0=ALU.mult, op1=ALU.add), reads=[s, G], writes=[G])
            S_.op("dve", lambda e: e.reduce_max(out=m2, in_=sel2, axis=AX.X), reads=[G], writes=[s])
            S_.op("dve", lambda e: e.tensor_scalar(out=o2, in0=sel2, scalar1=m2, scalar2=None, op0=ALU.is_equal),
                  reads=[s, G], writes=[G])
            S_.op("dve", lambda e: e.tensor_scalar(out=nm1, in0=m1, scalar1=-1.0, scalar2=None, op0=ALU.mult),
                  reads=[s], writes=[s])
            S_.op("act", lambda e: e.activation(out=dd, in_=m2, func=AF.Exp, bias=nm1), reads=[s], writes=[s])
            S_.op("dve", lambda e: e.tensor_scalar(out=p1, in0=dd, scalar1=1.0, scalar2=None, op0=ALU.add),
                  reads=[s], writes=[s])
            S_.op("dve", lambda e: e.reciprocal(out=p1, in_=p1), reads=[s], writes=[s])
            S_.op("dve", lambda e: e.tensor_tensor(out=p2, in0=dd, in1=p1, op=ALU.mult), reads=[s], writes=[s])
            S_.op("dve", lambda e: e.tensor_tensor(out=g1, in0=p1, in1=pgp, op=ALU.mult), reads=[s], writes=[s])
            S_.op("dve", lambda e: e.tensor_tensor(out=g2, in0=p2, in1=pgp, op=ALU.mult), reads=[s], writes=[s])
            S_.op("dve", lambda e: e.tensor_scalar(out=g8, in0=o1, scalar1=g1, scalar2=None, op0=ALU.mult),
                  reads=[s, G], writes=[G])
            S_.op("dve", lambda e: e.scalar_tensor_tensor(out=g8, in0=o2, scalar=g2, in1=g8,
                                                          op0=ALU.mult, op1=ALU.add), reads=[s, G], writes=[G])
            S_.op("dve", lambda e: e.tensor_copy(out=sel, in_=g8), reads=[G], writes=[s])
            for g in range(4):
                S_.op("dve", lambda e, g=g: e.tensor_scalar(
                    out=G[:, t, 8 * g:8 * g + 8], in0=sel, scalar1=s[:, 40 + g:41 + g], scalar2=None,
                    op0=ALU.mult), reads=[s], writes=[G])
        S_.dma("sp", par[0][:], par_d[P_GATE2], writes=[par[0]])
        ui = 0
        ti = 0
        stage = [par[1], par[2], scr]
        sctr = [0]

        def load_cast(dst_ap, dst_buf, src_ap, ceng):
            st = stage[sctr[0] % 3]
            sctr[0] += 1
            S_.dma("sp", st[:], src_ap, writes=[st])
            if ceng == "act":
                S_.op("act", lambda e: e.activation(out=dst_ap, in_=st[:], func=AF.Identity), reads=[st], writes=[dst_buf])
            else:
                S_.op(ceng, lambda e: e.tensor_copy(out=dst_ap, in_=st[:]), reads=[st], writes=[dst_buf])

        for ex in range(n_experts):
            wb = wdb[ex % 2]
            for hc in range(4):
                load_cast(wb[:, hc * D:(hc + 1) * D], wb, wd_d[ex][:, hc * D:(hc + 1) * D], "pool")
            for hc in range(4):
                gb = gub[ui % 2]
                ceng = "act" if ui % 2 == 0 else "dve"
                for j in range(2):
                    load_cast(gb[:, j * 2048:(j + 1) * 2048], gb, wgu_d[ex * 4 + hc][:, j * 2048:(j + 1) * 2048], ceng)
                gv = gb[:].rearrange("p (j k c) -> p j k c", j=2, k=KC)
                ab = aTb[(ex % 2) * 4 + hc]
                for half in range(TB // 512):
                    ts_ = slice(half * 512, (half + 1) * 512)
                    pgb, pub = pgu[(2 * ui + half) % 2 * 2], pgu[(2 * ui + half) % 2 * 2 + 1]
                    for k in range(KC):
                        S_.op("pe", lambda e, k=k, gv=gv, pgb=pgb, ts_=ts_: e.matmul(
                            pgb[:], lhsT=gv[:, 0, k, :], rhs=hT[:, k, ts_],
                            start=(k == 0), stop=(k == KC - 1)), reads=[gb, hT], writes=[pgb])
                    for k in range(KC):
                        S_.op("pe", lambda e, k=k, gv=gv, pub=pub, ts_=ts_: e.matmul(
                            pub[:], lhsT=gv[:, 1, k, :], rhs=hT[:, k, ts_],
                            start=(k == 0), stop=(k == KC - 1)), reads=[gb, hT], writes=[pub])
                    sg = tmpy[ti % 2]
                    ti += 1
                    S_.op("act", lambda e, sg=sg, pgb=pgb: e.activation(out=sg[:], in_=pgb[:], func=AF.Silu),
                          reads=[pgb], writes=[sg])
                    S_.op("dve", lambda e, pub=pub, ab=ab, sg=sg, ts_=ts_: e.tensor_tensor(
                        out=ab[:, ts_], in0=sg[:], in1=pub[:], op=ALU.mult), reads=[sg, pub], writes=[ab])
                ui += 1
            abs_ = [aTb[(ex % 2) * 4 + hc] for hc in range(4)]
            for t in range(TTB):
                for n in range(4):
                    pb = py[pyi % 4]
                    pyi += 1
                    for hc in range(4):
                        S_.op("pe", lambda e, hc=hc, t=t, n=n, pb=pb, wb=wb, abs_=abs_: e.matmul(
                            pb[:], lhsT=abs_[hc][:, t * 128:(t + 1) * 128],
                            rhs=wb[:, hc * D + n * 512: hc * D + (n + 1) * 512],
                            start=(hc == 0), stop=(hc == 3)), reads=[abs_[hc], wb], writes=[pb])
                    cs = slice(n * 512, (n + 1) * 512)
                    yt = tmpy[ti % 2]
                    ti += 1
                    S_.op("act", lambda e, pb=pb, yt=yt, t=t, ex=ex: e.activation(
                        out=yt[:], in_=pb[:], func=AF.Identity, scale=G[:, t, ex:ex + 1]), reads=[pb, G], writes=[yt])
                    S_.op("dve", lambda e, yt=yt, cs=cs: e.tensor_tensor(
                        out=yt[:], in0=yt[:], in1=par[0][:, cs], op=ALU.mult), reads=[yt, par[0]], writes=[yt])
                    S_.op("pool", lambda e, t=t, cs=cs, yt=yt: e.tensor_tensor(
                        out=acc[t][:, cs], in0=acc[t][:, cs], in1=yt[:], op=ALU.add),
                        reads=[yt, acc[t]], writes=[acc[t]])
        S_.dma("sp", par[1][:], par_d[P_LNG2], writes=[par[1]])
        S_.dma("sp", par[2][:], par_d[P_LNB2], writes=[par[2]])
        for t in range(TTB):
            ln_tile(acc[t], par[1], par[1][:], par[2], par[2][:], sm[t % 2])
            S_.dma("act", out_d[t], acc[t][:], reads=[acc[t]])
        S_.finish()
    return nc


def run_l3b(a_tok, wp, x_tok, rows, wrc, brc, wrf, brf, wgu, wdp, trace=False):
    if "l3b" not in _NC_CACHE:
        _NC_CACHE["l3b"] = build_l3b()
    nc = _NC_CACHE["l3b"]
    wpp = np.ascontiguousarray(wp.reshape(KC, 128, 4, 512).transpose(2, 1, 0, 3)).reshape(4, 128, KC * 512)
    wr = np.ascontiguousarray(np.concatenate([wrc, wrf], axis=1).reshape(KC, 128, 36).transpose(1, 0, 2))
    br = bc128(np.concatenate([brc, brf]))
    ident = np.eye(128, dtype=np.float32)
    pars = [np.ascontiguousarray(np.stack([bc128(v) for v in rows[b]])) for b in range(B)]
    in_maps = []
    TC = NTOK // NCORES
    for c in range(NCORES):
        b = c // (NCORES // B)
        a_c = a_tok[c * TC:(c + 1) * TC]
        aT = np.ascontiguousarray(a_c.reshape(TB, KC, 128).transpose(2, 1, 0))
        xc = np.ascontiguousarray(x_tok[c * TC:(c + 1) * TC]).reshape(TTB, 128, D)
        in_maps.append({"aT": aT, "wp": wpp, "x": xc, "par": pars[b], "wr": wr, "br": br,
                        "wgu": wgu, "wd": wdp, "ident": ident})
    res = run_bass_kernel_spmd(nc, in_maps, core_ids=list(range(NCORES)), trace=trace)
    out = np.concatenate([res.results[c]["out"].reshape(TC, D) for c in range(NCORES)], axis=0)
    if trace:
        return out, res
    return out
```

```python
import numpy as np
from contextlib import ExitStack
import concourse.bass as bass
import concourse.mybir as mybir
from concourse.bass_utils import run_bass_kernel_spmd

F32 = mybir.dt.float32
F32R = mybir.dt.float32r
BF16 = mybir.dt.bfloat16
I32 = mybir.dt.int32
AF = mybir.ActivationFunctionType
ALU = mybir.AluOpType
AX = mybir.AxisListType

NCORES = 8
D = 2048
KC = D // 128
B = 2
S = 4096
NTOK = B * S
HD = 128
NH = 16
NG = 3
DILS = (1, 4, 16)
RADIUS = 64
ROT = 32
THETA = 500000.0
NE = 32
EPG = 8
NGRP = 4
ED = 512
EPS = 1e-5
ALPHA = 4.0 ** 0.25

SAME_ENG_SYNC = True


class Buf:
    __slots__ = ("t", "name", "w", "r", "dsem", "dcnt")

    def __init__(self, t, name):
        self.t = t
        self.name = name
        self.w = None
        self.r = []
        self.dsem = None
        self.dcnt = 0

    def __getitem__(self, k):
        return self.t[k]


class Sched:
    def __init__(self, nc, es):
        self.nc = nc
        self.es = es
        self.eng = {"pe": nc.tensor, "dve": nc.vector, "act": nc.scalar,
                    "pool": nc.gpsimd, "sp": nc.sync}
        self.sem = {}
        self.cnt = {}
        self.seen = {}
        for k in self.eng:
            self.sem[k] = es.enter_context(nc.semaphore("s_" + k))
            self.cnt[k] = 0
            self.seen[k] = {}
        self.nbuf = 0
        self.out_bufs = []

    def sbuf(self, shape, dtype=F32, name=None):
        self.nbuf += 1
        name = name or f"sb{self.nbuf}"
        t = self.es.enter_context(self.nc.sbuf_tensor(name, list(shape), dtype))
        return Buf(t, name)

    def psum(self, shape, dtype=F32, name=None):
        self.nbuf += 1
        name = name or f"ps{self.nbuf}"
        t = self.es.enter_context(self.nc.psum_tensor(name, list(shape), dtype))
        return Buf(t, name)

    def view(self, name="v"):
        return Buf(None, name)

    def _collect(self, eng, reads, writes):
        need = {}

        def add(dep, raw):
            if dep is None:
                return
            if dep[0] == "dma":
                key = ("d", dep[1])
                sem, val = dep[1], dep[2]
            else:
                e2, val = dep
                if e2 == eng:
                    if eng == "pe" or not raw or not SAME_ENG_SYNC:
                        return
                key = ("e", e2)
                sem = self.sem[e2]
            if need.get(key, (None, 0))[1] < val:
                need[key] = (sem, val)

        for b in reads:
            add(b.w, True)
        for b in writes:
            add(b.w, True)
            for r in b.r:
                add(r, False)
        return need

    def _emit_waits(self, eng, need):
        E = self.eng[eng]
        seen = self.seen[eng]
        for key, (sem, val) in need.items():
            if seen.get(key, 0) < val:
                E.wait_ge(sem, val)
                seen[key] = val

    def op(self, eng, fn, reads=(), writes=()):
        need = self._collect(eng, reads, writes)
        self._emit_waits(eng, need)
        ins = fn(self.eng[eng])
        self.cnt[eng] += 1
        ins.then_inc(self.sem[eng], 1)
        me = (eng, self.cnt[eng])
        for b in reads:
            b.r.append(me)
        for b in writes:
            b.w = me
            b.r = []
        return ins

    def dma(self, q, out, in_, reads=(), writes=(), track=None, **kw):
        need = self._collect("dmaq_" + q, reads, writes)
        self._emit_waits(q, need)
        b = track or (writes[0] if writes else reads[0])
        if b.dsem is None:
            b.dsem = self.es.enter_context(self.nc.semaphore("d_" + b.name))
            self.out_bufs.append(b)
        ins = self.eng[q].dma_start(out=out, in_=in_, **kw)
        b.dcnt += 16
        ins.then_inc(b.dsem, 16)
        me = ("dma", b.dsem, b.dcnt)
        for x in reads:
            x.r.append(me)
        for x in writes:
            x.w = me
            x.r = []
        return ins

    def mark_output(self, b):
        if b not in self.out_bufs:
            self.out_bufs.append(b)

    def finish(self):
        E = self.eng["sp"]
        for b in self.out_bufs:
            if b.dsem is not None:
                E.wait_ge(b.dsem, b.dcnt)


def r32(ap):
    return ap.bitcast(F32R)


L1_COLS = 3072


def build_l1():
    nc = bass.Bass("TRN2", target_bir_lowering=False)
    cT = nc.dram_tensor("cT", [128, KC, B], F32, kind="ExternalInput").ap()
    w = nc.dram_tensor("w", [KC, 128, L1_COLS], F32, kind="ExternalInput").ap()
    bias = nc.dram_tensor("bias", [B, L1_COLS], F32, kind="ExternalInput").ap()
    out = nc.dram_tensor("out", [B, L1_COLS], F32, kind="ExternalOutput").ap()
    with ExitStack() as es:
        S_ = Sched(nc, es)
        sc = S_.sbuf([128, KC, B], name="sc")
        bt = S_.sbuf([B, L1_COLS], name="bt")
        ot = S_.sbuf([B, L1_COLS], name="ot")
        NB = 4
        wb = [S_.sbuf([128, L1_COLS], name=f"wb{i}") for i in range(NB)]
        ps = [S_.psum([128, 512], name=f"ps{i}") for i in range(6)]
        S_.dma("sp", sc[:], cT, writes=[sc])
        S_.dma("sp", bt[:], bias, writes=[bt])
        S_.op("act", lambda e: e.activation(out=sc[:], in_=sc[:], func=AF.Silu),
              reads=[sc], writes=[sc])
        for k in range(KC):
            wk = wb[k % NB]
            S_.dma("sp" if k % 2 == 0 else "pool", wk[:], w[k], writes=[wk])
            for n in range(6):
                S_.op("pe", lambda e, n=n, k=k, wk=wk: e.matmul(
                    ps[n][0:B, :], lhsT=sc[:, k, :], rhs=wk[:, n * 512:(n + 1) * 512],
                    start=(k == 0), stop=(k == KC - 1)),
                    reads=[sc, wk], writes=[ps[n]])
        for n in range(6):
            S_.op("dve", lambda e, n=n: e.tensor_tensor(
                out=ot[:, n * 512:(n + 1) * 512], in0=ps[n][0:B, :],
                in1=bt[:, n * 512:(n + 1) * 512], op=ALU.add),
                reads=[ps[n], bt], writes=[ot])
        S_.dma("sp", out, ot[:], reads=[ot])
        S_.mark_output(ot)
        S_.finish()
    return nc


def run_l1(c, ada_w, ada_b):
    nc = build_l1()
    cT = np.ascontiguousarray(c.T.reshape(KC, 128, B).transpose(1, 0, 2))
    in_maps = []
    for core in range(NCORES):
        s = core // 2
        i, j = s // 2, s % 2
        c0 = (core % 2) * L1_COLS
        wsl = np.ascontiguousarray(ada_w[i, j][:, c0:c0 + L1_COLS]).reshape(KC, 128, L1_COLS)
        bsl = np.ascontiguousarray(np.broadcast_to(ada_b[i, j][c0:c0 + L1_COLS], (B, L1_COLS)))
        in_maps.append({"cT": cT, "w": wsl, "bias": bsl})
    res = run_bass_kernel_spmd(nc, in_maps, core_ids=list(range(NCORES)))
    mod = np.zeros((2, 2, B, 3 * D), np.float32)
    for core in range(NCORES):
        s = core // 2
        i, j = s // 2, s % 2
        c0 = (core % 2) * L1_COLS
        mod[i, j][:, c0:c0 + L1_COLS] = res.results[core]["out"]
    return mod


NPASS = 2
TP = 512
TT = TP // 128
P_GATE1, P_LNG1, P_LNB1, P_SC2, P_SH2, P_GATE2, P_LNG2, P_LNB2 = range(8)


def build_l3(n_experts=NE):
    nc = bass.Bass("TRN2", target_bir_lowering=False)
    aT_d = nc.dram_tensor("aT", [NPASS, 128, KC, TP], F32R, kind="ExternalInput").ap()
    wp_d = nc.dram_tensor("wp", [4, 128, KC * 512], F32R, kind="ExternalInput").ap()
    x_d = nc.dram_tensor("x", [NPASS, TT, 128, D], F32, kind="ExternalInput").ap()
    par_d = nc.dram_tensor("par", [8, 128, D], F32, kind="ExternalInput").ap()
    wr_d = nc.dram_tensor("wr", [128, KC, 36], F32, kind="ExternalInput").ap()
    br_d = nc.dram_tensor("br", [128, 36], F32, kind="ExternalInput").ap()
    wgu_d = nc.dram_tensor("wgu", [NE * 4, 128, 2 * KC * 128], F32R, kind="ExternalInput").ap()
    wd_d = nc.dram_tensor("wd", [NE, 128, 4 * D], F32R, kind="ExternalInput").ap()
    id_d = nc.dram_tensor("ident", [128, 128], F32, kind="ExternalInput").ap()
    out_d = nc.dram_tensor("out", [NPASS, TT, 128, D], F32, kind="ExternalOutput").ap()

    with ExitStack() as es:
        S_ = Sched(nc, es)
        hT = S_.sbuf([128, KC, TP], name="hT")
        acc = [S_.sbuf([128, D], name=f"acc{t}") for t in range(TT)]
        wdb = [S_.sbuf([128, 4 * D], name=f"wdb{i}") for i in range(2)]
        gub = [S_.sbuf([128, 2 * KC * 128], name=f"gub{i}") for i in range(2)]
        aTb = [S_.sbuf([128, TP], name=f"aTb{i}") for i in range(8)]
        par = [S_.sbuf([128, D], name=f"par{i}") for i in range(3)]
        scr = gub[0]
        sh2 = gub[1]
        wr = S_.sbuf([128, KC, 36], name="wr_sb")
        br = S_.sbuf([128, 36], name="br_sb")
        ident = S_.sbuf([128, 128], name="ident_sb")
        G = S_.sbuf([128, TT, NE], name="G")
        sm = [S_.sbuf([128, 64], name=f"sm{i}") for i in range(2)]
        ps = [S_.psum([128, 512], name=f"psb{i}") for i in range(8)]
        pg, pu, py = ps[0:2], ps[2:4], ps[4:8]

        S_.dma("sp", wr[:], wr_d, writes=[wr])
        S_.dma("sp", br[:], br_d, writes=[br])
        S_.dma("sp", ident[:], id_d, writes=[ident])

        def ln_tile(xb, gb, gap, bb, bap, smb):
            s1, s2, mean, msq, var, std, rstd, nmr = [smb[:, i:i + 1] for i in range(8)]
            S_.op("act", lambda e: e.activation(out=r32(scr[:, 0:D]), in_=xb[:], func=AF.Identity, accum_out=s1),
                  reads=[xb], writes=[scr, smb])
            S_.op("act", lambda e: e.activation(out=r32(scr[:, 0:D]), in_=xb[:], func=AF.Square, accum_out=s2),
                  reads=[xb], writes=[scr, smb])
            S_.op("dve", lambda e: e.tensor_scalar(out=mean, in0=s1, scalar1=1.0 / D, scalar2=None, op0=ALU.mult),
                  reads=[smb], writes=[smb])
            S_.op("dve", lambda e: e.tensor_tensor(out=msq, in0=mean, in1=mean, op=ALU.mult),
                  reads=[smb], writes=[smb])
            S_.op("dve", lambda e: e.scalar_tensor_tensor(out=var, in0=s2, scalar=1.0 / D, in1=msq,
                                                          op0=ALU.mult, op1=ALU.subtract),
                  reads=[smb], writes=[smb])
            S_.op("dve", lambda e: e.tensor_scalar(out=var, in0=var, scalar1=EPS, scalar2=None, op0=ALU.add),
                  reads=[smb], writes=[smb])
            S_.op("act", lambda e: e.activation(out=std, in_=var, func=AF.Sqrt), reads=[smb], writes=[smb])
            S_.op("dve", lambda e: e.reciprocal(out=rstd, in_=std), reads=[smb], writes=[smb])
            S_.op("dve", lambda e: e.tensor_scalar(out=nmr, in0=mean, scalar1=rstd, scalar2=-1.0,
                                                   op0=ALU.mult, op1=ALU.mult), reads=[smb], writes=[smb])
            S_.op("act", lambda e: e.activation(out=xb[:], in_=xb[:], func=AF.Identity, scale=rstd, bias=nmr),
                  reads=[xb, smb], writes=[xb])
            S_.op("dve", lambda e: e.tensor_tensor(out=xb[:], in0=xb[:], in1=gap, op=ALU.mult),
                  reads=[xb, gb], writes=[xb])
            S_.op("dve", lambda e: e.tensor_tensor(out=xb[:], in0=xb[:], in1=bap, op=ALU.add),
                  reads=[xb, bb], writes=[xb])

        pyi = 0
        for pz in range(NPASS):
            S_.dma("pool", r32(hT[:]), aT_d[pz], writes=[hT])
            for t in range(TT):
                S_.dma("sp", acc[t][:], x_d[pz, t], writes=[acc[t]])
            S_.dma("sp", par[0][:], par_d[P_GATE1], writes=[par[0]])
            S_.dma("sp", par[1][:], par_d[P_LNG1], writes=[par[1]])
            S_.dma("sp", par[2][:], par_d[P_LNB1], writes=[par[2]])
            for n in range(4):
                wb = wdb[n % 2]
                S_.dma("pool", r32(wb[:]), wp_d[n], writes=[wb])
                wv = wb[:].rearrange("p (k c) -> p k c", k=KC)
                for t in range(TT):
                    pb = py[pyi % 4]
                    pyi += 1
                    for k in range(KC):
                        S_.op("pe", lambda e, k=k, t=t, pb=pb, wv=wv: e.matmul(
                            pb[:], lhsT=r32(hT[:, k, t * 128:(t + 1) * 128]), rhs=r32(wv[:, k, :]),
                            start=(k == 0), stop=(k == KC - 1)), reads=[hT, wb], writes=[pb])
                    cs = slice(n * 512, (n + 1) * 512)
                    sg = aTb[(n * TT + t) % 8]
                    S_.op("dve", lambda e, pb=pb, sg=sg, cs=cs: e.tensor_tensor(
                        out=r32(sg[:]), in0=pb[:], in1=par[0][:, cs], op=ALU.mult),
                        reads=[pb, par[0]], writes=[sg])
                    S_.op("dve", lambda e, t=t, sg=sg, cs=cs: e.scalar_tensor_tensor(
                        out=acc[t][:, cs], in0=acc[t][:, cs], scalar=ALPHA, in1=sg[:],
                        op0=ALU.mult, op1=ALU.add), reads=[acc[t], sg], writes=[acc[t]])
            for t in range(TT):
                ln_tile(acc[t], par[1], par[1][:], par[2], par[2][:], sm[t % 2])
            S_.dma("sp", par[0][:], par_d[P_SC2], writes=[par[0]])
            S_.dma("pool", r32(sh2[:, 0:D]), par_d[P_SH2], writes=[sh2])
            S_.op("dve", lambda e: e.tensor_scalar(out=par[0][:], in0=par[0][:], scalar1=1.0, scalar2=None,
                                                    op0=ALU.add), reads=[par[0]], writes=[par[0]])
            for t in range(TT):
                S_.op("dve", lambda e, t=t: e.tensor_tensor(out=r32(scr[:, 0:D]), in0=acc[t][:], in1=par[0][:], op=ALU.mult),
                      reads=[acc[t], par[0]], writes=[scr])
                S_.op("dve", lambda e: e.tensor_tensor(out=r32(scr[:, 0:D]), in0=scr[:, 0:D], in1=sh2[:, 0:D], op=ALU.add),
                      reads=[scr, sh2], writes=[scr])
                S_.op("dve", lambda e, t=t: e.tensor_scalar(out=acc[t][:], in0=acc[t][:], scalar1=ALPHA,
                                                             scalar2=None, op0=ALU.mult),
                      reads=[acc[t]], writes=[acc[t]])
                for k0 in range(0, KC, 4):
                    pb = py[pyi % 4]
                    pyi += 1
                    for j in range(4):
                        k = k0 + j
                        S_.op("pe", lambda e, j=j, k=k, pb=pb: e.transpose(
                            out=pb[:, j * 128:(j + 1) * 128], in_=scr[:, k * 128:(k + 1) * 128],
                            identity=ident[:]), reads=[scr, ident], writes=[pb])
                    S_.op("act", lambda e, k0=k0, t=t, pb=pb: e.activation(
                        out=r32(hT[:, k0:k0 + 4, t * 128:(t + 1) * 128]),
                        in_=pb[:].rearrange("p (a b) -> p a b", a=4), func=AF.Identity),
                        reads=[pb], writes=[hT])
            for t in range(TT):
                pb = py[pyi % 4]
                pyi += 1
                for k in range(KC):
                    S_.op("pe", lambda e, k=k, t=t, pb=pb: e.matmul(
                        pb[:, 0:36], lhsT=hT[:, k, t * 128:(t + 1) * 128], rhs=wr[:, k, :],
                        start=(k == 0), stop=(k == KC - 1)), reads=[hT, wr], writes=[pb])
                s = sm[t % 2]
                lg = s[:, 0:36]
                m4, nm4, s4, pgp = s[:, 36:37], s[:, 37:38], s[:, 38:39], s[:, 39:40]
                oh4 = s[:, 40:44]
                e4 = s[:, 44:48]
                sel = s[:, 48:56]
                S_.op("dve", lambda e: e.tensor_tensor(out=lg, in0=pb[:, 0:36], in1=br[:], op=ALU.add),
                      reads=[pb, br], writes=[s])
                S_.op("dve", lambda e: e.reduce_max(out=m4, in_=s[:, 0:4], axis=AX.X), reads=[s], writes=[s])
                S_.op("dve", lambda e: e.tensor_scalar(out=nm4, in0=m4, scalar1=-1.0, scalar2=None, op0=ALU.mult),
                      reads=[s], writes=[s])
                S_.op("act", lambda e: e.activation(out=e4, in_=s[:, 0:4], func=AF.Exp, bias=nm4, accum_out=s4),
                      reads=[s], writes=[s])
                S_.op("dve", lambda e: e.reciprocal(out=pgp, in_=s4), reads=[s], writes=[s])
                S_.op("dve", lambda e: e.tensor_scalar(out=oh4, in0=s[:, 0:4], scalar1=m4, scalar2=None,
                                                       op0=ALU.is_equal), reads=[s], writes=[s])
                S_.op("dve", lambda e: e.tensor_scalar(out=sel, in0=s[:, 4:12], scalar1=s[:, 40:41], scalar2=None,
                                                       op0=ALU.mult), reads=[s], writes=[s])
                for g in range(1, 4):
                    S_.op("dve", lambda e, g=g: e.scalar_tensor_tensor(
                        out=sel, in0=s[:, 4 + 8 * g:12 + 8 * g], scalar=s[:, 40 + g:41 + g], in1=sel,
                        op0=ALU.mult, op1=ALU.add), reads=[s], writes=[s])
                s2b = sm[t % 2]
                m1, m2, nm1, dd, p1, p2 = [s[:, 56 + i:57 + i] for i in range(6)]
                g1, g2 = s[:, 62:63], s[:, 63:64]
                o1 = G[:, t, 0:8]
                o2 = G[:, t, 8:16]
                sel2 = G[:, t, 16:24]
                g8 = G[:, t, 24:32]
                S_.op("dve", lambda e: e.reduce_max(out=m1, in_=sel, axis=AX.X), reads=[s], writes=[s])
                S_.op("dve", lambda e: e.tensor_scalar(out=o1, in0=sel, scalar1=m1, scalar2=None, op0=ALU.is_equal),
                      reads=[s], writes=[G])
                S_.op("dve", lambda e: e.scalar_tensor_tensor(out=sel2, in0=o1, scalar=-1e30, in1=sel,
                                                              op0=ALU.mult, op1=ALU.add), reads=[s, G], writes=[G])
                S_.op("dve", lambda e: e.reduce_max(out=m2, in_=sel2, axis=AX.X), reads=[G], writes=[s])
                S_.op("dve", lambda e: e.tensor_scalar(out=o2, in0=sel2, scalar1=m2, scalar2=None, op0=ALU.is_equal),
                      reads=[s, G], writes=[G])
                S_.op("dve", lambda e: e.tensor_scalar(out=nm1, in0=m1, scalar1=-1.0, scalar2=None, op0=ALU.mult),
                      reads=[s], writes=[s])
                S_.op("act", lambda e: e.activation(out=dd, in_=m2, func=AF.Exp, bias=nm1), reads=[s], writes=[s])
                S_.op("dve", lambda e: e.tensor_scalar(out=p1, in0=dd, scalar1=1.0, scalar2=None, op0=ALU.add),
                      reads=[s], writes=[s])
                S_.op("dve", lambda e: e.reciprocal(out=p1, in_=p1), reads=[s], writes=[s])
                S_.op("dve", lambda e: e.tensor_tensor(out=p2, in0=dd, in1=p1, op=ALU.mult), reads=[s], writes=[s])
                S_.op("dve", lambda e: e.tensor_tensor(out=g1, in0=p1, in1=pgp, op=ALU.mult), reads=[s], writes=[s])
                S_.op("dve", lambda e: e.tensor_tensor(out=g2, in0=p2, in1=pgp, op=ALU.mult), reads=[s], writes=[s])
                S_.op("dve", lambda e: e.tensor_scalar(out=g8, in0=o1, scalar1=g1, scalar2=None, op0=ALU.mult),
                      reads=[s, G], writes=[G])
                S_.op("dve", lambda e: e.scalar_tensor_tensor(out=g8, in0=o2, scalar=g2, in1=g8,
                                                              op0=ALU.mult, op1=ALU.add), reads=[s, G], writes=[G])
                S_.op("dve", lambda e: e.tensor_copy(out=sel, in_=g8), reads=[G], writes=[s])
                for g in range(4):
                    S_.op("dve", lambda e, g=g: e.tensor_scalar(
                        out=G[:, t, 8 * g:8 * g + 8], in0=sel, scalar1=s[:, 40 + g:41 + g], scalar2=None,
                        op0=ALU.mult), reads=[s], writes=[G])
            S_.dma("sp", par[0][:], par_d[P_GATE2], writes=[par[0]])
            ui = 0
            S_.dma("pool", r32(wdb[0][:]), wd_d[0], writes=[wdb[0]])
            for ex in range(n_experts):
                wb = wdb[ex % 2]
                pre = {}
                for hc in range(2):
                    gbp = gub[(ui + hc) % 2]
                    S_.dma("pool", r32(gbp[:]), wgu_d[ex * 4 + hc], writes=[gbp])
                    pre[hc] = gbp
                for hc in range(4):
                    S_.op("pool", lambda e, hc=hc, wb=wb: e.tensor_tensor(
                        out=r32(wb[:, hc * D:(hc + 1) * D]), in0=wb[:, hc * D:(hc + 1) * D], in1=par[0][:], op=ALU.mult),
                        reads=[wb, par[0]], writes=[wb])
                if ex + 1 < n_experts:
                    wbn = wdb[(ex + 1) % 2]
                    S_.dma("pool", r32(wbn[:]), wd_d[ex + 1], writes=[wbn])
                for hc in range(4):
                    gb = gub[ui % 2]
                    if hc not in pre:
                        S_.dma("pool", r32(gb[:]), wgu_d[ex * 4 + hc], writes=[gb])
                    gv = gb[:].rearrange("p (j k c) -> p j k c", j=2, k=KC)
                    pgb, pub = pg[ui % 2], pu[ui % 2]
                    for k in range(KC):
                        S_.op("pe", lambda e, k=k, gv=gv, pgb=pgb: e.matmul(
                            pgb[:], lhsT=r32(gv[:, 0, k, :]), rhs=r32(hT[:, k, :]),
                            start=(k == 0), stop=(k == KC - 1)), reads=[gb, hT], writes=[pgb])
                    for k in range(KC):
                        S_.op("pe", lambda e, k=k, gv=gv, pub=pub: e.matmul(
                            pub[:], lhsT=r32(gv[:, 1, k, :]), rhs=r32(hT[:, k, :]),
                            start=(k == 0), stop=(k == KC - 1)), reads=[gb, hT], writes=[pub])
                    ab = aTb[(ex % 2) * 4 + hc]
                    S_.op("act", lambda e, ab=ab, pgb=pgb: e.activation(out=r32(ab[:]), in_=pgb[:], func=AF.Silu),
                          reads=[pgb], writes=[ab])
                    S_.op("dve", lambda e, pub=pub, ab=ab: e.tensor_tensor(
                        out=r32(ab[:]), in0=ab[:], in1=pub[:], op=ALU.mult), reads=[ab, pub], writes=[ab])
                    ui += 1
                abs_ = [aTb[(ex % 2) * 4 + hc] for hc in range(4)]
                for t in range(TT):
                    for n in range(4):
                        pb = py[pyi % 4]
                        pyi += 1
                        for hc in range(4):
                            S_.op("pe", lambda e, hc=hc, t=t, n=n, pb=pb, wb=wb, abs_=abs_: e.matmul(
                                pb[:], lhsT=r32(abs_[hc][:, t * 128:(t + 1) * 128]),
                                rhs=r32(wb[:, hc * D + n * 512: hc * D + (n + 1) * 512]),
                                start=(hc == 0), stop=(hc == 3)), reads=[abs_[hc], wb], writes=[pb])
                        cs = slice(n * 512, (n + 1) * 512)
                        S_.op("dve", lambda e, t=t, cs=cs, pb=pb, ex=ex: e.scalar_tensor_tensor(
                            out=acc[t][:, cs], in0=pb[:], scalar=G[:, t, ex:ex + 1], in1=acc[t][:, cs],
                            op0=ALU.mult, op1=ALU.add), reads=[pb, G, acc[t]], writes=[acc[t]])
            S_.dma("sp", par[1][:], par_d[P_LNG2], writes=[par[1]])
            S_.dma("sp", par[2][:], par_d[P_LNB2], writes=[par[2]])
            for t in range(TT):
                ln_tile(acc[t], par[1], par[1][:], par[2], par[2][:], sm[t % 2])
                S_.dma("act", out_d[pz, t], acc[t][:], reads=[acc[t]])
                S_.mark_output(acc[t])
        S_.finish()
    return nc


def pack_experts(wg, wu, wd):
    st = np.stack([wg, wu], axis=1).reshape(NE, 2, KC, 128, 4, 128)
    wgu = np.ascontiguousarray(st.transpose(0, 4, 3, 1, 2, 5)).reshape(NE * 4, 128, 2 * KC * 128)
    wdp = np.ascontiguousarray(wd.reshape(NE, 4, 128, D).transpose(0, 2, 1, 3)).reshape(NE, 128, 4 * D)
    return wgu, wdp


def bc128(v):
    return np.ascontiguousarray(np.broadcast_to(v, (128,) + v.shape))


_NC_CACHE = {}


def run_l3(a_tok, wp, x_tok, rows, wrc, brc, wrf, brf, wgu, wdp, trace=False):
    if "l3" not in _NC_CACHE:
        _NC_CACHE["l3"] = build_l3()
    nc = _NC_CACHE["l3"]
    wpp = np.ascontiguousarray(wp.reshape(KC, 128, 4, 512).transpose(2, 1, 0, 3)).reshape(4, 128, KC * 512)
    wr = np.ascontiguousarray(np.concatenate([wrc, wrf], axis=1).reshape(KC, 128, 36).transpose(1, 0, 2))
    br = bc128(np.concatenate([brc, brf]))
    ident = np.eye(128, dtype=np.float32)
    pars = [np.ascontiguousarray(np.stack([bc128(v) for v in rows[b]])) for b in range(B)]
    in_maps = []
    TC = NTOK // NCORES
    for c in range(NCORES):
        b = c // (NCORES // B)
        a_c = a_tok[c * TC:(c + 1) * TC]
        aT = np.ascontiguousarray(a_c.reshape(NPASS, TP, KC, 128).transpose(0, 3, 2, 1))
        xc = np.ascontiguousarray(x_tok[c * TC:(c + 1) * TC]).reshape(NPASS, TT, 128, D)
        in_maps.append({"aT": aT, "wp": wpp, "x": xc, "par": pars[b], "wr": wr, "br": br,
                        "wgu": wgu, "wd": wdp, "ident": ident})
    res = run_bass_kernel_spmd(nc, in_maps, core_ids=list(range(NCORES)), trace=trace)
    out = np.concatenate([res.results[c]["out"].reshape(TC, D) for c in range(NCORES)], axis=0)
    if trace:
        return out, res
    return out


HPC = 4
BLK = 256
NBLK = S // BLK
QB = 512
SM_SCALE = HD ** -0.5
TWO_PI_HI = 6.28125
TWO_PI_LO = 2.0 * np.pi - 6.28125


def build_l2(hpc=HPC, do_tab=True, do_proj=True, do_attn=True, nblk=NBLK, nqb=S // QB, do_sel=True, do_rot=True, do_v=True):
    nc = bass.Bass("TRN2", target_bir_lowering=False)
    xT_d = nc.dram_tensor("xT", [NBLK, 128, KC * BLK], F32R, kind="ExternalInput").ap()
    mod_d = nc.dram_tensor("mod", [128, 2 * KC], F32, kind="ExternalInput").ap()
    wqk_d = nc.dram_tensor("wqk", [HPC, 128, 6 * KC * 128], F32R, kind="ExternalInput").ap()
    wv_d = nc.dram_tensor("wv", [HPC, 128, KC * 384], F32R, kind="ExternalInput").ap()
    pos_d = nc.dram_tensor("pos", [32, S], I32, kind="ExternalInput").ap()
    invf_d = nc.dram_tensor("invf", [32, 1], F32, kind="ExternalInput").ap()
    rot_d = nc.dram_tensor("rot", [128, 128], F32, kind="ExternalInput").ap()
    msk_d = nc.dram_tensor("msk", [2, 128, QB], F32, kind="ExternalInput").ap()
    o_d = nc.dram_tensor("o", [HPC, S // 128, 128, HD], F32, kind="ExternalOutput").ap()

    with ExitStack() as es:
        S_ = Sched(nc, es)
        xb = [S_.sbuf([128, KC * BLK], name=f"xb{i}") for i in range(2)]
        wqk = S_.sbuf([128, 6 * KC * 128], name="wqk_sb")
        wv = S_.sbuf([128, KC * 384], name="wv_sb")
        QT = [S_.sbuf([128, S], BF16, name=f"QT{g}") for g in range(NG)]
        KT = [S_.sbuf([128, S], BF16, name=f"KT{g}") for g in range(NG)]
        V = S_.sbuf([128, S // 128, NG, HD + 1], BF16, name="V")
        cosT = S_.sbuf([128, S], BF16, name="cosT")
        sinT = S_.sbuf([128, S], BF16, name="sinT")
        mod = S_.sbuf([128, 2 * KC], name="mod_sb")
        invf = S_.sbuf([32, 1], name="invf_sb")
        rotf = S_.sbuf([128, 128], name="rotf")
        rotb = S_.sbuf([128, 128], BF16, name="rotb")
        mskf = S_.sbuf([128, QB], name="mskf")
        msk = [S_.sbuf([128, QB], BF16, name=f"msk{i}") for i in range(2)]
        PT = [S_.sbuf([128, QB], BF16, name=f"PT{i}") for i in range(4)]
        t1 = [S_.sbuf([128, BLK], name=f"t1_{i}") for i in range(2)]
        t2 = [S_.sbuf([128, BLK], name=f"t2_{i}") for i in range(2)]
        osb = [S_.sbuf([128, HD], name=f"osb{i}") for i in range(2)]
        rden = [S_.sbuf([128, 1], name=f"rden{i}") for i in range(2)]
        ps = [S_.psum([128, 512], name=f"psb{i}") for i in range(8)]

        S_.dma("sp", mod[:], mod_d, writes=[mod])
        S_.dma("sp", invf[:], invf_d, writes=[invf])
        S_.dma("sp", rotf[:], rot_d, writes=[rotf])
        S_.op("dve", lambda e: e.tensor_copy(out=rotb[:], in_=rotf[:]), reads=[rotf], writes=[rotb])
        for i in range(2):
            S_.dma("sp", mskf[:], msk_d[i], writes=[mskf])
            S_.op("dve", lambda e, i=i: e.tensor_copy(out=msk[i][:], in_=mskf[:]), reads=[mskf], writes=[msk[i]])
        S_.op("pool", lambda e: e.memset(V[:], 1.0), writes=[V])
        S_.op("pool", lambda e: e.memset(cosT[:], 1.0), writes=[cosT])
        S_.op("pool", lambda e: e.memset(sinT[:], 0.0), writes=[sinT])
        S_.op("dve", lambda e: e.tensor_scalar(out=mod[:, 0:KC], in0=mod[:, 0:KC], scalar1=1.0, scalar2=None,
                                               op0=ALU.add), reads=[mod], writes=[mod])

        for ch in range(S // BLK if do_tab else 0):
            cs = slice(ch * BLK, (ch + 1) * BLK)
            bA, bB, bC, bM = t1[0], t1[1], t2[0], t2[1]
            posi = bA[0:32, :].bitcast(I32)
            ang = bA[0:32, :]
            tq = bB[0:32, :]
            ni = bB[0:32, :].bitcast(I32)
            nf = bB[0:32, :]
            rr = bC[0:32, :]
            mk = bM[0:32, :]
            S_.dma("sp", posi, pos_d[:, cs], writes=[bA])
            S_.op("dve", lambda e: e.tensor_copy(out=ang, in_=posi), reads=[bA], writes=[bA])
            S_.op("dve", lambda e: e.tensor_scalar(out=ang, in0=ang, scalar1=invf[:, 0:1], scalar2=None, op0=ALU.mult),
                  reads=[bA, invf], writes=[bA])
            for which, tab in ((0, sinT), (1, cosT)):
                off = 0.0 if which == 0 else float(np.pi / 2)
                S_.op("dve", lambda e, off=off: e.tensor_scalar(out=tq, in0=ang, scalar1=off, scalar2=float(1 / (2 * np.pi)),
                                                                op0=ALU.add, op1=ALU.mult), reads=[bA], writes=[bB])
                S_.op("dve", lambda e: e.tensor_copy(out=ni, in_=tq), reads=[bB], writes=[bB])
                S_.op("dve", lambda e: e.tensor_copy(out=nf, in_=ni), reads=[bB], writes=[bB])
                S_.op("dve", lambda e, off=off: e.tensor_scalar(out=rr, in0=ang, scalar1=off, scalar2=None, op0=ALU.add),
                      reads=[bA], writes=[bC])
                S_.op("dve", lambda e: e.scalar_tensor_tensor(out=rr, in0=nf, scalar=-TWO_PI_HI, in1=rr,
                                                              op0=ALU.mult, op1=ALU.add), reads=[bC, bB], writes=[bC])
                S_.op("dve", lambda e: e.scalar_tensor_tensor(out=rr, in0=nf, scalar=-TWO_PI_LO, in1=rr,
                                                              op0=ALU.mult, op1=ALU.add), reads=[bC, bB], writes=[bC])
                S_.op("dve", lambda e: e.tensor_scalar(out=mk, in0=rr, scalar1=float(np.pi), scalar2=float(-2 * np.pi),
                                                       op0=ALU.is_gt, op1=ALU.mult), reads=[bC], writes=[bM])
                S_.op("dve", lambda e: e.tensor_tensor(out=rr, in0=rr, in1=mk, op=ALU.add), reads=[bC, bM], writes=[bC])
                S_.op("dve", lambda e: e.tensor_scalar(out=mk, in0=rr, scalar1=float(-np.pi), scalar2=float(2 * np.pi),
                                                       op0=ALU.is_lt, op1=ALU.mult), reads=[bC], writes=[bM])
                S_.op("dve", lambda e: e.tensor_tensor(out=rr, in0=rr, in1=mk, op=ALU.add), reads=[bC, bM], writes=[bC])
                S_.op("dve", lambda e: e.tensor_scalar(out=rr, in0=rr, scalar1=float(np.pi), scalar2=float(-np.pi),
                                                       op0=ALU.min, op1=ALU.max), reads=[bC], writes=[bC])
                S_.op("act", lambda e, tab=tab, cs=cs: e.activation(out=tab[0:32, cs], in_=rr, func=AF.Sin),
                      reads=[bC], writes=[tab])

        pi_ = [0]
        pti = 0
        oi = 0
        for hl in range(hpc):
            S_.dma("pool", r32(wqk[:]), wqk_d[hl], writes=[wqk])
            S_.dma("pool", r32(wv[:]), wv_d[hl], writes=[wv])
            wq = wqk[:].rearrange("p (j k c) -> p j k c", j=6, k=KC)
            wvv = wv[:].rearrange("p (k c) -> p k c", k=KC)
            for blk in range(nblk if do_proj else 0):
                x_ = xb[blk % 2]
                S_.dma("pool", r32(x_[:]), xT_d[blk], writes=[x_])
                xv = x_[:].rearrange("p (k t) -> p k t", k=KC)
                for k in range(KC):
                    if k % 2 == 0:
                        S_.op("act", lambda e, k=k, xv=xv: e.activation(
                            out=r32(xv[:, k, :]), in_=xv[:, k, :], func=AF.Identity,
                            scale=mod[:, k:k + 1], bias=mod[:, KC + k:KC + k + 1]), reads=[x_, mod], writes=[x_])
                    else:
                        S_.op("dve", lambda e, k=k, xv=xv: e.tensor_scalar(
                            out=r32(xv[:, k, :]), in0=xv[:, k, :], scalar1=mod[:, k:k + 1],
                            scalar2=mod[:, KC + k:KC + k + 1], op0=ALU.mult, op1=ALU.add),
                            reads=[x_, mod], writes=[x_])
                cs = slice(blk * BLK, (blk + 1) * BLK)

                def proj(j):
                    g, isk = j // 2, j % 2
                    dst = (KT if isk else QT)[g]
                    pq = ps[pi_[0] % 4]
                    pr = ps[4 + (pi_[0] % 2)]
                    pi_[0] += 1
                    for k in range(KC):
                        S_.op("pe", lambda e, k=k, j=j, pq=pq: e.matmul(
                            pq[:, 0:BLK], lhsT=r32(wq[:, j, k, :]), rhs=r32(xv[:, k, :]),
                            start=(k == 0), stop=(k == KC - 1)), reads=[wqk, x_], writes=[pq])
                    S_.op("act", lambda e, dst=dst, pq=pq: e.activation(
                        out=dst[:, cs], in_=pq[:, 0:BLK], func=AF.Identity), reads=[pq], writes=[dst])
                    return (j, dst, pq, pr)

                def rot(st):
                    j, dst, pq, pr = st
                    S_.op("pe", lambda e: e.matmul(
                        pr[:, 0:BLK], lhsT=rotb[:], rhs=dst[:, cs], start=True, stop=True),
                        reads=[rotb, dst], writes=[pr])
                    a1, a2 = t1[j % 2], t2[j % 2]
                    S_.op("dve", lambda e: e.tensor_tensor(
                        out=a1[:], in0=pq[:, 0:BLK], in1=cosT[:, cs], op=ALU.mult),
                        reads=[pq, cosT, dst], writes=[a1])
                    S_.op("dve", lambda e: e.tensor_tensor(
                        out=a2[:], in0=pr[:, 0:BLK], in1=sinT[:, cs], op=ALU.mult),
                        reads=[pr, sinT], writes=[a2])
                    S_.op("dve", lambda e: e.tensor_tensor(
                        out=dst[:, cs], in0=a1[:], in1=a2[:], op=ALU.add), reads=[a1, a2], writes=[dst])

                def vproj(tt):
                    pv = ps[6 + tt % 2]
                    for k in range(KC):
                        S_.op("pe", lambda e, k=k: e.matmul(
                            pv[:, 0:384], lhsT=r32(xv[:, k, tt * 128:(tt + 1) * 128]), rhs=r32(wvv[:, k, :]),
                            start=(k == 0), stop=(k == KC - 1)), reads=[x_, wv], writes=[pv])
                    tile_i = blk * (BLK // 128) + tt
                    S_.op("act", lambda e: e.activation(
                        out=V[:, tile_i, :, 0:HD], in_=pv[:, 0:384].rearrange("p (g c) -> p g c", g=NG),
                        func=AF.Identity), reads=[pv], writes=[V])

                prev = None
                for j in range(6):
                    cur = proj(j)
                    if prev is not None:
                        rot(prev)
                    prev = cur
                vproj(0)
                rot(prev)
                vproj(1)
            if not do_attn and hl == 0:
                S_.op("dve", lambda e: e.tensor_copy(out=osb[0][:], in_=QT[0][:, 0:HD]), reads=[QT[0], KT[0], V, cosT, sinT], writes=[osb[0]])
                S_.dma("sp", o_d[0, 0], osb[0][:], reads=[osb[0]])
            for qb in range(nqb if do_attn else 0):
                work = []
                for g, d in enumerate(DILS):
                    W_ = RADIUS * d
                    for kt in range(S // 128):
                        dl = kt * 128 - qb * QB
                        if dl - (QB - 1) <= W_ and dl + 127 >= -W_:
                            work.append((g, d, kt, dl))
                po = ps[4:8]
                qs = slice(qb * QB, (qb + 1) * QB)
                nw = len(work)
                pbase = pti
                pti += nw

                def issue_s(wi):
                    g, d, kt, dl = work[wi]
                    W_ = RADIUS * d
                    pS = ps[wi % 4]
                    S_.op("pe", lambda e: e.matmul(
                        pS[:], lhsT=KT[g][:, kt * 128:(kt + 1) * 128], rhs=QT[g][:, qs], start=True, stop=True),
                        reads=[KT[g], QT[g]], writes=[pS])
                    P_ = PT[(pbase + wi) % 4]
                    S_.op("act", lambda e: e.activation(out=P_[:], in_=pS[:], func=AF.Exp, scale=SM_SCALE),
                          reads=[pS], writes=[P_])
                    if g > 0:
                        S_.op("dve", lambda e: e.tensor_tensor(out=P_[:], in0=P_[:], in1=msk[g - 1][:],
                                                               op=ALU.mult), reads=[P_, msk[g - 1]], writes=[P_])
                    if do_sel and dl - (QB - 1) < -W_:
                        S_.op("pool", lambda e: e.affine_select(
                            out=P_[:], in_=P_[:], pattern=[[-1, QB]], compare_op=ALU.is_ge, fill=0.0,
                            base=dl + W_, channel_multiplier=1), reads=[P_], writes=[P_])
                    if do_sel and dl + 127 > W_:
                        S_.op("pool", lambda e: e.affine_select(
                            out=P_[:], in_=P_[:], pattern=[[1, QB]], compare_op=ALU.is_ge, fill=0.0,
                            base=W_ - dl, channel_multiplier=-1), reads=[P_], writes=[P_])

                def issue_pv(wi):
                    g, d, kt, dl = work[wi]
                    P_ = PT[(pbase + wi) % 4]
                    for sub in range(4):
                        S_.op("pe", lambda e, sub=sub: e.matmul(
                            po[sub][:, 0:HD + 1], lhsT=P_[:, sub * 128:(sub + 1) * 128], rhs=V[:, kt, g, :],
                            start=(wi == 0), stop=(wi == nw - 1)), reads=[P_, V], writes=[po[sub]])

                LA = 3
                for wi in range(min(LA, nw)):
                    issue_s(wi)
                for wi in range(nw):
                    if wi + LA < nw:
                        issue_s(wi + LA)
                    issue_pv(wi)
                for sub in range(4):
                    ob, rd = osb[oi % 2], rden[oi % 2]
                    oi += 1
                    S_.op("dve", lambda e, rd=rd, sub=sub: e.reciprocal(out=rd[:], in_=po[sub][:, HD:HD + 1]),
                          reads=[po[sub]], writes=[rd])
                    S_.op("dve", lambda e, rd=rd, ob=ob, sub=sub: e.tensor_scalar(
                        out=ob[:], in0=po[sub][:, 0:HD], scalar1=rd[:, 0:1], scalar2=None, op0=ALU.mult),
                        reads=[po[sub], rd], writes=[ob])
                    S_.dma("sp", o_d[hl, qb * 4 + sub], ob[:], reads=[ob])
                    S_.mark_output(ob)
        S_.finish()
    return nc


def rot_matrix():
    R = np.zeros((128, 128), np.float32)
    for i in range(16):
        R[16 + i, i] = -1.0
        R[i, 16 + i] = 1.0
    return R


def run_l2(x, positions, w_qkv, mod0, trace=False):
    if "l2" not in _NC_CACHE:
        _NC_CACHE["l2"] = build_l2()
    nc = _NC_CACHE["l2"]
    invf = (THETA ** (-np.arange(0, ROT, 2, dtype=np.float32) / ROT)).astype(np.float32)
    invf32 = np.concatenate([invf, invf]).reshape(32, 1).astype(np.float32)
    kl = np.arange(128)[:, None]
    ql = np.arange(QB)[None, :]
    msk = np.stack([((kl - ql) % d == 0).astype(np.float32) for d in DILS[1:]])
    rot = rot_matrix()
    w5 = w_qkv.reshape(KC, 128, NG, 3, NH, HD)
    xTs = []
    for b in range(B):
        xt = x[b].T.reshape(KC, 128, NBLK, BLK)
        xTs.append(np.ascontiguousarray(xt.transpose(2, 1, 0, 3)).reshape(NBLK, 128, KC * BLK))
    in_maps = []
    for c in range(NCORES):
        b, hg = c // 4, c % 4
        hs = slice(hg * HPC, (hg + 1) * HPC)
        wqk = w5[:, :, :, 0:2, hs, :]
        wqk = np.ascontiguousarray(wqk.transpose(4, 1, 2, 3, 0, 5)).reshape(HPC, 128, 6 * KC * 128)
        wv = w5[:, :, :, 2, hs, :]
        wv = np.ascontiguousarray(wv.transpose(3, 1, 0, 2, 4)).reshape(HPC, 128, KC * 384)
        shift, scale = mod0[b, 0:D], mod0[b, D:2 * D]
        modc = np.concatenate([scale.reshape(KC, 128).T, shift.reshape(KC, 128).T], axis=1).astype(np.float32)
        pos = np.ascontiguousarray(np.broadcast_to(positions[b].astype(np.int32), (32, S)))
        in_maps.append({"xT": xTs[b], "mod": np.ascontiguousarray(modc), "wqk": wqk, "wv": wv, "pos": pos,
                        "invf": invf32, "rot": rot, "msk": msk})
    res = run_bass_kernel_spmd(nc, in_maps, core_ids=list(range(NCORES)), trace=trace)
    o = np.zeros((B, S, NH, HD), np.float32)
    for c in range(NCORES):
        b, hg = c // 4, c % 4
        oc = res.results[c]["o"].reshape(HPC, S, HD)
        o[b, :, hg * HPC:(hg + 1) * HPC, :] = oc.transpose(1, 0, 2)
    o = o.reshape(NTOK, D)
    if trace:
        return o, res
    return o


def sched_barrier(S_):
    engs = list(S_.eng.keys())
    for en in engs:
        E = S_.eng[en]
        for x in engs:
            if x != en and S_.cnt[x] > 0 and S_.seen[en].get(("e", x), 0) < S_.cnt[x]:
                E.wait_ge(S_.sem[x], S_.cnt[x])
                S_.seen[en][("e", x)] = S_.cnt[x]
        for b in S_.out_bufs:
            if b.dsem is not None and S_.seen[en].get(("d", b.dsem), 0) < b.dcnt:
                E.wait_ge(b.dsem, b.dcnt)
                S_.seen[en][("d", b.dsem)] = b.dcnt


GC = 512
NST = S // 128


def build_l4():
    nc = bass.Bass("TRN2", target_bir_lowering=False)
    xT_d = nc.dram_tensor("xT", [NBLK, 128, KC * BLK], F32R, kind="ExternalInput").ap()
    mod_d = nc.dram_tensor("mod", [128, 2 * KC], F32, kind="ExternalInput").ap()
    win_d = nc.dram_tensor("win", [128, KC * GC], F32R, kind="ExternalInput").ap()
    cc_d = nc.dram_tensor("cc", [2, 128, 4 * GC], F32R, kind="ExternalInput").ap()
    dft_d = nc.dram_tensor("dft", [S // QB, 16, 128, 4 * QB], F32R, kind="ExternalInput").ap()
    y_d = nc.dram_tensor("yT", [4, 128, S], F32, kind="ExternalOutput").ap()
    with ExitStack() as es:
        S_ = Sched(nc, es)
        AB = [S_.sbuf([128, NST, GC], name=f"AB{i}") for i in range(2)]
        win = S_.sbuf([128, KC * GC], name="win_sb")
        xb = [S_.sbuf([128, KC * BLK], name=f"xb{i}") for i in range(1)]
        uT = S_.sbuf([128, 4, BLK], name="uT")
        ccs = [S_.sbuf([128, 4 * GC], name=f"ccs{i}") for i in range(2)]
        mod = S_.sbuf([128, 2 * KC], name="mod_sb")
        ysb = [S_.sbuf([128, QB], name=f"ysb{i}") for i in range(2)]
        ps = [S_.psum([128, 512], name=f"psb{i}") for i in range(8)]
        S_.dma("sp", mod[:], mod_d, writes=[mod])
        S_.op("dve", lambda e: e.tensor_scalar(out=mod[:, 0:KC], in0=mod[:, 0:KC], scalar1=1.0, scalar2=None,
                                               op0=ALU.add), reads=[mod], writes=[mod])
        S_.dma("pool", r32(win[:]), win_d, writes=[win])
        for i in range(2):
            S_.dma("pool", r32(ccs[i][:]), cc_d[i], writes=[ccs[i]])
        wv = win[:].rearrange("p (k c) -> p k c", k=KC)
        pi_ = 0
        for blk in range(NBLK):
            x_ = xb[0]
            S_.dma("pool", r32(x_[:]), xT_d[blk], writes=[x_])
            xv = x_[:].rearrange("p (k t) -> p k t", k=KC)
            for k in range(KC):
                if k % 2 == 0:
                    S_.op("act", lambda e, k=k, xv=xv: e.activation(
                        out=r32(xv[:, k, :]), in_=xv[:, k, :], func=AF.Identity,
                        scale=mod[:, k:k + 1], bias=mod[:, KC + k:KC + k + 1]), reads=[x_, mod], writes=[x_])
                else:
                    S_.op("dve", lambda e, k=k, xv=xv: e.tensor_scalar(
                        out=r32(xv[:, k, :]), in0=xv[:, k, :], scalar1=mod[:, k:k + 1],
                        scalar2=mod[:, KC + k:KC + k + 1], op0=ALU.mult, op1=ALU.add),
                        reads=[x_, mod], writes=[x_])
            for cc in range(4):
                pu = ps[pi_ % 4]
                pi_ += 1
                for k in range(KC):
                    S_.op("pe", lambda e, k=k, cc=cc, pu=pu, xv=xv: e.matmul(
                        pu[:, 0:BLK], lhsT=r32(wv[:, k, cc * 128:(cc + 1) * 128]), rhs=r32(xv[:, k, :]),
                        start=(k == 0), stop=(k == KC - 1)), reads=[win, x_], writes=[pu])
                S_.op("act", lambda e, cc=cc, pu=pu: e.activation(out=r32(uT[:, cc, :]), in_=pu[:, 0:BLK],
                                                                  func=AF.Identity), reads=[pu], writes=[uT])
            for tt in range(BLK // 128):
                st = blk * (BLK // 128) + tt
                for i in range(2):
                    pa = ps[4 + (2 * tt + i) % 4]
                    cv = ccs[i][:].rearrange("p (c m) -> p c m", c=4)
                    for cc in range(4):
                        S_.op("pe", lambda e, cc=cc, tt=tt, pa=pa, cv=cv: e.matmul(
                            pa[:], lhsT=r32(uT[:, cc, tt * 128:(tt + 1) * 128]), rhs=r32(cv[:, cc, :]),
                            start=(cc == 0), stop=(cc == 3)), reads=[uT, ccs[i]], writes=[pa])
                    if i == 0:
                        S_.op("act", lambda e, pa=pa, st=st: e.activation(out=r32(AB[0][:, st, :]), in_=pa[:],
                                                                          func=AF.Identity), reads=[pa], writes=[AB[0]])
                    else:
                        S_.op("dve", lambda e, pa=pa, st=st: e.tensor_copy(out=r32(AB[1][:, st, :]), in_=pa[:]),
                              reads=[pa], writes=[AB[1]])
        sched_barrier(S_)
        dpc = [Buf(win.t, f"dpc{i}") for i in range(4)]
        yi = 0
        di = 0
        for kb in range(S // QB):
            for pc in range(16):
                sg, cs_ = pc // 2, pc % 2
                db = dpc[di % 4]
                dcol = slice((di % 4) * 2048, (di % 4 + 1) * 2048)
                di += 1
                S_.dma("pool", r32(win.t[:, dcol]), dft_d[kb, pc], writes=[db])
                dv = win.t[:, dcol].rearrange("p (s k) -> p s k", s=4)
                for s4 in range(4):
                    st = sg * 4 + s4
                    for mc in range(4):
                        first = (pc == 0 and s4 == 0)
                        last = (pc == 15 and s4 == 3)
                        S_.op("pe", lambda e, mc=mc, st=st, s4=s4, cs_=cs_, dv=dv, first=first, last=last: e.matmul(
                            ps[mc][:], lhsT=r32(AB[cs_][:, st, mc * 128:(mc + 1) * 128]), rhs=r32(dv[:, s4, :]),
                            start=first, stop=last), reads=[AB[cs_], db], writes=[ps[mc]])
            for mc in range(4):
                yb = ysb[yi % 2]
                yi += 1
                if mc % 2 == 0:
                    S_.op("act", lambda e, mc=mc, yb=yb: e.activation(out=yb[:], in_=ps[mc][:], func=AF.Identity),
                          reads=[ps[mc]], writes=[yb])
                else:
                    S_.op("dve", lambda e, mc=mc, yb=yb: e.tensor_copy(out=yb[:], in_=ps[mc][:]),
                          reads=[ps[mc]], writes=[yb])
                S_.dma("sp", y_d[mc, :, kb * QB:(kb + 1) * QB], yb[:], reads=[yb])
        S_.finish()
    return nc


def dft_tables():
    n = np.arange(S)
    angS = 2 * np.pi * ((n[:, None] * n[None, :]) % S) / S
    CS = (np.cos(angS) / np.sqrt(S)).astype(np.float32)
    SS = (-np.sin(angS) / np.sqrt(S)).astype(np.float32)
    m = np.arange(GC)
    angC = 2 * np.pi * ((m[:, None] * m[None, :]) % GC) / GC
    CC = (np.cos(angC) / np.sqrt(GC)).astype(np.float32)
    SC = (np.sin(angC) / np.sqrt(GC)).astype(np.float32)
    M = np.stack([CS, SS])
    M = M.reshape(2, 8, 4, 128, S // QB, QB)
    dft = np.ascontiguousarray(M.transpose(4, 1, 0, 3, 2, 5)).reshape(S // QB, 16, 128, 4 * QB)
    cc = np.stack([CC, SC]).reshape(2, 4, 128, GC)
    cc = np.ascontiguousarray(cc.transpose(0, 2, 1, 3)).reshape(2, 128, 4 * GC)
    return dft, cc


def run_l4(x_tok, w_in, mod0):
    if "l4" not in _NC_CACHE:
        _NC_CACHE["l4"] = build_l4()
    nc = _NC_CACHE["l4"]
    dft, cc = dft_tables()
    x = x_tok.reshape(B, S, D)
    xTs = []
    for b in range(B):
        xt = x[b].T.reshape(KC, 128, NBLK, BLK)
        xTs.append(np.ascontiguousarray(xt.transpose(2, 1, 0, 3)).reshape(NBLK, 128, KC * BLK))
    in_maps = []
    for c in range(NCORES):
        b, g = c // 4, c % 4
        wi = w_in[:, g * GC:(g + 1) * GC].reshape(KC, 128, GC)
        wi = np.ascontiguousarray(wi.transpose(1, 0, 2)).reshape(128, KC * GC)
        shift, scale = mod0[b, 0:D], mod0[b, D:2 * D]
        modc = np.ascontiguousarray(np.concatenate([scale.reshape(KC, 128).T, shift.reshape(KC, 128).T], axis=1))
        in_maps.append({"xT": xTs[b], "mod": modc.astype(np.float32), "win": wi, "cc": cc, "dft": dft})
    res = run_bass_kernel_spmd(nc, in_maps, core_ids=list(range(NCORES)))
    y = np.zeros((B, S, D), np.float32)
    for c in range(NCORES):
        b, g = c // 4, c % 4
        yT = res.results[c]["yT"].reshape(GC, S)
        y[b, :, g * GC:(g + 1) * GC] = yT.T
    return y.reshape(NTOK, D)


def kernel(x, c, positions, ada_w, ada_b, attn_w_qkv, attn_w_o, fnet_w_in, fnet_w_out,
           ln_g, ln_b, router_coarse_w, router_coarse_b, router_fine_w, router_fine_b,
           expert_w_gate, expert_w_up, expert_w_down):
    f = lambda a: np.asarray(a, dtype=np.float32)
    x, c = f(x), f(c)
    positions = np.asarray(positions).astype(np.int32)
    ada_w, ada_b = f(ada_w), f(ada_b)
    ln_g, ln_b = f(ln_g), f(ln_b)
    mod = run_l1(c, ada_w, ada_b)
    x_tok = x.reshape(NTOK, D)
    for i in range(2):
        m0, m1 = mod[i, 0], mod[i, 1]
        if i == 0:
            a_tok = run_l2(x, positions, f(attn_w_qkv[0]), m0)
            wp = f(attn_w_o[0])
        else:
            a_tok = run_l4(x_tok, f(fnet_w_in[0]), m0)
            wp = f(fnet_w_out[0])
        rows = [[m0[b, 2 * D:3 * D], ln_g[i, 0], ln_b[i, 0], m1[b, D:2 * D], m1[b, 0:D], m1[b, 2 * D:3 * D],
                 ln_g[i, 1], ln_b[i, 1]] for b in range(B)]
        wgu, wdp = pack_experts(f(expert_w_gate[i]), f(expert_w_up[i]), f(expert_w_down[i]))
        x_tok = run_l3(a_tok, wp, x_tok, rows, f(router_coarse_w[i]), f(router_coarse_b[i]),
                       f(router_fine_w[i]), f(router_fine_b[i]), wgu, wdp)
        del wgu, wdp
    return x_tok.reshape(B, S, D).astype(np.float32)


TB = 1024
TTB = TB // 128
DMA_CAST = dict(max_dma_last_dim=4096)


def build_l3b(n_experts=NE):
    nc = bass.Bass("TRN2", target_bir_lowering=False)
    aT_d = nc.dram_tensor("aT", [128, KC, TB], F32, kind="ExternalInput").ap()
    wp_d = nc.dram_tensor("wp", [4, 128, KC * 512], F32, kind="ExternalInput").ap()
    x_d = nc.dram_tensor("x", [TTB, 128, D], F32, kind="ExternalInput").ap()
    par_d = nc.dram_tensor("par", [8, 128, D], F32, kind="ExternalInput").ap()
    wr_d = nc.dram_tensor("wr", [128, KC, 36], F32, kind="ExternalInput").ap()
    br_d = nc.dram_tensor("br", [128, 36], F32, kind="ExternalInput").ap()
    wgu_d = nc.dram_tensor("wgu", [n_experts * 4, 128, 2 * KC * 128], F32, kind="ExternalInput").ap()
    wd_d = nc.dram_tensor("wd", [n_experts, 128, 4 * D], F32, kind="ExternalInput").ap()
    id_d = nc.dram_tensor("ident", [128, 128], F32, kind="ExternalInput").ap()
    out_d = nc.dram_tensor("out", [TTB, 128, D], F32, kind="ExternalOutput").ap()

    with ExitStack() as es:
        S_ = Sched(nc, es)
        hT = S_.sbuf([128, KC, TB], BF16, name="hT")
        acc = [S_.sbuf([128, D], name=f"acc{t}") for t in range(TTB)]
        wdb = [S_.sbuf([128, 4 * D], BF16, name=f"wdb{i}") for i in range(2)]
        gub = [S_.sbuf([128, 2 * KC * 128], BF16, name=f"gub{i}") for i in range(2)]
        aTb = [S_.sbuf([128, TB], BF16, name=f"aTb{i}") for i in range(8)]
        par = [S_.sbuf([128, D], name=f"par{i}") for i in range(3)]
        scr = S_.sbuf([128, D], name="scr")
        sh2 = par[1]
        hTfb = par[2]
        tmpy = [S_.sbuf([128, 512], name=f"tmpy{i}") for i in range(2)]
        wr = S_.sbuf([128, KC, 36], name="wr_sb")
        br = S_.sbuf([128, 36], name="br_sb")
        ident = S_.sbuf([128, 128], name="ident_sb")
        G = S_.sbuf([128, TTB, NE], name="G")
        sm = [S_.sbuf([128, 64], name=f"sm{i}") for i in range(2)]
        ps = [S_.psum([128, 512], name=f"psb{i}") for i in range(8)]
        pgu, py = ps[0:4], ps[4:8]

        S_.dma("sp", wr[:], wr_d, writes=[wr])
        S_.dma("sp", br[:], br_d, writes=[br])
        S_.dma("sp", ident[:], id_d, writes=[ident])

        def ln_tile(xb, gb, gap, bb, bap, smb):
            s1, s2, mean, msq, var, std, rstd, nmr = [smb[:, i:i + 1] for i in range(8)]
            S_.op("act", lambda e: e.activation(out=scr[:], in_=xb[:], func=AF.Identity, accum_out=s1),
                  reads=[xb], writes=[scr, smb])
            S_.op("act", lambda e: e.activation(out=scr[:], in_=xb[:], func=AF.Square, accum_out=s2),
                  reads=[xb], writes=[scr, smb])
            S_.op("dve", lambda e: e.tensor_scalar(out=mean, in0=s1, scalar1=1.0 / D, scalar2=None, op0=ALU.mult),
                  reads=[smb], writes=[smb])
            S_.op("dve", lambda e: e.tensor_tensor(out=msq, in0=mean, in1=mean, op=ALU.mult),
                  reads=[smb], writes=[smb])
            S_.op("dve", lambda e: e.scalar_tensor_tensor(out=var, in0=s2, scalar=1.0 / D, in1=msq,
                                                          op0=ALU.mult, op1=ALU.subtract),
                  reads=[smb], writes=[smb])
            S_.op("dve", lambda e: e.tensor_scalar(out=var, in0=var, scalar1=EPS, scalar2=None, op0=ALU.add),
                  reads=[smb], writes=[smb])
            S_.op("act", lambda e: e.activation(out=std, in_=var, func=AF.Sqrt), reads=[smb], writes=[smb])
            S_.op("dve", lambda e: e.reciprocal(out=rstd, in_=std), reads=[smb], writes=[smb])
            S_.op("dve", lambda e: e.tensor_scalar(out=nmr, in0=mean, scalar1=rstd, scalar2=-1.0,
                                                   op0=ALU.mult, op1=ALU.mult), reads=[smb], writes=[smb])
            S_.op("act", lambda e: e.activation(out=xb[:], in_=xb[:], func=AF.Identity, scale=rstd, bias=nmr),
                  reads=[xb, smb], writes=[xb])
            S_.op("pool", lambda e: e.tensor_tensor(out=xb[:], in0=xb[:], in1=gap, op=ALU.mult),
                  reads=[xb, gb], writes=[xb])
            S_.op("pool", lambda e: e.tensor_tensor(out=xb[:], in0=xb[:], in1=bap, op=ALU.add),
                  reads=[xb, bb], writes=[xb])

        pyi = 0
        for k in range(KC):
            S_.dma("pool", hT[:, k, :], aT_d[:, k, :], writes=[hT], **DMA_CAST)
        for t in range(TTB):
            S_.dma("sp", acc[t][:], x_d[t], writes=[acc[t]])
        S_.dma("sp", par[0][:], par_d[P_GATE1], writes=[par[0]])
        S_.dma("sp", par[1][:], par_d[P_LNG1], writes=[par[1]])
        S_.dma("sp", par[2][:], par_d[P_LNB1], writes=[par[2]])
        for n in range(4):
            wb = wdb[n % 2]
            S_.dma("pool", wb[:], wp_d[n], writes=[wb], **DMA_CAST)
            wv = wb[:].rearrange("p (k c) -> p k c", k=KC)
            for t in range(TTB):
                pb = py[pyi % 4]
                pyi += 1
                for k in range(KC):
                    S_.op("pe", lambda e, k=k, t=t, pb=pb, wv=wv: e.matmul(
                        pb[:], lhsT=hT[:, k, t * 128:(t + 1) * 128], rhs=wv[:, k, :],
                        start=(k == 0), stop=(k == KC - 1)), reads=[hT, wb], writes=[pb])
                cs = slice(n * 512, (n + 1) * 512)
                sg = tmpy[(n * TTB + t) % 2]
                S_.op("dve", lambda e, pb=pb, sg=sg, cs=cs: e.tensor_tensor(
                    out=sg[:], in0=pb[:], in1=par[0][:, cs], op=ALU.mult),
                    reads=[pb, par[0]], writes=[sg])
                S_.op("pool", lambda e, t=t, sg=sg, cs=cs: e.scalar_tensor_tensor(
                    out=acc[t][:, cs], in0=acc[t][:, cs], scalar=ALPHA, in1=sg[:],
                    op0=ALU.mult, op1=ALU.add), reads=[acc[t], sg], writes=[acc[t]]) if False else \
                    S_.op("dve", lambda e, t=t, sg=sg, cs=cs: e.scalar_tensor_tensor(
                        out=acc[t][:, cs], in0=acc[t][:, cs], scalar=ALPHA, in1=sg[:],
                        op0=ALU.mult, op1=ALU.add), reads=[acc[t], sg], writes=[acc[t]])
        for t in range(TTB):
            ln_tile(acc[t], par[1], par[1][:], par[2], par[2][:], sm[t % 2])
        S_.dma("sp", par[0][:], par_d[P_SC2], writes=[par[0]])
        S_.dma("sp", sh2[:], par_d[P_SH2], writes=[sh2])
        S_.op("pool", lambda e: e.tensor_scalar(out=par[0][:], in0=par[0][:], scalar1=1.0, scalar2=None,
                                                op0=ALU.add), reads=[par[0]], writes=[par[0]])
        for t in range(TTB):
            S_.op("dve", lambda e, t=t: e.tensor_tensor(out=scr[:], in0=acc[t][:], in1=par[0][:], op=ALU.mult),
                  reads=[acc[t], par[0]], writes=[scr])
            S_.op("dve", lambda e: e.tensor_tensor(out=scr[:], in0=scr[:], in1=sh2[:], op=ALU.add),
                  reads=[scr, sh2], writes=[scr])
            S_.op("pool", lambda e, t=t: e.tensor_scalar(out=acc[t][:], in0=acc[t][:], scalar1=ALPHA,
                                                         scalar2=None, op0=ALU.mult),
                  reads=[acc[t]], writes=[acc[t]])
            for k0 in range(0, KC, 4):
                pb = py[pyi % 4]
                pyi += 1
                for j in range(4):
                    k = k0 + j
                    S_.op("pe", lambda e, j=j, k=k, pb=pb: e.transpose(
                        out=pb[:, j * 128:(j + 1) * 128], in_=scr[:, k * 128:(k + 1) * 128],
                        identity=ident[:]), reads=[scr, ident], writes=[pb])
                S_.op("act", lambda e, k0=k0, t=t, pb=pb: e.activation(
                    out=hT[:, k0:k0 + 4, t * 128:(t + 1) * 128],
                    in_=pb[:].rearrange("p (a b) -> p a b", a=4), func=AF.Identity),
                    reads=[pb], writes=[hT])
                S_.op("act", lambda e, k0=k0, pb=pb: e.activation(
                    out=hTfb[:].rearrange("p (k t) -> p k t", k=KC)[:, k0:k0 + 4, :],
                    in_=pb[:].rearrange("p (a b) -> p a b", a=4), func=AF.Identity),
                    reads=[pb], writes=[hTfb])
            pb = py[pyi % 4]
            pyi += 1
            for k in range(KC):
                S_.op("pe", lambda e, k=k, pb=pb: e.matmul(
                    pb[:, 0:36], lhsT=hTfb[:, k * 128:(k + 1) * 128], rhs=wr[:, k, :],
                    start=(k == 0), stop=(k == KC - 1)), reads=[hTfb, wr], writes=[pb])
            s = sm[t % 2]
            lg = s[:, 0:36]
            m4, nm4, s4, pgp = s[:, 36:37], s[:, 37:38], s[:, 38:39], s[:, 39:40]
            oh4 = s[:, 40:44]
            e4 = s[:, 44:48]
            sel = s[:, 48:56]
            S_.op("dve", lambda e: e.tensor_tensor(out=lg, in0=pb[:, 0:36], in1=br[:], op=ALU.add),
                  reads=[pb, br], writes=[s])
            S_.op("dve", lambda e: e.reduce_max(out=m4, in_=s[:, 0:4], axis=AX.X), reads=[s], writes=[s])
            S_.op("dve", lambda e: e.tensor_scalar(out=nm4, in0=m4, scalar1=-1.0, scalar2=None, op0=ALU.mult),
                  reads=[s], writes=[s])
            S_.op("act", lambda e: e.activation(out=e4, in_=s[:, 0:4], func=AF.Exp, bias=nm4, accum_out=s4),
                  reads=[s], writes=[s])
            S_.op("dve", lambda e: e.reciprocal(out=pgp, in_=s4), reads=[s], writes=[s])
            S_.op("dve", lambda e: e.tensor_scalar(out=oh4, in0=s[:, 0:4], scalar1=m4, scalar2=None,
                                                   op0=ALU.is_equal), reads=[s], writes=[s])
            S_.op("dve", lambda e: e.tensor_scalar(out=sel, in0=s[:, 4:12], scalar1=s[:, 40:41], scalar2=None,
                                                   op0=ALU.mult), reads=[s], writes=[s])
            for g in range(1, 4):
                S_.op("dve", lambda e, g=g: e.scalar_tensor_tensor(
                    out=sel, in0=s[:, 4 + 8 * g:12 + 8 * g], scalar=s[:, 40 + g:41 + g], in1=sel,
                    op0=ALU.mult, op1=ALU.add), reads=[s], writes=[s])
            m1, m2, nm1, dd, p1, p2 = [s[:, 56 + i:57 + i] for i in range(6)]
            g1, g2 = s[:, 62:63], s[:, 63:64]
            o1 = G[:, t, 0:8]
            o2 = G[:, t, 8:16]
            sel2 = G[:, t, 16:24]
            g8 = G[:, t, 24:32]
            S_.op("dve", lambda e: e.reduce_max(out=m1, in_=sel, axis=AX.X), reads=[s], writes=[s])
            S_.op("dve", lambda e: e.tensor_scalar(out=o1, in0=sel, scalar1=m1, scalar2=None, op0=ALU.is_equal),
                  reads=[s], writes=[G])
            S_.op("dve", lambda e: e.scalar_tensor_tensor(out=sel2, in0=o1, scalar=-1e30, in1=sel,
                                                          op0=ALU.mult, op1=ALU.add), reads=[s, G], writes=[G])
            S_.op("dve", lambda e: e.reduce_max(out=m2, in_=sel2, axis=AX.X), reads=[G], writes=[s])
            S_.op("dve", lambda e: e.tensor_scalar(out=o2, in0=sel2, scalar1=m2, scalar2=None, op0=ALU.is_equal),
                  reads=[s, G], writes=[G])
            S_.op("dve", lambda e: e.tensor_scalar(out=nm1, in0=m1, scalar1=-1.0, scalar2=None, op0=ALU.mult),
                  reads=[s], writes=[s])
            S_.op("act", lambda e: e.activation(out=dd, in_=m2, func=AF.Exp, bias=nm1), reads=[s], writes=[s])
            S_.op("dve", lambda e: e.tensor_scalar(out=p1, in0=dd, scalar1=1.0, scalar2=None, op0=ALU.add),
                  reads=[s], writes=[s])
            S_.op("dve", lambda e: e.reciprocal(out=p1, in_=p1), reads=[s], writes=[s])
            S_.op("dve", lambda e: e.tensor_tensor(out=p2, in0=dd, in1=p1, op=ALU.mult), reads=[s], writes=[s])
            S_.op("dve", lambda e: e.tensor_tensor(out=g1, in0=p1, in1=pgp, op=ALU.mult), reads=[s], writes=[s])
            S_.op("dve", lambda e: e.tensor_tensor(out=g2, in0=p2, in1=pgp, op=ALU.mult), reads=[s], writes=[s])
            S_.op("dve", lambda e: e.tensor_scalar(out=g8, in0=o1, scalar1=g1, scalar2=None, op0=ALU.mult),
                  reads=[s, G], writes=[G])
            S_.op("dve", lambda e: e.scalar_tensor_tensor(out=g8, in0=o2, scalar=g2, in1=g8,
                                                          op0=ALU.mult, op1=ALU.add), reads=[s, G], writes=[G])
            S_.op("dve", lambda e: e.tensor_copy(out=sel, in_=g8), reads=[G], writes=[s])
            for g in range(4):
                S_.op("dve", lambda e, g=g: e.tensor_scalar(
                    out=G[:, t, 8 * g:8 * g + 8], in0=sel, scalar1=s[:, 40 + g:41 + g], scalar2=None,
                    op0=ALU.mult), reads=[s], writes=[G])
        S_.dma("sp", par[0][:], par_d[P_GATE2], writes=[par[0]])
        ui = 0
        ti = 0
        stage = [par[1], par[2], scr]
        sctr = [0]

        def load_cast(dst_ap, dst_buf, src_ap, ceng):
            st = stage[sctr[0] % 3]
            sctr[0] += 1
            S_.dma("sp", st[:], src_ap, writes=[st])
            if ceng == "act":
                S_.op("act", lambda e: e.activation(out=dst_ap, in_=st[:], func=AF.Identity), reads=[st], writes=[dst_buf])
            else:
                S_.op(ceng, lambda e: e.tensor_copy(out=dst_ap, in_=st[:]), reads=[st], writes=[dst_buf])

        for ex in range(n_experts):
            wb = wdb[ex % 2]
            for hc in range(4):
                load_cast(wb[:, hc * D:(hc + 1) * D], wb, wd_d[ex][:, hc * D:(hc + 1) * D], "pool")
            for hc in range(4):
                gb = gub[ui % 2]
                ceng = "act" if ui % 2 == 0 else "dve"
                for j in range(2):
                    load_cast(gb[:, j * 2048:(j + 1) * 2048], gb, wgu_d[ex * 4 + hc][:, j * 2048:(j + 1) * 2048], ceng)
                gv = gb[:].rearrange("p (j k c) -> p j k c", j=2, k=KC)
                ab = aTb[(ex % 2) * 4 + hc]
                for half in range(TB // 512):
                    ts_ = slice(half * 512, (half + 1) * 512)
                    pgb, pub = pgu[(2 * ui + half) % 2 * 2], pgu[(2 * ui + half) % 2 * 2 + 1]
                    for k in range(KC):
                        S_.op("pe", lambda e, k=k, gv=gv, pgb=pgb, ts_=ts_: e.matmul(
                            pgb[:], lhsT=gv[:, 0, k, :], rhs=hT[:, k, ts_],
                            start=(k == 0), stop=(k == KC - 1)), reads=[gb, hT], writes=[pgb])
                    for k in range(KC):
                        S_.op("pe", lambda e, k=k, gv=gv, pub=pub, ts_=ts_: e.matmul(
                            pub[:], lhsT=gv[:, 1, k, :], rhs=hT[:, k, ts_],
                            start=(k == 0), stop=(k == KC - 1)), reads=[gb, hT], writes=[pub])
                    sg = tmpy[ti % 2]
                    ti += 1
                    S_.op("act", lambda e, sg=sg, pgb=pgb: e.activation(out=sg[:], in_=pgb[:], func=AF.Silu),
                          reads=[pgb], writes=[sg])
                    S_.op("dve", lambda e, pub=pub, ab=ab, sg=sg, ts_=ts_: e.tensor_tensor(
                        out=ab[:, ts_], in0=sg[:], in1=pub[:], op=ALU.mult), reads=[sg, pub], writes=[ab])
                ui += 1
            abs_ = [aTb[(ex % 2) * 4 + hc] for hc in range(4)]
            for t in range(TTB):
                for n in range(4):
                    pb = py[pyi % 4]
                    pyi += 1
                    for hc in range(4):
                        S_.op("pe", lambda e, hc=hc, t=t, n=n, pb=pb, wb=wb, abs_=abs_: e.matmul(
                            pb[:], lhsT=abs_[hc][:, t * 128:(t + 1) * 128],
                            rhs=wb[:, hc * D + n * 512: hc * D + (n + 1) * 512],
                            start=(hc == 0), stop=(hc == 3)), reads=[abs_[hc], wb], writes=[pb])
                    cs = slice(n * 512, (n + 1) * 512)
                    yt = tmpy[ti % 2]
                    ti += 1
                    S_.op("act", lambda e, pb=pb, yt=yt, t=t, ex=ex: e.activation(
                        out=yt[:], in_=pb[:], func=AF.Identity, scale=G[:, t, ex:ex + 1]), reads=[pb, G], writes=[yt])
                    S_.op("dve", lambda e, yt=yt, cs=cs: e.tensor_tensor(
                        out=yt[:], in0=yt[:], in1=par[0][:, cs], op=ALU.mult), reads=[yt, par[0]], writes=[yt])
                    S_.op("pool", lambda e, t=t, cs=cs, yt=yt: e.tensor_tensor(
                        out=acc[t][:, cs], in0=acc[t][:, cs], in1=yt[:], op=ALU.add),
                        reads=[yt, acc[t]], writes=[acc[t]])
        S_.dma("sp", par[1][:], par_d[P_LNG2], writes=[par[1]])
        S_.dma("sp", par[2][:], par_d[P_LNB2], writes=[par[2]])
        for t in range(TTB):
            ln_tile(acc[t], par[1], par[1][:], par[2], par[2][:], sm[t % 2])
            S_.dma("act", out_d[t], acc[t][:], reads=[acc[t]])
        S_.finish()
    return nc


def run_l3b(a_tok, wp, x_tok, rows, wrc, brc, wrf, brf, wgu, wdp, trace=False):
    if "l3b" not in _NC_CACHE:
        _NC_CACHE["l3b"] = build_l3b()
    nc = _NC_CACHE["l3b"]
    wpp = np.ascontiguousarray(wp.reshape(KC, 128, 4, 512).transpose(2, 1, 0, 3)).reshape(4, 128, KC * 512)
    wr = np.ascontiguousarray(np.concatenate([wrc, wrf], axis=1).reshape(KC, 128, 36).transpose(1, 0, 2))
    br = bc128(np.concatenate([brc, brf]))
    ident = np.eye(128, dtype=np.float32)
    pars = [np.ascontiguousarray(np.stack([bc128(v) for v in rows[b]])) for b in range(B)]
    in_maps = []
    TC = NTOK // NCORES
    for c in range(NCORES):
        b = c // (NCORES // B)
        a_c = a_tok[c * TC:(c + 1) * TC]
        aT = np.ascontiguousarray(a_c.reshape(TB, KC, 128).transpose(2, 1, 0))
        xc = np.ascontiguousarray(x_tok[c * TC:(c + 1) * TC]).reshape(TTB, 128, D)
        in_maps.append({"aT": aT, "wp": wpp, "x": xc, "par": pars[b], "wr": wr, "br": br,
                        "wgu": wgu, "wd": wdp, "ident": ident})
    res = run_bass_kernel_spmd(nc, in_maps, core_ids=list(range(NCORES)), trace=trace)
    out = np.concatenate([res.results[c]["out"].reshape(TC, D) for c in range(NCORES)], axis=0)
    if trace:
        return out, res
    return out
```

```python
import numpy as np
from contextlib import ExitStack
import concourse.bass as bass
import concourse.mybir as mybir
from concourse.bass_utils import run_bass_kernel_spmd

F32 = mybir.dt.float32
F32R = mybir.dt.float32r
BF16 = mybir.dt.bfloat16
I32 = mybir.dt.int32
AF = mybir.ActivationFunctionType
ALU = mybir.AluOpType
AX = mybir.AxisListType

NCORES = 8
D = 2048
KC = D // 128
B = 2
S = 4096
NTOK = B * S
HD = 128
NH = 16
NG = 3
DILS = (1, 4, 16)
RADIUS = 64
ROT = 32
THETA = 500000.0
NE = 32
EPG = 8
NGRP = 4
ED = 512
EPS = 1e-5
ALPHA = 4.0 ** 0.25

SAME_ENG_SYNC = True


class Buf:
    __slots__ = ("t", "name", "w", "r", "dsem", "dcnt")

    def __init__(self, t, name):
        self.t = t
        self.name = name
        self.w = None
        self.r = []
        self.dsem = None
        self.dcnt = 0

    def __getitem__(self, k):
        return self.t[k]


class Sched:
    def __init__(self, nc, es):
        self.nc = nc
        self.es = es
        self.eng = {"pe": nc.tensor, "dve": nc.vector, "act": nc.scalar,
                    "pool": nc.gpsimd, "sp": nc.sync}
        self.sem = {}
        self.cnt = {}
        self.seen = {}
        for k in self.eng:
            self.sem[k] = es.enter_context(nc.semaphore("s_" + k))
            self.cnt[k] = 0
            self.seen[k] = {}
        self.nbuf = 0
        self.out_bufs = []

    def sbuf(self, shape, dtype=F32, name=None):
        self.nbuf += 1
        name = name or f"sb{self.nbuf}"
        t = self.es.enter_context(self.nc.sbuf_tensor(name, list(shape), dtype))
        return Buf(t, name)

    def psum(self, shape, dtype=F32, name=None):
        self.nbuf += 1
        name = name or f"ps{self.nbuf}"
        t = self.es.enter_context(self.nc.psum_tensor(name, list(shape), dtype))
        return Buf(t, name)

    def view(self, name="v"):
        return Buf(None, name)

    def _collect(self, eng, reads, writes):
        need = {}

        def add(dep, raw):
            if dep is None:
                return
            if dep[0] == "dma":
                key = ("d", dep[1])
                sem, val = dep[1], dep[2]
            else:
                e2, val = dep
                if e2 == eng:
                    if eng == "pe" or not raw or not SAME_ENG_SYNC:
                        return
                key = ("e", e2)
                sem = self.sem[e2]
            if need.get(key, (None, 0))[1] < val:
                need[key] = (sem, val)

        for b in reads:
            add(b.w, True)
        for b in writes:
            add(b.w, True)
            for r in b.r:
                add(r, False)
        return need

    def _emit_waits(self, eng, need):
        E = self.eng[eng]
        seen = self.seen[eng]
        for key, (sem, val) in need.items():
            if seen.get(key, 0) < val:
                E.wait_ge(sem, val)
                seen[key] = val

    def op(self, eng, fn, reads=(), writes=()):
        need = self._collect(eng, reads, writes)
        self._emit_waits(eng, need)
        ins = fn(self.eng[eng])
        self.cnt[eng] += 1
        ins.then_inc(self.sem[eng], 1)
        me = (eng, self.cnt[eng])
        for b in reads:
            b.r.append(me)
        for b in writes:
            b.w = me
            b.r = []
        return ins

    def dma(self, q, out, in_, reads=(), writes=(), track=None, **kw):
        need = self._collect("dmaq_" + q, reads, writes)
        self._emit_waits(q, need)
        b = track or (writes[0] if writes else reads[0])
        if b.dsem is None:
            b.dsem = self.es.enter_context(self.nc.semaphore("d_" + b.name))
            self.out_bufs.append(b)
        ins = self.eng[q].dma_start(out=out, in_=in_, **kw)
        b.dcnt += 16
        ins.then_inc(b.dsem, 16)
        me = ("dma", b.dsem, b.dcnt)
        for x in reads:
            x.r.append(me)
        for x in writes:
            x.w = me
            x.r = []
        return ins

    def mark_output(self, b):
        if b not in self.out_bufs:
            self.out_bufs.append(b)

    def finish(self):
        E = self.eng["sp"]
        for b in self.out_bufs:
            if b.dsem is not None:
                E.wait_ge(b.dsem, b.dcnt)


def r32(ap):
    return ap.bitcast(F32R)


L1_COLS = 3072


def build_l1():
    nc = bass.Bass("TRN2", target_bir_lowering=False)
    cT = nc.dram_tensor("cT", [128, KC, B], F32, kind="ExternalInput").ap()
    w = nc.dram_tensor("w", [KC, 128, L1_COLS], F32, kind="ExternalInput").ap()
    bias = nc.dram_tensor("bias", [B, L1_COLS], F32, kind="ExternalInput").ap()
    out = nc.dram_tensor("out", [B, L1_COLS], F32, kind="ExternalOutput").ap()
    with ExitStack() as es:
        S_ = Sched(nc, es)
        sc = S_.sbuf([128, KC, B], name="sc")
        bt = S_.sbuf([B, L1_COLS], name="bt")
        ot = S_.sbuf([B, L1_COLS], name="ot")
        NB = 4
        wb = [S_.sbuf([128, L1_COLS], name=f"wb{i}") for i in range(NB)]
        ps = [S_.psum([128, 512], name=f"ps{i}") for i in range(6)]
        S_.dma("sp", sc[:], cT, writes=[sc])
        S_.dma("sp", bt[:], bias, writes=[bt])
        S_.op("act", lambda e: e.activation(out=sc[:], in_=sc[:], func=AF.Silu),
              reads=[sc], writes=[sc])
        for k in range(KC):
            wk = wb[k % NB]
            S_.dma("sp" if k % 2 == 0 else "pool", wk[:], w[k], writes=[wk])
            for n in range(6):
                S_.op("pe", lambda e, n=n, k=k, wk=wk: e.matmul(
                    ps[n][0:B, :], lhsT=sc[:, k, :], rhs=wk[:, n * 512:(n + 1) * 512],
                    start=(k == 0), stop=(k == KC - 1)),
                    reads=[sc, wk], writes=[ps[n]])
        for n in range(6):
            S_.op("dve", lambda e, n=n: e.tensor_tensor(
                out=ot[:, n * 512:(n + 1) * 512], in0=ps[n][0:B, :],
                in1=bt[:, n * 512:(n + 1) * 512], op=ALU.add),
                reads=[ps[n], bt], writes=[ot])
        S_.dma("sp", out, ot[:], reads=[ot])
        S_.mark_output(ot)
        S_.finish()
    return nc


def run_l1(c, ada_w, ada_b):
    nc = build_l1()
    cT = np.ascontiguousarray(c.T.reshape(KC, 128, B).transpose(1, 0, 2))
    in_maps = []
    for core in range(NCORES):
        s = core // 2
        i, j = s // 2, s % 2
        c0 = (core % 2) * L1_COLS
        wsl = np.ascontiguousarray(ada_w[i, j][:, c0:c0 + L1_COLS]).reshape(KC, 128, L1_COLS)
        bsl = np.ascontiguousarray(np.broadcast_to(ada_b[i, j][c0:c0 + L1_COLS], (B, L1_COLS)))
        in_maps.append({"cT": cT, "w": wsl, "bias": bsl})
    res = run_bass_kernel_spmd(nc, in_maps, core_ids=list(range(NCORES)))
    mod = np.zeros((2, 2, B, 3 * D), np.float32)
    for core in range(NCORES):
        s = core // 2
        i, j = s // 2, s % 2
        c0 = (core % 2) * L1_COLS
        mod[i, j][:, c0:c0 + L1_COLS] = res.results[core]["out"]
    return mod


NPASS = 2
TP = 512
TT = TP // 128
P_GATE1, P_LNG1, P_LNB1, P_SC2, P_SH2, P_GATE2, P_LNG2, P_LNB2 = range(8)


def build_l3(n_experts=NE):
    nc = bass.Bass("TRN2", target_bir_lowering=False)
    aT_d = nc.dram_tensor("aT", [NPASS, 128, KC, TP], F32R, kind="ExternalInput").ap()
    wp_d = nc.dram_tensor("wp", [4, 128, KC * 512], F32R, kind="ExternalInput").ap()
    x_d = nc.dram_tensor("x", [NPASS, TT, 128, D], F32, kind="ExternalInput").ap()
    par_d = nc.dram_tensor("par", [8, 128, D], F32, kind="ExternalInput").ap()
    wr_d = nc.dram_tensor("wr", [128, KC, 36], F32, kind="ExternalInput").ap()
    br_d = nc.dram_tensor("br", [128, 36], F32, kind="ExternalInput").ap()
    wgu_d = nc.dram_tensor("wgu", [NE * 4, 128, 2 * KC * 128], F32R, kind="ExternalInput").ap()
    wd_d = nc.dram_tensor("wd", [NE, 128, 4 * D], F32R, kind="ExternalInput").ap()
    id_d = nc.dram_tensor("ident", [128, 128], F32, kind="ExternalInput").ap()
    out_d = nc.dram_tensor("out", [NPASS, TT, 128, D], F32, kind="ExternalOutput").ap()

    with ExitStack() as es:
        S_ = Sched(nc, es)
        hT = S_.sbuf([128, KC, TP], name="hT")
        acc = [S_.sbuf([128, D], name=f"acc{t}") for t in range(TT)]
        wdb = [S_.sbuf([128, 4 * D], name=f"wdb{i}") for i in range(2)]
        gub = [S_.sbuf([128, 2 * KC * 128], name=f"gub{i}") for i in range(2)]
        aTb = [S_.sbuf([128, TP], name=f"aTb{i}") for i in range(8)]
        par = [S_.sbuf([128, D], name=f"par{i}") for i in range(3)]
        scr = gub[0]
        sh2 = gub[1]
        wr = S_.sbuf([128, KC, 36], name="wr_sb")
        br = S_.sbuf([128, 36], name="br_sb")
        ident = S_.sbuf([128, 128], name="ident_sb")
        G = S_.sbuf([128, TT, NE], name="G")
        sm = [S_.sbuf([128, 64], name=f"sm{i}") for i in range(2)]
        ps = [S_.psum([128, 512], name=f"psb{i}") for i in range(8)]
        pg, pu, py = ps[0:2], ps[2:4], ps[4:8]

        S_.dma("sp", wr[:], wr_d, writes=[wr])
        S_.dma("sp", br[:], br_d, writes=[br])
        S_.dma("sp", ident[:], id_d, writes=[ident])

        def ln_tile(xb, gb, gap, bb, bap, smb):
            s1, s2, mean, msq, var, std, rstd, nmr = [smb[:, i:i + 1] for i in range(8)]
            S_.op("act", lambda e: e.activation(out=r32(scr[:, 0:D]), in_=xb[:], func=AF.Identity, accum_out=s1),
                  reads=[xb], writes=[scr, smb])
            S_.op("act", lambda e: e.activation(out=r32(scr[:, 0:D]), in_=xb[:], func=AF.Square, accum_out=s2),
                  reads=[xb], writes=[scr, smb])
            S_.op("dve", lambda e: e.tensor_scalar(out=mean, in0=s1, scalar1=1.0 / D, scalar2=None, op0=ALU.mult),
                  reads=[smb], writes=[smb])
            S_.op("dve", lambda e: e.tensor_tensor(out=msq, in0=mean, in1=mean, op=ALU.mult),
                  reads=[smb], writes=[smb])
            S_.op("dve", lambda e: e.scalar_tensor_tensor(out=var, in0=s2, scalar=1.0 / D, in1=msq,
                                                          op0=ALU.mult, op1=ALU.subtract),
                  reads=[smb], writes=[smb])
            S_.op("dve", lambda e: e.tensor_scalar(out=var, in0=var, scalar1=EPS, scalar2=None, op0=ALU.add),
                  reads=[smb], writes=[smb])
            S_.op("act", lambda e: e.activation(out=std, in_=var, func=AF.Sqrt), reads=[smb], writes=[smb])
            S_.op("dve", lambda e: e.reciprocal(out=rstd, in_=std), reads=[smb], writes=[smb])
            S_.op("dve", lambda e: e.tensor_scalar(out=nmr, in0=mean, scalar1=rstd, scalar2=-1.0,
                                                   op0=ALU.mult, op1=ALU.mult), reads=[smb], writes=[smb])
            S_.op("act", lambda e: e.activation(out=xb[:], in_=xb[:], func=AF.Identity, scale=rstd, bias=nmr),
                  reads=[xb, smb], writes=[xb])
            S_.op("dve", lambda e: e.tensor_tensor(out=xb[:], in0=xb[:], in1=gap, op=ALU.mult),
                  reads=[xb, gb], writes=[xb])
            S_.op("dve", lambda e: e.tensor_tensor(out=xb[:], in0=xb[:], in1=bap, op=ALU.add),
                  reads=[xb, bb], writes=[xb])

        pyi = 0
        for pz in range(NPASS):
            S_.dma("pool", r32(hT[:]), aT_d[pz], writes=[hT])
            for t in range(TT):
                S_.dma("sp", acc[t][:], x_d[pz, t], writes=[acc[t]])
            S_.dma("sp", par[0][:], par_d[P_GATE1], writes=[par[0]])
            S_.dma("sp", par[1][:], par_d[P_LNG1], writes=[par[1]])
            S_.dma("sp", par[2][:], par_d[P_LNB1], writes=[par[2]])
            for n in range(4):
                wb = wdb[n % 2]
                S_.dma("pool", r32(wb[:]), wp_d[n], writes=[wb])
                wv = wb[:].rearrange("p (k c) -> p k c", k=KC)
                for t in range(TT):
                    pb = py[pyi % 4]
                    pyi += 1
                    for k in range(KC):
                        S_.op("pe", lambda e, k=k, t=t, pb=pb, wv=wv: e.matmul(
                            pb[:], lhsT=r32(hT[:, k, t * 128:(t + 1) * 128]), rhs=r32(wv[:, k, :]),
                            start=(k == 0), stop=(k == KC - 1)), reads=[hT, wb], writes=[pb])
                    cs = slice(n * 512, (n + 1) * 512)
                    sg = aTb[(n * TT + t) % 8]
                    S_.op("dve", lambda e, pb=pb, sg=sg, cs=cs: e.tensor_tensor(
                        out=r32(sg[:]), in0=pb[:], in1=par[0][:, cs], op=ALU.mult),
                        reads=[pb, par[0]], writes=[sg])
                    S_.op("dve", lambda e, t=t, sg=sg, cs=cs: e.scalar_tensor_tensor(
                        out=acc[t][:, cs], in0=acc[t][:, cs], scalar=ALPHA, in1=sg[:],
                        op0=ALU.mult, op1=ALU.add), reads=[acc[t], sg], writes=[acc[t]])
            for t in range(TT):
                ln_tile(acc[t], par[1], par[1][:], par[2], par[2][:], sm[t % 2])
            S_.dma("sp", par[0][:], par_d[P_SC2], writes=[par[0]])
            S_.dma("pool", r32(sh2[:, 0:D]), par_d[P_SH2], writes=[sh2])
            S_.op("dve", lambda e: e.tensor_scalar(out=par[0][:], in0=par[0][:], scalar1=1.0, scalar2=None,
                                                    op0=ALU.add), reads=[par[0]], writes=[par[0]])
            for t in range(TT):
                S_.op("dve", lambda e, t=t: e.tensor_tensor(out=r32(scr[:, 0:D]), in0=acc[t][:], in1=par[0][:], op=ALU.mult),
                      reads=[acc[t], par[0]], writes=[scr])
                S_.op("dve", lambda e: e.tensor_tensor(out=r32(scr[:, 0:D]), in0=scr[:, 0:D], in1=sh2[:, 0:D], op=ALU.add),
                      reads=[scr, sh2], writes=[scr])
                S_.op("dve", lambda e, t=t: e.tensor_scalar(out=acc[t][:], in0=acc[t][:], scalar1=ALPHA,
                                                             scalar2=None, op0=ALU.mult),
                      reads=[acc[t]], writes=[acc[t]])
                for k0 in range(0, KC, 4):
                    pb = py[pyi % 4]
                    pyi += 1
                    for j in range(4):
                        k = k0 + j
                        S_.op("pe", lambda e, j=j, k=k, pb=pb: e.transpose(
                            out=pb[:, j * 128:(j + 1) * 128], in_=scr[:, k * 128:(k + 1) * 128],
                            identity=ident[:]), reads=[scr, ident], writes=[pb])
                    S_.op("act", lambda e, k0=k0, t=t, pb=pb: e.activation(
                        out=r32(hT[:, k0:k0 + 4, t * 128:(t + 1) * 128]),
                        in_=pb[:].rearrange("p (a b) -> p a b", a=4), func=AF.Identity),
                        reads=[pb], writes=[hT])
            for t in range(TT):
                pb = py[pyi % 4]
                pyi += 1
                for k in range(KC):
                    S_.op("pe", lambda e, k=k, t=t, pb=pb: e.matmul(
                        pb[:, 0:36], lhsT=hT[:, k, t * 128:(t + 1) * 128], rhs=wr[:, k, :],
                        start=(k == 0), stop=(k == KC - 1)), reads=[hT, wr], writes=[pb])
                s = sm[t % 2]
                lg = s[:, 0:36]
                m4, nm4, s4, pgp = s[:, 36:37], s[:, 37:38], s[:, 38:39], s[:, 39:40]
                oh4 = s[:, 40:44]
                e4 = s[:, 44:48]
                sel = s[:, 48:56]
                S_.op("dve", lambda e: e.tensor_tensor(out=lg, in0=pb[:, 0:36], in1=br[:], op=ALU.add),
                      reads=[pb, br], writes=[s])
                S_.op("dve", lambda e: e.reduce_max(out=m4, in_=s[:, 0:4], axis=AX.X), reads=[s], writes=[s])
                S_.op("dve", lambda e: e.tensor_scalar(out=nm4, in0=m4, scalar1=-1.0, scalar2=None, op0=ALU.mult),
                      reads=[s], writes=[s])
                S_.op("act", lambda e: e.activation(out=e4, in_=s[:, 0:4], func=AF.Exp, bias=nm4, accum_out=s4),
                      reads=[s], writes=[s])
                S_.op("dve", lambda e: e.reciprocal(out=pgp, in_=s4), reads=[s], writes=[s])
                S_.op("dve", lambda e: e.tensor_scalar(out=oh4, in0=s[:, 0:4], scalar1=m4, scalar2=None,
                                                       op0=ALU.is_equal), reads=[s], writes=[s])
                S_.op("dve", lambda e: e.tensor_scalar(out=sel, in0=s[:, 4:12], scalar1=s[:, 40:41], scalar2=None,
                                                       op0=ALU.mult), reads=[s], writes=[s])
                for g in range(1, 4):
                    S_.op("dve", lambda e, g=g: e.scalar_tensor_tensor(
                        out=sel, in0=s[:, 4 + 8 * g:12 + 8 * g], scalar=s[:, 40 + g:41 + g], in1=sel,
                        op0=ALU.mult, op1=ALU.add), reads=[s], writes=[s])
                s2b = sm[t % 2]
                m1, m2, nm1, dd, p1, p2 = [s[:, 56 + i:57 + i] for i in range(6)]
                g1, g2 = s[:, 62:63], s[:, 63:64]
                o1 = G[:, t, 0:8]
                o2 = G[:, t, 8:16]
                sel2 = G[:, t, 16:24]
                g8 = G[:, t, 24:32]
                S_.op("dve", lambda e: e.reduce_max(out=m1, in_=sel, axis=AX.X), reads=[s], writes=[s])
                S_.op("dve", lambda e: e.tensor_scalar(out=o1, in0=sel, scalar1=m1, scalar2=None, op0=ALU.is_equal),
                      reads=[s], writes=[G])
                S_.op("dve", lambda e: e.scalar_tensor_tensor(out=sel2, in0=o1, scalar=-1e30, in1=sel,
                                                              op0=ALU.mult, op1=ALU.add), reads=[s, G], writes=[G])
                S_.op("dve", lambda e: e.reduce_max(out=m2, in_=sel2, axis=AX.X), reads=[G], writes=[s])
                S_.op("dve", lambda e: e.tensor_scalar(out=o2, in0=sel2, scalar1=m2, scalar2=None, op0=ALU.is_equal),
                      reads=[s, G], writes=[G])
                S_.op("dve", lambda e: e.tensor_scalar(out=nm1, in0=m1, scalar1=-1.0, scalar2=None, op0=ALU.mult),
                      reads=[s], writes=[s])
                S_.op("act", lambda e: e.activation(out=dd, in_=m2, func=AF.Exp, bias=nm1), reads=[s], writes=[s])
                S_.op("dve", lambda e: e.tensor_scalar(out=p1, in0=dd, scalar1=1.0, scalar2=None, op0=ALU.add),
                      reads=[s], writes=[s])
                S_.op("dve", lambda e: e.reciprocal(out=p1, in_=p1), reads=[s], writes=[s])
                S_.op("dve", lambda e: e.tensor_tensor(out=p2, in0=dd, in1=p1, op=ALU.mult), reads=[s], writes=[s])
                S_.op("dve", lambda e: e.tensor_tensor(out=g1, in0=p1, in1=pgp, op=ALU.mult), reads=[s], writes=[s])
                S_.op("dve", lambda e: e.tensor_tensor(out=g2, in0=p2, in1=pgp, op=ALU.mult), reads=[s], writes=[s])
                S_.op("dve", lambda e: e.tensor_scalar(out=g8, in0=o1, scalar1=g1, scalar2=None, op0=ALU.mult),
                      reads=[s, G], writes=[G])
                S_.op("dve", lambda e: e.scalar_tensor_tensor(out=g8, in0=o2, scalar=g2, in1=g8,
                                                              op0=ALU.mult, op1=ALU.add), reads=[s, G], writes=[G])
                S_.op("dve", lambda e: e.tensor_copy(out=sel, in_=g8), reads=[G], writes=[s])
                for g in range(4):
                    S_.op("dve", lambda e, g=g: e.tensor_scalar(
                        out=G[:, t, 8 * g:8 * g + 8], in0=sel, scalar1=s[:, 40 + g:41 + g], scalar2=None,
                        op0=ALU.mult), reads=[s], writes=[G])
            S_.dma("sp", par[0][:], par_d[P_GATE2], writes=[par[0]])
            ui = 0
            S_.dma("pool", r32(wdb[0][:]), wd_d[0], writes=[wdb[0]])
            for ex in range(n_experts):
                wb = wdb[ex % 2]
                pre = {}
                for hc in range(2):
                    gbp = gub[(ui + hc) % 2]
                    S_.dma("pool", r32(gbp[:]), wgu_d[ex * 4 + hc], writes=[gbp])
                    pre[hc] = gbp
                for hc in range(4):
                    S_.op("pool", lambda e, hc=hc, wb=wb: e.tensor_tensor(
                        out=r32(wb[:, hc * D:(hc + 1) * D]), in0=wb[:, hc * D:(hc + 1) * D], in1=par[0][:], op=ALU.mult),
                        reads=[wb, par[0]], writes=[wb])
                if ex + 1 < n_experts:
                    wbn = wdb[(ex + 1) % 2]
                    S_.dma("pool", r32(wbn[:]), wd_d[ex + 1], writes=[wbn])
                for hc in range(4):
                    gb = gub[ui % 2]
                    if hc not in pre:
                        S_.dma("pool", r32(gb[:]), wgu_d[ex * 4 + hc], writes=[gb])
                    gv = gb[:].rearrange("p (j k c) -> p j k c", j=2, k=KC)
                    pgb, pub = pg[ui % 2], pu[ui % 2]
                    for k in range(KC):
                        S_.op("pe", lambda e, k=k, gv=gv, pgb=pgb: e.matmul(
                            pgb[:], lhsT=r32(gv[:, 0, k, :]), rhs=r32(hT[:, k, :]),
                            start=(k == 0), stop=(k == KC - 1)), reads=[gb, hT], writes=[pgb])
                    for k in range(KC):
                        S_.op("pe", lambda e, k=k, gv=gv, pub=pub: e.matmul(
                            pub[:], lhsT=r32(gv[:, 1, k, :]), rhs=r32(hT[:, k, :]),
                            start=(k == 0), stop=(k == KC - 1)), reads=[gb, hT], writes=[pub])
                    ab = aTb[(ex % 2) * 4 + hc]
                    S_.op("act", lambda e, ab=ab, pgb=pgb: e.activation(out=r32(ab[:]), in_=pgb[:], func=AF.Silu),
                          reads=[pgb], writes=[ab])
                    S_.op("dve", lambda e, pub=pub, ab=ab: e.tensor_tensor(
                        out=r32(ab[:]), in0=ab[:], in1=pub[:], op=ALU.mult), reads=[ab, pub], writes=[ab])
                    ui += 1
                abs_ = [aTb[(ex % 2) * 4 + hc] for hc in range(4)]
                for t in range(TT):
                    for n in range(4):
                        pb = py[pyi % 4]
                        pyi += 1
                        for hc in range(4):
                            S_.op("pe", lambda e, hc=hc, t=t, n=n, pb=pb, wb=wb, abs_=abs_: e.matmul(
                                pb[:], lhsT=r32(abs_[hc][:, t * 128:(t + 1) * 128]),
                                rhs=r32(wb[:, hc * D + n * 512: hc * D + (n + 1) * 512]),
                                start=(hc == 0), stop=(hc == 3)), reads=[abs_[hc], wb], writes=[pb])
                        cs = slice(n * 512, (n + 1) * 512)
                        S_.op("dve", lambda e, t=t, cs=cs, pb=pb, ex=ex: e.scalar_tensor_tensor(
                            out=acc[t][:, cs], in0=pb[:], scalar=G[:, t, ex:ex + 1], in1=acc[t][:, cs],
                            op0=ALU.mult, op1=ALU.add), reads=[pb, G, acc[t]], writes=[acc[t]])
            S_.dma("sp", par[1][:], par_d[P_LNG2], writes=[par[1]])
            S_.dma("sp", par[2][:], par_d[P_LNB2], writes=[par[2]])
            for t in range(TT):
                ln_tile(acc[t], par[1], par[1][:], par[2], par[2][:], sm[t % 2])
                S_.dma("act", out_d[pz, t], acc[t][:], reads=[acc[t]])
                S_.mark_output(acc[t])
        S_.finish()
    return nc


def pack_experts(wg, wu, wd):
    st = np.stack([wg, wu], axis=1).reshape(NE, 2, KC, 128, 4, 128)
    wgu = np.ascontiguousarray(st.transpose(0, 4, 3, 1, 2, 5)).reshape(NE * 4, 128, 2 * KC * 128)
    wdp = np.ascontiguousarray(wd.reshape(NE, 4, 128, D).transpose(0, 2, 1, 3)).reshape(NE, 128, 4 * D)
    return wgu, wdp


def bc128(v):
    return np.ascontiguousarray(np.broadcast_to(v, (128,) + v.shape))


_NC_CACHE = {}


def run_l3(a_tok, wp, x_tok, rows, wrc, brc, wrf, brf, wgu, wdp, trace=False):
    if "l3" not in _NC_CACHE:
        _NC_CACHE["l3"] = build_l3()
    nc = _NC_CACHE["l3"]
    wpp = np.ascontiguousarray(wp.reshape(KC, 128, 4, 512).transpose(2, 1, 0, 3)).reshape(4, 128, KC * 512)
    wr = np.ascontiguousarray(np.concatenate([wrc, wrf], axis=1).reshape(KC, 128, 36).transpose(1, 0, 2))
    br = bc128(np.concatenate([brc, brf]))
    ident = np.eye(128, dtype=np.float32)
    pars = [np.ascontiguousarray(np.stack([bc128(v) for v in rows[b]])) for b in range(B)]
    in_maps = []
    TC = NTOK // NCORES
    for c in range(NCORES):
        b = c // (NCORES // B)
        a_c = a_tok[c * TC:(c + 1) * TC]
        aT = np.ascontiguousarray(a_c.reshape(NPASS, TP, KC, 128).transpose(0, 3, 2, 1))
        xc = np.ascontiguousarray(x_tok[c * TC:(c + 1) * TC]).reshape(NPASS, TT, 128, D)
        in_maps.append({"aT": aT, "wp": wpp, "x": xc, "par": pars[b], "wr": wr, "br": br,
                        "wgu": wgu, "wd": wdp, "ident": ident})
    res = run_bass_kernel_spmd(nc, in_maps, core_ids=list(range(NCORES)), trace=trace)
    out = np.concatenate([res.results[c]["out"].reshape(TC, D) for c in range(NCORES)], axis=0)
    if trace:
        return out, res
    return out


HPC = 4
BLK = 256
NBLK = S // BLK
QB = 512
SM_SCALE = HD ** -0.5
TWO_PI_HI = 6.28125
TWO_PI_LO = 2.0 * np.pi - 6.28125


def build_l2(hpc=HPC, do_tab=True, do_proj=True, do_attn=True, nblk=NBLK, nqb=S // QB, do_sel=True, do_rot=True, do_v=True):
    nc = bass.Bass("TRN2", target_bir_lowering=False)
    xT_d = nc.dram_tensor("xT", [NBLK, 128, KC * BLK], F32R, kind="ExternalInput").ap()
    mod_d = nc.dram_tensor("mod", [128, 2 * KC], F32, kind="ExternalInput").ap()
    wqk_d = nc.dram_tensor("wqk", [HPC, 128, 6 * KC * 128], F32R, kind="ExternalInput").ap()
    wv_d = nc.dram_tensor("wv", [HPC, 128, KC * 384], F32R, kind="ExternalInput").ap()
    pos_d = nc.dram_tensor("pos", [32, S], I32, kind="ExternalInput").ap()
    invf_d = nc.dram_tensor("invf", [32, 1], F32, kind="ExternalInput").ap()
    rot_d = nc.dram_tensor("rot", [128, 128], F32, kind="ExternalInput").ap()
    msk_d = nc.dram_tensor("msk", [2, 128, QB], F32, kind="ExternalInput").ap()
    o_d = nc.dram_tensor("o", [HPC, S // 128, 128, HD], F32, kind="ExternalOutput").ap()

    with ExitStack() as es:
        S_ = Sched(nc, es)
        xb = [S_.sbuf([128, KC * BLK], name=f"xb{i}") for i in range(2)]
        wqk = S_.sbuf([128, 6 * KC * 128], name="wqk_sb")
        wv = S_.sbuf([128, KC * 384], name="wv_sb")
        QT = [S_.sbuf([128, S], BF16, name=f"QT{g}") for g in range(NG)]
        KT = [S_.sbuf([128, S], BF16, name=f"KT{g}") for g in range(NG)]
        V = S_.sbuf([128, S // 128, NG, HD + 1], BF16, name="V")
        cosT = S_.sbuf([128, S], BF16, name="cosT")
        sinT = S_.sbuf([128, S], BF16, name="sinT")
        mod = S_.sbuf([128, 2 * KC], name="mod_sb")
        invf = S_.sbuf([32, 1], name="invf_sb")
        rotf = S_.sbuf([128, 128], name="rotf")
        rotb = S_.sbuf([128, 128], BF16, name="rotb")
        mskf = S_.sbuf([128, QB], name="mskf")
        msk = [S_.sbuf([128, QB], BF16, name=f"msk{i}") for i in range(2)]
        PT = [S_.sbuf([128, QB], BF16, name=f"PT{i}") for i in range(4)]
        t1 = [S_.sbuf([128, BLK], name=f"t1_{i}") for i in range(2)]
        t2 = [S_.sbuf([128, BLK], name=f"t2_{i}") for i in range(2)]
        osb = [S_.sbuf([128, HD], name=f"osb{i}") for i in range(2)]
        rden = [S_.sbuf([128, 1], name=f"rden{i}") for i in range(2)]
        ps = [S_.psum([128, 512], name=f"psb{i}") for i in range(8)]

        S_.dma("sp", mod[:], mod_d, writes=[mod])
        S_.dma("sp", invf[:], invf_d, writes=[invf])
        S_.dma("sp", rotf[:], rot_d, writes=[rotf])
        S_.op("dve", lambda e: e.tensor_copy(out=rotb[:], in_=rotf[:]), reads=[rotf], writes=[rotb])
        for i in range(2):
            S_.dma("sp", mskf[:], msk_d[i], writes=[mskf])
            S_.op("dve", lambda e, i=i: e.tensor_copy(out=msk[i][:], in_=mskf[:]), reads=[mskf], writes=[msk[i]])
        S_.op("pool", lambda e: e.memset(V[:], 1.0), writes=[V])
        S_.op("pool", lambda e: e.memset(cosT[:], 1.0), writes=[cosT])
        S_.op("pool", lambda e: e.memset(sinT[:], 0.0), writes=[sinT])
        S_.op("dve", lambda e: e.tensor_scalar(out=mod[:, 0:KC], in0=mod[:, 0:KC], scalar1=1.0, scalar2=None,
                                               op0=ALU.add), reads=[mod], writes=[mod])

        for ch in range(S // BLK if do_tab else 0):
            cs = slice(ch * BLK, (ch + 1) * BLK)
            bA, bB, bC, bM = t1[0], t1[1], t2[0], t2[1]
            posi = bA[0:32, :].bitcast(I32)
            ang = bA[0:32, :]
            tq = bB[0:32, :]
            ni = bB[0:32, :].bitcast(I32)
            nf = bB[0:32, :]
            rr = bC[0:32, :]
            mk = bM[0:32, :]
            S_.dma("sp", posi, pos_d[:, cs], writes=[bA])
            S_.op("dve", lambda e: e.tensor_copy(out=ang, in_=posi), reads=[bA], writes=[bA])
            S_.op("dve", lambda e: e.tensor_scalar(out=ang, in0=ang, scalar1=invf[:, 0:1], scalar2=None, op0=ALU.mult),
                  reads=[bA, invf], writes=[bA])
            for which, tab in ((0, sinT), (1, cosT)):
                off = 0.0 if which == 0 else float(np.pi / 2)
                S_.op("dve", lambda e, off=off: e.tensor_scalar(out=tq, in0=ang, scalar1=off, scalar2=float(1 / (2 * np.pi)),
                                                                op0=ALU.add, op1=ALU.mult), reads=[bA], writes=[bB])
                S_.op("dve", lambda e: e.tensor_copy(out=ni, in_=tq), reads=[bB], writes=[bB])
                S_.op("dve", lambda e: e.tensor_copy(out=nf, in_=ni), reads=[bB], writes=[bB])
                S_.op("dve", lambda e, off=off: e.tensor_scalar(out=rr, in0=ang, scalar1=off, scalar2=None, op0=ALU.add),
                      reads=[bA], writes=[bC])
                S_.op("dve", lambda e: e.scalar_tensor_tensor(out=rr, in0=nf, scalar=-TWO_PI_HI, in1=rr,
                                                              op0=ALU.mult, op1=ALU.add), reads=[bC, bB], writes=[bC])
                S_.op("dve", lambda e: e.scalar_tensor_tensor(out=rr, in0=nf, scalar=-TWO_PI_LO, in1=rr,
                                                              op0=ALU.mult, op1=ALU.add), reads=[bC, bB], writes=[bC])
                S_.op("dve", lambda e: e.tensor_scalar(out=mk, in0=rr, scalar1=float(np.pi), scalar2=float(-2 * np.pi),
                                                       op0=ALU.is_gt, op1=ALU.mult), reads=[bC], writes=[bM])
                S_.op("dve", lambda e: e.tensor_tensor(out=rr, in0=rr, in1=mk, op=ALU.add), reads=[bC, bM], writes=[bC])
                S_.op("dve", lambda e: e.tensor_scalar(out=mk, in0=rr, scalar1=float(-np.pi), scalar2=float(2 * np.pi),
                                                       op0=ALU.is_lt, op1=ALU.mult), reads=[bC], writes=[bM])
                S_.op("dve", lambda e: e.tensor_tensor(out=rr, in0=rr, in1=mk, op=ALU.add), reads=[bC, bM], writes=[bC])
                S_.op("dve", lambda e: e.tensor_scalar(out=rr, in0=rr, scalar1=float(np.pi), scalar2=float(-np.pi),
                                                       op0=ALU.min, op1=ALU.max), reads=[bC], writes=[bC])
                S_.op("act", lambda e, tab=tab, cs=cs: e.activation(out=tab[0:32, cs], in_=rr, func=AF.Sin),
                      reads=[bC], writes=[tab])

        pi_ = [0]
        pti = 0
        oi = 0
        pref = set()
        xk = [[Buf(xb[i].t, f"xk{i}_{k}") for k in range(KC)] for i in range(2)]

        def load_x(bi, blk):
            for q4 in range(4):
                cols = slice(q4 * 4 * BLK, (q4 + 1) * 4 * BLK)
                S_.dma("pool", r32(xb[bi].t[:, cols]), xT_d[blk][:, cols], writes=xk[bi][4 * q4:4 * q4 + 4])

        for hl in range(hpc):
            if ("w", hl) not in pref:
                S_.dma("pool", r32(wqk[:]), wqk_d[hl], writes=[wqk])
                S_.dma("pool", r32(wv[:]), wv_d[hl], writes=[wv])
            wq = wqk[:].rearrange("p (j k c) -> p j k c", j=6, k=KC)
            wvv = wv[:].rearrange("p (k c) -> p k c", k=KC)
            for blk in range(nblk if do_proj else 0):
                x_ = xb[blk % 2]
                xk_ = xk[blk % 2]
                if ("x", hl, blk) not in pref:
                    load_x(blk % 2, blk)
                xv = x_[:].rearrange("p (k t) -> p k t", k=KC)
                for k in range(KC):
                    if k % 2 == 0:
                        S_.op("act", lambda e, k=k, xv=xv: e.activation(
                            out=r32(xv[:, k, :]), in_=xv[:, k, :], func=AF.Identity,
                            scale=mod[:, k:k + 1], bias=mod[:, KC + k:KC + k + 1]), reads=[xk_[k], mod], writes=[xk_[k]])
                    else:
                        S_.op("dve", lambda e, k=k, xv=xv: e.tensor_scalar(
                            out=r32(xv[:, k, :]), in0=xv[:, k, :], scalar1=mod[:, k:k + 1],
                            scalar2=mod[:, KC + k:KC + k + 1], op0=ALU.mult, op1=ALU.add),
                            reads=[xk_[k], mod], writes=[xk_[k]])
                cs = slice(blk * BLK, (blk + 1) * BLK)

                def proj(j):
                    g, isk = j // 2, j % 2
                    dst = (KT if isk else QT)[g]
                    pq = ps[pi_[0] % 4]
                    pr = ps[4 + (pi_[0] % 2)]
                    pi_[0] += 1
                    for k in range(KC):
                        S_.op("pe", lambda e, k=k, j=j, pq=pq: e.matmul(
                            pq[:, 0:BLK], lhsT=r32(wq[:, j, k, :]), rhs=r32(xv[:, k, :]),
                            start=(k == 0), stop=(k == KC - 1)), reads=[wqk, xk_[k]], writes=[pq])
                    S_.op("act", lambda e, dst=dst, pq=pq: e.activation(
                        out=dst[:, cs], in_=pq[:, 0:BLK], func=AF.Identity), reads=[pq], writes=[dst])
                    return (j, dst, pq, pr)

                def rot(st):
                    j, dst, pq, pr = st
                    S_.op("pe", lambda e: e.matmul(
                        pr[:, 0:BLK], lhsT=rotb[:], rhs=dst[:, cs], start=True, stop=True),
                        reads=[rotb, dst], writes=[pr])
                    a1, a2 = t1[j % 2], t2[j % 2]
                    S_.op("dve", lambda e: e.tensor_tensor(
                        out=a1[:], in0=pq[:, 0:BLK], in1=cosT[:, cs], op=ALU.mult),
                        reads=[pq, cosT, dst], writes=[a1])
                    S_.op("dve", lambda e: e.tensor_tensor(
                        out=a2[:], in0=pr[:, 0:BLK], in1=sinT[:, cs], op=ALU.mult),
                        reads=[pr, sinT], writes=[a2])
                    S_.op("dve", lambda e: e.tensor_tensor(
                        out=dst[:, cs], in0=a1[:], in1=a2[:], op=ALU.add), reads=[a1, a2], writes=[dst])

                def vproj(tt):
                    pv = ps[6 + tt % 2]
                    for k in range(KC):
                        S_.op("pe", lambda e, k=k: e.matmul(
                            pv[:, 0:384], lhsT=r32(xv[:, k, tt * 128:(tt + 1) * 128]), rhs=r32(wvv[:, k, :]),
                            start=(k == 0), stop=(k == KC - 1)), reads=[xk_[k], wv], writes=[pv])
                    tile_i = blk * (BLK // 128) + tt
                    S_.op("act", lambda e: e.activation(
                        out=V[:, tile_i, :, 0:HD], in_=pv[:, 0:384].rearrange("p (g c) -> p g c", g=NG),
                        func=AF.Identity), reads=[pv], writes=[V])

                prev = None
                for j in range(6):
                    cur = proj(j)
                    if prev is not None:
                        rot(prev)
                    prev = cur
                vproj(0)
                rot(prev)
                vproj(1)
            if do_proj and do_attn and hl + 1 < hpc and nblk >= 2:
                S_.dma("pool", r32(wqk[:]), wqk_d[hl + 1], writes=[wqk])
                S_.dma("pool", r32(wv[:]), wv_d[hl + 1], writes=[wv])
                pref.add(("w", hl + 1))
                for pb_ in range(2):
                    load_x(pb_, pb_)
                    pref.add(("x", hl + 1, pb_))
            if not do_attn and hl == 0:
                S_.op("dve", lambda e: e.tensor_copy(out=osb[0][:], in_=QT[0][:, 0:HD]), reads=[QT[0], KT[0], V, cosT, sinT], writes=[osb[0]])
                S_.dma("sp", o_d[0, 0], osb[0][:], reads=[osb[0]])
            for qb in range(nqb if do_attn else 0):
                work = []
                for g, d in enumerate(DILS):
                    W_ = RADIUS * d
                    for kt in range(S // 128):
                        dl = kt * 128 - qb * QB
                        if dl - (QB - 1) <= W_ and dl + 127 >= -W_:
                            work.append((g, d, kt, dl))
                po = ps[4:8]
                qs = slice(qb * QB, (qb + 1) * QB)
                nw = len(work)
                pbase = pti
                pti += nw

                def issue_s(wi):
                    g, d, kt, dl = work[wi]
                    W_ = RADIUS * d
                    pS = ps[wi % 4]
                    S_.op("pe", lambda e: e.matmul(
                        pS[:], lhsT=KT[g][:, kt * 128:(kt + 1) * 128], rhs=QT[g][:, qs], start=True, stop=True),
                        reads=[KT[g], QT[g]], writes=[pS])
                    P_ = PT[(pbase + wi) % 4]
                    S_.op("act", lambda e: e.activation(out=P_[:], in_=pS[:], func=AF.Exp, scale=SM_SCALE),
                          reads=[pS], writes=[P_])
                    if g > 0:
                        S_.op("dve", lambda e: e.tensor_tensor(out=P_[:], in0=P_[:], in1=msk[g - 1][:],
                                                               op=ALU.mult), reads=[P_, msk[g - 1]], writes=[P_])
                    if do_sel and dl - (QB - 1) < -W_:
                        S_.op("pool", lambda e: e.affine_select(
                            out=P_[:], in_=P_[:], pattern=[[-1, QB]], compare_op=ALU.is_ge, fill=0.0,
                            base=dl + W_, channel_multiplier=1), reads=[P_], writes=[P_])
                    if do_sel and dl + 127 > W_:
                        S_.op("pool", lambda e: e.affine_select(
                            out=P_[:], in_=P_[:], pattern=[[1, QB]], compare_op=ALU.is_ge, fill=0.0,
                            base=W_ - dl, channel_multiplier=-1), reads=[P_], writes=[P_])

                def issue_pv(wi):
                    g, d, kt, dl = work[wi]
                    P_ = PT[(pbase + wi) % 4]
                    for sub in range(4):
                        S_.op("pe", lambda e, sub=sub: e.matmul(
                            po[sub][:, 0:HD + 1], lhsT=P_[:, sub * 128:(sub + 1) * 128], rhs=V[:, kt, g, :],
                            start=(wi == 0), stop=(wi == nw - 1)), reads=[P_, V], writes=[po[sub]])

                LA = 3
                for wi in range(min(LA, nw)):
                    issue_s(wi)
                for wi in range(nw):
                    if wi + LA < nw:
                        issue_s(wi + LA)
                    issue_pv(wi)
                for sub in range(4):
                    ob, rd = osb[oi % 2], rden[oi % 2]
                    oi += 1
                    S_.op("dve", lambda e, rd=rd, sub=sub: e.reciprocal(out=rd[:], in_=po[sub][:, HD:HD + 1]),
                          reads=[po[sub]], writes=[rd])
                    S_.op("dve", lambda e, rd=rd, ob=ob, sub=sub: e.tensor_scalar(
                        out=ob[:], in0=po[sub][:, 0:HD], scalar1=rd[:, 0:1], scalar2=None, op0=ALU.mult),
                        reads=[po[sub], rd], writes=[ob])
                    S_.dma("sp", o_d[hl, qb * 4 + sub], ob[:], reads=[ob])
                    S_.mark_output(ob)
        S_.finish()
    return nc


def rot_matrix():
    R = np.zeros((128, 128), np.float32)
    for i in range(16):
        R[16 + i, i] = -1.0
        R[i, 16 + i] = 1.0
    return R


def run_l2(x, positions, w_qkv, mod0, trace=False):
    if "l2" not in _NC_CACHE:
        _NC_CACHE["l2"] = build_l2()
    nc = _NC_CACHE["l2"]
    invf = (THETA ** (-np.arange(0, ROT, 2, dtype=np.float32) / ROT)).astype(np.float32)
    invf32 = np.concatenate([invf, invf]).reshape(32, 1).astype(np.float32)
    kl = np.arange(128)[:, None]
    ql = np.arange(QB)[None, :]
    msk = np.stack([((kl - ql) % d == 0).astype(np.float32) for d in DILS[1:]])
    rot = rot_matrix()
    w5 = w_qkv.reshape(KC, 128, NG, 3, NH, HD)
    xTs = []
    for b in range(B):
        xt = x[b].T.reshape(KC, 128, NBLK, BLK)
        xTs.append(np.ascontiguousarray(xt.transpose(2, 1, 0, 3)).reshape(NBLK, 128, KC * BLK))
    in_maps = []
    for c in range(NCORES):
        b, hg = c // 4, c % 4
        hs = slice(hg * HPC, (hg + 1) * HPC)
        wqk = w5[:, :, :, 0:2, hs, :]
        wqk = np.ascontiguousarray(wqk.transpose(4, 1, 2, 3, 0, 5)).reshape(HPC, 128, 6 * KC * 128)
        wv = w5[:, :, :, 2, hs, :]
        wv = np.ascontiguousarray(wv.transpose(3, 1, 0, 2, 4)).reshape(HPC, 128, KC * 384)
        shift, scale = mod0[b, 0:D], mod0[b, D:2 * D]
        modc = np.concatenate([scale.reshape(KC, 128).T, shift.reshape(KC, 128).T], axis=1).astype(np.float32)
        pos = np.ascontiguousarray(np.broadcast_to(positions[b].astype(np.int32), (32, S)))
        in_maps.append({"xT": xTs[b], "mod": np.ascontiguousarray(modc), "wqk": wqk, "wv": wv, "pos": pos,
                        "invf": invf32, "rot": rot, "msk": msk})
    res = run_bass_kernel_spmd(nc, in_maps, core_ids=list(range(NCORES)), trace=trace)
    o = np.zeros((B, S, NH, HD), np.float32)
    for c in range(NCORES):
        b, hg = c // 4, c % 4
        oc = res.results[c]["o"].reshape(HPC, S, HD)
        o[b, :, hg * HPC:(hg + 1) * HPC, :] = oc.transpose(1, 0, 2)
    o = o.reshape(NTOK, D)
    if trace:
        return o, res
    return o


def sched_barrier(S_):
    engs = list(S_.eng.keys())
    for en in engs:
        E = S_.eng[en]
        for x in engs:
            if x != en and S_.cnt[x] > 0 and S_.seen[en].get(("e", x), 0) < S_.cnt[x]:
                E.wait_ge(S_.sem[x], S_.cnt[x])
                S_.seen[en][("e", x)] = S_.cnt[x]
        for b in S_.out_bufs:
            if b.dsem is not None and S_.seen[en].get(("d", b.dsem), 0) < b.dcnt:
                E.wait_ge(b.dsem, b.dcnt)
                S_.seen[en][("d", b.dsem)] = b.dcnt


GC = 512
NST = S // 128


def build_l4():
    nc = bass.Bass("TRN2", target_bir_lowering=False)
    xT_d = nc.dram_tensor("xT", [NBLK, 128, KC * BLK], F32R, kind="ExternalInput").ap()
    mod_d = nc.dram_tensor("mod", [128, 2 * KC], F32, kind="ExternalInput").ap()
    win_d = nc.dram_tensor("win", [128, KC * GC], F32R, kind="ExternalInput").ap()
    cc_d = nc.dram_tensor("cc", [2, 128, 4 * GC], F32R, kind="ExternalInput").ap()
    dft_d = nc.dram_tensor("dft", [S // QB, 16, 128, 4 * QB], F32R, kind="ExternalInput").ap()
    y_d = nc.dram_tensor("yT", [4, 128, S], F32, kind="ExternalOutput").ap()
    with ExitStack() as es:
        S_ = Sched(nc, es)
        AB = [S_.sbuf([128, NST, GC], name=f"AB{i}") for i in range(2)]
        win = S_.sbuf([128, KC * GC], name="win_sb")
        xb = [S_.sbuf([128, KC * BLK], name=f"xb{i}") for i in range(1)]
        uT = S_.sbuf([128, 4, BLK], name="uT")
        ccs = [S_.sbuf([128, 4 * GC], name=f"ccs{i}") for i in range(2)]
        mod = S_.sbuf([128, 2 * KC], name="mod_sb")
        ysb = [S_.sbuf([128, QB], name=f"ysb{i}") for i in range(2)]
        ps = [S_.psum([128, 512], name=f"psb{i}") for i in range(8)]
        S_.dma("sp", mod[:], mod_d, writes=[mod])
        S_.op("dve", lambda e: e.tensor_scalar(out=mod[:, 0:KC], in0=mod[:, 0:KC], scalar1=1.0, scalar2=None,
                                               op0=ALU.add), reads=[mod], writes=[mod])
        S_.dma("pool", r32(win[:]), win_d, writes=[win])
        for i in range(2):
            S_.dma("pool", r32(ccs[i][:]), cc_d[i], writes=[ccs[i]])
        wv = win[:].rearrange("p (k c) -> p k c", k=KC)
        pi_ = 0
        xk = [Buf(xb[0].t, f"xk_{k}") for k in range(KC)]
        for blk in range(NBLK):
            x_ = xb[0]
            for q4 in range(4):
                cols = slice(q4 * 4 * BLK, (q4 + 1) * 4 * BLK)
                S_.dma("pool", r32(x_.t[:, cols]), xT_d[blk][:, cols], writes=xk[4 * q4:4 * q4 + 4])
            xv = x_[:].rearrange("p (k t) -> p k t", k=KC)
            for k in range(KC):
                if k % 2 == 0:
                    S_.op("act", lambda e, k=k, xv=xv: e.activation(
                        out=r32(xv[:, k, :]), in_=xv[:, k, :], func=AF.Identity,
                        scale=mod[:, k:k + 1], bias=mod[:, KC + k:KC + k + 1]), reads=[xk[k], mod], writes=[xk[k]])
                else:
                    S_.op("dve", lambda e, k=k, xv=xv: e.tensor_scalar(
                        out=r32(xv[:, k, :]), in0=xv[:, k, :], scalar1=mod[:, k:k + 1],
                        scalar2=mod[:, KC + k:KC + k + 1], op0=ALU.mult, op1=ALU.add),
                        reads=[xk[k], mod], writes=[xk[k]])
            for k in range(KC):
                for cc in range(4):
                    S_.op("pe", lambda e, k=k, cc=cc, xv=xv: e.matmul(
                        ps[cc][:, 0:BLK], lhsT=r32(wv[:, k, cc * 128:(cc + 1) * 128]), rhs=r32(xv[:, k, :]),
                        start=(k == 0), stop=(k == KC - 1)), reads=[win, xk[k]], writes=[ps[cc]])
            for cc in range(4):
                if cc % 2 == 0:
                    S_.op("act", lambda e, cc=cc: e.activation(out=r32(uT[:, cc, :]), in_=ps[cc][:, 0:BLK],
                                                               func=AF.Identity), reads=[ps[cc]], writes=[uT])
                else:
                    S_.op("dve", lambda e, cc=cc: e.tensor_copy(out=r32(uT[:, cc, :]), in_=ps[cc][:, 0:BLK]),
                          reads=[ps[cc]], writes=[uT])
            for tt in range(BLK // 128):
                st = blk * (BLK // 128) + tt
                for i in range(2):
                    pa = ps[4 + (2 * tt + i) % 4]
                    cv = ccs[i][:].rearrange("p (c m) -> p c m", c=4)
                    for cc in range(4):
                        S_.op("pe", lambda e, cc=cc, tt=tt, pa=pa, cv=cv: e.matmul(
                            pa[:], lhsT=r32(uT[:, cc, tt * 128:(tt + 1) * 128]), rhs=r32(cv[:, cc, :]),
                            start=(cc == 0), stop=(cc == 3)), reads=[uT, ccs[i]], writes=[pa])
                    if i == 0:
                        S_.op("act", lambda e, pa=pa, st=st: e.activation(out=r32(AB[0][:, st, :]), in_=pa[:],
                                                                          func=AF.Identity), reads=[pa], writes=[AB[0]])
                    else:
                        S_.op("dve", lambda e, pa=pa, st=st: e.tensor_copy(out=r32(AB[1][:, st, :]), in_=pa[:]),
                              reads=[pa], writes=[AB[1]])
        sched_barrier(S_)
        dpc = [Buf(win.t, f"dpc{i}") for i in range(4)]
        yi = 0
        di = 0
        for kb in range(S // QB):
            for pc in range(16):
                sg, cs_ = pc // 2, pc % 2
                db = dpc[di % 4]
                dcol = slice((di % 4) * 2048, (di % 4 + 1) * 2048)
                di += 1
                S_.dma("pool", r32(win.t[:, dcol]), dft_d[kb, pc], writes=[db])
                dv = win.t[:, dcol].rearrange("p (s k) -> p s k", s=4)
                for s4 in range(4):
                    st = sg * 4 + s4
                    for mc in range(4):
                        first = (pc == 0 and s4 == 0)
                        last = (pc == 15 and s4 == 3)
                        S_.op("pe", lambda e, mc=mc, st=st, s4=s4, cs_=cs_, dv=dv, first=first, last=last: e.matmul(
                            ps[mc][:], lhsT=r32(AB[cs_][:, st, mc * 128:(mc + 1) * 128]), rhs=r32(dv[:, s4, :]),
                            start=first, stop=last), reads=[AB[cs_], db], writes=[ps[mc]])
            for mc in range(4):
                yb = ysb[yi % 2]
                yi += 1
                if mc % 2 == 0:
                    S_.op("act", lambda e, mc=mc, yb=yb: e.activation(out=yb[:], in_=ps[mc][:], func=AF.Identity),
                          reads=[ps[mc]], writes=[yb])
                else:
                    S_.op("dve", lambda e, mc=mc, yb=yb: e.tensor_copy(out=yb[:], in_=ps[mc][:]),
                          reads=[ps[mc]], writes=[yb])
                S_.dma("sp", y_d[mc, :, kb * QB:(kb + 1) * QB], yb[:], reads=[yb])
        S_.finish()
    return nc


def dft_tables():
    n = np.arange(S)
    angS = 2 * np.pi * ((n[:, None] * n[None, :]) % S) / S
    CS = (np.cos(angS) / np.sqrt(S)).astype(np.float32)
    SS = (-np.sin(angS) / np.sqrt(S)).astype(np.float32)
    m = np.arange(GC)
    angC = 2 * np.pi * ((m[:, None] * m[None, :]) % GC) / GC
    CC = (np.cos(angC) / np.sqrt(GC)).astype(np.float32)
    SC = (np.sin(angC) / np.sqrt(GC)).astype(np.float32)
    M = np.stack([CS, SS])
    M = M.reshape(2, 8, 4, 128, S // QB, QB)
    dft = np.ascontiguousarray(M.transpose(4, 1, 0, 3, 2, 5)).reshape(S // QB, 16, 128, 4 * QB)
    cc = np.stack([CC, SC]).reshape(2, 4, 128, GC)
    cc = np.ascontiguousarray(cc.transpose(0, 2, 1, 3)).reshape(2, 128, 4 * GC)
    return dft, cc


def run_l4(x_tok, w_in, mod0):
    if "l4" not in _NC_CACHE:
        _NC_CACHE["l4"] = build_l4()
    nc = _NC_CACHE["l4"]
    dft, cc = dft_tables()
    x = x_tok.reshape(B, S, D)
    xTs = []
    for b in range(B):
        xt = x[b].T.reshape(KC, 128, NBLK, BLK)
        xTs.append(np.ascontiguousarray(xt.transpose(2, 1, 0, 3)).reshape(NBLK, 128, KC * BLK))
    in_maps = []
    for c in range(NCORES):
        b, g = c // 4, c % 4
        wi = w_in[:, g * GC:(g + 1) * GC].reshape(KC, 128, GC)
        wi = np.ascontiguousarray(wi.transpose(1, 0, 2)).reshape(128, KC * GC)
        shift, scale = mod0[b, 0:D], mod0[b, D:2 * D]
        modc = np.ascontiguousarray(np.concatenate([scale.reshape(KC, 128).T, shift.reshape(KC, 128).T], axis=1))
        in_maps.append({"xT": xTs[b], "mod": modc.astype(np.float32), "win": wi, "cc": cc, "dft": dft})
    res = run_bass_kernel_spmd(nc, in_maps, core_ids=list(range(NCORES)))
    y = np.zeros((B, S, D), np.float32)
    for c in range(NCORES):
        b, g = c // 4, c % 4
        yT = res.results[c]["yT"].reshape(GC, S)
        y[b, :, g * GC:(g + 1) * GC] = yT.T
    return y.reshape(NTOK, D)


def kernel(x, c, positions, ada_w, ada_b, attn_w_qkv, attn_w_o, fnet_w_in, fnet_w_out,
           ln_g, ln_b, router_coarse_w, router_coarse_b, router_fine_w, router_fine_b,
           expert_w_gate, expert_w_up, expert_w_down):
    f = lambda a: np.asarray(a, dtype=np.float32)
    x, c = f(x), f(c)
    positions = np.asarray(positions).astype(np.int32)
    ada_w, ada_b = f(ada_w), f(ada_b)
    ln_g, ln_b = f(ln_g), f(ln_b)
    mod = run_l1(c, ada_w, ada_b)
    x_tok = x.reshape(NTOK, D)
    for i in range(2):
        m0, m1 = mod[i, 0], mod[i, 1]
        if i == 0:
            a_tok = run_l2(x, positions, f(attn_w_qkv[0]), m0)
            wp = f(attn_w_o[0])
        else:
            a_tok = run_l4(x_tok, f(fnet_w_in[0]), m0)
            wp = f(fnet_w_out[0])
        rows = [[m0[b, 2 * D:3 * D], ln_g[i, 0], ln_b[i, 0], m1[b, D:2 * D], m1[b, 0:D], m1[b, 2 * D:3 * D],
                 ln_g[i, 1], ln_b[i, 1]] for b in range(B)]
        wgu, wdp = pack_experts(f(expert_w_gate[i]), f(expert_w_up[i]), f(expert_w_down[i]))
        x_tok = run_l3(a_tok, wp, x_tok, rows, f(router_coarse_w[i]), f(router_coarse_b[i]),
                       f(router_fine_w[i]), f(router_fine_b[i]), wgu, wdp)
        del wgu, wdp
    return x_tok.reshape(B, S, D).astype(np.float32)


TB = 1024
TTB = TB // 128
DMA_CAST = dict(max_dma_last_dim=4096)


def build_l3b(n_experts=NE):
    nc = bass.Bass("TRN2", target_bir_lowering=False)
    aT_d = nc.dram_tensor("aT", [128, KC, TB], F32, kind="ExternalInput").ap()
    wp_d = nc.dram_tensor("wp", [4, 128, KC * 512], F32, kind="ExternalInput").ap()
    x_d = nc.dram_tensor("x", [TTB, 128, D], F32, kind="ExternalInput").ap()
    par_d = nc.dram_tensor("par", [8, 128, D], F32, kind="ExternalInput").ap()
    wr_d = nc.dram_tensor("wr", [128, KC, 36], F32, kind="ExternalInput").ap()
    br_d = nc.dram_tensor("br", [128, 36], F32, kind="ExternalInput").ap()
    wgu_d = nc.dram_tensor("wgu", [n_experts * 4, 128, 2 * KC * 128], F32, kind="ExternalInput").ap()
    wd_d = nc.dram_tensor("wd", [n_experts, 128, 4 * D], F32, kind="ExternalInput").ap()
    id_d = nc.dram_tensor("ident", [128, 128], F32, kind="ExternalInput").ap()
    out_d = nc.dram_tensor("out", [TTB, 128, D], F32, kind="ExternalOutput").ap()

    with ExitStack() as es:
        S_ = Sched(nc, es)
        hT = S_.sbuf([128, KC, TB], BF16, name="hT")
        acc = [S_.sbuf([128, D], name=f"acc{t}") for t in range(TTB)]
        wdb = [S_.sbuf([128, 4 * D], BF16, name=f"wdb{i}") for i in range(2)]
        gub = [S_.sbuf([128, 2 * KC * 128], BF16, name=f"gub{i}") for i in range(2)]
        aTb = [S_.sbuf([128, TB], BF16, name=f"aTb{i}") for i in range(8)]
        par = [S_.sbuf([128, D], name=f"par{i}") for i in range(3)]
        scr = S_.sbuf([128, D], name="scr")
        sh2 = par[1]
        hTfb = par[2]
        tmpy = [S_.sbuf([128, 512], name=f"tmpy{i}") for i in range(2)]
        wr = S_.sbuf([128, KC, 36], name="wr_sb")
        br = S_.sbuf([128, 36], name="br_sb")
        ident = S_.sbuf([128, 128], name="ident_sb")
        G = S_.sbuf([128, TTB, NE], name="G")
        sm = [S_.sbuf([128, 64], name=f"sm{i}") for i in range(2)]
        ps = [S_.psum([128, 512], name=f"psb{i}") for i in range(8)]
        pgu, py = ps[0:4], ps[4:8]

        S_.dma("sp", wr[:], wr_d, writes=[wr])
        S_.dma("sp", br[:], br_d, writes=[br])
        S_.dma("sp", ident[:], id_d, writes=[ident])

        def ln_tile(xb, gb, gap, bb, bap, smb):
            s1, s2, mean, msq, var, std, rstd, nmr = [smb[:, i:i + 1] for i in range(8)]
            S_.op("act", lambda e: e.activation(out=scr[:], in_=xb[:], func=AF.Identity, accum_out=s1),
                  reads=[xb], writes=[scr, smb])
            S_.op("act", lambda e: e.activation(out=scr[:], in_=xb[:], func=AF.Square, accum_out=s2),
                  reads=[xb], writes=[scr, smb])
            S_.op("dve", lambda e: e.tensor_scalar(out=mean, in0=s1, scalar1=1.0 / D, scalar2=None, op0=ALU.mult),
                  reads=[smb], writes=[smb])
            S_.op("dve", lambda e: e.tensor_tensor(out=msq, in0=mean, in1=mean, op=ALU.mult),
                  reads=[smb], writes=[smb])
            S_.op("dve", lambda e: e.scalar_tensor_tensor(out=var, in0=s2, scalar=1.0 / D, in1=msq,
                                                          op0=ALU.mult, op1=ALU.subtract),
                  reads=[smb], writes=[smb])
            S_.op("dve", lambda e: e.tensor_scalar(out=var, in0=var, scalar1=EPS, scalar2=None, op0=ALU.add),
                  reads=[smb], writes=[smb])
            S_.op("act", lambda e: e.activation(out=std, in_=var, func=AF.Sqrt), reads=[smb], writes=[smb])
            S_.op("dve", lambda e: e.reciprocal(out=rstd, in_=std), reads=[smb], writes=[smb])
            S_.op("dve", lambda e: e.tensor_scalar(out=nmr, in0=mean, scalar1=rstd, scalar2=-1.0,
                                                   op0=ALU.mult, op1=ALU.mult), reads=[smb], writes=[smb])
            S_.op("act", lambda e: e.activation(out=xb[:], in_=xb[:], func=AF.Identity, scale=rstd, bias=nmr),
                  reads=[xb, smb], writes=[xb])
            S_.op("pool", lambda e: e.tensor_tensor(out=xb[:], in0=xb[:], in1=gap, op=ALU.mult),
                  reads=[xb, gb], writes=[xb])
            S_.op("pool", lambda e: e.tensor_tensor(out=xb[:], in0=xb[:], in1=bap, op=ALU.add),
                  reads=[xb, bb], writes=[xb])

        pyi = 0
        for k in range(KC):
            S_.dma("pool", hT[:, k, :], aT_d[:, k, :], writes=[hT], **DMA_CAST)
        for t in range(TTB):
            S_.dma("sp", acc[t][:], x_d[t], writes=[acc[t]])
        S_.dma("sp", par[0][:], par_d[P_GATE1], writes=[par[0]])
        S_.dma("sp", par[1][:], par_d[P_LNG1], writes=[par[1]])
        S_.dma("sp", par[2][:], par_d[P_LNB1], writes=[par[2]])
        for n in range(4):
            wb = wdb[n % 2]
            S_.dma("pool", wb[:], wp_d[n], writes=[wb], **DMA_CAST)
            wv = wb[:].rearrange("p (k c) -> p k c", k=KC)
            for t in range(TTB):
                pb = py[pyi % 4]
                pyi += 1
                for k in range(KC):
                    S_.op("pe", lambda e, k=k, t=t, pb=pb, wv=wv: e.matmul(
                        pb[:], lhsT=hT[:, k, t * 128:(t + 1) * 128], rhs=wv[:, k, :],
                        start=(k == 0), stop=(k == KC - 1)), reads=[hT, wb], writes=[pb])
                cs = slice(n * 512, (n + 1) * 512)
                sg = tmpy[(n * TTB + t) % 2]
                S_.op("dve", lambda e, pb=pb, sg=sg, cs=cs: e.tensor_tensor(
                    out=sg[:], in0=pb[:], in1=par[0][:, cs], op=ALU.mult),
                    reads=[pb, par[0]], writes=[sg])
                S_.op("pool", lambda e, t=t, sg=sg, cs=cs: e.scalar_tensor_tensor(
                    out=acc[t][:, cs], in0=acc[t][:, cs], scalar=ALPHA, in1=sg[:],
                    op0=ALU.mult, op1=ALU.add), reads=[acc[t], sg], writes=[acc[t]]) if False else \
                    S_.op("dve", lambda e, t=t, sg=sg, cs=cs: e.scalar_tensor_tensor(
                        out=acc[t][:, cs], in0=acc[t][:, cs], scalar=ALPHA, in1=sg[:],
                        op0=ALU.mult, op1=ALU.add), reads=[acc[t], sg], writes=[acc[t]])
        for t in range(TTB):
            ln_tile(acc[t], par[1], par[1][:], par[2], par[2][:], sm[t % 2])
        S_.dma("sp", par[0][:], par_d[P_SC2], writes=[par[0]])
        S_.dma("sp", sh2[:], par_d[P_SH2], writes=[sh2])
        S_.op("pool", lambda e: e.tensor_scalar(out=par[0][:], in0=par[0][:], scalar1=1.0, scalar2=None,
                                                op0=ALU.add), reads=[par[0]], writes=[par[0]])
        for t in range(TTB):
            S_.op("dve", lambda e, t=t: e.tensor_tensor(out=scr[:], in0=acc[t][:], in1=par[0][:], op=ALU.mult),
                  reads=[acc[t], par[0]], writes=[scr])
            S_.op("dve", lambda e: e.tensor_tensor(out=scr[:], in0=scr[:], in1=sh2[:], op=ALU.add),
                  reads=[scr, sh2], writes=[scr])
            S_.op("pool", lambda e, t=t: e.tensor_scalar(out=acc[t][:], in0=acc[t][:], scalar1=ALPHA,
                                                         scalar2=None, op0=ALU.mult),
                  reads=[acc[t]], writes=[acc[t]])
            for k0 in range(0, KC, 4):
                pb = py[pyi % 4]
                pyi += 1
                for j in range(4):
                    k = k0 + j
                    S_.op("pe", lambda e, j=j, k=k, pb=pb: e.transpose(
                        out=pb[:, j * 128:(j + 1) * 128], in_=scr[:, k * 128:(k + 1) * 128],
                        identity=ident[:]), reads=[scr, ident], writes=[pb])
                S_.op("act", lambda e, k0=k0, t=t, pb=pb: e.activation(
                    out=hT[:, k0:k0 + 4, t * 128:(t + 1) * 128],
                    in_=pb[:].rearrange("p (a b) -> p a b", a=4), func=AF.Identity),
                    reads=[pb], writes=[hT])
                S_.op("act", lambda e, k0=k0, pb=pb: e.activation(
                    out=hTfb[:].rearrange("p (k t) -> p k t", k=KC)[:, k0:k0 + 4, :],
                    in_=pb[:].rearrange("p (a b) -> p a b", a=4), func=AF.Identity),
                    reads=[pb], writes=[hTfb])
            pb = py[pyi % 4]
            pyi += 1
            for k in range(KC):
                S_.op("pe", lambda e, k=k, pb=pb: e.matmul(
                    pb[:, 0:36], lhsT=hTfb[:, k * 128:(k + 1) * 128], rhs=wr[:, k, :],
                    start=(k == 0), stop=(k == KC - 1)), reads=[hTfb, wr], writes=[pb])
            s = sm[t % 2]
            lg = s[:, 0:36]
            m4, nm4, s4, pgp = s[:, 36:37], s[:, 37:38], s[:, 38:39], s[:, 39:40]
            oh4 = s[:, 40:44]
            e4 = s[:, 44:48]
            sel = s[:, 48:56]
            S_.op("dve", lambda e: e.tensor_tensor(out=lg, in0=pb[:, 0:36], in1=br[:], op=ALU.add),
                  reads=[pb, br], writes=[s])
            S_.op("dve", lambda e: e.reduce_max(out=m4, in_=s[:, 0:4], axis=AX.X), reads=[s], writes=[s])
            S_.op("dve", lambda e: e.tensor_scalar(out=nm4, in0=m4, scalar1=-1.0, scalar2=None, op0=ALU.mult),
                  reads=[s], writes=[s])
            S_.op("act", lambda e: e.activation(out=e4, in_=s[:, 0:4], func=AF.Exp, bias=nm4, accum_out=s4),
                  reads=[s], writes=[s])
            S_.op("dve", lambda e: e.reciprocal(out=pgp, in_=s4), reads=[s], writes=[s])
            S_.op("dve", lambda e: e.tensor_scalar(out=oh4, in0=s[:, 0:4], scalar1=m4, scalar2=None,
                                                   op0=ALU.is_equal), reads=[s], writes=[s])
            S_.op("dve", lambda e: e.tensor_scalar(out=sel, in0=s[:, 4:12], scalar1=s[:, 40:41], scalar2=None,
                                                   op0=ALU.mult), reads=[s], writes=[s])
            for g in range(1, 4):
                S_.op("dve", lambda e, g=g: e.scalar_tensor_tensor(
                    out=sel, in0=s[:, 4 + 8 * g:12 + 8 * g], scalar=s[:, 40 + g:41 + g], in1=sel,
                    op0=ALU.mult, op1=ALU.add), reads=[s], writes=[s])
            m1, m2, nm1, dd, p1, p2 = [s[:, 56 + i:57 + i] for i in range(6)]
            g1, g2 = s[:, 62:63], s[:, 63:64]
            o1 = G[:, t, 0:8]
            o2 = G[:, t, 8:16]
            sel2 = G[:, t, 16:24]
            g8 = G[:, t, 24:32]
            S_.op("dve", lambda e: e.reduce_max(out=m1, in_=sel, axis=AX.X), reads=[s], writes=[s])
            S_.op("dve", lambda e: e.tensor_scalar(out=o1, in0=sel, scalar1=m1, scalar2=None, op0=ALU.is_equal),
                  reads=[s], writes=[G])
            S_.op("dve", lambda e: e.scalar_tensor_tensor(out=sel2, in0=o1, scalar=-1e30, in1=sel,
                                                          op0=ALU.mult, op1=ALU.add), reads=[s, G], writes=[G])
            S_.op("dve", lambda e: e.reduce_max(out=m2, in_=sel2, axis=AX.X), reads=[G], writes=[s])
            S_.op("dve", lambda e: e.tensor_scalar(out=o2, in0=sel2, scalar1=m2, scalar2=None, op0=ALU.is_equal),
                  reads=[s, G], writes=[G])
            S_.op("dve", lambda e: e.tensor_scalar(out=nm1, in0=m1, scalar1=-1.0, scalar2=None, op0=ALU.mult),
                  reads=[s], writes=[s])
            S_.op("act", lambda e: e.activation(out=dd, in_=m2, func=AF.Exp, bias=nm1), reads=[s], writes=[s])
            S_.op("dve", lambda e: e.tensor_scalar(out=p1, in0=dd, scalar1=1.0, scalar2=None, op0=ALU.add),
                  reads=[s], writes=[s])
            S_.op("dve", lambda e: e.reciprocal(out=p1, in_=p1), reads=[s], writes=[s])
            S_.op("dve", lambda e: e.tensor_tensor(out=p2, in0=dd, in1=p1, op=ALU.mult), reads=[s], writes=[s])
            S_.op("dve", lambda e: e.tensor_tensor(out=g1, in0=p1, in1=pgp, op=ALU.mult), reads=[s], writes=[s])
            S_.op("dve", lambda e: e.tensor_tensor(out=g2, in0=p2, in1=pgp, op=ALU.mult), reads=[s], writes=[s])
            S_.op("dve", lambda e: e.tensor_scalar(out=g8, in0=o1, scalar1=g1, scalar2=None, op0=ALU.mult),
                  reads=[s, G], writes=[G])
            S_.op("dve", lambda e: e.scalar_tensor_tensor(out=g8, in0=o2, scalar=g2, in1=g8,
                                                          op0=ALU.mult, op1=ALU.add), reads=[s, G], writes=[G])
            S_.op("dve", lambda e: e.tensor_copy(out=sel, in_=g8), reads=[G], writes=[s])
            for g in range(4):
                S_.op("dve", lambda e, g=g: e.tensor_scalar(
                    out=G[:, t, 8 * g:8 * g + 8], in0=sel, scalar1=s[:, 40 + g:41 + g], scalar2=None,
                    op0=ALU.mult), reads=[s], writes=[G])
        S_.dma("sp", par[0][:], par_d[P_GATE2], writes=[par[0]])
        ui = 0
        ti = 0
        stage = [par[1], par[2], scr]
        sctr = [0]

        def load_cast(dst_ap, dst_buf, src_ap, ceng):
            st = stage[sctr[0] % 3]
            sctr[0] += 1
            S_.dma("sp", st[:], src_ap, writes=[st])
            if ceng == "act":
                S_.op("act", lambda e: e.activation(out=dst_ap, in_=st[:], func=AF.Identity), reads=[st], writes=[dst_buf])
            else:
                S_.op(ceng, lambda e: e.tensor_copy(out=dst_ap, in_=st[:]), reads=[st], writes=[dst_buf])

        for ex in range(n_experts):
            wb = wdb[ex % 2]
            for hc in range(4):
                load_cast(wb[:, hc * D:(hc + 1) * D], wb, wd_d[ex][:, hc * D:(hc + 1) * D], "pool")
            for hc in range(4):
                gb = gub[ui % 2]
                ceng = "act" if ui % 2 == 0 else "dve"
                for j in range(2):
                    load_cast(gb[:, j * 2048:(j + 1) * 2048], gb, wgu_d[ex * 4 + hc][:, j * 2048:(j + 1) * 2048], ceng)
                gv = gb[:].rearrange("p (j k c) -> p j k c", j=2, k=KC)
                ab = aTb[(ex % 2) * 4 + hc]
                for half in range(TB // 512):
                    ts_ = slice(half * 512, (half + 1) * 512)
                    pgb, pub = pgu[(2 * ui + half) % 2 * 2], pgu[(2 * ui + half) % 2 * 2 + 1]
                    for k in range(KC):
                        S_.op("pe", lambda e, k=k, gv=gv, pgb=pgb, ts_=ts_: e.matmul(
                            pgb[:], lhsT=gv[:, 0, k, :], rhs=hT[:, k, ts_],
                            start=(k == 0), stop=(k == KC - 1)), reads=[gb, hT], writes=[pgb])
                    for k in range(KC):
                        S_.op("pe", lambda e, k=k, gv=gv, pub=pub, ts_=ts_: e.matmul(
                            pub[:], lhsT=gv[:, 1, k, :], rhs=hT[:, k, ts_],
                            start=(k == 0), stop=(k == KC - 1)), reads=[gb, hT], writes=[pub])
                    sg = tmpy[ti % 2]
                    ti += 1
                    S_.op("act", lambda e, sg=sg, pgb=pgb: e.activation(out=sg[:], in_=pgb[:], func=AF.Silu),
                          reads=[pgb], writes=[sg])
                    S_.op("dve", lambda e, pub=pub, ab=ab, sg=sg, ts_=ts_: e.tensor_tensor(
                        out=ab[:, ts_], in0=sg[:], in1=pub[:], op=ALU.mult), reads=[sg, pub], writes=[ab])
                ui += 1
            abs_ = [aTb[(ex % 2) * 4 + hc] for hc in range(4)]
            for t in range(TTB):
                for n in range(4):
                    pb = py[pyi % 4]
                    pyi += 1
                    for hc in range(4):
                        S_.op("pe", lambda e, hc=hc, t=t, n=n, pb=pb, wb=wb, abs_=abs_: e.matmul(
                            pb[:], lhsT=abs_[hc][:, t * 128:(t + 1) * 128],
                            rhs=wb[:, hc * D + n * 512: hc * D + (n + 1) * 512],
                            start=(hc == 0), stop=(hc == 3)), reads=[abs_[hc], wb], writes=[pb])
                    cs = slice(n * 512, (n + 1) * 512)
                    yt = tmpy[ti % 2]
                    ti += 1
                    S_.op("act", lambda e, pb=pb, yt=yt, t=t, ex=ex: e.activation(
                        out=yt[:], in_=pb[:], func=AF.Identity, scale=G[:, t, ex:ex + 1]), reads=[pb, G], writes=[yt])
                    S_.op("dve", lambda e, yt=yt, cs=cs: e.tensor_tensor(
                        out=yt[:], in0=yt[:], in1=par[0][:, cs], op=ALU.mult), reads=[yt, par[0]], writes=[yt])
                    S_.op("pool", lambda e, t=t, cs=cs, yt=yt: e.tensor_tensor(
                        out=acc[t][:, cs], in0=acc[t][:, cs], in1=yt[:], op=ALU.add),
                        reads=[yt, acc[t]], writes=[acc[t]])
        S_.dma("sp", par[1][:], par_d[P_LNG2], writes=[par[1]])
        S_.dma("sp", par[2][:], par_d[P_LNB2], writes=[par[2]])
        for t in range(TTB):
            ln_tile(acc[t], par[1], par[1][:], par[2], par[2][:], sm[t % 2])
            S_.dma("act", out_d[t], acc[t][:], reads=[acc[t]])
        S_.finish()
    return nc


def run_l3b(a_tok, wp, x_tok, rows, wrc, brc, wrf, brf, wgu, wdp, trace=False):
    if "l3b" not in _NC_CACHE:
        _NC_CACHE["l3b"] = build_l3b()
    nc = _NC_CACHE["l3b"]
    wpp = np.ascontiguousarray(wp.reshape(KC, 128, 4, 512).transpose(2, 1, 0, 3)).reshape(4, 128, KC * 512)
    wr = np.ascontiguousarray(np.concatenate([wrc, wrf], axis=1).reshape(KC, 128, 36).transpose(1, 0, 2))
    br = bc128(np.concatenate([brc, brf]))
    ident = np.eye(128, dtype=np.float32)
    pars = [np.ascontiguousarray(np.stack([bc128(v) for v in rows[b]])) for b in range(B)]
    in_maps = []
    TC = NTOK // NCORES
    for c in range(NCORES):
        b = c // (NCORES // B)
        a_c = a_tok[c * TC:(c + 1) * TC]
        aT = np.ascontiguousarray(a_c.reshape(TB, KC, 128).transpose(2, 1, 0))
        xc = np.ascontiguousarray(x_tok[c * TC:(c + 1) * TC]).reshape(TTB, 128, D)
        in_maps.append({"aT": aT, "wp": wpp, "x": xc, "par": pars[b], "wr": wr, "br": br,
                        "wgu": wgu, "wd": wdp, "ident": ident})
    res = run_bass_kernel_spmd(nc, in_maps, core_ids=list(range(NCORES)), trace=trace)
    out = np.concatenate([res.results[c]["out"].reshape(TC, D) for c in range(NCORES)], axis=0)
    if trace:
        return out, res
    return out
```

```python
import numpy as np
from contextlib import ExitStack
import concourse.bass as bass
import concourse.mybir as mybir
from concourse.bass_utils import run_bass_kernel_spmd

F32 = mybir.dt.float32
F32R = mybir.dt.float32r
BF16 = mybir.dt.bfloat16
I32 = mybir.dt.int32
AF = mybir.ActivationFunctionType
ALU = mybir.AluOpType
AX = mybir.AxisListType

NCORES = 8
D = 2048
KC = D // 128
B = 2
S = 4096
NTOK = B * S
HD = 128
NH = 16
NG = 3
DILS = (1, 4, 16)
RADIUS = 64
ROT = 32
THETA = 500000.0
NE = 32
EPG = 8
NGRP = 4
ED = 512
EPS = 1e-5
ALPHA = 4.0 ** 0.25

SAME_ENG_SYNC = True


class Buf:
    __slots__ = ("t", "name", "w", "r", "dsem", "dcnt")

    def __init__(self, t, name):
        self.t = t
        self.name = name
        self.w = None
        self.r = []
        self.dsem = None
        self.dcnt = 0

    def __getitem__(self, k):
        return self.t[k]


class Sched:
    def __init__(self, nc, es):
        self.nc = nc
        self.es = es
        self.eng = {"pe": nc.tensor, "dve": nc.vector, "act": nc.scalar,
                    "pool": nc.gpsimd, "sp": nc.sync}
        self.sem = {}
        self.cnt = {}
        self.seen = {}
        for k in self.eng:
            self.sem[k] = es.enter_context(nc.semaphore("s_" + k))
            self.cnt[k] = 0
            self.seen[k] = {}
        self.nbuf = 0
        self.out_bufs = []

    def sbuf(self, shape, dtype=F32, name=None):
        self.nbuf += 1
        name = name or f"sb{self.nbuf}"
        t = self.es.enter_context(self.nc.sbuf_tensor(name, list(shape), dtype))
        return Buf(t, name)

    def psum(self, shape, dtype=F32, name=None):
        self.nbuf += 1
        name = name or f"ps{self.nbuf}"
        t = self.es.enter_context(self.nc.psum_tensor(name, list(shape), dtype))
        return Buf(t, name)

    def view(self, name="v"):
        return Buf(None, name)

    def _collect(self, eng, reads, writes):
        need = {}

        def add(dep, raw):
            if dep is None:
                return
            if dep[0] == "dma":
                key = ("d", dep[1])
                sem, val = dep[1], dep[2]
            else:
                e2, val = dep
                if e2 == eng:
                    if eng == "pe" or not raw or not SAME_ENG_SYNC:
                        return
                key = ("e", e2)
                sem = self.sem[e2]
            if need.get(key, (None, 0))[1] < val:
                need[key] = (sem, val)

        for b in reads:
            add(b.w, True)
        for b in writes:
            add(b.w, True)
            for r in b.r:
                add(r, False)
        return need

    def _emit_waits(self, eng, need):
        E = self.eng[eng]
        seen = self.seen[eng]
        for key, (sem, val) in need.items():
            if seen.get(key, 0) < val:
                E.wait_ge(sem, val)
                seen[key] = val

    def op(self, eng, fn, reads=(), writes=()):
        need = self._collect(eng, reads, writes)
        self._emit_waits(eng, need)
        ins = fn(self.eng[eng])
        self.cnt[eng] += 1
        ins.then_inc(self.sem[eng], 1)
        me = (eng, self.cnt[eng])
        for b in reads:
            b.r.append(me)
        for b in writes:
            b.w = me
            b.r = []
        return ins

    def dma(self, q, out, in_, reads=(), writes=(), track=None, **kw):
        need = self._collect("dmaq_" + q, reads, writes)
        self._emit_waits(q, need)
        b = track or (writes[0] if writes else reads[0])
        if b.dsem is None:
            b.dsem = self.es.enter_context(self.nc.semaphore("d_" + b.name))
            self.out_bufs.append(b)
        ins = self.eng[q].dma_start(out=out, in_=in_, **kw)
        b.dcnt += 16
        ins.then_inc(b.dsem, 16)
        me = ("dma", b.dsem, b.dcnt)
        for x in reads:
            x.r.append(me)
        for x in writes:
            x.w = me
            x.r = []
        return ins

    def mark_output(self, b):
        if b not in self.out_bufs:
            self.out_bufs.append(b)

    def finish(self):
        E = self.eng["sp"]
        for b in self.out_bufs:
            if b.dsem is not None:
                E.wait_ge(b.dsem, b.dcnt)


def r32(ap):
    return ap.bitcast(F32R)


L1_COLS = 3072


def build_l1():
    nc = bass.Bass("TRN2", target_bir_lowering=False)
    cT = nc.dram_tensor("cT", [128, KC, B], F32, kind="ExternalInput").ap()
    w = nc.dram_tensor("w", [KC, 128, L1_COLS], F32, kind="ExternalInput").ap()
    bias = nc.dram_tensor("bias", [B, L1_COLS], F32, kind="ExternalInput").ap()
    out = nc.dram_tensor("out", [B, L1_COLS], F32, kind="ExternalOutput").ap()
    with ExitStack() as es:
        S_ = Sched(nc, es)
        sc = S_.sbuf([128, KC, B], name="sc")
        bt = S_.sbuf([B, L1_COLS], name="bt")
        ot = S_.sbuf([B, L1_COLS], name="ot")
        NB = 4
        wb = [S_.sbuf([128, L1_COLS], name=f"wb{i}") for i in range(NB)]
        ps = [S_.psum([128, 512], name=f"ps{i}") for i in range(6)]
        S_.dma("sp", sc[:], cT, writes=[sc])
        S_.dma("sp", bt[:], bias, writes=[bt])
        S_.op("act", lambda e: e.activation(out=sc[:], in_=sc[:], func=AF.Silu),
              reads=[sc], writes=[sc])
        for k in range(KC):
            wk = wb[k % NB]
            S_.dma("sp" if k % 2 == 0 else "pool", wk[:], w[k], writes=[wk])
            for n in range(6):
                S_.op("pe", lambda e, n=n, k=k, wk=wk: e.matmul(
                    ps[n][0:B, :], lhsT=sc[:, k, :], rhs=wk[:, n * 512:(n + 1) * 512],
                    start=(k == 0), stop=(k == KC - 1)),
                    reads=[sc, wk], writes=[ps[n]])
        for n in range(6):
            S_.op("dve", lambda e, n=n: e.tensor_tensor(
                out=ot[:, n * 512:(n + 1) * 512], in0=ps[n][0:B, :],
                in1=bt[:, n * 512:(n + 1) * 512], op=ALU.add),
                reads=[ps[n], bt], writes=[ot])
        S_.dma("sp", out, ot[:], reads=[ot])
        S_.mark_output(ot)
        S_.finish()
    return nc


def run_l1(c, ada_w, ada_b):
    nc = build_l1()
    cT = np.ascontiguousarray(c.T.reshape(KC, 128, B).transpose(1, 0, 2))
    in_maps = []
    for core in range(NCORES):
        s = core // 2
        i, j = s // 2, s % 2
        c0 = (core % 2) * L1_COLS
        wsl = np.ascontiguousarray(ada_w[i, j][:, c0:c0 + L1_COLS]).reshape(KC, 128, L1_COLS)
        bsl = np.ascontiguousarray(np.broadcast_to(ada_b[i, j][c0:c0 + L1_COLS], (B, L1_COLS)))
        in_maps.append({"cT": cT, "w": wsl, "bias": bsl})
    res = run_bass_kernel_spmd(nc, in_maps, core_ids=list(range(NCORES)))
    mod = np.zeros((2, 2, B, 3 * D), np.float32)
    for core in range(NCORES):
        s = core // 2
        i, j = s // 2, s % 2
        c0 = (core % 2) * L1_COLS
        mod[i, j][:, c0:c0 + L1_COLS] = res.results[core]["out"]
    return mod


NPASS = 2
TP = 512
TT = TP // 128
P_GATE1, P_LNG1, P_LNB1, P_SC2, P_SH2, P_GATE2, P_LNG2, P_LNB2 = range(8)


def build_l3(n_experts=NE):
    nc = bass.Bass("TRN2", target_bir_lowering=False)
    aT_d = nc.dram_tensor("aT", [NPASS, 128, KC, TP], F32R, kind="ExternalInput").ap()
    wp_d = nc.dram_tensor("wp", [4, 128, KC * 512], F32R, kind="ExternalInput").ap()
    x_d = nc.dram_tensor("x", [NPASS, TT, 128, D], F32, kind="ExternalInput").ap()
    par_d = nc.dram_tensor("par", [8, 128, D], F32, kind="ExternalInput").ap()
    wr_d = nc.dram_tensor("wr", [128, KC, 36], F32, kind="ExternalInput").ap()
    br_d = nc.dram_tensor("br", [128, 36], F32, kind="ExternalInput").ap()
    wgu_d = nc.dram_tensor("wgu", [NE * 4, 128, 2 * KC * 128], F32R, kind="ExternalInput").ap()
    wd_d = nc.dram_tensor("wd", [NE, 128, 4 * D], F32R, kind="ExternalInput").ap()
    id_d = nc.dram_tensor("ident", [128, 128], F32, kind="ExternalInput").ap()
    out_d = nc.dram_tensor("out", [NPASS, TT, 128, D], F32, kind="ExternalOutput").ap()

    with ExitStack() as es:
        S_ = Sched(nc, es)
        hT = S_.sbuf([128, KC, TP], name="hT")
        acc = [S_.sbuf([128, D], name=f"acc{t}") for t in range(TT)]
        wdb = [S_.sbuf([128, 4 * D], name=f"wdb{i}") for i in range(2)]
        gub = [S_.sbuf([128, 2 * KC * 128], name=f"gub{i}") for i in range(2)]
        aTb = [S_.sbuf([128, TP], name=f"aTb{i}") for i in range(8)]
        par = [S_.sbuf([128, D], name=f"par{i}") for i in range(3)]
        gG = [Buf(gub[i].t, f"gG{i}") for i in range(2)]
        gU = [Buf(gub[i].t, f"gU{i}") for i in range(2)]
        scr = gG[0]
        sh2 = gG[1]
        wr = S_.sbuf([128, KC, 36], name="wr_sb")
        br = S_.sbuf([128, 36], name="br_sb")
        ident = S_.sbuf([128, 128], name="ident_sb")
        G = S_.sbuf([128, TT, NE], name="G")
        sm = [S_.sbuf([128, 64], name=f"sm{i}") for i in range(2)]
        ps = [S_.psum([128, 512], name=f"psb{i}") for i in range(8)]
        pg, pu, py = ps[0:2], ps[2:4], ps[4:8]

        S_.dma("sp", wr[:], wr_d, writes=[wr])
        S_.dma("sp", br[:], br_d, writes=[br])
        S_.dma("sp", ident[:], id_d, writes=[ident])

        def ln_tile(xb, gb, gap, bb, bap, smb):
            s1, s2, mean, msq, var, std, rstd, nmr = [smb[:, i:i + 1] for i in range(8)]
            S_.op("act", lambda e: e.activation(out=r32(scr[:, 0:D]), in_=xb[:], func=AF.Identity, accum_out=s1),
                  reads=[xb], writes=[scr, smb])
            S_.op("act", lambda e: e.activation(out=r32(scr[:, 0:D]), in_=xb[:], func=AF.Square, accum_out=s2),
                  reads=[xb], writes=[scr, smb])
            S_.op("dve", lambda e: e.tensor_scalar(out=mean, in0=s1, scalar1=1.0 / D, scalar2=None, op0=ALU.mult),
                  reads=[smb], writes=[smb])
            S_.op("dve", lambda e: e.tensor_tensor(out=msq, in0=mean, in1=mean, op=ALU.mult),
                  reads=[smb], writes=[smb])
            S_.op("dve", lambda e: e.scalar_tensor_tensor(out=var, in0=s2, scalar=1.0 / D, in1=msq,
                                                          op0=ALU.mult, op1=ALU.subtract),
                  reads=[smb], writes=[smb])
            S_.op("dve", lambda e: e.tensor_scalar(out=var, in0=var, scalar1=EPS, scalar2=None, op0=ALU.add),
                  reads=[smb], writes=[smb])
            S_.op("act", lambda e: e.activation(out=std, in_=var, func=AF.Sqrt), reads=[smb], writes=[smb])
            S_.op("dve", lambda e: e.reciprocal(out=rstd, in_=std), reads=[smb], writes=[smb])
            S_.op("dve", lambda e: e.tensor_scalar(out=nmr, in0=mean, scalar1=rstd, scalar2=-1.0,
                                                   op0=ALU.mult, op1=ALU.mult), reads=[smb], writes=[smb])
            S_.op("act", lambda e: e.activation(out=xb[:], in_=xb[:], func=AF.Identity, scale=rstd, bias=nmr),
                  reads=[xb, smb], writes=[xb])
            S_.op("dve", lambda e: e.tensor_tensor(out=xb[:], in0=xb[:], in1=gap, op=ALU.mult),
                  reads=[xb, gb], writes=[xb])
            S_.op("dve", lambda e: e.tensor_tensor(out=xb[:], in0=xb[:], in1=bap, op=ALU.add),
                  reads=[xb, bb], writes=[xb])

        pyi = 0
        for pz in range(NPASS):
            S_.dma("pool", r32(hT[:]), aT_d[pz], writes=[hT])
            for t in range(TT):
                S_.dma("sp", acc[t][:], x_d[pz, t], writes=[acc[t]])
            S_.dma("sp", par[0][:], par_d[P_GATE1], writes=[par[0]])
            S_.dma("sp", par[1][:], par_d[P_LNG1], writes=[par[1]])
            S_.dma("sp", par[2][:], par_d[P_LNB1], writes=[par[2]])
            for n in range(4):
                wb = wdb[n % 2]
                S_.dma("pool", r32(wb[:]), wp_d[n], writes=[wb])
                wv = wb[:].rearrange("p (k c) -> p k c", k=KC)
                for t in range(TT):
                    pb = py[pyi % 4]
                    pyi += 1
                    for k in range(KC):
                        S_.op("pe", lambda e, k=k, t=t, pb=pb, wv=wv: e.matmul(
                            pb[:], lhsT=r32(hT[:, k, t * 128:(t + 1) * 128]), rhs=r32(wv[:, k, :]),
                            start=(k == 0), stop=(k == KC - 1)), reads=[hT, wb], writes=[pb])
                    cs = slice(n * 512, (n + 1) * 512)
                    sg = aTb[(n * TT + t) % 8]
                    S_.op("dve", lambda e, pb=pb, sg=sg, cs=cs: e.tensor_tensor(
                        out=r32(sg[:]), in0=pb[:], in1=par[0][:, cs], op=ALU.mult),
                        reads=[pb, par[0]], writes=[sg])
                    S_.op("dve", lambda e, t=t, sg=sg, cs=cs: e.scalar_tensor_tensor(
                        out=acc[t][:, cs], in0=acc[t][:, cs], scalar=ALPHA, in1=sg[:],
                        op0=ALU.mult, op1=ALU.add), reads=[acc[t], sg], writes=[acc[t]])
            for t in range(TT):
                ln_tile(acc[t], par[1], par[1][:], par[2], par[2][:], sm[t % 2])
            S_.dma("sp", par[0][:], par_d[P_SC2], writes=[par[0]])
            S_.dma("pool", r32(sh2[:, 0:D]), par_d[P_SH2], writes=[sh2])
            S_.op("dve", lambda e: e.tensor_scalar(out=par[0][:], in0=par[0][:], scalar1=1.0, scalar2=None,
                                                    op0=ALU.add), reads=[par[0]], writes=[par[0]])
            for t in range(TT):
                S_.op("dve", lambda e, t=t: e.tensor_tensor(out=r32(scr[:, 0:D]), in0=acc[t][:], in1=par[0][:], op=ALU.mult),
                      reads=[acc[t], par[0]], writes=[scr])
                S_.op("dve", lambda e: e.tensor_tensor(out=r32(scr[:, 0:D]), in0=scr[:, 0:D], in1=sh2[:, 0:D], op=ALU.add),
                      reads=[scr, sh2], writes=[scr])
                S_.op("dve", lambda e, t=t: e.tensor_scalar(out=acc[t][:], in0=acc[t][:], scalar1=ALPHA,
                                                             scalar2=None, op0=ALU.mult),
                      reads=[acc[t]], writes=[acc[t]])
                for k0 in range(0, KC, 4):
                    pb = py[pyi % 4]
                    pyi += 1
                    for j in range(4):
                        k = k0 + j
                        S_.op("pe", lambda e, j=j, k=k, pb=pb: e.transpose(
                            out=pb[:, j * 128:(j + 1) * 128], in_=scr[:, k * 128:(k + 1) * 128],
                            identity=ident[:]), reads=[scr, ident], writes=[pb])
                    S_.op("act", lambda e, k0=k0, t=t, pb=pb: e.activation(
                        out=r32(hT[:, k0:k0 + 4, t * 128:(t + 1) * 128]),
                        in_=pb[:].rearrange("p (a b) -> p a b", a=4), func=AF.Identity),
                        reads=[pb], writes=[hT])
            for t in range(TT):
                pb = py[pyi % 4]
                pyi += 1
                for k in range(KC):
                    S_.op("pe", lambda e, k=k, t=t, pb=pb: e.matmul(
                        pb[:, 0:36], lhsT=hT[:, k, t * 128:(t + 1) * 128], rhs=wr[:, k, :],
                        start=(k == 0), stop=(k == KC - 1)), reads=[hT, wr], writes=[pb])
                s = sm[t % 2]
                lg = s[:, 0:36]
                m4, nm4, s4, pgp = s[:, 36:37], s[:, 37:38], s[:, 38:39], s[:, 39:40]
                oh4 = s[:, 40:44]
                e4 = s[:, 44:48]
                sel = s[:, 48:56]
                S_.op("dve", lambda e: e.tensor_tensor(out=lg, in0=pb[:, 0:36], in1=br[:], op=ALU.add),
                      reads=[pb, br], writes=[s])
                S_.op("dve", lambda e: e.reduce_max(out=m4, in_=s[:, 0:4], axis=AX.X), reads=[s], writes=[s])
                S_.op("dve", lambda e: e.tensor_scalar(out=nm4, in0=m4, scalar1=-1.0, scalar2=None, op0=ALU.mult),
                      reads=[s], writes=[s])
                S_.op("act", lambda e: e.activation(out=e4, in_=s[:, 0:4], func=AF.Exp, bias=nm4, accum_out=s4),
                      reads=[s], writes=[s])
                S_.op("dve", lambda e: e.reciprocal(out=pgp, in_=s4), reads=[s], writes=[s])
                S_.op("dve", lambda e: e.tensor_scalar(out=oh4, in0=s[:, 0:4], scalar1=m4, scalar2=None,
                                                       op0=ALU.is_equal), reads=[s], writes=[s])
                S_.op("dve", lambda e: e.tensor_scalar(out=sel, in0=s[:, 4:12], scalar1=s[:, 40:41], scalar2=None,
                                                       op0=ALU.mult), reads=[s], writes=[s])
                for g in range(1, 4):
                    S_.op("dve", lambda e, g=g: e.scalar_tensor_tensor(
                        out=sel, in0=s[:, 4 + 8 * g:12 + 8 * g], scalar=s[:, 40 + g:41 + g], in1=sel,
                        op0=ALU.mult, op1=ALU.add), reads=[s], writes=[s])
                s2b = sm[t % 2]
                m1, m2, nm1, dd, p1, p2 = [s[:, 56 + i:57 + i] for i in range(6)]
                g1, g2 = s[:, 62:63], s[:, 63:64]
                o1 = G[:, t, 0:8]
                o2 = G[:, t, 8:16]
                sel2 = G[:, t, 16:24]
                g8 = G[:, t, 24:32]
                S_.op("dve", lambda e: e.reduce_max(out=m1, in_=sel, axis=AX.X), reads=[s], writes=[s])
                S_.op("dve", lambda e: e.tensor_scalar(out=o1, in0=sel, scalar1=m1, scalar2=None, op0=ALU.is_equal),
                      reads=[s], writes=[G])
                S_.op("dve", lambda e: e.scalar_tensor_tensor(out=sel2, in0=o1, scalar=-1e30, in1=sel,
                                                              op0=ALU.mult, op1=ALU.add), reads=[s, G], writes=[G])
                S_.op("dve", lambda e: e.reduce_max(out=m2, in_=sel2, axis=AX.X), reads=[G], writes=[s])
                S_.op("dve", lambda e: e.tensor_scalar(out=o2, in0=sel2, scalar1=m2, scalar2=None, op0=ALU.is_equal),
                      reads=[s, G], writes=[G])
                S_.op("dve", lambda e: e.tensor_scalar(out=nm1, in0=m1, scalar1=-1.0, scalar2=None, op0=ALU.mult),
                      reads=[s], writes=[s])
                S_.op("act", lambda e: e.activation(out=dd, in_=m2, func=AF.Exp, bias=nm1), reads=[s], writes=[s])
                S_.op("dve", lambda e: e.tensor_scalar(out=p1, in0=dd, scalar1=1.0, scalar2=None, op0=ALU.add),
                      reads=[s], writes=[s])
                S_.op("dve", lambda e: e.reciprocal(out=p1, in_=p1), reads=[s], writes=[s])
                S_.op("dve", lambda e: e.tensor_tensor(out=p2, in0=dd, in1=p1, op=ALU.mult), reads=[s], writes=[s])
                S_.op("dve", lambda e: e.tensor_tensor(out=g1, in0=p1, in1=pgp, op=ALU.mult), reads=[s], writes=[s])
                S_.op("dve", lambda e: e.tensor_tensor(out=g2, in0=p2, in1=pgp, op=ALU.mult), reads=[s], writes=[s])
                S_.op("dve", lambda e: e.tensor_scalar(out=g8, in0=o1, scalar1=g1, scalar2=None, op0=ALU.mult),
                      reads=[s, G], writes=[G])
                S_.op("dve", lambda e: e.scalar_tensor_tensor(out=g8, in0=o2, scalar=g2, in1=g8,
                                                              op0=ALU.mult, op1=ALU.add), reads=[s, G], writes=[G])
                S_.op("dve", lambda e: e.tensor_copy(out=sel, in_=g8), reads=[G], writes=[s])
                for g in range(4):
                    S_.op("dve", lambda e, g=g: e.tensor_scalar(
                        out=G[:, t, 8 * g:8 * g + 8], in0=sel, scalar1=s[:, 40 + g:41 + g], scalar2=None,
                        op0=ALU.mult), reads=[s], writes=[G])
            S_.dma("sp", par[0][:], par_d[P_GATE2], writes=[par[0]])
            ui = 0
            S_.dma("pool", r32(wdb[0][:]), wd_d[0], writes=[wdb[0]])

            def load_unit(u):
                bi = u % 2
                S_.dma("pool", r32(gub[bi].t[:, 0:2048]), wgu_d[u][:, 0:2048], writes=[gG[bi]])
                S_.dma("pool", r32(gub[bi].t[:, 2048:4096]), wgu_d[u][:, 2048:4096], writes=[gU[bi]])

            ubase = ui
            load_unit(0)
            for ex in range(n_experts):
                wb = wdb[ex % 2]
                for hc in range(4):
                    u = ex * 4 + hc
                    bi = u % 2
                    if u + 1 < n_experts * 4:
                        load_unit(u + 1)
                    S_.op("pool", lambda e, hc=hc, wb=wb: e.tensor_tensor(
                        out=r32(wb[:, hc * D:(hc + 1) * D]), in0=wb[:, hc * D:(hc + 1) * D], in1=par[0][:], op=ALU.mult),
                        reads=[wb, par[0]], writes=[wb])
                    if hc == 1 and ex + 1 < n_experts:
                        wbn = wdb[(ex + 1) % 2]
                        S_.dma("pool", r32(wbn[:]), wd_d[ex + 1], writes=[wbn])
                    gv = gub[bi].t[:].rearrange("p (j k c) -> p j k c", j=2, k=KC)
                    pgb, pub = pg[ui % 2], pu[ui % 2]
                    for k in range(KC):
                        S_.op("pe", lambda e, k=k, gv=gv, pgb=pgb: e.matmul(
                            pgb[:], lhsT=r32(gv[:, 0, k, :]), rhs=r32(hT[:, k, :]),
                            start=(k == 0), stop=(k == KC - 1)), reads=[gG[bi], hT], writes=[pgb])
                    for k in range(KC):
                        S_.op("pe", lambda e, k=k, gv=gv, pub=pub: e.matmul(
                            pub[:], lhsT=r32(gv[:, 1, k, :]), rhs=r32(hT[:, k, :]),
                            start=(k == 0), stop=(k == KC - 1)), reads=[gU[bi], hT], writes=[pub])
                    ab = aTb[(ex % 2) * 4 + hc]
                    S_.op("act", lambda e, ab=ab, pgb=pgb: e.activation(out=r32(ab[:]), in_=pgb[:], func=AF.Silu),
                          reads=[pgb], writes=[ab])
                    S_.op("dve", lambda e, pub=pub, ab=ab: e.tensor_tensor(
                        out=r32(ab[:]), in0=ab[:], in1=pub[:], op=ALU.mult), reads=[ab, pub], writes=[ab])
                    ui += 1
                abs_ = [aTb[(ex % 2) * 4 + hc] for hc in range(4)]
                for t in range(TT):
                    for n in range(4):
                        pb = py[pyi % 4]
                        pyi += 1
                        for hc in range(4):
                            S_.op("pe", lambda e, hc=hc, t=t, n=n, pb=pb, wb=wb, abs_=abs_: e.matmul(
                                pb[:], lhsT=r32(abs_[hc][:, t * 128:(t + 1) * 128]),
                                rhs=r32(wb[:, hc * D + n * 512: hc * D + (n + 1) * 512]),
                                start=(hc == 0), stop=(hc == 3)), reads=[abs_[hc], wb], writes=[pb])
                        cs = slice(n * 512, (n + 1) * 512)
                        S_.op("dve", lambda e, t=t, cs=cs, pb=pb, ex=ex: e.scalar_tensor_tensor(
                            out=acc[t][:, cs], in0=pb[:], scalar=G[:, t, ex:ex + 1], in1=acc[t][:, cs],
                            op0=ALU.mult, op1=ALU.add), reads=[pb, G, acc[t]], writes=[acc[t]])
            S_.dma("sp", par[1][:], par_d[P_LNG2], writes=[par[1]])
            S_.dma("sp", par[2][:], par_d[P_LNB2], writes=[par[2]])
            for t in range(TT):
                ln_tile(acc[t], par[1], par[1][:], par[2], par[2][:], sm[t % 2])
                S_.dma("act", out_d[pz, t], acc[t][:], reads=[acc[t]])
                S_.mark_output(acc[t])
        S_.finish()
    return nc


def pack_experts(wg, wu, wd):
    st = np.stack([wg, wu], axis=1).reshape(NE, 2, KC, 128, 4, 128)
    wgu = np.ascontiguousarray(st.transpose(0, 4, 3, 1, 2, 5)).reshape(NE * 4, 128, 2 * KC * 128)
    wdp = np.ascontiguousarray(wd.reshape(NE, 4, 128, D).transpose(0, 2, 1, 3)).reshape(NE, 128, 4 * D)
    return wgu, wdp


def bc128(v):
    return np.ascontiguousarray(np.broadcast_to(v, (128,) + v.shape))


_NC_CACHE = {}


def run_l3(a_tok, wp, x_tok, rows, wrc, brc, wrf, brf, wgu, wdp, trace=False):
    if "l3" not in _NC_CACHE:
        _NC_CACHE["l3"] = build_l3()
    nc = _NC_CACHE["l3"]
    wpp = np.ascontiguousarray(wp.reshape(KC, 128, 4, 512).transpose(2, 1, 0, 3)).reshape(4, 128, KC * 512)
    wr = np.ascontiguousarray(np.concatenate([wrc, wrf], axis=1).reshape(KC, 128, 36).transpose(1, 0, 2))
    br = bc128(np.concatenate([brc, brf]))
    ident = np.eye(128, dtype=np.float32)
    pars = [np.ascontiguousarray(np.stack([bc128(v) for v in rows[b]])) for b in range(B)]
    in_maps = []
    TC = NTOK // NCORES
    for c in range(NCORES):
        b = c // (NCORES // B)
        a_c = a_tok[c * TC:(c + 1) * TC]
        aT = np.ascontiguousarray(a_c.reshape(NPASS, TP, KC, 128).transpose(0, 3, 2, 1))
        xc = np.ascontiguousarray(x_tok[c * TC:(c + 1) * TC]).reshape(NPASS, TT, 128, D)
        in_maps.append({"aT": aT, "wp": wpp, "x": xc, "par": pars[b], "wr": wr, "br": br,
                        "wgu": wgu, "wd": wdp, "ident": ident})
    res = run_bass_kernel_spmd(nc, in_maps, core_ids=list(range(NCORES)), trace=trace)
    out = np.concatenate([res.results[c]["out"].reshape(TC, D) for c in range(NCORES)], axis=0)
    if trace:
        return out, res
    return out


HPC = 4
BLK = 256
NBLK = S // BLK
QB = 512
SM_SCALE = HD ** -0.5
TWO_PI_HI = 6.28125
TWO_PI_LO = 2.0 * np.pi - 6.28125


def build_l2(hpc=HPC, do_tab=True, do_proj=True, do_attn=True, nblk=NBLK, nqb=S // QB, do_sel=True, do_rot=True, do_v=True):
    nc = bass.Bass("TRN2", target_bir_lowering=False)
    xT_d = nc.dram_tensor("xT", [NBLK, 128, KC * BLK], F32R, kind="ExternalInput").ap()
    mod_d = nc.dram_tensor("mod", [128, 2 * KC], F32, kind="ExternalInput").ap()
    wqk_d = nc.dram_tensor("wqk", [HPC, 128, 6 * KC * 128], F32R, kind="ExternalInput").ap()
    wv_d = nc.dram_tensor("wv", [HPC, 128, KC * 384], F32R, kind="ExternalInput").ap()
    pos_d = nc.dram_tensor("pos", [32, S], I32, kind="ExternalInput").ap()
    invf_d = nc.dram_tensor("invf", [32, 1], F32, kind="ExternalInput").ap()
    rot_d = nc.dram_tensor("rot", [128, 128], F32, kind="ExternalInput").ap()
    msk_d = nc.dram_tensor("msk", [2, 128, QB], F32, kind="ExternalInput").ap()
    o_d = nc.dram_tensor("o", [HPC, S // 128, 128, HD], F32, kind="ExternalOutput").ap()

    with ExitStack() as es:
        S_ = Sched(nc, es)
        xb = [S_.sbuf([128, KC * BLK], name=f"xb{i}") for i in range(2)]
        wqk = S_.sbuf([128, 6 * KC * 128], name="wqk_sb")
        wv = S_.sbuf([128, KC * 384], name="wv_sb")
        QT = [S_.sbuf([128, S], BF16, name=f"QT{g}") for g in range(NG)]
        KT = [S_.sbuf([128, S], BF16, name=f"KT{g}") for g in range(NG)]
        V = S_.sbuf([128, S // 128, NG, HD + 1], BF16, name="V")
        cosT = S_.sbuf([128, S], BF16, name="cosT")
        sinT = S_.sbuf([128, S], BF16, name="sinT")
        mod = S_.sbuf([128, 2 * KC], name="mod_sb")
        invf = S_.sbuf([32, 1], name="invf_sb")
        rotf = S_.sbuf([128, 128], name="rotf")
        rotb = S_.sbuf([128, 128], BF16, name="rotb")
        mskf = S_.sbuf([128, QB], name="mskf")
        msk = [S_.sbuf([128, QB], BF16, name=f"msk{i}") for i in range(2)]
        PT = [S_.sbuf([128, QB], BF16, name=f"PT{i}") for i in range(4)]
        t1 = [S_.sbuf([128, BLK], name=f"t1_{i}") for i in range(2)]
        t2 = [S_.sbuf([128, BLK], name=f"t2_{i}") for i in range(2)]
        osb = [S_.sbuf([128, HD], name=f"osb{i}") for i in range(2)]
        rden = [S_.sbuf([128, 1], name=f"rden{i}") for i in range(2)]
        ps = [S_.psum([128, 512], name=f"psb{i}") for i in range(8)]

        S_.dma("sp", mod[:], mod_d, writes=[mod])
        S_.dma("sp", invf[:], invf_d, writes=[invf])
        S_.dma("sp", rotf[:], rot_d, writes=[rotf])
        S_.op("dve", lambda e: e.tensor_copy(out=rotb[:], in_=rotf[:]), reads=[rotf], writes=[rotb])
        for i in range(2):
            S_.dma("sp", mskf[:], msk_d[i], writes=[mskf])
            S_.op("dve", lambda e, i=i: e.tensor_copy(out=msk[i][:], in_=mskf[:]), reads=[mskf], writes=[msk[i]])
        S_.op("pool", lambda e: e.memset(V[:], 1.0), writes=[V])
        S_.op("pool", lambda e: e.memset(cosT[:], 1.0), writes=[cosT])
        S_.op("pool", lambda e: e.memset(sinT[:], 0.0), writes=[sinT])
        S_.op("dve", lambda e: e.tensor_scalar(out=mod[:, 0:KC], in0=mod[:, 0:KC], scalar1=1.0, scalar2=None,
                                               op0=ALU.add), reads=[mod], writes=[mod])

        for ch in range(S // BLK if do_tab else 0):
            cs = slice(ch * BLK, (ch + 1) * BLK)
            bA, bB, bC, bM = t1[0], t1[1], t2[0], t2[1]
            posi = bA[0:32, :].bitcast(I32)
            ang = bA[0:32, :]
            tq = bB[0:32, :]
            ni = bB[0:32, :].bitcast(I32)
            nf = bB[0:32, :]
            rr = bC[0:32, :]
            mk = bM[0:32, :]
            S_.dma("sp", posi, pos_d[:, cs], writes=[bA])
            S_.op("dve", lambda e: e.tensor_copy(out=ang, in_=posi), reads=[bA], writes=[bA])
            S_.op("dve", lambda e: e.tensor_scalar(out=ang, in0=ang, scalar1=invf[:, 0:1], scalar2=None, op0=ALU.mult),
                  reads=[bA, invf], writes=[bA])
            for which, tab in ((0, sinT), (1, cosT)):
                off = 0.0 if which == 0 else float(np.pi / 2)
                S_.op("dve", lambda e, off=off: e.tensor_scalar(out=tq, in0=ang, scalar1=off, scalar2=float(1 / (2 * np.pi)),
                                                                op0=ALU.add, op1=ALU.mult), reads=[bA], writes=[bB])
                S_.op("dve", lambda e: e.tensor_copy(out=ni, in_=tq), reads=[bB], writes=[bB])
                S_.op("dve", lambda e: e.tensor_copy(out=nf, in_=ni), reads=[bB], writes=[bB])
                S_.op("dve", lambda e, off=off: e.tensor_scalar(out=rr, in0=ang, scalar1=off, scalar2=None, op0=ALU.add),
                      reads=[bA], writes=[bC])
                S_.op("dve", lambda e: e.scalar_tensor_tensor(out=rr, in0=nf, scalar=-TWO_PI_HI, in1=rr,
                                                              op0=ALU.mult, op1=ALU.add), reads=[bC, bB], writes=[bC])
                S_.op("dve", lambda e: e.scalar_tensor_tensor(out=rr, in0=nf, scalar=-TWO_PI_LO, in1=rr,
                                                              op0=ALU.mult, op1=ALU.add), reads=[bC, bB], writes=[bC])
                S_.op("dve", lambda e: e.tensor_scalar(out=mk, in0=rr, scalar1=float(np.pi), scalar2=float(-2 * np.pi),
                                                       op0=ALU.is_gt, op1=ALU.mult), reads=[bC], writes=[bM])
                S_.op("dve", lambda e: e.tensor_tensor(out=rr, in0=rr, in1=mk, op=ALU.add), reads=[bC, bM], writes=[bC])
                S_.op("dve", lambda e: e.tensor_scalar(out=mk, in0=rr, scalar1=float(-np.pi), scalar2=float(2 * np.pi),
                                                       op0=ALU.is_lt, op1=ALU.mult), reads=[bC], writes=[bM])
                S_.op("dve", lambda e: e.tensor_tensor(out=rr, in0=rr, in1=mk, op=ALU.add), reads=[bC, bM], writes=[bC])
                S_.op("dve", lambda e: e.tensor_scalar(out=rr, in0=rr, scalar1=float(np.pi), scalar2=float(-np.pi),
                                                       op0=ALU.min, op1=ALU.max), reads=[bC], writes=[bC])
                S_.op("act", lambda e, tab=tab, cs=cs: e.activation(out=tab[0:32, cs], in_=rr, func=AF.Sin),
                      reads=[bC], writes=[tab])

        pi_ = [0]
        pti = 0
        oi = 0
        pref = set()
        xk = [[Buf(xb[i].t, f"xk{i}_{k}") for k in range(KC)] for i in range(2)]

        def load_x(bi, blk):
            for q4 in range(4):
                cols = slice(q4 * 4 * BLK, (q4 + 1) * 4 * BLK)
                S_.dma("pool", r32(xb[bi].t[:, cols]), xT_d[blk][:, cols], writes=xk[bi][4 * q4:4 * q4 + 4])

        for hl in range(hpc):
            if ("w", hl) not in pref:
                S_.dma("pool", r32(wqk[:]), wqk_d[hl], writes=[wqk])
                S_.dma("pool", r32(wv[:]), wv_d[hl], writes=[wv])
            wq = wqk[:].rearrange("p (j k c) -> p j k c", j=6, k=KC)
            wvv = wv[:].rearrange("p (k c) -> p k c", k=KC)
            for blk in range(nblk if do_proj else 0):
                x_ = xb[blk % 2]
                xk_ = xk[blk % 2]
                if ("x", hl, blk) not in pref:
                    load_x(blk % 2, blk)
                xv = x_[:].rearrange("p (k t) -> p k t", k=KC)
                for k in range(KC):
                    if k % 2 == 0:
                        S_.op("act", lambda e, k=k, xv=xv: e.activation(
                            out=r32(xv[:, k, :]), in_=xv[:, k, :], func=AF.Identity,
                            scale=mod[:, k:k + 1], bias=mod[:, KC + k:KC + k + 1]), reads=[xk_[k], mod], writes=[xk_[k]])
                    else:
                        S_.op("dve", lambda e, k=k, xv=xv: e.tensor_scalar(
                            out=r32(xv[:, k, :]), in0=xv[:, k, :], scalar1=mod[:, k:k + 1],
                            scalar2=mod[:, KC + k:KC + k + 1], op0=ALU.mult, op1=ALU.add),
                            reads=[xk_[k], mod], writes=[xk_[k]])
                cs = slice(blk * BLK, (blk + 1) * BLK)

                def proj(j):
                    g, isk = j // 2, j % 2
                    dst = (KT if isk else QT)[g]
                    pq = ps[pi_[0] % 4]
                    pr = ps[4 + (pi_[0] % 2)]
                    pi_[0] += 1
                    for k in range(KC):
                        S_.op("pe", lambda e, k=k, j=j, pq=pq: e.matmul(
                            pq[:, 0:BLK], lhsT=r32(wq[:, j, k, :]), rhs=r32(xv[:, k, :]),
                            start=(k == 0), stop=(k == KC - 1)), reads=[wqk, xk_[k]], writes=[pq])
                    S_.op("act", lambda e, dst=dst, pq=pq: e.activation(
                        out=dst[:, cs], in_=pq[:, 0:BLK], func=AF.Identity), reads=[pq], writes=[dst])
                    return (j, dst, pq, pr)

                def rot(st):
                    j, dst, pq, pr = st
                    S_.op("pe", lambda e: e.matmul(
                        pr[:, 0:BLK], lhsT=rotb[:], rhs=dst[:, cs], start=True, stop=True),
                        reads=[rotb, dst], writes=[pr])
                    a1, a2 = t1[j % 2], t2[j % 2]
                    S_.op("dve", lambda e: e.tensor_tensor(
                        out=a1[:], in0=pq[:, 0:BLK], in1=cosT[:, cs], op=ALU.mult),
                        reads=[pq, cosT, dst], writes=[a1])
                    S_.op("dve", lambda e: e.tensor_tensor(
                        out=a2[:], in0=pr[:, 0:BLK], in1=sinT[:, cs], op=ALU.mult),
                        reads=[pr, sinT], writes=[a2])
                    S_.op("dve", lambda e: e.tensor_tensor(
                        out=dst[:, cs], in0=a1[:], in1=a2[:], op=ALU.add), reads=[a1, a2], writes=[dst])

                def vproj(tt):
                    pv = ps[6 + tt % 2]
                    for k in range(KC):
                        S_.op("pe", lambda e, k=k: e.matmul(
                            pv[:, 0:384], lhsT=r32(xv[:, k, tt * 128:(tt + 1) * 128]), rhs=r32(wvv[:, k, :]),
                            start=(k == 0), stop=(k == KC - 1)), reads=[xk_[k], wv], writes=[pv])
                    tile_i = blk * (BLK // 128) + tt
                    S_.op("act", lambda e: e.activation(
                        out=V[:, tile_i, :, 0:HD], in_=pv[:, 0:384].rearrange("p (g c) -> p g c", g=NG),
                        func=AF.Identity), reads=[pv], writes=[V])

                prev = None
                for j in range(6):
                    cur = proj(j)
                    if prev is not None:
                        rot(prev)
                    prev = cur
                vproj(0)
                rot(prev)
                vproj(1)
            if do_proj and do_attn and hl + 1 < hpc and nblk >= 2:
                S_.dma("pool", r32(wqk[:]), wqk_d[hl + 1], writes=[wqk])
                S_.dma("pool", r32(wv[:]), wv_d[hl + 1], writes=[wv])
                pref.add(("w", hl + 1))
                for pb_ in range(2):
                    load_x(pb_, pb_)
                    pref.add(("x", hl + 1, pb_))
            if not do_attn and hl == 0:
                S_.op("dve", lambda e: e.tensor_copy(out=osb[0][:], in_=QT[0][:, 0:HD]), reads=[QT[0], KT[0], V, cosT, sinT], writes=[osb[0]])
                S_.dma("sp", o_d[0, 0], osb[0][:], reads=[osb[0]])
            for qb in range(nqb if do_attn else 0):
                work = []
                for g, d in enumerate(DILS):
                    W_ = RADIUS * d
                    for kt in range(S // 128):
                        dl = kt * 128 - qb * QB
                        if dl - (QB - 1) <= W_ and dl + 127 >= -W_:
                            work.append((g, d, kt, dl))
                po = ps[4:8]
                qs = slice(qb * QB, (qb + 1) * QB)
                nw = len(work)
                pbase = pti
                pti += nw

                def issue_s(wi):
                    g, d, kt, dl = work[wi]
                    W_ = RADIUS * d
                    pS = ps[wi % 4]
                    S_.op("pe", lambda e: e.matmul(
                        pS[:], lhsT=KT[g][:, kt * 128:(kt + 1) * 128], rhs=QT[g][:, qs], start=True, stop=True),
                        reads=[KT[g], QT[g]], writes=[pS])
                    P_ = PT[(pbase + wi) % 4]
                    S_.op("act", lambda e: e.activation(out=P_[:], in_=pS[:], func=AF.Exp, scale=SM_SCALE),
                          reads=[pS], writes=[P_])
                    if g > 0:
                        S_.op("dve", lambda e: e.tensor_tensor(out=P_[:], in0=P_[:], in1=msk[g - 1][:],
                                                               op=ALU.mult), reads=[P_, msk[g - 1]], writes=[P_])
                    if do_sel and dl - (QB - 1) < -W_:
                        S_.op("pool", lambda e: e.affine_select(
                            out=P_[:], in_=P_[:], pattern=[[-1, QB]], compare_op=ALU.is_ge, fill=0.0,
                            base=dl + W_, channel_multiplier=1), reads=[P_], writes=[P_])
                    if do_sel and dl + 127 > W_:
                        S_.op("pool", lambda e: e.affine_select(
                            out=P_[:], in_=P_[:], pattern=[[1, QB]], compare_op=ALU.is_ge, fill=0.0,
                            base=W_ - dl, channel_multiplier=-1), reads=[P_], writes=[P_])

                def issue_pv(wi):
                    g, d, kt, dl = work[wi]
                    P_ = PT[(pbase + wi) % 4]
                    for sub in range(4):
                        S_.op("pe", lambda e, sub=sub: e.matmul(
                            po[sub][:, 0:HD + 1], lhsT=P_[:, sub * 128:(sub + 1) * 128], rhs=V[:, kt, g, :],
                            start=(wi == 0), stop=(wi == nw - 1)), reads=[P_, V], writes=[po[sub]])

                LA = 3
                for wi in range(min(LA, nw)):
                    issue_s(wi)
                for wi in range(nw):
                    if wi + LA < nw:
                        issue_s(wi + LA)
                    issue_pv(wi)
                for sub in range(4):
                    ob, rd = osb[oi % 2], rden[oi % 2]
                    oi += 1
                    S_.op("dve", lambda e, rd=rd, sub=sub: e.reciprocal(out=rd[:], in_=po[sub][:, HD:HD + 1]),
                          reads=[po[sub]], writes=[rd])
                    S_.op("dve", lambda e, rd=rd, ob=ob, sub=sub: e.tensor_scalar(
                        out=ob[:], in0=po[sub][:, 0:HD], scalar1=rd[:, 0:1], scalar2=None, op0=ALU.mult),
                        reads=[po[sub], rd], writes=[ob])
                    S_.dma("sp", o_d[hl, qb * 4 + sub], ob[:], reads=[ob])
                    S_.mark_output(ob)
        S_.finish()
    return nc


def rot_matrix():
    R = np.zeros((128, 128), np.float32)
    for i in range(16):
        R[16 + i, i] = -1.0
        R[i, 16 + i] = 1.0
    return R


def run_l2(x, positions, w_qkv, mod0, trace=False):
    if "l2" not in _NC_CACHE:
        _NC_CACHE["l2"] = build_l2()
    nc = _NC_CACHE["l2"]
    invf = (THETA ** (-np.arange(0, ROT, 2, dtype=np.float32) / ROT)).astype(np.float32)
    invf32 = np.concatenate([invf, invf]).reshape(32, 1).astype(np.float32)
    kl = np.arange(128)[:, None]
    ql = np.arange(QB)[None, :]
    msk = np.stack([((kl - ql) % d == 0).astype(np.float32) for d in DILS[1:]])
    rot = rot_matrix()
    w5 = w_qkv.reshape(KC, 128, NG, 3, NH, HD)
    xTs = []
    for b in range(B):
        xt = x[b].T.reshape(KC, 128, NBLK, BLK)
        xTs.append(np.ascontiguousarray(xt.transpose(2, 1, 0, 3)).reshape(NBLK, 128, KC * BLK))
    in_maps = []
    for c in range(NCORES):
        b, hg = c // 4, c % 4
        hs = slice(hg * HPC, (hg + 1) * HPC)
        wqk = w5[:, :, :, 0:2, hs, :]
        wqk = np.ascontiguousarray(wqk.transpose(4, 1, 2, 3, 0, 5)).reshape(HPC, 128, 6 * KC * 128)
        wv = w5[:, :, :, 2, hs, :]
        wv = np.ascontiguousarray(wv.transpose(3, 1, 0, 2, 4)).reshape(HPC, 128, KC * 384)
        shift, scale = mod0[b, 0:D], mod0[b, D:2 * D]
        modc = np.concatenate([scale.reshape(KC, 128).T, shift.reshape(KC, 128).T], axis=1).astype(np.float32)
        pos = np.ascontiguousarray(np.broadcast_to(positions[b].astype(np.int32), (32, S)))
        in_maps.append({"xT": xTs[b], "mod": np.ascontiguousarray(modc), "wqk": wqk, "wv": wv, "pos": pos,
                        "invf": invf32, "rot": rot, "msk": msk})
    res = run_bass_kernel_spmd(nc, in_maps, core_ids=list(range(NCORES)), trace=trace)
    o = np.zeros((B, S, NH, HD), np.float32)
    for c in range(NCORES):
        b, hg = c // 4, c % 4
        oc = res.results[c]["o"].reshape(HPC, S, HD)
        o[b, :, hg * HPC:(hg + 1) * HPC, :] = oc.transpose(1, 0, 2)
    o = o.reshape(NTOK, D)
    if trace:
        return o, res
    return o


def sched_barrier(S_):
    engs = list(S_.eng.keys())
    for en in engs:
        E = S_.eng[en]
        for x in engs:
            if x != en and S_.cnt[x] > 0 and S_.seen[en].get(("e", x), 0) < S_.cnt[x]:
                E.wait_ge(S_.sem[x], S_.cnt[x])
                S_.seen[en][("e", x)] = S_.cnt[x]
        for b in S_.out_bufs:
            if b.dsem is not None and S_.seen[en].get(("d", b.dsem), 0) < b.dcnt:
                E.wait_ge(b.dsem, b.dcnt)
                S_.seen[en][("d", b.dsem)] = b.dcnt


GC = 512
NST = S // 128


def build_l4():
    nc = bass.Bass("TRN2", target_bir_lowering=False)
    xT_d = nc.dram_tensor("xT", [NBLK, 128, KC * BLK], F32R, kind="ExternalInput").ap()
    mod_d = nc.dram_tensor("mod", [128, 2 * KC], F32, kind="ExternalInput").ap()
    win_d = nc.dram_tensor("win", [128, KC * GC], F32R, kind="ExternalInput").ap()
    cc_d = nc.dram_tensor("cc", [2, 128, 4 * GC], F32R, kind="ExternalInput").ap()
    dft_d = nc.dram_tensor("dft", [S // QB, 16, 128, 4 * QB], F32R, kind="ExternalInput").ap()
    y_d = nc.dram_tensor("yT", [4, 128, S], F32, kind="ExternalOutput").ap()
    with ExitStack() as es:
        S_ = Sched(nc, es)
        AB = [S_.sbuf([128, NST, GC], name=f"AB{i}") for i in range(2)]
        win = S_.sbuf([128, KC * GC], name="win_sb")
        xb = [S_.sbuf([128, KC * BLK], name=f"xb{i}") for i in range(1)]
        uT = S_.sbuf([128, 4, BLK], name="uT")
        ccs = [S_.sbuf([128, 4 * GC], name=f"ccs{i}") for i in range(2)]
        mod = S_.sbuf([128, 2 * KC], name="mod_sb")
        ysb = [S_.sbuf([128, QB], name=f"ysb{i}") for i in range(2)]
        ps = [S_.psum([128, 512], name=f"psb{i}") for i in range(8)]
        S_.dma("sp", mod[:], mod_d, writes=[mod])
        S_.op("dve", lambda e: e.tensor_scalar(out=mod[:, 0:KC], in0=mod[:, 0:KC], scalar1=1.0, scalar2=None,
                                               op0=ALU.add), reads=[mod], writes=[mod])
        S_.dma("pool", r32(win[:]), win_d, writes=[win])
        for i in range(2):
            S_.dma("pool", r32(ccs[i][:]), cc_d[i], writes=[ccs[i]])
        wv = win[:].rearrange("p (k c) -> p k c", k=KC)
        pi_ = 0
        xk = [Buf(xb[0].t, f"xk_{k}") for k in range(KC)]
        for blk in range(NBLK):
            x_ = xb[0]
            for q4 in range(4):
                cols = slice(q4 * 4 * BLK, (q4 + 1) * 4 * BLK)
                S_.dma("pool", r32(x_.t[:, cols]), xT_d[blk][:, cols], writes=xk[4 * q4:4 * q4 + 4])
            xv = x_[:].rearrange("p (k t) -> p k t", k=KC)
            for k in range(KC):
                if k % 2 == 0:
                    S_.op("act", lambda e, k=k, xv=xv: e.activation(
                        out=r32(xv[:, k, :]), in_=xv[:, k, :], func=AF.Identity,
                        scale=mod[:, k:k + 1], bias=mod[:, KC + k:KC + k + 1]), reads=[xk[k], mod], writes=[xk[k]])
                else:
                    S_.op("dve", lambda e, k=k, xv=xv: e.tensor_scalar(
                        out=r32(xv[:, k, :]), in0=xv[:, k, :], scalar1=mod[:, k:k + 1],
                        scalar2=mod[:, KC + k:KC + k + 1], op0=ALU.mult, op1=ALU.add),
                        reads=[xk[k], mod], writes=[xk[k]])
            for k in range(KC):
                for cc in range(4):
                    S_.op("pe", lambda e, k=k, cc=cc, xv=xv: e.matmul(
                        ps[cc][:, 0:BLK], lhsT=r32(wv[:, k, cc * 128:(cc + 1) * 128]), rhs=r32(xv[:, k, :]),
                        start=(k == 0), stop=(k == KC - 1)), reads=[win, xk[k]], writes=[ps[cc]])
            for cc in range(4):
                if cc % 2 == 0:
                    S_.op("act", lambda e, cc=cc: e.activation(out=r32(uT[:, cc, :]), in_=ps[cc][:, 0:BLK],
                                                               func=AF.Identity), reads=[ps[cc]], writes=[uT])
                else:
                    S_.op("dve", lambda e, cc=cc: e.tensor_copy(out=r32(uT[:, cc, :]), in_=ps[cc][:, 0:BLK]),
                          reads=[ps[cc]], writes=[uT])
            for tt in range(BLK // 128):
                st = blk * (BLK // 128) + tt
                for i in range(2):
                    pa = ps[4 + (2 * tt + i) % 4]
                    cv = ccs[i][:].rearrange("p (c m) -> p c m", c=4)
                    for cc in range(4):
                        S_.op("pe", lambda e, cc=cc, tt=tt, pa=pa, cv=cv: e.matmul(
                            pa[:], lhsT=r32(uT[:, cc, tt * 128:(tt + 1) * 128]), rhs=r32(cv[:, cc, :]),
                            start=(cc == 0), stop=(cc == 3)), reads=[uT, ccs[i]], writes=[pa])
                    if i == 0:
                        S_.op("act", lambda e, pa=pa, st=st: e.activation(out=r32(AB[0][:, st, :]), in_=pa[:],
                                                                          func=AF.Identity), reads=[pa], writes=[AB[0]])
                    else:
                        S_.op("dve", lambda e, pa=pa, st=st: e.tensor_copy(out=r32(AB[1][:, st, :]), in_=pa[:]),
                              reads=[pa], writes=[AB[1]])
        sched_barrier(S_)
        dpc = [Buf(win.t, f"dpc{i}") for i in range(4)]
        yi = 0
        di = 0
        for kb in range(S // QB):
            for pc in range(16):
                sg, cs_ = pc // 2, pc % 2
                db = dpc[di % 4]
                dcol = slice((di % 4) * 2048, (di % 4 + 1) * 2048)
                di += 1
                S_.dma("pool", r32(win.t[:, dcol]), dft_d[kb, pc], writes=[db])
                dv = win.t[:, dcol].rearrange("p (s k) -> p s k", s=4)
                for s4 in range(4):
                    st = sg * 4 + s4
                    for mc in range(4):
                        first = (pc == 0 and s4 == 0)
                        last = (pc == 15 and s4 == 3)
                        S_.op("pe", lambda e, mc=mc, st=st, s4=s4, cs_=cs_, dv=dv, first=first, last=last: e.matmul(
                            ps[mc][:], lhsT=r32(AB[cs_][:, st, mc * 128:(mc + 1) * 128]), rhs=r32(dv[:, s4, :]),
                            start=first, stop=last), reads=[AB[cs_], db], writes=[ps[mc]])
            for mc in range(4):
                yb = ysb[yi % 2]
                yi += 1
                if mc % 2 == 0:
                    S_.op("act", lambda e, mc=mc, yb=yb: e.activation(out=yb[:], in_=ps[mc][:], func=AF.Identity),
                          reads=[ps[mc]], writes=[yb])
                else:
                    S_.op("dve", lambda e, mc=mc, yb=yb: e.tensor_copy(out=yb[:], in_=ps[mc][:]),
                          reads=[ps[mc]], writes=[yb])
                S_.dma("sp", y_d[mc, :, kb * QB:(kb + 1) * QB], yb[:], reads=[yb])
        S_.finish()
    return nc


def dft_tables():
    n = np.arange(S)
    angS = 2 * np.pi * ((n[:, None] * n[None, :]) % S) / S
    CS = (np.cos(angS) / np.sqrt(S)).astype(np.float32)
    SS = (-np.sin(angS) / np.sqrt(S)).astype(np.float32)
    m = np.arange(GC)
    angC = 2 * np.pi * ((m[:, None] * m[None, :]) % GC) / GC
    CC = (np.cos(angC) / np.sqrt(GC)).astype(np.float32)
    SC = (np.sin(angC) / np.sqrt(GC)).astype(np.float32)
    M = np.stack([CS, SS])
    M = M.reshape(2, 8, 4, 128, S // QB, QB)
    dft = np.ascontiguousarray(M.transpose(4, 1, 0, 3, 2, 5)).reshape(S // QB, 16, 128, 4 * QB)
    cc = np.stack([CC, SC]).reshape(2, 4, 128, GC)
    cc = np.ascontiguousarray(cc.transpose(0, 2, 1, 3)).reshape(2, 128, 4 * GC)
    return dft, cc


def run_l4(x_tok, w_in, mod0):
    if "l4" not in _NC_CACHE:
        _NC_CACHE["l4"] = build_l4()
    nc = _NC_CACHE["l4"]
    dft, cc = dft_tables()
    x = x_tok.reshape(B, S, D)
    xTs = []
    for b in range(B):
        xt = x[b].T.reshape(KC, 128, NBLK, BLK)
        xTs.append(np.ascontiguousarray(xt.transpose(2, 1, 0, 3)).reshape(NBLK, 128, KC * BLK))
    in_maps = []
    for c in range(NCORES):
        b, g = c // 4, c % 4
        wi = w_in[:, g * GC:(g + 1) * GC].reshape(KC, 128, GC)
        wi = np.ascontiguousarray(wi.transpose(1, 0, 2)).reshape(128, KC * GC)
        shift, scale = mod0[b, 0:D], mod0[b, D:2 * D]
        modc = np.ascontiguousarray(np.concatenate([scale.reshape(KC, 128).T, shift.reshape(KC, 128).T], axis=1))
        in_maps.append({"xT": xTs[b], "mod": modc.astype(np.float32), "win": wi, "cc": cc, "dft": dft})
    res = run_bass_kernel_spmd(nc, in_maps, core_ids=list(range(NCORES)))
    y = np.zeros((B, S, D), np.float32)
    for c in range(NCORES):
        b, g = c // 4, c % 4
        yT = res.results[c]["yT"].reshape(GC, S)
        y[b, :, g * GC:(g + 1) * GC] = yT.T
    return y.reshape(NTOK, D)


def kernel(x, c, positions, ada_w, ada_b, attn_w_qkv, attn_w_o, fnet_w_in, fnet_w_out,
           ln_g, ln_b, router_coarse_w, router_coarse_b, router_fine_w, router_fine_b,
           expert_w_gate, expert_w_up, expert_w_down):
    f = lambda a: np.asarray(a, dtype=np.float32)
    x, c = f(x), f(c)
    positions = np.asarray(positions).astype(np.int32)
    ada_w, ada_b = f(ada_w), f(ada_b)
    ln_g, ln_b = f(ln_g), f(ln_b)
    mod = run_l1(c, ada_w, ada_b)
    x_tok = x.reshape(NTOK, D)
    for i in range(2):
        m0, m1 = mod[i, 0], mod[i, 1]
        if i == 0:
            a_tok = run_l2(x, positions, f(attn_w_qkv[0]), m0)
            wp = f(attn_w_o[0])
        else:
            a_tok = run_l4(x_tok, f(fnet_w_in[0]), m0)
            wp = f(fnet_w_out[0])
        rows = [[m0[b, 2 * D:3 * D], ln_g[i, 0], ln_b[i, 0], m1[b, D:2 * D], m1[b, 0:D], m1[b, 2 * D:3 * D],
                 ln_g[i, 1], ln_b[i, 1]] for b in range(B)]
        wgu, wdp = pack_experts(f(expert_w_gate[i]), f(expert_w_up[i]), f(expert_w_down[i]))
        x_tok = run_l3(a_tok, wp, x_tok, rows, f(router_coarse_w[i]), f(router_coarse_b[i]),
                       f(router_fine_w[i]), f(router_fine_b[i]), wgu, wdp)
        del wgu, wdp
    return x_tok.reshape(B, S, D).astype(np.float32)


TB = 1024
TTB = TB // 128
DMA_CAST = dict(max_dma_last_dim=4096)


def build_l3b(n_experts=NE):
    nc = bass.Bass("TRN2", target_bir_lowering=False)
    aT_d = nc.dram_tensor("aT", [128, KC, TB], F32, kind="ExternalInput").ap()
    wp_d = nc.dram_tensor("wp", [4, 128, KC * 512], F32, kind="ExternalInput").ap()
    x_d = nc.dram_tensor("x", [TTB, 128, D], F32, kind="ExternalInput").ap()
    par_d = nc.dram_tensor("par", [8, 128, D], F32, kind="ExternalInput").ap()
    wr_d = nc.dram_tensor("wr", [128, KC, 36], F32, kind="ExternalInput").ap()
    br_d = nc.dram_tensor("br", [128, 36], F32, kind="ExternalInput").ap()
    wgu_d = nc.dram_tensor("wgu", [n_experts * 4, 128, 2 * KC * 128], F32, kind="ExternalInput").ap()
    wd_d = nc.dram_tensor("wd", [n_experts, 128, 4 * D], F32, kind="ExternalInput").ap()
    id_d = nc.dram_tensor("ident", [128, 128], F32, kind="ExternalInput").ap()
    out_d = nc.dram_tensor("out", [TTB, 128, D], F32, kind="ExternalOutput").ap()

    with ExitStack() as es:
        S_ = Sched(nc, es)
        hT = S_.sbuf([128, KC, TB], BF16, name="hT")
        acc = [S_.sbuf([128, D], name=f"acc{t}") for t in range(TTB)]
        wdb = [S_.sbuf([128, 4 * D], BF16, name=f"wdb{i}") for i in range(2)]
        gub = [S_.sbuf([128, 2 * KC * 128], BF16, name=f"gub{i}") for i in range(2)]
        aTb = [S_.sbuf([128, TB], BF16, name=f"aTb{i}") for i in range(8)]
        par = [S_.sbuf([128, D], name=f"par{i}") for i in range(3)]
        scr = S_.sbuf([128, D], name="scr")
        sh2 = par[1]
        hTfb = par[2]
        tmpy = [S_.sbuf([128, 512], name=f"tmpy{i}") for i in range(2)]
        wr = S_.sbuf([128, KC, 36], name="wr_sb")
        br = S_.sbuf([128, 36], name="br_sb")
        ident = S_.sbuf([128, 128], name="ident_sb")
        G = S_.sbuf([128, TTB, NE], name="G")
        sm = [S_.sbuf([128, 64], name=f"sm{i}") for i in range(2)]
        ps = [S_.psum([128, 512], name=f"psb{i}") for i in range(8)]
        pgu, py = ps[0:4], ps[4:8]

        S_.dma("sp", wr[:], wr_d, writes=[wr])
        S_.dma("sp", br[:], br_d, writes=[br])
        S_.dma("sp", ident[:], id_d, writes=[ident])

        def ln_tile(xb, gb, gap, bb, bap, smb):
            s1, s2, mean, msq, var, std, rstd, nmr = [smb[:, i:i + 1] for i in range(8)]
            S_.op("act", lambda e: e.activation(out=scr[:], in_=xb[:], func=AF.Identity, accum_out=s1),
                  reads=[xb], writes=[scr, smb])
            S_.op("act", lambda e: e.activation(out=scr[:], in_=xb[:], func=AF.Square, accum_out=s2),
                  reads=[xb], writes=[scr, smb])
            S_.op("dve", lambda e: e.tensor_scalar(out=mean, in0=s1, scalar1=1.0 / D, scalar2=None, op0=ALU.mult),
                  reads=[smb], writes=[smb])
            S_.op("dve", lambda e: e.tensor_tensor(out=msq, in0=mean, in1=mean, op=ALU.mult),
                  reads=[smb], writes=[smb])
            S_.op("dve", lambda e: e.scalar_tensor_tensor(out=var, in0=s2, scalar=1.0 / D, in1=msq,
                                                          op0=ALU.mult, op1=ALU.subtract),
                  reads=[smb], writes=[smb])
            S_.op("dve", lambda e: e.tensor_scalar(out=var, in0=var, scalar1=EPS, scalar2=None, op0=ALU.add),
                  reads=[smb], writes=[smb])
            S_.op("act", lambda e: e.activation(out=std, in_=var, func=AF.Sqrt), reads=[smb], writes=[smb])
            S_.op("dve", lambda e: e.reciprocal(out=rstd, in_=std), reads=[smb], writes=[smb])
            S_.op("dve", lambda e: e.tensor_scalar(out=nmr, in0=mean, scalar1=rstd, scalar2=-1.0,
                                                   op0=ALU.mult, op1=ALU.mult), reads=[smb], writes=[smb])
            S_.op("act", lambda e: e.activation(out=xb[:], in_=xb[:], func=AF.Identity, scale=rstd, bias=nmr),
                  reads=[xb, smb], writes=[xb])
            S_.op("pool", lambda e: e.tensor_tensor(out=xb[:], in0=xb[:], in1=gap, op=ALU.mult),
                  reads=[xb, gb], writes=[xb])
            S_.op("pool", lambda e: e.tensor_tensor(out=xb[:], in0=xb[:], in1=bap, op=ALU.add),
                  reads=[xb, bb], writes=[xb])

        pyi = 0
        for k in range(KC):
            S_.dma("pool", hT[:, k, :], aT_d[:, k, :], writes=[hT], **DMA_CAST)
        for t in range(TTB):
            S_.dma("sp", acc[t][:], x_d[t], writes=[acc[t]])
        S_.dma("sp", par[0][:], par_d[P_GATE1], writes=[par[0]])
        S_.dma("sp", par[1][:], par_d[P_LNG1], writes=[par[1]])
        S_.dma("sp", par[2][:], par_d[P_LNB1], writes=[par[2]])
        for n in range(4):
            wb = wdb[n % 2]
            S_.dma("pool", wb[:], wp_d[n], writes=[wb], **DMA_CAST)
            wv = wb[:].rearrange("p (k c) -> p k c", k=KC)
            for t in range(TTB):
                pb = py[pyi % 4]
                pyi += 1
                for k in range(KC):
                    S_.op("pe", lambda e, k=k, t=t, pb=pb, wv=wv: e.matmul(
                        pb[:], lhsT=hT[:, k, t * 128:(t + 1) * 128], rhs=wv[:, k, :],
                        start=(k == 0), stop=(k == KC - 1)), reads=[hT, wb], writes=[pb])
                cs = slice(n * 512, (n + 1) * 512)
                sg = tmpy[(n * TTB + t) % 2]
                S_.op("dve", lambda e, pb=pb, sg=sg, cs=cs: e.tensor_tensor(
                    out=sg[:], in0=pb[:], in1=par[0][:, cs], op=ALU.mult),
                    reads=[pb, par[0]], writes=[sg])
                S_.op("pool", lambda e, t=t, sg=sg, cs=cs: e.scalar_tensor_tensor(
                    out=acc[t][:, cs], in0=acc[t][:, cs], scalar=ALPHA, in1=sg[:],
                    op0=ALU.mult, op1=ALU.add), reads=[acc[t], sg], writes=[acc[t]]) if False else \
                    S_.op("dve", lambda e, t=t, sg=sg, cs=cs: e.scalar_tensor_tensor(
                        out=acc[t][:, cs], in0=acc[t][:, cs], scalar=ALPHA, in1=sg[:],
                        op0=ALU.mult, op1=ALU.add), reads=[acc[t], sg], writes=[acc[t]])
        for t in range(TTB):
            ln_tile(acc[t], par[1], par[1][:], par[2], par[2][:], sm[t % 2])
        S_.dma("sp", par[0][:], par_d[P_SC2], writes=[par[0]])
        S_.dma("sp", sh2[:], par_d[P_SH2], writes=[sh2])
        S_.op("pool", lambda e: e.tensor_scalar(out=par[0][:], in0=par[0][:], scalar1=1.0, scalar2=None,
                                                op0=ALU.add), reads=[par[0]], writes=[par[0]])
        for t in range(TTB):
            S_.op("dve", lambda e, t=t: e.tensor_tensor(out=scr[:], in0=acc[t][:], in1=par[0][:], op=ALU.mult),
                  reads=[acc[t], par[0]], writes=[scr])
            S_.op("dve", lambda e: e.tensor_tensor(out=scr[:], in0=scr[:], in1=sh2[:], op=ALU.add),
                  reads=[scr, sh2], writes=[scr])
            S_.op("pool", lambda e, t=t: e.tensor_scalar(out=acc[t][:], in0=acc[t][:], scalar1=ALPHA,
                                                         scalar2=None, op0=ALU.mult),
                  reads=[acc[t]], writes=[acc[t]])
            for k0 in range(0, KC, 4):
                pb = py[pyi % 4]
                pyi += 1
                for j in range(4):
                    k = k0 + j
                    S_.op("pe", lambda e, j=j, k=k, pb=pb: e.transpose(
                        out=pb[:, j * 128:(j + 1) * 128], in_=scr[:, k * 128:(k + 1) * 128],
                        identity=ident[:]), reads=[scr, ident], writes=[pb])
                S_.op("act", lambda e, k0=k0, t=t, pb=pb: e.activation(
                    out=hT[:, k0:k0 + 4, t * 128:(t + 1) * 128],
                    in_=pb[:].rearrange("p (a b) -> p a b", a=4), func=AF.Identity),
                    reads=[pb], writes=[hT])
                S_.op("act", lambda e, k0=k0, pb=pb: e.activation(
                    out=hTfb[:].rearrange("p (k t) -> p k t", k=KC)[:, k0:k0 + 4, :],
                    in_=pb[:].rearrange("p (a b) -> p a b", a=4), func=AF.Identity),
                    reads=[pb], writes=[hTfb])
            pb = py[pyi % 4]
            pyi += 1
            for k in range(KC):
                S_.op("pe", lambda e, k=k, pb=pb: e.matmul(
                    pb[:, 0:36], lhsT=hTfb[:, k * 128:(k + 1) * 128], rhs=wr[:, k, :],
                    start=(k == 0), stop=(k == KC - 1)), reads=[hTfb, wr], writes=[pb])
            s = sm[t % 2]
            lg = s[:, 0:36]
            m4, nm4, s4, pgp = s[:, 36:37], s[:, 37:38], s[:, 38:39], s[:, 39:40]
            oh4 = s[:, 40:44]
            e4 = s[:, 44:48]
            sel = s[:, 48:56]
            S_.op("dve", lambda e: e.tensor_tensor(out=lg, in0=pb[:, 0:36], in1=br[:], op=ALU.add),
                  reads=[pb, br], writes=[s])
            S_.op("dve", lambda e: e.reduce_max(out=m4, in_=s[:, 0:4], axis=AX.X), reads=[s], writes=[s])
            S_.op("dve", lambda e: e.tensor_scalar(out=nm4, in0=m4, scalar1=-1.0, scalar2=None, op0=ALU.mult),
                  reads=[s], writes=[s])
            S_.op("act", lambda e: e.activation(out=e4, in_=s[:, 0:4], func=AF.Exp, bias=nm4, accum_out=s4),
                  reads=[s], writes=[s])
            S_.op("dve", lambda e: e.reciprocal(out=pgp, in_=s4), reads=[s], writes=[s])
            S_.op("dve", lambda e: e.tensor_scalar(out=oh4, in0=s[:, 0:4], scalar1=m4, scalar2=None,
                                                   op0=ALU.is_equal), reads=[s], writes=[s])
            S_.op("dve", lambda e: e.tensor_scalar(out=sel, in0=s[:, 4:12], scalar1=s[:, 40:41], scalar2=None,
                                                   op0=ALU.mult), reads=[s], writes=[s])
            for g in range(1, 4):
                S_.op("dve", lambda e, g=g: e.scalar_tensor_tensor(
                    out=sel, in0=s[:, 4 + 8 * g:12 + 8 * g], scalar=s[:, 40 + g:41 + g], in1=sel,
                    op0=ALU.mult, op1=ALU.add), reads=[s], writes=[s])
            m1, m2, nm1, dd, p1, p2 = [s[:, 56 + i:57 + i] for i in range(6)]
            g1, g2 = s[:, 62:63], s[:, 63:64]
            o1 = G[:, t, 0:8]
            o2 = G[:, t, 8:16]
            sel2 = G[:, t, 16:24]
            g8 = G[:, t, 24:32]
            S_.op("dve", lambda e: e.reduce_max(out=m1, in_=sel, axis=AX.X), reads=[s], writes=[s])
            S_.op("dve", lambda e: e.tensor_scalar(out=o1, in0=sel, scalar1=m1, scalar2=None, op0=ALU.is_equal),
                  reads=[s], writes=[G])
            S_.op("dve", lambda e: e.scalar_tensor_tensor(out=sel2, in0=o1, scalar=-1e30, in1=sel,
                                                          op0=ALU.mult, op1=ALU.add), reads=[s, G], writes=[G])
            S_.op("dve", lambda e: e.reduce_max(out=m2, in_=sel2, axis=AX.X), reads=[G], writes=[s])
            S_.op("dve", lambda e: e.tensor_scalar(out=o2, in0=sel2, scalar1=m2, scalar2=None, op0=ALU.is_equal),
                  reads=[s, G], writes=[G])
            S_.op("dve", lambda e: e.tensor_scalar(out=nm1, in0=m1, scalar1=-1.0, scalar2=None, op0=ALU.mult),
                  reads=[s], writes=[s])
            S_.op("act", lambda e: e.activation(out=dd, in_=m2, func=AF.Exp, bias=nm1), reads=[s], writes=[s])
            S_.op("dve", lambda e: e.tensor_scalar(out=p1, in0=dd, scalar1=1.0, scalar2=None, op0=ALU.add),
                  reads=[s], writes=[s])
            S_.op("dve", lambda e: e.reciprocal(out=p1, in_=p1), reads=[s], writes=[s])
            S_.op("dve", lambda e: e.tensor_tensor(out=p2, in0=dd, in1=p1, op=ALU.mult), reads=[s], writes=[s])
            S_.op("dve", lambda e: e.tensor_tensor(out=g1, in0=p1, in1=pgp, op=ALU.mult), reads=[s], writes=[s])
            S_.op("dve", lambda e: e.tensor_tensor(out=g2, in0=p2, in1=pgp, op=ALU.mult), reads=[s], writes=[s])
            S_.op("dve", lambda e: e.tensor_scalar(out=g8, in0=o1, scalar1=g1, scalar2=None, op0=ALU.mult),
                  reads=[s, G], writes=[G])
            S_.op("dve", lambda e: e.scalar_tensor_tensor(out=g8, in0=o2, scalar=g2, in1=g8,
                                                          op0=ALU.mult, op1=ALU.add), reads=[s, G], writes=[G])
            S_.op("dve", lambda e: e.tensor_copy(out=sel, in_=g8), reads=[G], writes=[s])
            for g in range(4):
                S_.op("dve", lambda e, g=g: e.tensor_scalar(
                    out=G[:, t, 8 * g:8 * g + 8], in0=sel, scalar1=s[:, 40 + g:41 + g], scalar2=None,
                    op0=ALU.mult), reads=[s], writes=[G])
        S_.dma("sp", par[0][:], par_d[P_GATE2], writes=[par[0]])
        ui = 0
        ti = 0
        stage = [par[1], par[2], scr]
        sctr = [0]

        def load_cast(dst_ap, dst_buf, src_ap, ceng):
            st = stage[sctr[0] % 3]
            sctr[0] += 1
            S_.dma("sp", st[:], src_ap, writes=[st])
            if ceng == "act":
                S_.op("act", lambda e: e.activation(out=dst_ap, in_=st[:], func=AF.Identity), reads=[st], writes=[dst_buf])
            else:
                S_.op(ceng, lambda e: e.tensor_copy(out=dst_ap, in_=st[:]), reads=[st], writes=[dst_buf])

        for ex in range(n_experts):
            wb = wdb[ex % 2]
            for hc in range(4):
                load_cast(wb[:, hc * D:(hc + 1) * D], wb, wd_d[ex][:, hc * D:(hc + 1) * D], "pool")
            for hc in range(4):
                gb = gub[ui % 2]
                ceng = "act" if ui % 2 == 0 else "dve"
                for j in range(2):
                    load_cast(gb[:, j * 2048:(j + 1) * 2048], gb, wgu_d[ex * 4 + hc][:, j * 2048:(j + 1) * 2048], ceng)
                gv = gb[:].rearrange("p (j k c) -> p j k c", j=2, k=KC)
                ab = aTb[(ex % 2) * 4 + hc]
                for half in range(TB // 512):
                    ts_ = slice(half * 512, (half + 1) * 512)
                    pgb, pub = pgu[(2 * ui + half) % 2 * 2], pgu[(2 * ui + half) % 2 * 2 + 1]
                    for k in range(KC):
                        S_.op("pe", lambda e, k=k, gv=gv, pgb=pgb, ts_=ts_: e.matmul(
                            pgb[:], lhsT=gv[:, 0, k, :], rhs=hT[:, k, ts_],
                            start=(k == 0), stop=(k == KC - 1)), reads=[gb, hT], writes=[pgb])
                    for k in range(KC):
                        S_.op("pe", lambda e, k=k, gv=gv, pub=pub, ts_=ts_: e.matmul(
                            pub[:], lhsT=gv[:, 1, k, :], rhs=hT[:, k, ts_],
                            start=(k == 0), stop=(k == KC - 1)), reads=[gb, hT], writes=[pub])
                    sg = tmpy[ti % 2]
                    ti += 1
                    S_.op("act", lambda e, sg=sg, pgb=pgb: e.activation(out=sg[:], in_=pgb[:], func=AF.Silu),
                          reads=[pgb], writes=[sg])
                    S_.op("dve", lambda e, pub=pub, ab=ab, sg=sg, ts_=ts_: e.tensor_tensor(
                        out=ab[:, ts_], in0=sg[:], in1=pub[:], op=ALU.mult), reads=[sg, pub], writes=[ab])
                ui += 1
            abs_ = [aTb[(ex % 2) * 4 + hc] for hc in range(4)]
            for t in range(TTB):
                for n in range(4):
                    pb = py[pyi % 4]
                    pyi += 1
                    for hc in range(4):
                        S_.op("pe", lambda e, hc=hc, t=t, n=n, pb=pb, wb=wb, abs_=abs_: e.matmul(
                            pb[:], lhsT=abs_[hc][:, t * 128:(t + 1) * 128],
                            rhs=wb[:, hc * D + n * 512: hc * D + (n + 1) * 512],
                            start=(hc == 0), stop=(hc == 3)), reads=[abs_[hc], wb], writes=[pb])
                    cs = slice(n * 512, (n + 1) * 512)
                    yt = tmpy[ti % 2]
                    ti += 1
                    S_.op("act", lambda e, pb=pb, yt=yt, t=t, ex=ex: e.activation(
                        out=yt[:], in_=pb[:], func=AF.Identity, scale=G[:, t, ex:ex + 1]), reads=[pb, G], writes=[yt])
                    S_.op("dve", lambda e, yt=yt, cs=cs: e.tensor_tensor(
                        out=yt[:], in0=yt[:], in1=par[0][:, cs], op=ALU.mult), reads=[yt, par[0]], writes=[yt])
                    S_.op("pool", lambda e, t=t, cs=cs, yt=yt: e.tensor_tensor(
                        out=acc[t][:, cs], in0=acc[t][:, cs], in1=yt[:], op=ALU.add),
                        reads=[yt, acc[t]], writes=[acc[t]])
        S_.dma("sp", par[1][:], par_d[P_LNG2], writes=[par[1]])
        S_.dma("sp", par[2][:], par_d[P_LNB2], writes=[par[2]])
        for t in range(TTB):
            ln_tile(acc[t], par[1], par[1][:], par[2], par[2][:], sm[t % 2])
            S_.dma("act", out_d[t], acc[t][:], reads=[acc[t]])
        S_.finish()
    return nc


def run_l3b(a_tok, wp, x_tok, rows, wrc, brc, wrf, brf, wgu, wdp, trace=False):
    if "l3b" not in _NC_CACHE:
        _NC_CACHE["l3b"] = build_l3b()
    nc = _NC_CACHE["l3b"]
    wpp = np.ascontiguousarray(wp.reshape(KC, 128, 4, 512).transpose(2, 1, 0, 3)).reshape(4, 128, KC * 512)
    wr = np.ascontiguousarray(np.concatenate([wrc, wrf], axis=1).reshape(KC, 128, 36).transpose(1, 0, 2))
    br = bc128(np.concatenate([brc, brf]))
    ident = np.eye(128, dtype=np.float32)
    pars = [np.ascontiguousarray(np.stack([bc128(v) for v in rows[b]])) for b in range(B)]
    in_maps = []
    TC = NTOK // NCORES
    for c in range(NCORES):
        b = c // (NCORES // B)
        a_c = a_tok[c * TC:(c + 1) * TC]
        aT = np.ascontiguousarray(a_c.reshape(TB, KC, 128).transpose(2, 1, 0))
        xc = np.ascontiguousarray(x_tok[c * TC:(c + 1) * TC]).reshape(TTB, 128, D)
        in_maps.append({"aT": aT, "wp": wpp, "x": xc, "par": pars[b], "wr": wr, "br": br,
                        "wgu": wgu, "wd": wdp, "ident": ident})
    res = run_bass_kernel_spmd(nc, in_maps, core_ids=list(range(NCORES)), trace=trace)
    out = np.concatenate([res.results[c]["out"].reshape(TC, D) for c in range(NCORES)], axis=0)
    if trace:
        return out, res
    return out
```

```python
import numpy as np
from contextlib import ExitStack
import concourse.bass as bass
import concourse.mybir as mybir
from concourse.bass_utils import run_bass_kernel_spmd

F32 = mybir.dt.float32
F32R = mybir.dt.float32r
BF16 = mybir.dt.bfloat16
I32 = mybir.dt.int32
AF = mybir.ActivationFunctionType
ALU = mybir.AluOpType
AX = mybir.AxisListType

NCORES = 8
D = 2048
KC = D // 128
B = 2
S = 4096
NTOK = B * S
HD = 128
NH = 16
NG = 3
DILS = (1, 4, 16)
RADIUS = 64
ROT = 32
THETA = 500000.0
NE = 32
EPG = 8
NGRP = 4
ED = 512
EPS = 1e-5
ALPHA = 4.0 ** 0.25

SAME_ENG_SYNC = True


class Buf:
    __slots__ = ("t", "name", "w", "r", "dsem", "dcnt")

    def __init__(self, t, name):
        self.t = t
        self.name = name
        self.w = None
        self.r = []
        self.dsem = None
        self.dcnt = 0

    def __getitem__(self, k):
        return self.t[k]


class Sched:
    def __init__(self, nc, es):
        self.nc = nc
        self.es = es
        self.eng = {"pe": nc.tensor, "dve": nc.vector, "act": nc.scalar,
                    "pool": nc.gpsimd, "sp": nc.sync}
        self.sem = {}
        self.cnt = {}
        self.seen = {}
        for k in self.eng:
            self.sem[k] = es.enter_context(nc.semaphore("s_" + k))
            self.cnt[k] = 0
            self.seen[k] = {}
        self.nbuf = 0
        self.out_bufs = []

    def sbuf(self, shape, dtype=F32, name=None):
        self.nbuf += 1
        name = name or f"sb{self.nbuf}"
        t = self.es.enter_context(self.nc.sbuf_tensor(name, list(shape), dtype))
        return Buf(t, name)

    def psum(self, shape, dtype=F32, name=None):
        self.nbuf += 1
        name = name or f"ps{self.nbuf}"
        t = self.es.enter_context(self.nc.psum_tensor(name, list(shape), dtype))
        return Buf(t, name)

    def view(self, name="v"):
        return Buf(None, name)

    def _collect(self, eng, reads, writes):
        need = {}

        def add(dep, raw):
            if dep is None:
                return
            if dep[0] == "dma":
                key = ("d", dep[1])
                sem, val = dep[1], dep[2]
            else:
                e2, val = dep
                if e2 == eng:
                    if eng == "pe" or not raw or not SAME_ENG_SYNC:
                        return
                key = ("e", e2)
                sem = self.sem[e2]
            if need.get(key, (None, 0))[1] < val:
                need[key] = (sem, val)

        for b in reads:
            add(b.w, True)
        for b in writes:
            add(b.w, True)
            for r in b.r:
                add(r, False)
        return need

    def _emit_waits(self, eng, need):
        E = self.eng[eng]
        seen = self.seen[eng]
        for key, (sem, val) in need.items():
            if seen.get(key, 0) < val:
                E.wait_ge(sem, val)
                seen[key] = val

    def op(self, eng, fn, reads=(), writes=()):
        need = self._collect(eng, reads, writes)
        self._emit_waits(eng, need)
        ins = fn(self.eng[eng])
        self.cnt[eng] += 1
        ins.then_inc(self.sem[eng], 1)
        me = (eng, self.cnt[eng])
        for b in reads:
            b.r.append(me)
        for b in writes:
            b.w = me
            b.r = []
        return ins

    def dma(self, q, out, in_, reads=(), writes=(), track=None, **kw):
        need = self._collect("dmaq_" + q, reads, writes)
        self._emit_waits(q, need)
        b = track or (writes[0] if writes else reads[0])
        if b.dsem is None:
            b.dsem = self.es.enter_context(self.nc.semaphore("d_" + b.name))
            self.out_bufs.append(b)
        ins = self.eng[q].dma_start(out=out, in_=in_, **kw)
        b.dcnt += 16
        ins.then_inc(b.dsem, 16)
        me = ("dma", b.dsem, b.dcnt)
        for x in reads:
            x.r.append(me)
        for x in writes:
            x.w = me
            x.r = []
        return ins

    def mark_output(self, b):
        if b not in self.out_bufs:
            self.out_bufs.append(b)

    def finish(self):
        E = self.eng["sp"]
        for b in self.out_bufs:
            if b.dsem is not None:
                E.wait_ge(b.dsem, b.dcnt)


def r32(ap):
    return ap.bitcast(F32R)


L1_COLS = 3072


def build_l1():
    nc = bass.Bass("TRN2", target_bir_lowering=False)
    cT = nc.dram_tensor("cT", [128, KC, B], F32, kind="ExternalInput").ap()
    w = nc.dram_tensor("w", [KC, 128, L1_COLS], F32, kind="ExternalInput").ap()
    bias = nc.dram_tensor("bias", [B, L1_COLS], F32, kind="ExternalInput").ap()
    out = nc.dram_tensor("out", [B, L1_COLS], F32, kind="ExternalOutput").ap()
    with ExitStack() as es:
        S_ = Sched(nc, es)
        sc = S_.sbuf([128, KC, B], name="sc")
        bt = S_.sbuf([B, L1_COLS], name="bt")
        ot = S_.sbuf([B, L1_COLS], name="ot")
        NB = 4
        wb = [S_.sbuf([128, L1_COLS], name=f"wb{i}") for i in range(NB)]
        ps = [S_.psum([128, 512], name=f"ps{i}") for i in range(6)]
        S_.dma("sp", sc[:], cT, writes=[sc])
        S_.dma("sp", bt[:], bias, writes=[bt])
        S_.op("act", lambda e: e.activation(out=sc[:], in_=sc[:], func=AF.Silu),
              reads=[sc], writes=[sc])
        for k in range(KC):
            wk = wb[k % NB]
            S_.dma("sp" if k % 2 == 0 else "pool", wk[:], w[k], writes=[wk])
            for n in range(6):
                S_.op("pe", lambda e, n=n, k=k, wk=wk: e.matmul(
                    ps[n][0:B, :], lhsT=sc[:, k, :], rhs=wk[:, n * 512:(n + 1) * 512],
                    start=(k == 0), stop=(k == KC - 1)),
                    reads=[sc, wk], writes=[ps[n]])
        for n in range(6):
            S_.op("dve", lambda e, n=n: e.tensor_tensor(
                out=ot[:, n * 512:(n + 1) * 512], in0=ps[n][0:B, :],
                in1=bt[:, n * 512:(n + 1) * 512], op=ALU.add),
                reads=[ps[n], bt], writes=[ot])
        S_.dma("sp", out, ot[:], reads=[ot])
        S_.mark_output(ot)
        S_.finish()
    return nc


def run_l1(c, ada_w, ada_b):
    nc = build_l1()
    cT = np.ascontiguousarray(c.T.reshape(KC, 128, B).transpose(1, 0, 2))
    in_maps = []
    for core in range(NCORES):
        s = core // 2
        i, j = s // 2, s % 2
        c0 = (core % 2) * L1_COLS
        wsl = np.ascontiguousarray(ada_w[i, j][:, c0:c0 + L1_COLS]).reshape(KC, 128, L1_COLS)
        bsl = np.ascontiguousarray(np.broadcast_to(ada_b[i, j][c0:c0 + L1_COLS], (B, L1_COLS)))
        in_maps.append({"cT": cT, "w": wsl, "bias": bsl})
    res = run_bass_kernel_spmd(nc, in_maps, core_ids=list(range(NCORES)))
    mod = np.zeros((2, 2, B, 3 * D), np.float32)
    for core in range(NCORES):
        s = core // 2
        i, j = s // 2, s % 2
        c0 = (core % 2) * L1_COLS
        mod[i, j][:, c0:c0 + L1_COLS] = res.results[core]["out"]
    return mod


NPASS = 2
TP = 512
TT = TP // 128
P_GATE1, P_LNG1, P_LNB1, P_SC2, P_SH2, P_GATE2, P_LNG2, P_LNB2 = range(8)


def build_l3(n_experts=NE):
    nc = bass.Bass("TRN2", target_bir_lowering=False)
    aT_d = nc.dram_tensor("aT", [NPASS, 128, KC, TP], F32R, kind="ExternalInput").ap()
    wp_d = nc.dram_tensor("wp", [4, 128, KC * 512], F32R, kind="ExternalInput").ap()
    x_d = nc.dram_tensor("x", [NPASS, TT, 128, D], F32, kind="ExternalInput").ap()
    par_d = nc.dram_tensor("par", [8, 128, D], F32, kind="ExternalInput").ap()
    wr_d = nc.dram_tensor("wr", [128, KC, 36], F32, kind="ExternalInput").ap()
    br_d = nc.dram_tensor("br", [128, 36], F32, kind="ExternalInput").ap()
    wgu_d = nc.dram_tensor("wgu", [NE * 4, 128, 2 * KC * 128], F32R, kind="ExternalInput").ap()
    wd_d = nc.dram_tensor("wd", [NE, 128, 4 * D], F32R, kind="ExternalInput").ap()
    id_d = nc.dram_tensor("ident", [128, 128], F32, kind="ExternalInput").ap()
    out_d = nc.dram_tensor("out", [NPASS, TT, 128, D], F32, kind="ExternalOutput").ap()

    with ExitStack() as es:
        S_ = Sched(nc, es)
        hT = S_.sbuf([128, KC, TP], name="hT")
        acc = [S_.sbuf([128, D], name=f"acc{t}") for t in range(TT)]
        wdb = [S_.sbuf([128, 4 * D], name=f"wdb{i}") for i in range(2)]
        gub = [S_.sbuf([128, 2 * KC * 128], name=f"gub{i}") for i in range(2)]
        aTb = [S_.sbuf([128, TP], name=f"aTb{i}") for i in range(8)]
        par = [S_.sbuf([128, D], name=f"par{i}") for i in range(3)]
        gG = [Buf(gub[i].t, f"gG{i}") for i in range(2)]
        gU = [Buf(gub[i].t, f"gU{i}") for i in range(2)]
        scr = gG[0]
        sh2 = gG[1]
        wr = S_.sbuf([128, KC, 36], name="wr_sb")
        br = S_.sbuf([128, 36], name="br_sb")
        ident = S_.sbuf([128, 128], name="ident_sb")
        G = S_.sbuf([128, TT, NE], name="G")
        sm = [S_.sbuf([128, 64], name=f"sm{i}") for i in range(2)]
        ps = [S_.psum([128, 512], name=f"psb{i}") for i in range(8)]
        pg, pu, py = ps[0:2], ps[2:4], ps[4:8]

        S_.dma("sp", wr[:], wr_d, writes=[wr])
        S_.dma("sp", br[:], br_d, writes=[br])
        S_.dma("sp", ident[:], id_d, writes=[ident])

        def ln_tile(xb, gb, gap, bb, bap, smb):
            s1, s2, mean, msq, var, std, rstd, nmr = [smb[:, i:i + 1] for i in range(8)]
            S_.op("act", lambda e: e.activation(out=r32(scr[:, 0:D]), in_=xb[:], func=AF.Identity, accum_out=s1),
                  reads=[xb], writes=[scr, smb])
            S_.op("act", lambda e: e.activation(out=r32(scr[:, 0:D]), in_=xb[:], func=AF.Square, accum_out=s2),
                  reads=[xb], writes=[scr, smb])
            S_.op("dve", lambda e: e.tensor_scalar(out=mean, in0=s1, scalar1=1.0 / D, scalar2=None, op0=ALU.mult),
                  reads=[smb], writes=[smb])
            S_.op("dve", lambda e: e.tensor_tensor(out=msq, in0=mean, in1=mean, op=ALU.mult),
                  reads=[smb], writes=[smb])
            S_.op("dve", lambda e: e.scalar_tensor_tensor(out=var, in0=s2, scalar=1.0 / D, in1=msq,
                                                          op0=ALU.mult, op1=ALU.subtract),
                  reads=[smb], writes=[smb])
            S_.op("dve", lambda e: e.tensor_scalar(out=var, in0=var, scalar1=EPS, scalar2=None, op0=ALU.add),
                  reads=[smb], writes=[smb])
            S_.op("act", lambda e: e.activation(out=std, in_=var, func=AF.Sqrt), reads=[smb], writes=[smb])
            S_.op("dve", lambda e: e.reciprocal(out=rstd, in_=std), reads=[smb], writes=[smb])
            S_.op("dve", lambda e: e.tensor_scalar(out=nmr, in0=mean, scalar1=rstd, scalar2=-1.0,
                                                   op0=ALU.mult, op1=ALU.mult), reads=[smb], writes=[smb])
            S_.op("act", lambda e: e.activation(out=xb[:], in_=xb[:], func=AF.Identity, scale=rstd, bias=nmr),
                  reads=[xb, smb], writes=[xb])
            S_.op("dve", lambda e: e.tensor_tensor(out=xb[:], in0=xb[:], in1=gap, op=ALU.mult),
                  reads=[xb, gb], writes=[xb])
            S_.op("dve", lambda e: e.tensor_tensor(out=xb[:], in0=xb[:], in1=bap, op=ALU.add),
                  reads=[xb, bb], writes=[xb])

        pyi = 0
        wpc = [[Buf(wdb[i].t, f"wpc{i}_{h}") for h in range(4)] for i in range(2)]
        for pz in range(NPASS):
            S_.dma("pool", r32(hT[:]), aT_d[pz], writes=[hT])
            for t in range(TT):
                S_.dma("sp", acc[t][:], x_d[pz, t], writes=[acc[t]])
            S_.dma("sp", par[0][:], par_d[P_GATE1], writes=[par[0]])
            S_.dma("sp", par[1][:], par_d[P_LNG1], writes=[par[1]])
            S_.dma("sp", par[2][:], par_d[P_LNB1], writes=[par[2]])
            for n in range(4):
                wb = wdb[n % 2]
                S_.dma("pool", r32(wb[:]), wp_d[n], writes=wpc[n % 2])
                wv = wb[:].rearrange("p (k c) -> p k c", k=KC)
                for t in range(TT):
                    pb = py[pyi % 4]
                    pyi += 1
                    for k in range(KC):
                        S_.op("pe", lambda e, k=k, t=t, pb=pb, wv=wv: e.matmul(
                            pb[:], lhsT=r32(hT[:, k, t * 128:(t + 1) * 128]), rhs=r32(wv[:, k, :]),
                            start=(k == 0), stop=(k == KC - 1)), reads=[hT] + wpc[n % 2], writes=[pb])
                    cs = slice(n * 512, (n + 1) * 512)
                    sg = aTb[(n * TT + t) % 8]
                    S_.op("dve", lambda e, pb=pb, sg=sg, cs=cs: e.tensor_tensor(
                        out=r32(sg[:]), in0=pb[:], in1=par[0][:, cs], op=ALU.mult),
                        reads=[pb, par[0]], writes=[sg])
                    S_.op("dve", lambda e, t=t, sg=sg, cs=cs: e.scalar_tensor_tensor(
                        out=acc[t][:, cs], in0=acc[t][:, cs], scalar=ALPHA, in1=sg[:],
                        op0=ALU.mult, op1=ALU.add), reads=[acc[t], sg], writes=[acc[t]])
            for t in range(TT):
                ln_tile(acc[t], par[1], par[1][:], par[2], par[2][:], sm[t % 2])
            S_.dma("sp", par[0][:], par_d[P_SC2], writes=[par[0]])
            S_.dma("pool", r32(sh2[:, 0:D]), par_d[P_SH2], writes=[sh2])
            S_.op("dve", lambda e: e.tensor_scalar(out=par[0][:], in0=par[0][:], scalar1=1.0, scalar2=None,
                                                    op0=ALU.add), reads=[par[0]], writes=[par[0]])
            for t in range(TT):
                S_.op("dve", lambda e, t=t: e.tensor_tensor(out=r32(scr[:, 0:D]), in0=acc[t][:], in1=par[0][:], op=ALU.mult),
                      reads=[acc[t], par[0]], writes=[scr])
                S_.op("dve", lambda e: e.tensor_tensor(out=r32(scr[:, 0:D]), in0=scr[:, 0:D], in1=sh2[:, 0:D], op=ALU.add),
                      reads=[scr, sh2], writes=[scr])
                S_.op("dve", lambda e, t=t: e.tensor_scalar(out=acc[t][:], in0=acc[t][:], scalar1=ALPHA,
                                                             scalar2=None, op0=ALU.mult),
                      reads=[acc[t]], writes=[acc[t]])
                for k0 in range(0, KC, 4):
                    pb = py[pyi % 4]
                    pyi += 1
                    for j in range(4):
                        k = k0 + j
                        S_.op("pe", lambda e, j=j, k=k, pb=pb: e.transpose(
                            out=pb[:, j * 128:(j + 1) * 128], in_=scr[:, k * 128:(k + 1) * 128],
                            identity=ident[:]), reads=[scr, ident], writes=[pb])
                    S_.op("act", lambda e, k0=k0, t=t, pb=pb: e.activation(
                        out=r32(hT[:, k0:k0 + 4, t * 128:(t + 1) * 128]),
                        in_=pb[:].rearrange("p (a b) -> p a b", a=4), func=AF.Identity),
                        reads=[pb], writes=[hT])
            for t in range(TT):
                pb = py[pyi % 4]
                pyi += 1
                for k in range(KC):
                    S_.op("pe", lambda e, k=k, t=t, pb=pb: e.matmul(
                        pb[:, 0:36], lhsT=hT[:, k, t * 128:(t + 1) * 128], rhs=wr[:, k, :],
                        start=(k == 0), stop=(k == KC - 1)), reads=[hT, wr], writes=[pb])
                s = sm[t % 2]
                lg = s[:, 0:36]
                m4, nm4, s4, pgp = s[:, 36:37], s[:, 37:38], s[:, 38:39], s[:, 39:40]
                oh4 = s[:, 40:44]
                e4 = s[:, 44:48]
                sel = s[:, 48:56]
                S_.op("dve", lambda e: e.tensor_tensor(out=lg, in0=pb[:, 0:36], in1=br[:], op=ALU.add),
                      reads=[pb, br], writes=[s])
                S_.op("dve", lambda e: e.reduce_max(out=m4, in_=s[:, 0:4], axis=AX.X), reads=[s], writes=[s])
                S_.op("dve", lambda e: e.tensor_scalar(out=nm4, in0=m4, scalar1=-1.0, scalar2=None, op0=ALU.mult),
                      reads=[s], writes=[s])
                S_.op("act", lambda e: e.activation(out=e4, in_=s[:, 0:4], func=AF.Exp, bias=nm4, accum_out=s4),
                      reads=[s], writes=[s])
                S_.op("dve", lambda e: e.reciprocal(out=pgp, in_=s4), reads=[s], writes=[s])
                S_.op("dve", lambda e: e.tensor_scalar(out=oh4, in0=s[:, 0:4], scalar1=m4, scalar2=None,
                                                       op0=ALU.is_equal), reads=[s], writes=[s])
                S_.op("dve", lambda e: e.tensor_scalar(out=sel, in0=s[:, 4:12], scalar1=s[:, 40:41], scalar2=None,
                                                       op0=ALU.mult), reads=[s], writes=[s])
                for g in range(1, 4):
                    S_.op("dve", lambda e, g=g: e.scalar_tensor_tensor(
                        out=sel, in0=s[:, 4 + 8 * g:12 + 8 * g], scalar=s[:, 40 + g:41 + g], in1=sel,
                        op0=ALU.mult, op1=ALU.add), reads=[s], writes=[s])
                s2b = sm[t % 2]
                m1, m2, nm1, dd, p1, p2 = [s[:, 56 + i:57 + i] for i in range(6)]
                g1, g2 = s[:, 62:63], s[:, 63:64]
                o1 = G[:, t, 0:8]
                o2 = G[:, t, 8:16]
                sel2 = G[:, t, 16:24]
                g8 = G[:, t, 24:32]
                S_.op("dve", lambda e: e.reduce_max(out=m1, in_=sel, axis=AX.X), reads=[s], writes=[s])
                S_.op("dve", lambda e: e.tensor_scalar(out=o1, in0=sel, scalar1=m1, scalar2=None, op0=ALU.is_equal),
                      reads=[s], writes=[G])
                S_.op("dve", lambda e: e.scalar_tensor_tensor(out=sel2, in0=o1, scalar=-1e30, in1=sel,
                                                              op0=ALU.mult, op1=ALU.add), reads=[s, G], writes=[G])
                S_.op("dve", lambda e: e.reduce_max(out=m2, in_=sel2, axis=AX.X), reads=[G], writes=[s])
                S_.op("dve", lambda e: e.tensor_scalar(out=o2, in0=sel2, scalar1=m2, scalar2=None, op0=ALU.is_equal),
                      reads=[s, G], writes=[G])
                S_.op("dve", lambda e: e.tensor_scalar(out=nm1, in0=m1, scalar1=-1.0, scalar2=None, op0=ALU.mult),
                      reads=[s], writes=[s])
                S_.op("act", lambda e: e.activation(out=dd, in_=m2, func=AF.Exp, bias=nm1), reads=[s], writes=[s])
                S_.op("dve", lambda e: e.tensor_scalar(out=p1, in0=dd, scalar1=1.0, scalar2=None, op0=ALU.add),
                      reads=[s], writes=[s])
                S_.op("dve", lambda e: e.reciprocal(out=p1, in_=p1), reads=[s], writes=[s])
                S_.op("dve", lambda e: e.tensor_tensor(out=p2, in0=dd, in1=p1, op=ALU.mult), reads=[s], writes=[s])
                S_.op("dve", lambda e: e.tensor_tensor(out=g1, in0=p1, in1=pgp, op=ALU.mult), reads=[s], writes=[s])
                S_.op("dve", lambda e: e.tensor_tensor(out=g2, in0=p2, in1=pgp, op=ALU.mult), reads=[s], writes=[s])
                S_.op("dve", lambda e: e.tensor_scalar(out=g8, in0=o1, scalar1=g1, scalar2=None, op0=ALU.mult),
                      reads=[s, G], writes=[G])
                S_.op("dve", lambda e: e.scalar_tensor_tensor(out=g8, in0=o2, scalar=g2, in1=g8,
                                                              op0=ALU.mult, op1=ALU.add), reads=[s, G], writes=[G])
                S_.op("dve", lambda e: e.tensor_copy(out=sel, in_=g8), reads=[G], writes=[s])
                for g in range(4):
                    S_.op("dve", lambda e, g=g: e.tensor_scalar(
                        out=G[:, t, 8 * g:8 * g + 8], in0=sel, scalar1=s[:, 40 + g:41 + g], scalar2=None,
                        op0=ALU.mult), reads=[s], writes=[G])
            S_.dma("sp", par[0][:], par_d[P_GATE2], writes=[par[0]])
            ui = 0

            def load_wd_piece(ex_, h):
                bi_ = ex_ % 2
                S_.dma("pool", r32(wdb[bi_].t[:, h * D:(h + 1) * D]), wd_d[ex_][:, h * D:(h + 1) * D],
                       writes=[wpc[bi_][h]])

            for h in range(4):
                load_wd_piece(0, h)

            def load_unit(u):
                bi = u % 2
                S_.dma("pool", r32(gub[bi].t[:, 0:2048]), wgu_d[u][:, 0:2048], writes=[gG[bi]])
                S_.dma("pool", r32(gub[bi].t[:, 2048:4096]), wgu_d[u][:, 2048:4096], writes=[gU[bi]])

            ubase = ui
            load_unit(0)
            for ex in range(n_experts):
                wb = wdb[ex % 2]
                for hc in range(4):
                    u = ex * 4 + hc
                    bi = u % 2
                    if u + 1 < n_experts * 4:
                        load_unit(u + 1)
                    wp_ = wpc[ex % 2][hc]
                    S_.op("pool", lambda e, hc=hc, wb=wb: e.tensor_tensor(
                        out=r32(wb[:, hc * D:(hc + 1) * D]), in0=wb[:, hc * D:(hc + 1) * D], in1=par[0][:], op=ALU.mult),
                        reads=[wp_, par[0]], writes=[wp_])
                    if ex + 1 < n_experts:
                        load_wd_piece(ex + 1, hc)
                    gv = gub[bi].t[:].rearrange("p (j k c) -> p j k c", j=2, k=KC)
                    pgb, pub = pg[ui % 2], pu[ui % 2]
                    for k in range(KC):
                        S_.op("pe", lambda e, k=k, gv=gv, pgb=pgb: e.matmul(
                            pgb[:], lhsT=r32(gv[:, 0, k, :]), rhs=r32(hT[:, k, :]),
                            start=(k == 0), stop=(k == KC - 1)), reads=[gG[bi], hT], writes=[pgb])
                    for k in range(KC):
                        S_.op("pe", lambda e, k=k, gv=gv, pub=pub: e.matmul(
                            pub[:], lhsT=r32(gv[:, 1, k, :]), rhs=r32(hT[:, k, :]),
                            start=(k == 0), stop=(k == KC - 1)), reads=[gU[bi], hT], writes=[pub])
                    ab = aTb[(ex % 2) * 4 + hc]
                    S_.op("act", lambda e, ab=ab, pgb=pgb: e.activation(out=r32(ab[:]), in_=pgb[:], func=AF.Silu),
                          reads=[pgb], writes=[ab])
                    S_.op("dve", lambda e, pub=pub, ab=ab: e.tensor_tensor(
                        out=r32(ab[:]), in0=ab[:], in1=pub[:], op=ALU.mult), reads=[ab, pub], writes=[ab])
                    ui += 1
                abs_ = [aTb[(ex % 2) * 4 + hc] for hc in range(4)]
                for t in range(TT):
                    for n in range(4):
                        pb = py[pyi % 4]
                        pyi += 1
                        for hc in range(4):
                            S_.op("pe", lambda e, hc=hc, t=t, n=n, pb=pb, wb=wb, abs_=abs_: e.matmul(
                                pb[:], lhsT=r32(abs_[hc][:, t * 128:(t + 1) * 128]),
                                rhs=r32(wb[:, hc * D + n * 512: hc * D + (n + 1) * 512]),
                                start=(hc == 0), stop=(hc == 3)), reads=[abs_[hc], wpc[ex % 2][hc]], writes=[pb])
                        cs = slice(n * 512, (n + 1) * 512)
                        S_.op("dve", lambda e, t=t, cs=cs, pb=pb, ex=ex: e.scalar_tensor_tensor(
                            out=acc[t][:, cs], in0=pb[:], scalar=G[:, t, ex:ex + 1], in1=acc[t][:, cs],
                            op0=ALU.mult, op1=ALU.add), reads=[pb, G, acc[t]], writes=[acc[t]])
            S_.dma("sp", par[1][:], par_d[P_LNG2], writes=[par[1]])
            S_.dma("sp", par[2][:], par_d[P_LNB2], writes=[par[2]])
            for t in range(TT):
                ln_tile(acc[t], par[1], par[1][:], par[2], par[2][:], sm[t % 2])
                S_.dma("act", out_d[pz, t], acc[t][:], reads=[acc[t]])
                S_.mark_output(acc[t])
        S_.finish()
    return nc


def pack_experts(wg, wu, wd):
    st = np.stack([wg, wu], axis=1).reshape(NE, 2, KC, 128, 4, 128)
    wgu = np.ascontiguousarray(st.transpose(0, 4, 3, 1, 2, 5)).reshape(NE * 4, 128, 2 * KC * 128)
    wdp = np.ascontiguousarray(wd.reshape(NE, 4, 128, D).transpose(0, 2, 1, 3)).reshape(NE, 128, 4 * D)
    return wgu, wdp


def bc128(v):
    return np.ascontiguousarray(np.broadcast_to(v, (128,) + v.shape))


_NC_CACHE = {}


def run_l3(a_tok, wp, x_tok, rows, wrc, brc, wrf, brf, wgu, wdp, trace=False):
    if "l3" not in _NC_CACHE:
        _NC_CACHE["l3"] = build_l3()
    nc = _NC_CACHE["l3"]
    wpp = np.ascontiguousarray(wp.reshape(KC, 128, 4, 512).transpose(2, 1, 0, 3)).reshape(4, 128, KC * 512)
    wr = np.ascontiguousarray(np.concatenate([wrc, wrf], axis=1).reshape(KC, 128, 36).transpose(1, 0, 2))
    br = bc128(np.concatenate([brc, brf]))
    ident = np.eye(128, dtype=np.float32)
    pars = [np.ascontiguousarray(np.stack([bc128(v) for v in rows[b]])) for b in range(B)]
    in_maps = []
    TC = NTOK // NCORES
    for c in range(NCORES):
        b = c // (NCORES // B)
        a_c = a_tok[c * TC:(c + 1) * TC]
        aT = np.ascontiguousarray(a_c.reshape(NPASS, TP, KC, 128).transpose(0, 3, 2, 1))
        xc = np.ascontiguousarray(x_tok[c * TC:(c + 1) * TC]).reshape(NPASS, TT, 128, D)
        in_maps.append({"aT": aT, "wp": wpp, "x": xc, "par": pars[b], "wr": wr, "br": br,
                        "wgu": wgu, "wd": wdp, "ident": ident})
    res = run_bass_kernel_spmd(nc, in_maps, core_ids=list(range(NCORES)), trace=trace)
    out = np.concatenate([res.results[c]["out"].reshape(TC, D) for c in range(NCORES)], axis=0)
    if trace:
        return out, res
    return out


HPC = 4
BLK = 256
NBLK = S // BLK
QB = 512
SM_SCALE = HD ** -0.5
TWO_PI_HI = 6.28125
TWO_PI_LO = 2.0 * np.pi - 6.28125


def build_l2(hpc=HPC, do_tab=True, do_proj=True, do_attn=True, nblk=NBLK, nqb=S // QB, do_sel=True, do_rot=True, do_v=True):
    nc = bass.Bass("TRN2", target_bir_lowering=False)
    xT_d = nc.dram_tensor("xT", [NBLK, 128, KC * BLK], F32R, kind="ExternalInput").ap()
    mod_d = nc.dram_tensor("mod", [128, 2 * KC], F32, kind="ExternalInput").ap()
    wqk_d = nc.dram_tensor("wqk", [HPC, 128, 6 * KC * 128], F32R, kind="ExternalInput").ap()
    wv_d = nc.dram_tensor("wv", [HPC, 128, KC * 384], F32R, kind="ExternalInput").ap()
    pos_d = nc.dram_tensor("pos", [32, S], I32, kind="ExternalInput").ap()
    invf_d = nc.dram_tensor("invf", [32, 1], F32, kind="ExternalInput").ap()
    rot_d = nc.dram_tensor("rot", [128, 128], F32, kind="ExternalInput").ap()
    msk_d = nc.dram_tensor("msk", [2, 128, QB], F32, kind="ExternalInput").ap()
    o_d = nc.dram_tensor("o", [HPC, S // 128, 128, HD], F32, kind="ExternalOutput").ap()

    with ExitStack() as es:
        S_ = Sched(nc, es)
        xb = [S_.sbuf([128, KC * BLK], name=f"xb{i}") for i in range(2)]
        wqk = S_.sbuf([128, 6 * KC * 128], name="wqk_sb")
        wv = S_.sbuf([128, KC * 384], name="wv_sb")
        QT = [S_.sbuf([128, S], BF16, name=f"QT{g}") for g in range(NG)]
        KT = [S_.sbuf([128, S], BF16, name=f"KT{g}") for g in range(NG)]
        V = S_.sbuf([128, S // 128, NG, HD + 1], BF16, name="V")
        cosT = S_.sbuf([128, S], BF16, name="cosT")
        sinT = S_.sbuf([128, S], BF16, name="sinT")
        mod = S_.sbuf([128, 2 * KC], name="mod_sb")
        invf = S_.sbuf([32, 1], name="invf_sb")
        rotf = S_.sbuf([128, 128], name="rotf")
        rotb = S_.sbuf([128, 128], BF16, name="rotb")
        mskf = S_.sbuf([128, QB], name="mskf")
        msk = [S_.sbuf([128, QB], BF16, name=f"msk{i}") for i in range(2)]
        PT = [S_.sbuf([128, QB], BF16, name=f"PT{i}") for i in range(4)]
        t1 = [S_.sbuf([128, BLK], name=f"t1_{i}") for i in range(2)]
        t2 = [S_.sbuf([128, BLK], name=f"t2_{i}") for i in range(2)]
        osb = [S_.sbuf([128, HD], name=f"osb{i}") for i in range(2)]
        rden = [S_.sbuf([128, 1], name=f"rden{i}") for i in range(2)]
        ps = [S_.psum([128, 512], name=f"psb{i}") for i in range(8)]

        S_.dma("sp", mod[:], mod_d, writes=[mod])
        S_.dma("sp", invf[:], invf_d, writes=[invf])
        S_.dma("sp", rotf[:], rot_d, writes=[rotf])
        S_.op("dve", lambda e: e.tensor_copy(out=rotb[:], in_=rotf[:]), reads=[rotf], writes=[rotb])
        for i in range(2):
            S_.dma("sp", mskf[:], msk_d[i], writes=[mskf])
            S_.op("dve", lambda e, i=i: e.tensor_copy(out=msk[i][:], in_=mskf[:]), reads=[mskf], writes=[msk[i]])
        S_.op("pool", lambda e: e.memset(V[:], 1.0), writes=[V])
        S_.op("pool", lambda e: e.memset(cosT[:], 1.0), writes=[cosT])
        S_.op("pool", lambda e: e.memset(sinT[:], 0.0), writes=[sinT])
        S_.op("dve", lambda e: e.tensor_scalar(out=mod[:, 0:KC], in0=mod[:, 0:KC], scalar1=1.0, scalar2=None,
                                               op0=ALU.add), reads=[mod], writes=[mod])

        for ch in range(S // BLK if do_tab else 0):
            cs = slice(ch * BLK, (ch + 1) * BLK)
            bA, bB, bC, bM = t1[0], t1[1], t2[0], t2[1]
            posi = bA[0:32, :].bitcast(I32)
            ang = bA[0:32, :]
            tq = bB[0:32, :]
            ni = bB[0:32, :].bitcast(I32)
            nf = bB[0:32, :]
            rr = bC[0:32, :]
            mk = bM[0:32, :]
            S_.dma("sp", posi, pos_d[:, cs], writes=[bA])
            S_.op("dve", lambda e: e.tensor_copy(out=ang, in_=posi), reads=[bA], writes=[bA])
            S_.op("dve", lambda e: e.tensor_scalar(out=ang, in0=ang, scalar1=invf[:, 0:1], scalar2=None, op0=ALU.mult),
                  reads=[bA, invf], writes=[bA])
            for which, tab in ((0, sinT), (1, cosT)):
                off = 0.0 if which == 0 else float(np.pi / 2)
                S_.op("dve", lambda e, off=off: e.tensor_scalar(out=tq, in0=ang, scalar1=off, scalar2=float(1 / (2 * np.pi)),
                                                                op0=ALU.add, op1=ALU.mult), reads=[bA], writes=[bB])
                S_.op("dve", lambda e: e.tensor_copy(out=ni, in_=tq), reads=[bB], writes=[bB])
                S_.op("dve", lambda e: e.tensor_copy(out=nf, in_=ni), reads=[bB], writes=[bB])
                S_.op("dve", lambda e, off=off: e.tensor_scalar(out=rr, in0=ang, scalar1=off, scalar2=None, op0=ALU.add),
                      reads=[bA], writes=[bC])
                S_.op("dve", lambda e: e.scalar_tensor_tensor(out=rr, in0=nf, scalar=-TWO_PI_HI, in1=rr,
                                                              op0=ALU.mult, op1=ALU.add), reads=[bC, bB], writes=[bC])
                S_.op("dve", lambda e: e.scalar_tensor_tensor(out=rr, in0=nf, scalar=-TWO_PI_LO, in1=rr,
                                                              op0=ALU.mult, op1=ALU.add), reads=[bC, bB], writes=[bC])
                S_.op("dve", lambda e: e.tensor_scalar(out=mk, in0=rr, scalar1=float(np.pi), scalar2=float(-2 * np.pi),
                                                       op0=ALU.is_gt, op1=ALU.mult), reads=[bC], writes=[bM])
                S_.op("dve", lambda e: e.tensor_tensor(out=rr, in0=rr, in1=mk, op=ALU.add), reads=[bC, bM], writes=[bC])
                S_.op("dve", lambda e: e.tensor_scalar(out=mk, in0=rr, scalar1=float(-np.pi), scalar2=float(2 * np.pi),
                                                       op0=ALU.is_lt, op1=ALU.mult), reads=[bC], writes=[bM])
                S_.op("dve", lambda e: e.tensor_tensor(out=rr, in0=rr, in1=mk, op=ALU.add), reads=[bC, bM], writes=[bC])
                S_.op("dve", lambda e: e.tensor_scalar(out=rr, in0=rr, scalar1=float(np.pi), scalar2=float(-np.pi),
                                                       op0=ALU.min, op1=ALU.max), reads=[bC], writes=[bC])
                S_.op("act", lambda e, tab=tab, cs=cs: e.activation(out=tab[0:32, cs], in_=rr, func=AF.Sin),
                      reads=[bC], writes=[tab])

        pi_ = [0]
        pti = 0
        oi = 0
        pref = set()
        xk = [[Buf(xb[i].t, f"xk{i}_{k}") for k in range(KC)] for i in range(2)]

        def load_x(bi, blk):
            for q4 in range(4):
                cols = slice(q4 * 4 * BLK, (q4 + 1) * 4 * BLK)
                S_.dma("pool", r32(xb[bi].t[:, cols]), xT_d[blk][:, cols], writes=xk[bi][4 * q4:4 * q4 + 4])

        for hl in range(hpc):
            if ("w", hl) not in pref:
                S_.dma("pool", r32(wqk[:]), wqk_d[hl], writes=[wqk])
                S_.dma("pool", r32(wv[:]), wv_d[hl], writes=[wv])
            wq = wqk[:].rearrange("p (j k c) -> p j k c", j=6, k=KC)
            wvv = wv[:].rearrange("p (k c) -> p k c", k=KC)
            for blk in range(nblk if do_proj else 0):
                x_ = xb[blk % 2]
                xk_ = xk[blk % 2]
                if ("x", hl, blk) not in pref:
                    load_x(blk % 2, blk)
                xv = x_[:].rearrange("p (k t) -> p k t", k=KC)
                for k in range(KC):
                    if k % 2 == 0:
                        S_.op("act", lambda e, k=k, xv=xv: e.activation(
                            out=r32(xv[:, k, :]), in_=xv[:, k, :], func=AF.Identity,
                            scale=mod[:, k:k + 1], bias=mod[:, KC + k:KC + k + 1]), reads=[xk_[k], mod], writes=[xk_[k]])
                    else:
                        S_.op("dve", lambda e, k=k, xv=xv: e.tensor_scalar(
                            out=r32(xv[:, k, :]), in0=xv[:, k, :], scalar1=mod[:, k:k + 1],
                            scalar2=mod[:, KC + k:KC + k + 1], op0=ALU.mult, op1=ALU.add),
                            reads=[xk_[k], mod], writes=[xk_[k]])
                cs = slice(blk * BLK, (blk + 1) * BLK)

                def proj(j):
                    g, isk = j // 2, j % 2
                    dst = (KT if isk else QT)[g]
                    pq = ps[pi_[0] % 4]
                    pr = ps[4 + (pi_[0] % 2)]
                    pi_[0] += 1
                    for k in range(KC):
                        S_.op("pe", lambda e, k=k, j=j, pq=pq: e.matmul(
                            pq[:, 0:BLK], lhsT=r32(wq[:, j, k, :]), rhs=r32(xv[:, k, :]),
                            start=(k == 0), stop=(k == KC - 1)), reads=[wqk, xk_[k]], writes=[pq])
                    S_.op("act", lambda e, dst=dst, pq=pq: e.activation(
                        out=dst[:, cs], in_=pq[:, 0:BLK], func=AF.Identity), reads=[pq], writes=[dst])
                    return (j, dst, pq, pr)

                def rot(st):
                    j, dst, pq, pr = st
                    S_.op("pe", lambda e: e.matmul(
                        pr[:, 0:BLK], lhsT=rotb[:], rhs=dst[:, cs], start=True, stop=True),
                        reads=[rotb, dst], writes=[pr])
                    a1, a2 = t1[j % 2], t2[j % 2]
                    S_.op("dve", lambda e: e.tensor_tensor(
                        out=a1[:], in0=pq[:, 0:BLK], in1=cosT[:, cs], op=ALU.mult),
                        reads=[pq, cosT, dst], writes=[a1])
                    S_.op("dve", lambda e: e.tensor_tensor(
                        out=a2[:], in0=pr[:, 0:BLK], in1=sinT[:, cs], op=ALU.mult),
                        reads=[pr, sinT], writes=[a2])
                    S_.op("dve", lambda e: e.tensor_tensor(
                        out=dst[:, cs], in0=a1[:], in1=a2[:], op=ALU.add), reads=[a1, a2], writes=[dst])

                def vproj(tt):
                    pv = ps[6 + tt % 2]
                    for k in range(KC):
                        S_.op("pe", lambda e, k=k: e.matmul(
                            pv[:, 0:384], lhsT=r32(xv[:, k, tt * 128:(tt + 1) * 128]), rhs=r32(wvv[:, k, :]),
                            start=(k == 0), stop=(k == KC - 1)), reads=[xk_[k], wv], writes=[pv])
                    tile_i = blk * (BLK // 128) + tt
                    S_.op("act", lambda e: e.activation(
                        out=V[:, tile_i, :, 0:HD], in_=pv[:, 0:384].rearrange("p (g c) -> p g c", g=NG),
                        func=AF.Identity), reads=[pv], writes=[V])

                prev = None
                for j in range(6):
                    cur = proj(j)
                    if prev is not None:
                        rot(prev)
                    prev = cur
                vproj(0)
                rot(prev)
                vproj(1)
            if do_proj and do_attn and hl + 1 < hpc and nblk >= 2:
                S_.dma("pool", r32(wqk[:]), wqk_d[hl + 1], writes=[wqk])
                S_.dma("pool", r32(wv[:]), wv_d[hl + 1], writes=[wv])
                pref.add(("w", hl + 1))
                for pb_ in range(2):
                    load_x(pb_, pb_)
                    pref.add(("x", hl + 1, pb_))
            if not do_attn and hl == 0:
                S_.op("dve", lambda e: e.tensor_copy(out=osb[0][:], in_=QT[0][:, 0:HD]), reads=[QT[0], KT[0], V, cosT, sinT], writes=[osb[0]])
                S_.dma("sp", o_d[0, 0], osb[0][:], reads=[osb[0]])
            for qb in range(nqb if do_attn else 0):
                work = []
                for g, d in enumerate(DILS):
                    W_ = RADIUS * d
                    for kt in range(S // 128):
                        dl = kt * 128 - qb * QB
                        if dl - (QB - 1) <= W_ and dl + 127 >= -W_:
                            work.append((g, d, kt, dl))
                po = ps[4:8]
                qs = slice(qb * QB, (qb + 1) * QB)
                nw = len(work)
                pbase = pti
                pti += nw

                def issue_s(wi):
                    g, d, kt, dl = work[wi]
                    W_ = RADIUS * d
                    pS = ps[wi % 4]
                    S_.op("pe", lambda e: e.matmul(
                        pS[:], lhsT=KT[g][:, kt * 128:(kt + 1) * 128], rhs=QT[g][:, qs], start=True, stop=True),
                        reads=[KT[g], QT[g]], writes=[pS])
                    P_ = PT[(pbase + wi) % 4]
                    S_.op("act", lambda e: e.activation(out=P_[:], in_=pS[:], func=AF.Exp, scale=SM_SCALE),
                          reads=[pS], writes=[P_])
                    if g > 0:
                        S_.op("dve", lambda e: e.tensor_tensor(out=P_[:], in0=P_[:], in1=msk[g - 1][:],
                                                               op=ALU.mult), reads=[P_, msk[g - 1]], writes=[P_])
                    if do_sel and dl - (QB - 1) < -W_:
                        S_.op("pool", lambda e: e.affine_select(
                            out=P_[:], in_=P_[:], pattern=[[-1, QB]], compare_op=ALU.is_ge, fill=0.0,
                            base=dl + W_, channel_multiplier=1), reads=[P_], writes=[P_])
                    if do_sel and dl + 127 > W_:
                        S_.op("pool", lambda e: e.affine_select(
                            out=P_[:], in_=P_[:], pattern=[[1, QB]], compare_op=ALU.is_ge, fill=0.0,
                            base=W_ - dl, channel_multiplier=-1), reads=[P_], writes=[P_])

                def issue_pv(wi):
                    g, d, kt, dl = work[wi]
                    P_ = PT[(pbase + wi) % 4]
                    for sub in range(4):
                        S_.op("pe", lambda e, sub=sub: e.matmul(
                            po[sub][:, 0:HD + 1], lhsT=P_[:, sub * 128:(sub + 1) * 128], rhs=V[:, kt, g, :],
                            start=(wi == 0), stop=(wi == nw - 1)), reads=[P_, V], writes=[po[sub]])

                LA = 3
                for wi in range(min(LA, nw)):
                    issue_s(wi)
                for wi in range(nw):
                    if wi + LA < nw:
                        issue_s(wi + LA)
                    issue_pv(wi)
                for sub in range(4):
                    ob, rd = osb[oi % 2], rden[oi % 2]
                    oi += 1
                    S_.op("dve", lambda e, rd=rd, sub=sub: e.reciprocal(out=rd[:], in_=po[sub][:, HD:HD + 1]),
                          reads=[po[sub]], writes=[rd])
                    S_.op("dve", lambda e, rd=rd, ob=ob, sub=sub: e.tensor_scalar(
                        out=ob[:], in0=po[sub][:, 0:HD], scalar1=rd[:, 0:1], scalar2=None, op0=ALU.mult),
                        reads=[po[sub], rd], writes=[ob])
                    S_.dma("sp", o_d[hl, qb * 4 + sub], ob[:], reads=[ob])
                    S_.mark_output(ob)
        S_.finish()
    return nc


def rot_matrix():
    R = np.zeros((128, 128), np.float32)
    for i in range(16):
        R[16 + i, i] = -1.0
        R[i, 16 + i] = 1.0
    return R


def run_l2(x, positions, w_qkv, mod0, trace=False):
    if "l2" not in _NC_CACHE:
        _NC_CACHE["l2"] = build_l2()
    nc = _NC_CACHE["l2"]
    invf = (THETA ** (-np.arange(0, ROT, 2, dtype=np.float32) / ROT)).astype(np.float32)
    invf32 = np.concatenate([invf, invf]).reshape(32, 1).astype(np.float32)
    kl = np.arange(128)[:, None]
    ql = np.arange(QB)[None, :]
    msk = np.stack([((kl - ql) % d == 0).astype(np.float32) for d in DILS[1:]])
    rot = rot_matrix()
    w5 = w_qkv.reshape(KC, 128, NG, 3, NH, HD)
    xTs = []
    for b in range(B):
        xt = x[b].T.reshape(KC, 128, NBLK, BLK)
        xTs.append(np.ascontiguousarray(xt.transpose(2, 1, 0, 3)).reshape(NBLK, 128, KC * BLK))
    in_maps = []
    for c in range(NCORES):
        b, hg = c // 4, c % 4
        hs = slice(hg * HPC, (hg + 1) * HPC)
        wqk = w5[:, :, :, 0:2, hs, :]
        wqk = np.ascontiguousarray(wqk.transpose(4, 1, 2, 3, 0, 5)).reshape(HPC, 128, 6 * KC * 128)
        wv = w5[:, :, :, 2, hs, :]
        wv = np.ascontiguousarray(wv.transpose(3, 1, 0, 2, 4)).reshape(HPC, 128, KC * 384)
        shift, scale = mod0[b, 0:D], mod0[b, D:2 * D]
        modc = np.concatenate([scale.reshape(KC, 128).T, shift.reshape(KC, 128).T], axis=1).astype(np.float32)
        pos = np.ascontiguousarray(np.broadcast_to(positions[b].astype(np.int32), (32, S)))
        in_maps.append({"xT": xTs[b], "mod": np.ascontiguousarray(modc), "wqk": wqk, "wv": wv, "pos": pos,
                        "invf": invf32, "rot": rot, "msk": msk})
    res = run_bass_kernel_spmd(nc, in_maps, core_ids=list(range(NCORES)), trace=trace)
    o = np.zeros((B, S, NH, HD), np.float32)
    for c in range(NCORES):
        b, hg = c // 4, c % 4
        oc = res.results[c]["o"].reshape(HPC, S, HD)
        o[b, :, hg * HPC:(hg + 1) * HPC, :] = oc.transpose(1, 0, 2)
    o = o.reshape(NTOK, D)
    if trace:
        return o, res
    return o


def sched_barrier(S_):
    engs = list(S_.eng.keys())
    for en in engs:
        E = S_.eng[en]
        for x in engs:
            if x != en and S_.cnt[x] > 0 and S_.seen[en].get(("e", x), 0) < S_.cnt[x]:
                E.wait_ge(S_.sem[x], S_.cnt[x])
                S_.seen[en][("e", x)] = S_.cnt[x]
        for b in S_.out_bufs:
            if b.dsem is not None and S_.seen[en].get(("d", b.dsem), 0) < b.dcnt:
                E.wait_ge(b.dsem, b.dcnt)
                S_.seen[en][("d", b.dsem)] = b.dcnt


GC = 512
NST = S // 128


def build_l4():
    nc = bass.Bass("TRN2", target_bir_lowering=False)
    xT_d = nc.dram_tensor("xT", [NBLK, 128, KC * BLK], F32R, kind="ExternalInput").ap()
    mod_d = nc.dram_tensor("mod", [128, 2 * KC], F32, kind="ExternalInput").ap()
    win_d = nc.dram_tensor("win", [128, KC * GC], F32R, kind="ExternalInput").ap()
    cc_d = nc.dram_tensor("cc", [2, 128, 4 * GC], F32R, kind="ExternalInput").ap()
    dft_d = nc.dram_tensor("dft", [S // QB, 16, 128, 4 * QB], F32R, kind="ExternalInput").ap()
    y_d = nc.dram_tensor("yT", [4, 128, S], F32, kind="ExternalOutput").ap()
    with ExitStack() as es:
        S_ = Sched(nc, es)
        AB = [S_.sbuf([128, NST, GC], name=f"AB{i}") for i in range(2)]
        win = S_.sbuf([128, KC * GC], name="win_sb")
        xb = [S_.sbuf([128, KC * BLK], name=f"xb{i}") for i in range(1)]
        uT = S_.sbuf([128, 4, BLK], name="uT")
        ccs = [S_.sbuf([128, 4 * GC], name=f"ccs{i}") for i in range(2)]
        mod = S_.sbuf([128, 2 * KC], name="mod_sb")
        ysb = [S_.sbuf([128, QB], name=f"ysb{i}") for i in range(2)]
        ps = [S_.psum([128, 512], name=f"psb{i}") for i in range(8)]
        S_.dma("sp", mod[:], mod_d, writes=[mod])
        S_.op("dve", lambda e: e.tensor_scalar(out=mod[:, 0:KC], in0=mod[:, 0:KC], scalar1=1.0, scalar2=None,
                                               op0=ALU.add), reads=[mod], writes=[mod])
        S_.dma("pool", r32(win[:]), win_d, writes=[win])
        for i in range(2):
            S_.dma("pool", r32(ccs[i][:]), cc_d[i], writes=[ccs[i]])
        wv = win[:].rearrange("p (k c) -> p k c", k=KC)
        pi_ = 0
        xk = [Buf(xb[0].t, f"xk_{k}") for k in range(KC)]
        for blk in range(NBLK):
            x_ = xb[0]
            for q4 in range(4):
                cols = slice(q4 * 4 * BLK, (q4 + 1) * 4 * BLK)
                S_.dma("pool", r32(x_.t[:, cols]), xT_d[blk][:, cols], writes=xk[4 * q4:4 * q4 + 4])
            xv = x_[:].rearrange("p (k t) -> p k t", k=KC)
            for k in range(KC):
                if k % 2 == 0:
                    S_.op("act", lambda e, k=k, xv=xv: e.activation(
                        out=r32(xv[:, k, :]), in_=xv[:, k, :], func=AF.Identity,
                        scale=mod[:, k:k + 1], bias=mod[:, KC + k:KC + k + 1]), reads=[xk[k], mod], writes=[xk[k]])
                else:
                    S_.op("dve", lambda e, k=k, xv=xv: e.tensor_scalar(
                        out=r32(xv[:, k, :]), in0=xv[:, k, :], scalar1=mod[:, k:k + 1],
                        scalar2=mod[:, KC + k:KC + k + 1], op0=ALU.mult, op1=ALU.add),
                        reads=[xk[k], mod], writes=[xk[k]])
            for k in range(KC):
                for cc in range(4):
                    S_.op("pe", lambda e, k=k, cc=cc, xv=xv: e.matmul(
                        ps[cc][:, 0:BLK], lhsT=r32(wv[:, k, cc * 128:(cc + 1) * 128]), rhs=r32(xv[:, k, :]),
                        start=(k == 0), stop=(k == KC - 1)), reads=[win, xk[k]], writes=[ps[cc]])
            for cc in range(4):
                if cc % 2 == 0:
                    S_.op("act", lambda e, cc=cc: e.activation(out=r32(uT[:, cc, :]), in_=ps[cc][:, 0:BLK],
                                                               func=AF.Identity), reads=[ps[cc]], writes=[uT])
                else:
                    S_.op("dve", lambda e, cc=cc: e.tensor_copy(out=r32(uT[:, cc, :]), in_=ps[cc][:, 0:BLK]),
                          reads=[ps[cc]], writes=[uT])
            for tt in range(BLK // 128):
                st = blk * (BLK // 128) + tt
                for i in range(2):
                    pa = ps[4 + (2 * tt + i) % 4]
                    cv = ccs[i][:].rearrange("p (c m) -> p c m", c=4)
                    for cc in range(4):
                        S_.op("pe", lambda e, cc=cc, tt=tt, pa=pa, cv=cv: e.matmul(
                            pa[:], lhsT=r32(uT[:, cc, tt * 128:(tt + 1) * 128]), rhs=r32(cv[:, cc, :]),
                            start=(cc == 0), stop=(cc == 3)), reads=[uT, ccs[i]], writes=[pa])
                    if i == 0:
                        S_.op("act", lambda e, pa=pa, st=st: e.activation(out=r32(AB[0][:, st, :]), in_=pa[:],
                                                                          func=AF.Identity), reads=[pa], writes=[AB[0]])
                    else:
                        S_.op("dve", lambda e, pa=pa, st=st: e.tensor_copy(out=r32(AB[1][:, st, :]), in_=pa[:]),
                              reads=[pa], writes=[AB[1]])
        sched_barrier(S_)
        dpc = [Buf(win.t, f"dpc{i}") for i in range(4)]
        yi = 0
        di = 0
        for kb in range(S // QB):
            for pc in range(16):
                sg, cs_ = pc // 2, pc % 2
                db = dpc[di % 4]
                dcol = slice((di % 4) * 2048, (di % 4 + 1) * 2048)
                di += 1
                S_.dma("pool", r32(win.t[:, dcol]), dft_d[kb, pc], writes=[db])
                dv = win.t[:, dcol].rearrange("p (s k) -> p s k", s=4)
                for s4 in range(4):
                    st = sg * 4 + s4
                    for mc in range(4):
                        first = (pc == 0 and s4 == 0)
                        last = (pc == 15 and s4 == 3)
                        S_.op("pe", lambda e, mc=mc, st=st, s4=s4, cs_=cs_, dv=dv, first=first, last=last: e.matmul(
                            ps[mc][:], lhsT=r32(AB[cs_][:, st, mc * 128:(mc + 1) * 128]), rhs=r32(dv[:, s4, :]),
                            start=first, stop=last), reads=[AB[cs_], db], writes=[ps[mc]])
            for mc in range(4):
                yb = ysb[yi % 2]
                yi += 1
                if mc % 2 == 0:
                    S_.op("act", lambda e, mc=mc, yb=yb: e.activation(out=yb[:], in_=ps[mc][:], func=AF.Identity),
                          reads=[ps[mc]], writes=[yb])
                else:
                    S_.op("dve", lambda e, mc=mc, yb=yb: e.tensor_copy(out=yb[:], in_=ps[mc][:]),
                          reads=[ps[mc]], writes=[yb])
                S_.dma("sp", y_d[mc, :, kb * QB:(kb + 1) * QB], yb[:], reads=[yb])
        S_.finish()
    return nc


def dft_tables():
    n = np.arange(S)
    angS = 2 * np.pi * ((n[:, None] * n[None, :]) % S) / S
    CS = (np.cos(angS) / np.sqrt(S)).astype(np.float32)
    SS = (-np.sin(angS) / np.sqrt(S)).astype(np.float32)
    m = np.arange(GC)
    angC = 2 * np.pi * ((m[:, None] * m[None, :]) % GC) / GC
    CC = (np.cos(angC) / np.sqrt(GC)).astype(np.float32)
    SC = (np.sin(angC) / np.sqrt(GC)).astype(np.float32)
    M = np.stack([CS, SS])
    M = M.reshape(2, 8, 4, 128, S // QB, QB)
    dft = np.ascontiguousarray(M.transpose(4, 1, 0, 3, 2, 5)).reshape(S // QB, 16, 128, 4 * QB)
    cc = np.stack([CC, SC]).reshape(2, 4, 128, GC)
    cc = np.ascontiguousarray(cc.transpose(0, 2, 1, 3)).reshape(2, 128, 4 * GC)
    return dft, cc


def run_l4(x_tok, w_in, mod0):
    if "l4" not in _NC_CACHE:
        _NC_CACHE["l4"] = build_l4()
    nc = _NC_CACHE["l4"]
    dft, cc = dft_tables()
    x = x_tok.reshape(B, S, D)
    xTs = []
    for b in range(B):
        xt = x[b].T.reshape(KC, 128, NBLK, BLK)
        xTs.append(np.ascontiguousarray(xt.transpose(2, 1, 0, 3)).reshape(NBLK, 128, KC * BLK))
    in_maps = []
    for c in range(NCORES):
        b, g = c // 4, c % 4
        wi = w_in[:, g * GC:(g + 1) * GC].reshape(KC, 128, GC)
        wi = np.ascontiguousarray(wi.transpose(1, 0, 2)).reshape(128, KC * GC)
        shift, scale = mod0[b, 0:D], mod0[b, D:2 * D]
        modc = np.ascontiguousarray(np.concatenate([scale.reshape(KC, 128).T, shift.reshape(KC, 128).T], axis=1))
        in_maps.append({"xT": xTs[b], "mod": modc.astype(np.float32), "win": wi, "cc": cc, "dft": dft})
    res = run_bass_kernel_spmd(nc, in_maps, core_ids=list(range(NCORES)))
    y = np.zeros((B, S, D), np.float32)
    for c in range(NCORES):
        b, g = c // 4, c % 4
        yT = res.results[c]["yT"].reshape(GC, S)
        y[b, :, g * GC:(g + 1) * GC] = yT.T
    return y.reshape(NTOK, D)


def kernel(x, c, positions, ada_w, ada_b, attn_w_qkv, attn_w_o, fnet_w_in, fnet_w_out,
           ln_g, ln_b, router_coarse_w, router_coarse_b, router_fine_w, router_fine_b,
           expert_w_gate, expert_w_up, expert_w_down):
    f = lambda a: np.asarray(a, dtype=np.float32)
    x, c = f(x), f(c)
    positions = np.asarray(positions).astype(np.int32)
    ada_w, ada_b = f(ada_w), f(ada_b)
    ln_g, ln_b = f(ln_g), f(ln_b)
    mod = run_l1(c, ada_w, ada_b)
    x_tok = x.reshape(NTOK, D)
    for i in range(2):
        m0, m1 = mod[i, 0], mod[i, 1]
        if i == 0:
            a_tok = run_l2(x, positions, f(attn_w_qkv[0]), m0)
            wp = f(attn_w_o[0])
        else:
            a_tok = run_l4(x_tok, f(fnet_w_in[0]), m0)
            wp = f(fnet_w_out[0])
        rows = [[m0[b, 2 * D:3 * D], ln_g[i, 0], ln_b[i, 0], m1[b, D:2 * D], m1[b, 0:D], m1[b, 2 * D:3 * D],
                 ln_g[i, 1], ln_b[i, 1]] for b in range(B)]
        wgu, wdp = pack_experts(f(expert_w_gate[i]), f(expert_w_up[i]), f(expert_w_down[i]))
        x_tok = run_l3(a_tok, wp, x_tok, rows, f(router_coarse_w[i]), f(router_coarse_b[i]),
                       f(router_fine_w[i]), f(router_fine_b[i]), wgu, wdp)
        del wgu, wdp
    return x_tok.reshape(B, S, D).astype(np.float32)


TB = 1024
TTB = TB // 128
DMA_CAST = dict(max_dma_last_dim=4096)


def build_l3b(n_experts=NE):
    nc = bass.Bass("TRN2", target_bir_lowering=False)
    aT_d = nc.dram_tensor("aT", [128, KC, TB], F32, kind="ExternalInput").ap()
    wp_d = nc.dram_tensor("wp", [4, 128, KC * 512], F32, kind="ExternalInput").ap()
    x_d = nc.dram_tensor("x", [TTB, 128, D], F32, kind="ExternalInput").ap()
    par_d = nc.dram_tensor("par", [8, 128, D], F32, kind="ExternalInput").ap()
    wr_d = nc.dram_tensor("wr", [128, KC, 36], F32, kind="ExternalInput").ap()
    br_d = nc.dram_tensor("br", [128, 36], F32, kind="ExternalInput").ap()
    wgu_d = nc.dram_tensor("wgu", [n_experts * 4, 128, 2 * KC * 128], F32, kind="ExternalInput").ap()
    wd_d = nc.dram_tensor("wd", [n_experts, 128, 4 * D], F32, kind="ExternalInput").ap()
    id_d = nc.dram_tensor("ident", [128, 128], F32, kind="ExternalInput").ap()
    out_d = nc.dram_tensor("out", [TTB, 128, D], F32, kind="ExternalOutput").ap()

    with ExitStack() as es:
        S_ = Sched(nc, es)
        hT = S_.sbuf([128, KC, TB], BF16, name="hT")
        acc = [S_.sbuf([128, D], name=f"acc{t}") for t in range(TTB)]
        wdb = [S_.sbuf([128, 4 * D], BF16, name=f"wdb{i}") for i in range(2)]
        gub = [S_.sbuf([128, 2 * KC * 128], BF16, name=f"gub{i}") for i in range(2)]
        aTb = [S_.sbuf([128, TB], BF16, name=f"aTb{i}") for i in range(8)]
        par = [S_.sbuf([128, D], name=f"par{i}") for i in range(3)]
        scr = S_.sbuf([128, D], name="scr")
        sh2 = par[1]
        hTfb = par[2]
        tmpy = [S_.sbuf([128, 512], name=f"tmpy{i}") for i in range(2)]
        wr = S_.sbuf([128, KC, 36], name="wr_sb")
        br = S_.sbuf([128, 36], name="br_sb")
        ident = S_.sbuf([128, 128], name="ident_sb")
        G = S_.sbuf([128, TTB, NE], name="G")
        sm = [S_.sbuf([128, 64], name=f"sm{i}") for i in range(2)]
        ps = [S_.psum([128, 512], name=f"psb{i}") for i in range(8)]
        pgu, py = ps[0:4], ps[4:8]

        S_.dma("sp", wr[:], wr_d, writes=[wr])
        S_.dma("sp", br[:], br_d, writes=[br])
        S_.dma("sp", ident[:], id_d, writes=[ident])

        def ln_tile(xb, gb, gap, bb, bap, smb):
            s1, s2, mean, msq, var, std, rstd, nmr = [smb[:, i:i + 1] for i in range(8)]
            S_.op("act", lambda e: e.activation(out=scr[:], in_=xb[:], func=AF.Identity, accum_out=s1),
                  reads=[xb], writes=[scr, smb])
            S_.op("act", lambda e: e.activation(out=scr[:], in_=xb[:], func=AF.Square, accum_out=s2),
                  reads=[xb], writes=[scr, smb])
            S_.op("dve", lambda e: e.tensor_scalar(out=mean, in0=s1, scalar1=1.0 / D, scalar2=None, op0=ALU.mult),
                  reads=[smb], writes=[smb])
            S_.op("dve", lambda e: e.tensor_tensor(out=msq, in0=mean, in1=mean, op=ALU.mult),
                  reads=[smb], writes=[smb])
            S_.op("dve", lambda e: e.scalar_tensor_tensor(out=var, in0=s2, scalar=1.0 / D, in1=msq,
                                                          op0=ALU.mult, op1=ALU.subtract),
                  reads=[smb], writes=[smb])
            S_.op("dve", lambda e: e.tensor_scalar(out=var, in0=var, scalar1=EPS, scalar2=None, op0=ALU.add),
                  reads=[smb], writes=[smb])
            S_.op("act", lambda e: e.activation(out=std, in_=var, func=AF.Sqrt), reads=[smb], writes=[smb])
            S_.op("dve", lambda e: e.reciprocal(out=rstd, in_=std), reads=[smb], writes=[smb])
            S_.op("dve", lambda e: e.tensor_scalar(out=nmr, in0=mean, scalar1=rstd, scalar2=-1.0,
                                                   op0=ALU.mult, op1=ALU.mult), reads=[smb], writes=[smb])
            S_.op("act", lambda e: e.activation(out=xb[:], in_=xb[:], func=AF.Identity, scale=rstd, bias=nmr),
                  reads=[xb, smb], writes=[xb])
            S_.op("pool", lambda e: e.tensor_tensor(out=xb[:], in0=xb[:], in1=gap, op=ALU.mult),
                  reads=[xb, gb], writes=[xb])
            S_.op("pool", lambda e: e.tensor_tensor(out=xb[:], in0=xb[:], in1=bap, op=ALU.add),
                  reads=[xb, bb], writes=[xb])

        pyi = 0
        for k in range(KC):
            S_.dma("pool", hT[:, k, :], aT_d[:, k, :], writes=[hT], **DMA_CAST)
        for t in range(TTB):
            S_.dma("sp", acc[t][:], x_d[t], writes=[acc[t]])
        S_.dma("sp", par[0][:], par_d[P_GATE1], writes=[par[0]])
        S_.dma("sp", par[1][:], par_d[P_LNG1], writes=[par[1]])
        S_.dma("sp", par[2][:], par_d[P_LNB1], writes=[par[2]])
        for n in range(4):
            wb = wdb[n % 2]
            S_.dma("pool", wb[:], wp_d[n], writes=[wb], **DMA_CAST)
            wv = wb[:].rearrange("p (k c) -> p k c", k=KC)
            for t in range(TTB):
                pb = py[pyi % 4]
                pyi += 1
                for k in range(KC):
                    S_.op("pe", lambda e, k=k, t=t, pb=pb, wv=wv: e.matmul(
                        pb[:], lhsT=hT[:, k, t * 128:(t + 1) * 128], rhs=wv[:, k, :],
                        start=(k == 0), stop=(k == KC - 1)), reads=[hT, wb], writes=[pb])
                cs = slice(n * 512, (n + 1) * 512)
                sg = tmpy[(n * TTB + t) % 2]
                S_.op("dve", lambda e, pb=pb, sg=sg, cs=cs: e.tensor_tensor(
                    out=sg[:], in0=pb[:], in1=par[0][:, cs], op=ALU.mult),
                    reads=[pb, par[0]], writes=[sg])
                S_.op("pool", lambda e, t=t, sg=sg, cs=cs: e.scalar_tensor_tensor(
                    out=acc[t][:, cs], in0=acc[t][:, cs], scalar=ALPHA, in1=sg[:],
                    op0=ALU.mult, op1=ALU.add), reads=[acc[t], sg], writes=[acc[t]]) if False else \
                    S_.op("dve", lambda e, t=t, sg=sg, cs=cs: e.scalar_tensor_tensor(
                        out=acc[t][:, cs], in0=acc[t][:, cs], scalar=ALPHA, in1=sg[:],
                        op0=ALU.mult, op1=ALU.add), reads=[acc[t], sg], writes=[acc[t]])
        for t in range(TTB):
            ln_tile(acc[t], par[1], par[1][:], par[2], par[2][:], sm[t % 2])
        S_.dma("sp", par[0][:], par_d[P_SC2], writes=[par[0]])
        S_.dma("sp", sh2[:], par_d[P_SH2], writes=[sh2])
        S_.op("pool", lambda e: e.tensor_scalar(out=par[0][:], in0=par[0][:], scalar1=1.0, scalar2=None,
                                                op0=ALU.add), reads=[par[0]], writes=[par[0]])
        for t in range(TTB):
            S_.op("dve", lambda e, t=t: e.tensor_tensor(out=scr[:], in0=acc[t][:], in1=par[0][:], op=ALU.mult),
                  reads=[acc[t], par[0]], writes=[scr])
            S_.op("dve", lambda e: e.tensor_tensor(out=scr[:], in0=scr[:], in1=sh2[:], op=ALU.add),
                  reads=[scr, sh2], writes=[scr])
            S_.op("pool", lambda e, t=t: e.tensor_scalar(out=acc[t][:], in0=acc[t][:], scalar1=ALPHA,
                                                         scalar2=None, op0=ALU.mult),
                  reads=[acc[t]], writes=[acc[t]])
            for k0 in range(0, KC, 4):
                pb = py[pyi % 4]
                pyi += 1
                for j in range(4):
                    k = k0 + j
                    S_.op("pe", lambda e, j=j, k=k, pb=pb: e.transpose(
                        out=pb[:, j * 128:(j + 1) * 128], in_=scr[:, k * 128:(k + 1) * 128],
                        identity=ident[:]), reads=[scr, ident], writes=[pb])
                S_.op("act", lambda e, k0=k0, t=t, pb=pb: e.activation(
                    out=hT[:, k0:k0 + 4, t * 128:(t + 1) * 128],
                    in_=pb[:].rearrange("p (a b) -> p a b", a=4), func=AF.Identity),
                    reads=[pb], writes=[hT])
                S_.op("act", lambda e, k0=k0, pb=pb: e.activation(
                    out=hTfb[:].rearrange("p (k t) -> p k t", k=KC)[:, k0:k0 + 4, :],
                    in_=pb[:].rearrange("p (a b) -> p a b", a=4), func=AF.Identity),
                    reads=[pb], writes=[hTfb])
            pb = py[pyi % 4]
            pyi += 1
            for k in range(KC):
                S_.op("pe", lambda e, k=k, pb=pb: e.matmul(
                    pb[:, 0:36], lhsT=hTfb[:, k * 128:(k + 1) * 128], rhs=wr[:, k, :],
                    start=(k == 0), stop=(k == KC - 1)), reads=[hTfb, wr], writes=[pb])
            s = sm[t % 2]
            lg = s[:, 0:36]
            m4, nm4, s4, pgp = s[:, 36:37], s[:, 37:38], s[:, 38:39], s[:, 39:40]
            oh4 = s[:, 40:44]
            e4 = s[:, 44:48]
            sel = s[:, 48:56]
            S_.op("dve", lambda e: e.tensor_tensor(out=lg, in0=pb[:, 0:36], in1=br[:], op=ALU.add),
                  reads=[pb, br], writes=[s])
            S_.op("dve", lambda e: e.reduce_max(out=m4, in_=s[:, 0:4], axis=AX.X), reads=[s], writes=[s])
            S_.op("dve", lambda e: e.tensor_scalar(out=nm4, in0=m4, scalar1=-1.0, scalar2=None, op0=ALU.mult),
                  reads=[s], writes=[s])
            S_.op("act", lambda e: e.activation(out=e4, in_=s[:, 0:4], func=AF.Exp, bias=nm4, accum_out=s4),
                  reads=[s], writes=[s])
            S_.op("dve", lambda e: e.reciprocal(out=pgp, in_=s4), reads=[s], writes=[s])
            S_.op("dve", lambda e: e.tensor_scalar(out=oh4, in0=s[:, 0:4], scalar1=m4, scalar2=None,
                                                   op0=ALU.is_equal), reads=[s], writes=[s])
            S_.op("dve", lambda e: e.tensor_scalar(out=sel, in0=s[:, 4:12], scalar1=s[:, 40:41], scalar2=None,
                                                   op0=ALU.mult), reads=[s], writes=[s])
            for g in range(1, 4):
                S_.op("dve", lambda e, g=g: e.scalar_tensor_tensor(
                    out=sel, in0=s[:, 4 + 8 * g:12 + 8 * g], scalar=s[:, 40 + g:41 + g], in1=sel,
                    op0=ALU.mult, op1=ALU.add), reads=[s], writes=[s])
            m1, m2, nm1, dd, p1, p2 = [s[:, 56 + i:57 + i] for i in range(6)]
            g1, g2 = s[:, 62:63], s[:, 63:64]
            o1 = G[:, t, 0:8]
            o2 = G[:, t, 8:16]
            sel2 = G[:, t, 16:24]
            g8 = G[:, t, 24:32]
            S_.op("dve", lambda e: e.reduce_max(out=m1, in_=sel, axis=AX.X), reads=[s], writes=[s])
            S_.op("dve", lambda e: e.tensor_scalar(out=o1, in0=sel, scalar1=m1, scalar2=None, op0=ALU.is_equal),
                  reads=[s], writes=[G])
            S_.op("dve", lambda e: e.scalar_tensor_tensor(out=sel2, in0=o1, scalar=-1e30, in1=sel,
                                                          op0=ALU.mult, op1=ALU.add), reads=[s, G], writes=[G])
            S_.op("dve", lambda e: e.reduce_max(out=m2, in_=sel2, axis=AX.X), reads=[G], writes=[s])
            S_.op("dve", lambda e: e.tensor_scalar(out=o2, in0=sel2, scalar1=m2, scalar2=None, op0=ALU.is_equal),
                  reads=[s, G], writes=[G])
            S_.op("dve", lambda e: e.tensor_scalar(out=nm1, in0=m1, scalar1=-1.0, scalar2=None, op0=ALU.mult),
                  reads=[s], writes=[s])
            S_.op("act", lambda e: e.activation(out=dd, in_=m2, func=AF.Exp, bias=nm1), reads=[s], writes=[s])
            S_.op("dve", lambda e: e.tensor_scalar(out=p1, in0=dd, scalar1=1.0, scalar2=None, op0=ALU.add),
                  reads=[s], writes=[s])
            S_.op("dve", lambda e: e.reciprocal(out=p1, in_=p1), reads=[s], writes=[s])
            S_.op("dve", lambda e: e.tensor_tensor(out=p2, in0=dd, in1=p1, op=ALU.mult), reads=[s], writes=[s])
            S_.op("dve", lambda e: e.tensor_tensor(out=g1, in0=p1, in1=pgp, op=ALU.mult), reads=[s], writes=[s])
            S_.op("dve", lambda e: e.tensor_tensor(out=g2, in0=p2, in1=pgp, op=ALU.mult), reads=[s], writes=[s])
            S_.op("dve", lambda e: e.tensor_scalar(out=g8, in0=o1, scalar1=g1, scalar2=None, op0=ALU.mult),
                  reads=[s, G], writes=[G])
            S_.op("dve", lambda e: e.scalar_tensor_tensor(out=g8, in0=o2, scalar=g2, in1=g8,
                                                          op0=ALU.mult, op1=ALU.add), reads=[s, G], writes=[G])
            S_.op("dve", lambda e: e.tensor_copy(out=sel, in_=g8), reads=[G], writes=[s])
            for g in range(4):
                S_.op("dve", lambda e, g=g: e.tensor_scalar(
                    out=G[:, t, 8 * g:8 * g + 8], in0=sel, scalar1=s[:, 40 + g:41 + g], scalar2=None,
                    op0=ALU.mult), reads=[s], writes=[G])
        S_.dma("sp", par[0][:], par_d[P_GATE2], writes=[par[0]])
        ui = 0
        ti = 0
        stage = [par[1], par[2], scr]
        sctr = [0]

        def load_cast(dst_ap, dst_buf, src_ap, ceng):
            st = stage[sctr[0] % 3]
            sctr[0] += 1
            S_.dma("sp", st[:], src_ap, writes=[st])
            if ceng == "act":
                S_.op("act", lambda e: e.activation(out=dst_ap, in_=st[:], func=AF.Identity), reads=[st], writes=[dst_buf])
            else:
                S_.op(ceng, lambda e: e.tensor_copy(out=dst_ap, in_=st[:]), reads=[st], writes=[dst_buf])

        for ex in range(n_experts):
            wb = wdb[ex % 2]
            for hc in range(4):
                load_cast(wb[:, hc * D:(hc + 1) * D], wb, wd_d[ex][:, hc * D:(hc + 1) * D], "pool")
            for hc in range(4):
                gb = gub[ui % 2]
                ceng = "act" if ui % 2 == 0 else "dve"
                for j in range(2):
                    load_cast(gb[:, j * 2048:(j + 1) * 2048], gb, wgu_d[ex * 4 + hc][:, j * 2048:(j + 1) * 2048], ceng)
                gv = gb[:].rearrange("p (j k c) -> p j k c", j=2, k=KC)
                ab = aTb[(ex % 2) * 4 + hc]
                for half in range(TB // 512):
                    ts_ = slice(half * 512, (half + 1) * 512)
                    pgb, pub = pgu[(2 * ui + half) % 2 * 2], pgu[(2 * ui + half) % 2 * 2 + 1]
                    for k in range(KC):
                        S_.op("pe", lambda e, k=k, gv=gv, pgb=pgb, ts_=ts_: e.matmul(
                            pgb[:], lhsT=gv[:, 0, k, :], rhs=hT[:, k, ts_],
                            start=(k == 0), stop=(k == KC - 1)), reads=[gb, hT], writes=[pgb])
                    for k in range(KC):
                        S_.op("pe", lambda e, k=k, gv=gv, pub=pub, ts_=ts_: e.matmul(
                            pub[:], lhsT=gv[:, 1, k, :], rhs=hT[:, k, ts_],
                            start=(k == 0), stop=(k == KC - 1)), reads=[gb, hT], writes=[pub])
                    sg = tmpy[ti % 2]
                    ti += 1
                    S_.op("act", lambda e, sg=sg, pgb=pgb: e.activation(out=sg[:], in_=pgb[:], func=AF.Silu),
                          reads=[pgb], writes=[sg])
                    S_.op("dve", lambda e, pub=pub, ab=ab, sg=sg, ts_=ts_: e.tensor_tensor(
                        out=ab[:, ts_], in0=sg[:], in1=pub[:], op=ALU.mult), reads=[sg, pub], writes=[ab])
                ui += 1
            abs_ = [aTb[(ex % 2) * 4 + hc] for hc in range(4)]
            for t in range(TTB):
                for n in range(4):
                    pb = py[pyi % 4]
                    pyi += 1
                    for hc in range(4):
                        S_.op("pe", lambda e, hc=hc, t=t, n=n, pb=pb, wb=wb, abs_=abs_: e.matmul(
                            pb[:], lhsT=abs_[hc][:, t * 128:(t + 1) * 128],
                            rhs=wb[:, hc * D + n * 512: hc * D + (n + 1) * 512],
                            start=(hc == 0), stop=(hc == 3)), reads=[abs_[hc], wb], writes=[pb])
                    cs = slice(n * 512, (n + 1) * 512)
                    yt = tmpy[ti % 2]
                    ti += 1
                    S_.op("act", lambda e, pb=pb, yt=yt, t=t, ex=ex: e.activation(
                        out=yt[:], in_=pb[:], func=AF.Identity, scale=G[:, t, ex:ex + 1]), reads=[pb, G], writes=[yt])
                    S_.op("dve", lambda e, yt=yt, cs=cs: e.tensor_tensor(
                        out=yt[:], in0=yt[:], in1=par[0][:, cs], op=ALU.mult), reads=[yt, par[0]], writes=[yt])
                    S_.op("pool", lambda e, t=t, cs=cs, yt=yt: e.tensor_tensor(
                        out=acc[t][:, cs], in0=acc[t][:, cs], in1=yt[:], op=ALU.add),
                        reads=[yt, acc[t]], writes=[acc[t]])
        S_.dma("sp", par[1][:], par_d[P_LNG2], writes=[par[1]])
        S_.dma("sp", par[2][:], par_d[P_LNB2], writes=[par[2]])
        for t in range(TTB):
            ln_tile(acc[t], par[1], par[1][:], par[2], par[2][:], sm[t % 2])
            S_.dma("act", out_d[t], acc[t][:], reads=[acc[t]])
        S_.finish()
    return nc


def run_l3b(a_tok, wp, x_tok, rows, wrc, brc, wrf, brf, wgu, wdp, trace=False):
    if "l3b" not in _NC_CACHE:
        _NC_CACHE["l3b"] = build_l3b()
    nc = _NC_CACHE["l3b"]
    wpp = np.ascontiguousarray(wp.reshape(KC, 128, 4, 512).transpose(2, 1, 0, 3)).reshape(4, 128, KC * 512)
    wr = np.ascontiguousarray(np.concatenate([wrc, wrf], axis=1).reshape(KC, 128, 36).transpose(1, 0, 2))
    br = bc128(np.concatenate([brc, brf]))
    ident = np.eye(128, dtype=np.float32)
    pars = [np.ascontiguousarray(np.stack([bc128(v) for v in rows[b]])) for b in range(B)]
    in_maps = []
    TC = NTOK // NCORES
    for c in range(NCORES):
        b = c // (NCORES // B)
        a_c = a_tok[c * TC:(c + 1) * TC]
        aT = np.ascontiguousarray(a_c.reshape(TB, KC, 128).transpose(2, 1, 0))
        xc = np.ascontiguousarray(x_tok[c * TC:(c + 1) * TC]).reshape(TTB, 128, D)
        in_maps.append({"aT": aT, "wp": wpp, "x": xc, "par": pars[b], "wr": wr, "br": br,
                        "wgu": wgu, "wd": wdp, "ident": ident})
    res = run_bass_kernel_spmd(nc, in_maps, core_ids=list(range(NCORES)), trace=trace)
    out = np.concatenate([res.results[c]["out"].reshape(TC, D) for c in range(NCORES)], axis=0)
    if trace:
        return out, res
    return out
```
